# Optimizing a Trainium2 kernel written in Bass

```python
import math
import jax, jax.numpy as jnp
from jax import lax
import numpy as np

D_MODEL = 1024
BATCH = 2
SEQ = 8192
DEPTH = 1

GRID_W = 64
CTX_LEN = 256
NORM_EPS = 1e-6
ROPE_BASE = 10000.0
MASK_VALUE = -1e30

DA_HEADS = 4
DA_HEAD_DIM = 64
Q_BLOCK = 128
WA_HEADS = 8
WA_KV_HEADS = 2
WA_GROUP = WA_HEADS // WA_KV_HEADS
WA_HEAD_DIM = 64
WINDOW = 128
BAND_BLOCK = WINDOW

QA_COLS = DA_HEADS * 2 * DA_HEAD_DIM
KA_COLS = DA_HEADS * 2 * DA_HEAD_DIM
VA_COLS = DA_HEADS * 2 * DA_HEAD_DIM
QB_COLS = WA_HEADS * WA_HEAD_DIM
KB_COLS = WA_KV_HEADS * WA_HEAD_DIM
VB_COLS = WA_KV_HEADS * WA_HEAD_DIM
IN_COLS = QA_COLS + KA_COLS + VA_COLS + QB_COLS + KB_COLS + VB_COLS
SPLIT_POINTS = [QA_COLS, QA_COLS + KA_COLS, QA_COLS + KA_COLS + VA_COLS,
                QA_COLS + KA_COLS + VA_COLS + QB_COLS,
                QA_COLS + KA_COLS + VA_COLS + QB_COLS + KB_COLS]
DA_OUT = DA_HEADS * 2 * DA_HEAD_DIM
WA_OUT = WA_HEADS * WA_HEAD_DIM
MIX_WIDTH = DA_OUT + WA_OUT

N_EXPERTS = 32
TOP_K = 4
D_EXPERT = 1024
SWIGLU_LIMIT = 7.0
SWIGLU_ALPHA = 1.702
EXPERT_BLOCK = 128

kernel_name = "hymba_diff_swa_moe_dit_block"


def rmsnorm(x, g):
    xf = x.astype(jnp.float32)
    y = xf * lax.rsqrt(jnp.mean(xf * xf, axis=-1, keepdims=True) + NORM_EPS)
    return (y * g.astype(jnp.float32)).astype(x.dtype)


def adaln(cvec, w_mod, b_mod):
    mod = jax.nn.silu(cvec) @ w_mod + b_mod
    return jnp.split(mod, 6, axis=-1)


def modulate(h, shift, scale):
    return h * (1.0 + scale) + shift


def rope_tables(n_tok, head_dim):
    rows_count = n_tok // GRID_W
    rows = jnp.repeat(jnp.arange(rows_count), GRID_W).astype(jnp.float32)
    cols = jnp.tile(jnp.arange(GRID_W), rows_count).astype(jnp.float32)
    nf = head_dim // 4
    inv = ROPE_BASE ** (-jnp.arange(nf, dtype=jnp.float32) / nf)
    ang_r = rows[:, None] * inv
    ang_c = cols[:, None] * inv
    return (jnp.cos(ang_r), jnp.sin(ang_r), jnp.cos(ang_c), jnp.sin(ang_c))


def _rotate(xh, cos, sin):
    nf = xh.shape[-1] // 2
    x1, x2 = xh[..., :nf], xh[..., nf:]
    return jnp.concatenate([x1 * cos - x2 * sin, x2 * cos + x1 * sin], axis=-1)


def axial_rope(x, tabs):
    d = x.shape[-1]
    da = d // 2
    shape = (1, x.shape[1]) + (1,) * (x.ndim - 3) + (da // 2,)
    cr, sr, cc, sc = [t.reshape(shape).astype(x.dtype) for t in tabs]
    return jnp.concatenate([_rotate(x[..., :da], cr, sr), _rotate(x[..., da:], cc, sc)], axis=-1)


def split_proj(p):
    B, N, _ = p.shape
    qa, ka, va, qb, kb, vb = jnp.split(p, SPLIT_POINTS, axis=-1)
    qa = qa.reshape(B, N, DA_HEADS, 2, DA_HEAD_DIM)
    ka = ka.reshape(B, N, DA_HEADS, 2, DA_HEAD_DIM)
    va = va.reshape(B, N, DA_HEADS, 2 * DA_HEAD_DIM)
    qb = qb.reshape(B, N, WA_KV_HEADS, WA_GROUP, WA_HEAD_DIM)
    kb = kb.reshape(B, N, WA_KV_HEADS, WA_HEAD_DIM)
    vb = vb.reshape(B, N, WA_KV_HEADS, WA_HEAD_DIM)
    return qa, ka, va, qb, kb, vb


def diff_attend(q, k, v, lam):
    s = jnp.einsum('bqhcd,bkhcd->bhcqk', q, k).astype(jnp.float32)
    p = jax.nn.softmax(s, axis=-1)
    a = p[:, :, 0] - lam * p[:, :, 1]
    return jnp.einsum('bhqk,bkhe->bqhe', a.astype(v.dtype), v)


def diff_attend_latent(q, k_all, v_all, lam):
    B, S, H, _, d = q.shape
    nqb = S // Q_BLOCK
    qb = jnp.moveaxis(q.reshape(B, nqb, Q_BLOCK, H, 2, d), 1, 0)
    out = lax.map(lambda qq: diff_attend(qq, k_all, v_all, lam), qb)
    return jnp.moveaxis(out, 0, 1).reshape(B, S, H, 2 * d)


def diff_post(o, subln_g, lam_init):
    B, N = o.shape[:2]
    return (rmsnorm(o, subln_g) * (1.0 - lam_init)).reshape(B, N, DA_OUT)


def window_gqa_latent(q, k, v, k_ctx, v_ctx, sink):
    B, S, KV, G, d = q.shape
    W = BAND_BLOCK
    nb = S // W
    pad = ((0, 0), (W, W), (0, 0), (0, 0))
    kp = jnp.pad(k, pad).reshape(B, nb + 2, W, KV, d)
    vp = jnp.pad(v, pad).reshape(B, nb + 2, W, KV, d)
    kband = jnp.concatenate([kp[:, :-2], kp[:, 1:-1], kp[:, 2:]], axis=2)
    vband = jnp.concatenate([vp[:, :-2], vp[:, 1:-1], vp[:, 2:]], axis=2)
    qbk = q.reshape(B, nb, W, KV, G, d)
    s_loc = jnp.einsum('bnqkgd,bnjkd->bnkgqj', qbk, kband).astype(jnp.float32)
    s_ctx = jnp.einsum('bnqkgd,bjkd->bnkgqj', qbk, k_ctx).astype(jnp.float32)
    n_i = jnp.arange(nb)[:, None, None]
    q_i = jnp.arange(W)[None, :, None]
    k_j = jnp.arange(3 * W)[None, None, :]
    kpos = (n_i - 1) * W + k_j
    rel = k_j - W - q_i
    valid = (jnp.abs(rel) <= WINDOW) & (kpos >= 0) & (kpos < S)
    s_loc = jnp.where(valid[None, :, None, None], s_loc, MASK_VALUE)
    sink_col = jnp.broadcast_to(sink.astype(jnp.float32).reshape(1, 1, KV, G, 1, 1),
                                s_loc.shape[:-1] + (1,))
    p = jax.nn.softmax(jnp.concatenate([s_loc, s_ctx, sink_col], axis=-1), axis=-1)
    p_loc = p[..., :3 * W].astype(v.dtype)
    p_ctx = p[..., 3 * W:-1].astype(v.dtype)
    o = (jnp.einsum('bnkgqj,bnjkd->bnqkgd', p_loc, vband)
         + jnp.einsum('bnkgqj,bjkd->bnqkgd', p_ctx, v_ctx))
    return o.reshape(B, S, WA_OUT)


def sink_attend_ctx(q, k, v, sink):
    B, C, KV, G, d = q.shape
    s = jnp.einsum('bqkgd,bjkd->bkgqj', q, k).astype(jnp.float32)
    sink_col = jnp.broadcast_to(sink.astype(jnp.float32).reshape(1, KV, G, 1, 1), s.shape[:-1] + (1,))
    p = jax.nn.softmax(jnp.concatenate([s, sink_col], axis=-1), axis=-1)[..., :-1]
    o = jnp.einsum('bkgqj,bjkd->bqkgd', p.astype(v.dtype), v)
    return o.reshape(B, C, WA_OUT)


def moe(x2d, w_r, b_r, w_gu, b_gu, w_dn, b_dn):
    T, D = x2d.shape
    logits = (x2d @ w_r + b_r).astype(jnp.float32)
    top_v, top_i = lax.top_k(logits, TOP_K)
    gates = jax.nn.softmax(top_v, axis=-1)
    flat_e = top_i.reshape(-1).astype(jnp.int32)
    n_assign = T * TOP_K
    order = jnp.argsort(flat_e)
    sorted_e = flat_e[order]
    counts = jnp.zeros((N_EXPERTS,), jnp.int32).at[flat_e].add(1)
    padded = ((counts + EXPERT_BLOCK - 1) // EXPERT_BLOCK) * EXPERT_BLOCK
    start = jnp.cumsum(counts) - counts
    pend = jnp.cumsum(padded)
    pstart = pend - padded
    rank = jnp.arange(n_assign, dtype=jnp.int32) - start[sorted_e]
    dest_sorted = (pstart[sorted_e] + rank).astype(jnp.int32)
    n_rows = n_assign + N_EXPERTS * EXPERT_BLOCK
    n_blocks = n_rows // EXPERT_BLOCK
    row_tok = jnp.full((n_rows,), T, jnp.int32).at[dest_sorted].set((order // TOP_K).astype(jnp.int32))
    block_start = jnp.arange(n_blocks, dtype=jnp.int32) * EXPERT_BLOCK
    block_exp = jnp.clip(jnp.searchsorted(pend, block_start, side='right'), 0, N_EXPERTS - 1)
    x_pad = jnp.concatenate([x2d, jnp.zeros((1, D), x2d.dtype)], axis=0)

    def expert_block(args):
        tok, e = args
        xb = x_pad[tok]
        h = xb @ w_gu[e] + b_gu[e]
        g = jnp.minimum(h[:, ::2], SWIGLU_LIMIT)
        lin = jnp.clip(h[:, 1::2], -SWIGLU_LIMIT, SWIGLU_LIMIT)
        a = g * jax.nn.sigmoid(SWIGLU_ALPHA * g) * (lin + 1.0)
        return a @ w_dn[e] + b_dn[e]

    rows = lax.map(expert_block, (row_tok.reshape(n_blocks, EXPERT_BLOCK), block_exp))
    rows = rows.reshape(n_rows, D)
    dest = jnp.zeros((n_assign,), jnp.int32).at[order].set(dest_sorted)
    y = rows[dest].reshape(T, TOP_K, D)
    return jnp.einsum('tk,tkd->td', gates.astype(y.dtype), y)


def setup_inputs(seed: int = 0) -> dict:
    key = jax.random.key(seed)
    ks = jax.random.split(key, 24)
    D, L, E, F = D_MODEL, DEPTH, N_EXPERTS, D_EXPERT
    nrm = lambda k, s: jax.random.normal(k, s, jnp.float32)
    return {
        "x": nrm(ks[0], (BATCH, SEQ, D)),
        "c": nrm(ks[1], (BATCH, D)),
        "ctx": nrm(ks[2], (BATCH, CTX_LEN, D)),
        "c_ctx": nrm(ks[3], (D,)),
        "w_mod": nrm(ks[4], (L, D, 6 * D)) * (0.5 * D ** -0.5),
        "b_mod": nrm(ks[5], (L, 6 * D)) * 0.02,
        "norm1_g": 1.0 + 0.05 * nrm(ks[6], (L, D)),
        "w_in": nrm(ks[7], (L, D, IN_COLS)) * D ** -0.5,
        "lam_q1": 0.1 * nrm(ks[8], (L, DA_HEAD_DIM)),
        "lam_k1": 0.1 * nrm(ks[9], (L, DA_HEAD_DIM)),
        "lam_q2": 0.1 * nrm(ks[10], (L, DA_HEAD_DIM)),
        "lam_k2": 0.1 * nrm(ks[11], (L, DA_HEAD_DIM)),
        "subln_g": 1.0 + 0.05 * nrm(ks[12], (L, 2 * DA_HEAD_DIM)),
        "sink": 0.5 * nrm(ks[13], (L, WA_HEADS)),
        "w_out": nrm(ks[14], (L, MIX_WIDTH, D)) * MIX_WIDTH ** -0.5,
        "norm2_g": 1.0 + 0.05 * nrm(ks[15], (L, D)),
        "w_router": nrm(ks[16], (L, D, E)) * D ** -0.5,
        "b_router": 0.01 * nrm(ks[17], (L, E)),
        "w_gate_up": nrm(ks[18], (L, E, D, 2 * F)) * D ** -0.5,
        "b_gate_up": 0.01 * nrm(ks[19], (L, E, 2 * F)),
        "w_down": nrm(ks[20], (L, E, F, D)) * F ** -0.5,
        "b_down": 0.01 * nrm(ks[21], (L, E, D)),
        "final_g": 1.0 + 0.05 * nrm(ks[22], (D,)),
    }


def reference(x, c, ctx, c_ctx, w_mod, b_mod, norm1_g, w_in, lam_q1, lam_k1, lam_q2, lam_k2,
              subln_g, sink, w_out, norm2_g, w_router, b_router, w_gate_up, b_gate_up,
              w_down, b_down, final_g):
    B, S, D = x.shape
    C = ctx.shape[1]
    ROWS = S // GRID_W
    tabs = rope_tables(ROWS * GRID_W, DA_HEAD_DIM)
    scale_a = DA_HEAD_DIM ** -0.5
    scale_b = WA_HEAD_DIM ** -0.5
    cs = ctx
    for l in range(DEPTH):
        lam_init = 0.8 - 0.6 * math.exp(-0.3 * l)
        sh1, sc1, g1, sh2, sc2, g2 = [m[:, None, :] for m in adaln(c, w_mod[l], b_mod[l])]
        csh1, csc1, cg1, csh2, csc2, cg2 = [m[None, None, :] for m in adaln(c_ctx, w_mod[l], b_mod[l])]

        hx = modulate(rmsnorm(x, norm1_g[l]), sh1, sc1)
        hc = modulate(rmsnorm(cs, norm1_g[l]), csh1, csc1)
        qa, ka, va, qb, kb, vb = split_proj(hx @ w_in[l])
        qac, kac, vac, qbc, kbc, vbc = split_proj(hc @ w_in[l])
        qa = axial_rope(qa * scale_a, tabs)
        ka = axial_rope(ka, tabs)
        qb = axial_rope(qb * scale_b, tabs)
        kb = axial_rope(kb, tabs)
        lam = (jnp.exp(jnp.sum(lam_q1[l].astype(jnp.float32) * lam_k1[l].astype(jnp.float32)))
               - jnp.exp(jnp.sum(lam_q2[l].astype(jnp.float32) * lam_k2[l].astype(jnp.float32)))
               + lam_init)
        ya = diff_attend_latent(qa, jnp.concatenate([ka, kac], axis=1),
                                jnp.concatenate([va, vac], axis=1), lam)
        ya = diff_post(ya, subln_g[l], lam_init)
        yb = window_gqa_latent(qb, kb, vb, kbc, vbc, sink[l])
        x = x + g1 * (jnp.concatenate([ya, yb], axis=-1) @ w_out[l])
        if l < DEPTH - 1:
            yac = diff_post(diff_attend(qac * scale_a, kac, vac, lam), subln_g[l], lam_init)
            ybc = sink_attend_ctx(qbc * scale_b, kbc, vbc, sink[l])
            cs = cs + cg1 * (jnp.concatenate([yac, ybc], axis=-1) @ w_out[l])

        hx2 = modulate(rmsnorm(x, norm2_g[l]), sh2, sc2)
        x = x + g2 * moe(hx2.reshape(B * S, D), w_router[l], b_router[l], w_gate_up[l],
                         b_gate_up[l], w_down[l], b_down[l]).reshape(B, S, D)
        if l < DEPTH - 1:
            hc2 = modulate(rmsnorm(cs, norm2_g[l]), csh2, csc2)
            cs = cs + cg2 * moe(hc2.reshape(B * C, D), w_router[l], b_router[l], w_gate_up[l],
                                b_gate_up[l], w_down[l], b_down[l]).reshape(B, C, D)
    return rmsnorm(x, final_g)
```

```python
import contextlib
import numpy as np
import concourse.bass as bass
import concourse.mybir as mybir
from concourse.bass_utils import run_bass_kernel_spmd

F32 = mybir.dt.float32; BF16 = mybir.dt.bfloat16; I32 = mybir.dt.int32; U8 = mybir.dt.uint8
U32 = mybir.dt.uint32
ALU = mybir.AluOpType; AF = mybir.ActivationFunctionType
AX = mybir.AxisListType
ENGS = ("pe", "act", "dve", "pool", "sp")
NDMASEM = 12

D = 1024; S_LEN = 8192; CTX = 256; NQ = 2048; NE = 32;
NT = [-(-min(2048, -(-8192 // (i + 1))) // 128) for i in range(NE)]
OFFT = [sum(NT[:i]) for i in range(NE)]
NSLOT = sum(NT) * 128; NSLOTP = NSLOT
QA, KA, VA, QB, KB, VB = 0, 512, 1024, 1536, 2048, 2176
NKT = 66
WROWS = 2560
EPS = 1e-6
LAM_INIT = 0.2


def L(method, *args, **kw):
    return lambda e: getattr(e, method)(*args, **kw)


class Buf:
    __slots__ = ("name", "w", "r", "excl")
    def __init__(self, name="", excl=False):
        self.name = name; self.w = None; self.r = []; self.excl = excl


class Op:
    __slots__ = ("eng", "fn", "deps", "dma", "signal", "sem", "val", "idx", "name")
    def __init__(self, eng, fn, dma, name):
        self.eng = eng; self.fn = fn; self.deps = []; self.dma = dma
        self.signal = False; self.sem = None; self.val = 0; self.idx = 0; self.name = name


class Sched:
    def __init__(self):
        self.ops = {e: [] for e in ENGS}
        self.all = []

    def op(self, eng, fn, reads=(), writes=(), dma=False, deps=(), name=""):
        o = Op(eng, fn, dma, name)
        ds = {}
        ex = [b for b in reads if b.excl]
        if ex:
            reads = [b for b in reads if not b.excl]; writes = list(writes) + ex
        for b in reads:
            if b.w is not None: ds[id(b.w)] = b.w
        for b in writes:
            if b.w is not None: ds[id(b.w)] = b.w
            lastr = {}
            for r in b.r:
                if r.dma: ds[id(r)] = r
                elif r.eng not in lastr or r.idx > lastr[r.eng].idx: lastr[r.eng] = r
            for r in lastr.values(): ds[id(r)] = r
        for d in deps: ds[id(d)] = d
        o.deps = list(ds.values())
        for b in reads: b.r.append(o)
        for b in writes:
            b.w = o; b.r = []
        o.idx = len(self.ops[eng]); self.ops[eng].append(o); self.all.append(o)
        return o

    def dma(self, eng, out, in_, reads=(), writes=(), name="", **kw):
        return self.op(eng, L("dma_start", out=out, in_=in_, **kw), reads, writes, dma=True, name=name)

    def barrier(self):
        lasts = []
        for e in ENGS:
            real = [o for o in self.ops[e] if o.fn is not None]
            comp = [o for o in real if not o.dma]
            if comp: lasts.append(comp[-1])
            lasts.extend([o for o in real if o.dma][-NDMASEM:])
        for e in ENGS:
            self.op(e, None, deps=lasts, name="barrier")

    def emit(self, nc):
        for o in self.all:
            for d in o.deps:
                if d.eng == "pe" and o.eng == "pe" and not d.dma:
                    continue
                d.signal = True
        with contextlib.ExitStack() as st:
            csem = {e: st.enter_context(nc.semaphore("c_" + e)) for e in ENGS}
            dsem = {e: [st.enter_context(nc.semaphore(f"d_{e}{i}")) for i in range(NDMASEM)]
                    for e in ("sp", "pool", "act")}
            for e in ENGS:
                cnt = 0; di = 0; dcnt = [0] * NDMASEM; dlast = [None] * NDMASEM
                for o in self.ops[e]:
                    if o.fn is None: continue
                    if o.dma:
                        o.signal = True
                        s = di % NDMASEM; di += 1
                        if dlast[s] is not None:
                            o.deps.append(dlast[s])
                        dcnt[s] += 16; o.sem = dsem[e][s]; o.val = dcnt[s]; dlast[s] = o
                    elif o.signal:
                        cnt += 1; o.sem = csem[e]; o.val = cnt
            blk = st.enter_context(nc.Block())
            engobj = {"pe": "tensor", "act": "scalar", "dve": "vector", "pool": "gpsimd", "sp": "sync"}

            def mk(e):
                def body(eng):
                    seen = {}
                    for o in self.ops[e]:
                        need = {}
                        for d in o.deps:
                            if d.fn is None: continue
                            if d.eng == "pe" and e == "pe" and not d.dma: continue
                            k = id(d.sem)
                            if seen.get(k, 0) >= d.val: continue
                            if k not in need or need[k][1] < d.val: need[k] = (d.sem, d.val)
                        for k, (s, v) in need.items():
                            eng.wait_ge(s, v); seen[k] = v
                        if o.fn is None: continue
                        inst = o.fn(eng)
                        if o.signal:
                            inst.then_inc(o.sem, 16 if o.dma else 1)
                return body
            for e in ENGS:
                if self.ops[e]:
                    getattr(blk, engobj[e])(mk(e))


def build_program(do_moe=True, dbg=False, stop_after=99):
    nc = bass.Bass("TRN2", target_bir_lowering=False)
    dt_ = nc.dram_tensor

    def din(name, shape, dt=F32):
        return dt_(name, list(shape), dt, kind="ExternalInput").ap()

    xkv = din("xkv", [S_LEN, D]); ctxd = din("ctx", [CTX, D]); cvec = din("cvec", [2, D])
    cosd = din("cosT", [128, S_LEN + CTX]); sind = din("sinT", [128, S_LEN + CTX])
    wmaskd = din("wmask", [128, 2]); constd = din("consts", [128, 9 * 128])
    w_mod = din("w_mod", [D, 6 * D]); b_mod = din("b_mod", [6 * D]); n1g = din("norm1_g", [D])
    w_in = din("w_in", [D, 2304]); lamd = din("lam", [4, 64]); sublnd = din("subln_g", [128])
    sinkd = din("sink", [8]); w_out = din("w_out", [D, D]); n2g = din("norm2_g", [D])
    w_r = din("w_router", [D, NE]); b_r = din("b_router", [NE]); fgd = din("final_g", [D])
    if do_moe:
        w_gu = din("w_gate_up", [NE, D, 2 * D]); b_gu = din("b_gate_up", [NE, 2 * D])
        w_dn = din("w_down", [NE, D, D]); b_dn = din("b_down", [NE, D])
    outd = dt_("out", [NQ, D], F32, kind="ExternalOutput").ap()
    kscr = dt_("kscr", [4, 9, 128, 1024], BF16).ap()
    vscr = dt_("vscr", [4, 9, 128, 8, 128], BF16).ap()
    x1scr = dt_("x1scr", [NQ, D], F32).ap()
    xe = dt_("xe", [NSLOTP, D], BF16).ap()
    ye = dt_("ye", [NSLOTP, D], F32).ap()
    if dbg:
        dbg_x1 = dt_("dbg_x1", [NQ, D], F32, kind="ExternalOutput").ap()
        dbg_yb = dt_("dbg_yb", [128, 4 * NQ], BF16, kind="ExternalOutput").ap()
        dbg_kb = dt_("dbg_kb", [128, WROWS + CTX], BF16, kind="ExternalOutput").ap()
        dbg_vb = dt_("dbg_vb", [128, 22 * 128], BF16, kind="ExternalOutput").ap()
        dbg_qb = dt_("dbg_qb", [128, 4 * WROWS], BF16, kind="ExternalOutput").ap()

    TOT = 206 * 1024
    arena = nc.alloc_sbuf_tensor("arena", [128, TOT], U8)
    psum = nc.alloc_psum_tensor("psum", [128, 8, 512], F32)
    off = [0]

    def alloc(shape, dt=F32):
        n = int(np.prod(shape[1:])) * (2 if dt == BF16 else 4)
        a = arena[0:shape[0], off[0]:off[0] + n].bitcast(dt)
        off[0] += (n + 63) // 64 * 64
        assert off[0] <= TOT, ("SBUF overflow", off[0])
        if len(shape) == 3:
            a = a.rearrange("p (a b) -> p a b", b=shape[2])
        elif len(shape) == 4:
            a = a.rearrange("p (a b c) -> p a b c", b=shape[2], c=shape[3])
        return a

    def pbank(b, n=1):
        return psum[:, b:b + n, :]

    def pflat(b, n=1):
        return psum[:, b:b + n, :].rearrange("p a b -> p (a b)")

    def pbf(b):
        return psum[:, b, :].bitcast(BF16).rearrange("p (a b) -> p a b", b=128)

    S = Sched()

    def finish():
        S.barrier()
        S.emit(nc)
        return nc
    PB = [Buf(f"psum{i}", excl=True) for i in range(8)]

    cst = alloc([128, 9, 128], F32)
    ident_f = cst[:, 0, :]; iota_f = cst[:, 5, 0:32]; pidx = cst[:, 6, :]; OFFrep = cst[0:32, 7, :]; Lmask = cst[0:32, 8, 0:32]
    cstb = alloc([128, 5, 128], BF16)
    ident_b = cstb[:, 0, :]; RT_b = cstb[:, 1, :]; U_b = cstb[:, 2, :]; triL = cstb[:, 3, :]; triR = cstb[:, 4, :]
    ones_b = alloc([128, 128], BF16); ones_f = alloc([128, 128], F32)
    A1T = alloc([128, 8]); B1T = alloc([128, 8]); A1cT = alloc([128, 8]); B1cT = alloc([128, 8])
    gsubp = alloc([128, 1]); neglam = alloc([128, 1]); sinkexp = alloc([128, 4]); wmask = alloc([128, 2])
    G1bc = alloc([128, D]); A2bc = alloc([128, D]); B2bc = alloc([128, D]); G2bc = alloc([128, D]); FGbc = alloc([128, D])
    desti = alloc([128, 64], I32); gates = alloc([128, 64]); basecnt = alloc([128, 32])
    ek_all = alloc([128, 64]); rk_all = alloc([128, 64]); Pm = alloc([32, 32]); permbc = alloc([128, 32])
    idxg = alloc([128, 32 * 8], I32); bdn_idx = alloc([128, 32], I32)
    junk = alloc([128, D], BF16); Bjunk = Buf("junk")
    Bc = Buf("consts"); Bvec = Buf("vecs"); Bbc = Buf("bcast"); Brt = Buf("route")

    S.dma("sp", cst.rearrange("p a b -> p (a b)"), constd, writes=[Bc])
    S.dma("sp", wmask, wmaskd, writes=[Bc])
    S.op("dve", L("tensor_copy", out=cstb, in_=cst[:, 0:5, :]), reads=[Bc], writes=[Bc])
    S.op("dve", L("memset", ones_b, 1.0), writes=[Bc])
    S.op("dve", L("memset", ones_f, 1.0), writes=[Bc])
    S.op("dve", L("memset", basecnt, 0.0), writes=[Brt])

    mark_persist = off[0]

    cT = alloc([128, 8, 2]); sg = alloc([128, 8, 2]); scT = alloc([128, 8, 2]); scT_b = alloc([128, 8, 2], BF16)
    screp = alloc([128, 8, 128], BF16)
    bmT = alloc([128, 16]); g1nT = alloc([128, 8]); modT = alloc([128, 16, 2])
    bmbc = alloc([128, 4 * D]); n2bc = alloc([128, D])
    lamt = alloc([1, 4, 64]); lamp = alloc([1, 2, 64]); lams = alloc([1, 2]); lamv = alloc([1, 2])
    wm = [alloc([128, 8, 1024], BF16) for _ in range(2)]
    Bwm = [Buf("wm0"), Buf("wm1")]; Bp0 = Buf("p0")

    for r_ in range(2):
        S.dma("sp", cT[:, :, r_], cvec[r_].rearrange("(c p) -> p c", p=128), writes=[Bp0], allow_slow_non_contiguous=True)
    S.dma("sp", bmT, b_mod[0:2048].rearrange("(c p) -> p c", p=128), writes=[Bp0], allow_slow_non_contiguous=True)
    S.dma("sp", g1nT, n1g.rearrange("(c p) -> p c", p=128), writes=[Bp0], allow_slow_non_contiguous=True)
    S.dma("sp", bmbc, b_mod[2048:6144].partition_broadcast(128), writes=[Bp0])
    S.dma("sp", n2bc, n2g.partition_broadcast(128), writes=[Bp0])
    S.dma("sp", FGbc, fgd.partition_broadcast(128), writes=[Bbc])
    S.dma("sp", gsubp, sublnd.rearrange("(p o) -> p o", o=1), writes=[Bvec])
    S.dma("sp", lamt.rearrange("p a b -> p (a b)"), lamd.rearrange("(o a) b -> o (a b)", o=1), writes=[Bp0])
    for g in range(2):
        S.dma("sp", sinkexp[g * 64:(g + 1) * 64, :], sinkd[g * 4:(g + 1) * 4].partition_broadcast(64), writes=[Bvec])
    wmv = w_mod.rearrange("(c p) n -> p c n", p=128)
    for i in range(2):
        S.dma("pool", wm[i], wmv[:, :, i * 1024:(i + 1) * 1024], writes=[Bwm[i]])

    S.op("act", L("activation", out=sg, in_=cT, func=AF.Sigmoid), reads=[Bp0], writes=[Bp0])
    S.op("dve", L("tensor_tensor", out=scT, in0=cT, in1=sg, op=ALU.mult), reads=[Bp0], writes=[Bp0])
    S.op("dve", L("tensor_copy", out=scT_b, in_=scT), reads=[Bp0], writes=[Bp0])
    S.op("dve", L("tensor_copy", out=screp, in_=scT[:, :, 0:1].to_broadcast([128, 8, 128])), reads=[Bp0], writes=[Bp0])
    S.op("dve", L("tensor_scalar", out=gsubp, in0=gsubp, scalar1=1.0 - LAM_INIT, scalar2=None, op0=ALU.mult),
         reads=[Bvec], writes=[Bvec])
    S.op("act", L("activation", out=sinkexp, in_=sinkexp, func=AF.Exp), reads=[Bvec], writes=[Bvec])
    S.op("dve", L("tensor_tensor", out=lamp, in0=lamt[:, 0:4:2, :], in1=lamt[:, 1:4:2, :], op=ALU.mult), reads=[Bp0], writes=[Bp0])
    S.op("dve", L("reduce_sum", out=lams, in_=lamp, axis=AX.X), reads=[Bp0], writes=[Bp0])
    S.op("act", L("activation", out=lamv, in_=lams, func=AF.Exp), reads=[Bp0], writes=[Bp0])
    S.op("dve", L("tensor_tensor", out=lamv[:, 0:1], in0=lamv[:, 1:2], in1=lamv[:, 0:1], op=ALU.subtract), reads=[Bp0], writes=[Bp0])
    S.op("dve", L("tensor_scalar", out=lamv[:, 0:1], in0=lamv[:, 0:1], scalar1=-LAM_INIT, scalar2=None, op0=ALU.add), reads=[Bp0], writes=[Bp0])
    S.op("pe", L("matmul", psum[:, 7, 0:1], lhsT=ones_f[0:1, :], rhs=lamv[:, 0:1], start=True, stop=True),
         reads=[Bp0, Bc], writes=[PB[7]])
    S.op("dve", L("tensor_copy", out=neglam, in_=psum[:, 7, 0:1]), reads=[PB[7]], writes=[Bvec])

    pm = psum[:, 0, 0:32].rearrange("p (a b) -> p a b", b=2)
    for blk in range(16):
        i = blk // 8
        for c in range(8):
            S.op("pe", L("matmul", pm[:, blk, :], lhsT=wm[i][:, c, (blk % 8) * 128:(blk % 8 + 1) * 128],
                                                              rhs=scT_b[:, c, :], start=(c == 0), stop=(c == 7)),
                 reads=[Bwm[i], Bp0], writes=[PB[0]])
    S.op("dve", L("tensor_tensor", out=modT, in0=pm, in1=bmT.unsqueeze(2).to_broadcast([128, 16, 2]), op=ALU.add),
         reads=[PB[0], Bp0], writes=[Bp0])
    for (AT, BT, r) in ((A1T, B1T, 0), (A1cT, B1cT, 1)):
        S.op("dve", L("scalar_tensor_tensor", out=AT, in0=modT[:, 8:16, r], scalar=1.0, in1=g1nT, op0=ALU.add, op1=ALU.mult),
             reads=[Bp0], writes=[Bvec])
        S.op("dve", L("tensor_copy", out=BT, in_=modT[:, 0:8, r]), reads=[Bp0], writes=[Bvec])
    dsts = [G1bc, B2bc, A2bc, G2bc]
    for q in range(4):
        i = q % 2
        S.dma("pool", wm[i], wmv[:, :, (2 + q) * 1024:(3 + q) * 1024], writes=[Bwm[i]])
        for hf in range(2):
            for c in range(8):
                S.op("pe", L("matmul", psum[:, 1 + hf, :], lhsT=screp[:, c, :], rhs=wm[i][:, c, hf * 512:(hf + 1) * 512],
                                                                start=(c == 0), stop=(c == 7)),
                     reads=[Bwm[i], Bp0], writes=[PB[1 + hf]])
        S.op("dve", L("tensor_tensor", out=dsts[q], in0=pflat(1, 2), in1=bmbc[:, q * 1024:(q + 1) * 1024], op=ALU.add),
             reads=[PB[1], PB[2], Bp0], writes=[Bbc])
    S.op("dve", L("scalar_tensor_tensor", out=A2bc, in0=A2bc, scalar=1.0, in1=n2bc, op0=ALU.add, op1=ALU.mult),
         reads=[Bbc, Bp0], writes=[Bbc])

    if stop_after == 0:
        return finish()
    S.barrier()
    off[0] = mark_persist

    QaT = alloc([128, 4, WROWS], BF16); QbT = alloc([128, 4, WROWS], BF16)
    KbT = alloc([128, WROWS + CTX], BF16); Vb = alloc([128, 22, 128], BF16)
    BQ = Buf("Q"); BKb = Buf("Kb"); BVb = Buf("Vb"); Bya = Buf("ya"); Byb = Buf("yb")
    Bks = Buf("kscr"); Bvs = Buf("vscr")
    mark_attn = off[0]

    win = alloc([128, 8, 2304], BF16); Bwin = Buf("win")
    xt = [alloc([128, D]) for _ in range(2)]; Bxt = [Buf("xt0"), Buf("xt1")]
    xn = [alloc([128, D], BF16) for _ in range(4)]; Bxn = [Buf(f"xn{i}") for i in range(4)]
    st4 = [alloc([128, 4]) for _ in range(4)]; Bst = [Buf(f"st{i}") for i in range(4)]
    evt = [alloc([128, 8, 128]) for _ in range(2)]; Bev = [Buf("ev0"), Buf("ev1")]
    hxT = [alloc([128, 8, 512], BF16) for _ in range(2)]; Bhx = [Buf("hx0"), Buf("hx1")]
    cosg = [alloc([128, 512]) for _ in range(2)]; sing = [alloc([128, 512]) for _ in range(2)]; Btab = [Buf("tab0"), Buf("tab1")]
    kbf = [alloc([128, 512], BF16) for _ in range(2)]; Bkbf = [Buf("kbf0"), Buf("kbf1")]
    rt1 = [alloc([128, 512]) for _ in range(2)]; rt2 = [alloc([128, 512]) for _ in range(2)]
    Brt1 = [Buf("rt1a"), Buf("rt1b")]; Brt2 = [Buf("rt2a"), Buf("rt2b")]
    kst = [alloc([128, 4, 512], BF16) for _ in range(2)]; Bkst = [Buf("kst0"), Buf("kst1")]
    vst = [alloc([128, 4, 4, 128], BF16) for _ in range(2)]; Bvst = [Buf("vst0"), Buf("vst1")]

    winv = w_in.rearrange("(c p) n -> p c n", p=128)
    S.dma("pool", win[:, :, 0:QB], winv[:, :, 0:QB], writes=[Bwin])
    S.dma("pool", win[:, :, KB:2304], winv[:, :, KB:2304], writes=[Bwin])
    for gi in range(4):
        for g in range(2):
            S.dma("pool", win[:, :, QB + gi * 128 + g * 64:QB + gi * 128 + (g + 1) * 64],
                  winv[:, :, QB + g * 256 + gi * 64:QB + g * 256 + (gi + 1) * 64], writes=[Bwin])
    import os
    rr = [0]

    pcyc = [0]

    def nextbank():
        b = 2 + pcyc[0] % 4; pcyc[0] += 1
        return b

    def projA(job, hb, tb):
        colbase, n, dst, wb = job
        i = rr[0] % 2; rr[0] += 1
        bk = nextbank()
        for c in range(8):
            S.op("pe", L("matmul", psum[:, bk, 0:n], lhsT=win[:, c, colbase:colbase + 128], rhs=hxT[hb][:, c, 0:n], start=(c == 0), stop=(c == 7)),
                 reads=[Bwin, Bhx[hb]], writes=[PB[bk]])
        S.op("act", L("activation", out=kbf[i][:, 0:n], in_=psum[:, bk, 0:n], func=AF.Copy), reads=[PB[bk]], writes=[Bkbf[i]])
        return (bk, i, n, dst, wb, tb)

    def ropeB(st):
        bk, i, n, dst, wb, tb = st
        S.op("pe", L("matmul", psum[:, 6 + i, 0:n], lhsT=RT_b, rhs=kbf[i][:, 0:n], start=True, stop=True), reads=[Bkbf[i], Bc], writes=[PB[6 + i]])
        S.op("dve", L("tensor_tensor", out=rt1[i][:, 0:n], in0=psum[:, bk, 0:n], in1=cosg[tb][:, 0:n], op=ALU.mult),
             reads=[PB[bk], Btab[tb]], writes=[Brt1[i]])
        S.op("dve", L("tensor_tensor", out=rt2[i][:, 0:n], in0=psum[:, 6 + i, 0:n], in1=sing[tb][:, 0:n], op=ALU.mult),
             reads=[PB[6 + i], Btab[tb]], writes=[Brt2[i]])
        S.op("pool", L("tensor_tensor", out=dst, in0=rt1[i][:, 0:n], in1=rt2[i][:, 0:n], op=ALU.add), reads=[Brt1[i], Brt2[i]], writes=wb)

    for G in range(17):
        ntile = 4 if G < 16 else 2
        n = ntile * 128
        hb = G % 2
        AT, BT = (A1T, B1T) if G < 16 else (A1cT, B1cT)
        tb = G % 2
        S.dma("sp", cosg[tb][:, 0:n], cosd[:, G * 512:G * 512 + n], writes=[Btab[tb]])
        S.dma("sp", sing[tb][:, 0:n], sind[:, G * 512:G * 512 + n], writes=[Btab[tb]])
        for t in range(ntile):
            gt = G * 4 + t
            xb = gt % 2; x4i = gt % 4
            src = xkv[gt * 128:(gt + 1) * 128, :] if G < 16 else ctxd[t * 128:(t + 1) * 128, :]
            S.dma("sp", xt[xb], src, writes=[Bxt[xb]])
            S.op("act", L("activation", out=junk, in_=xt[xb], func=AF.Square, accum_out=st4[x4i][:, 0:1]),
                 reads=[Bxt[xb]], writes=[Bjunk, Bst[x4i]])
            S.op("act", L("activation", out=st4[x4i][:, 1:2], in_=st4[x4i][:, 0:1], func=AF.Sqrt, scale=1.0 / D, bias=EPS),
                 reads=[Bst[x4i]], writes=[Bst[x4i]])
            S.op("dve", L("reciprocal", out=st4[x4i][:, 2:3], in_=st4[x4i][:, 1:2]), reads=[Bst[x4i]], writes=[Bst[x4i]])
            S.op("act", L("activation", out=xn[x4i], in_=xt[xb], func=AF.Copy, scale=st4[x4i][:, 2:3]),
                 reads=[Bxt[xb], Bst[x4i]], writes=[Bxn[x4i]])
            tbk = xb
            for c in range(8):
                S.op("pe", L("transpose", out=pbf(tbk)[:, c, :], in_=xn[x4i][:, c * 128:(c + 1) * 128], identity=ident_b),
                     reads=[Bxn[x4i], Bc], writes=[PB[tbk]])
            S.op("dve", L("tensor_tensor", out=evt[xb], in0=pbf(tbk), in1=AT.unsqueeze(2).to_broadcast([128, 8, 128]), op=ALU.mult),
                 reads=[PB[tbk], Bvec], writes=[Bev[xb]])
            S.op("pool", L("tensor_tensor", out=hxT[hb][:, :, t * 128:(t + 1) * 128], in0=evt[xb],
                           in1=BT.unsqueeze(2).to_broadcast([128, 8, 128]), op=ALU.add), reads=[Bev[xb], Bvec], writes=[Bhx[hb]])
        ch = G // 2 if G < 16 else 8
        ko = (G % 2) * 512 if G < 16 else 0
        sb = G % 2
        jobs = [(KA + h * 128, n, kst[sb][:, h, 0:n], [Bkst[sb]]) for h in range(4)]
        if G < 5 or G == 16:
            wo = G * 512 if G < 16 else WROWS
            jobs.append((KB, n, KbT[:, wo:wo + n], [BKb]))
        if G < 5:
            for (base, dstT) in ((QA, QaT), (QB, QbT)):
                for h in range(4):
                    jobs.append((base + h * 128, 512, dstT[:, h, G * 512:(G + 1) * 512], [BQ]))
        st_ = projA(jobs[0], hb, tb)
        for ji in range(len(jobs)):
            nxt = projA(jobs[ji + 1], hb, tb) if ji + 1 < len(jobs) else None
            ropeB(st_)
            st_ = nxt
            if ji == 3:
                for h in range(4):
                    S.dma("sp", kscr[h, ch, :, ko:ko + n], kst[sb][:, h, 0:n], reads=[Bkst[sb]], writes=[Buf()])
        for t in range(ntile):
            bk = nextbank()
            for c in range(8):
                S.op("pe", L("matmul", psum[:, bk, :], lhsT=hxT[hb][:, c, t * 128:(t + 1) * 128], rhs=win[:, c, VA:VA + 512],
                             start=(c == 0), stop=(c == 7)), reads=[Bwin, Bhx[hb]], writes=[PB[bk]])
            S.op("act", L("activation", out=vst[sb][:, :, t, :], in_=psum[:, bk, :].rearrange("p (h e) -> p h e", e=128), func=AF.Copy),
                 reads=[PB[bk]], writes=[Bvst[sb]])
        for h in range(4):
            tl0 = (G % 2) * 4 if G < 16 else 0
            S.dma("sp", vscr[h, ch, :, tl0:tl0 + ntile, :], vst[sb][:, h, 0:ntile, :], reads=[Bvst[sb]], writes=[Buf()])
        if G < 5 or G == 16:
            bk = nextbank()
            for t in range(ntile):
                for c in range(8):
                    S.op("pe", L("matmul", psum[:, bk, t * 128:(t + 1) * 128], lhsT=hxT[hb][:, c, t * 128:(t + 1) * 128],
                                 rhs=win[:, c, VB:VB + 128], start=(c == 0), stop=(c == 7)), reads=[Bwin, Bhx[hb]], writes=[PB[bk]])
            vt0 = G * 4 if G < 16 else 20
            S.op("act", L("activation", out=Vb[:, vt0:vt0 + ntile, :], in_=psum[:, bk, 0:n].rearrange("p (t e) -> p t e", e=128), func=AF.Copy),
                 reads=[PB[bk]], writes=[BVb])

    if stop_after == 1:
        return finish()
    S.barrier()
    off[0] = mark_attn
    yaT = alloc([128, 4, NQ], BF16); ybT = alloc([128, 4, NQ], BF16)
    mark_att2 = off[0]

    kch = [alloc([128, 1024], BF16) for _ in range(3)]; vch = [alloc([128, 8, 128], BF16) for _ in range(3)]
    Bkch = [Buf(f"kch{i}") for i in range(3)]; Bvch = [Buf(f"vch{i}") for i in range(3)]
    PT = [alloc([128, 1024], BF16) for _ in range(3)]; BPT = [Buf(f"PT{i}") for i in range(3)]
    r1 = alloc([128, 512]); r2 = alloc([128, 512]); t1 = alloc([128, 512]); t2 = alloc([128, 512])
    ot = alloc([128, 512]); osq = alloc([128, 512]); rs = alloc([128, 512]); zsb = alloc([64, 512])
    selA = alloc([64, 128]); selB = alloc([64, 128])
    o1sb = alloc([128, 512]); o2sb = alloc([128, 512])
    Bpp = [Buf(f"pp{i}") for i in range(10)]; Bsel = Buf("sel")
    S.op("dve", L("memset", selA, 0.0), writes=[Bsel]); S.op("dve", L("memset", selB, 0.0), writes=[Bsel])
    S.op("dve", L("memset", selA[0:32, :], 1.0 / 32), writes=[Bsel]); S.op("dve", L("memset", selB[32:64, :], 1.0 / 32), writes=[Bsel])
    if do_moe:
        zer = alloc([128, 4, D], BF16); Bzer = Buf("zer")
        S.op("pool", L("memset", zer, 0.0), writes=[Bzer])
        Bxe0 = []
        xev = xe[0:NSLOT, :].rearrange("(n p) d -> p n d", p=128)
        for i in range(NSLOT // 128 // 4):
            bz = Buf(); Bxe0.append(bz)
            S.dma("pool", xev[:, i * 4:(i + 1) * 4, :], zer, reads=[Bzer], writes=[bz])

    chunks = [(ci, 8) for ci in range(8)] + [(8, 2)]
    seq = [(h, qb, ci, nt) for h in range(4) for qb in range(4) for (ci, nt) in chunks]

    def issue_load(j):
        if j >= len(seq): return
        h, qb, ci, nt = seq[j]
        bi = j % 3
        S.dma("sp", kch[bi][:, 0:nt * 128], kscr[h, ci, :, 0:nt * 128], reads=[Bks], writes=[Bkch[bi]])
        S.dma("sp", vch[bi][:, 0:nt, :], vscr[h, ci, :, 0:nt, :], reads=[Bvs], writes=[Bvch[bi]])

    blocks = []
    for h in range(4):
        for qb in range(4):
            units = []
            for j0, (ci, nt) in enumerate(chunks):
                j = (h * 4 + qb) * 9 + j0
                for tt in range(nt):
                    units.append((j, tt, ci))
            blocks.append((h, qb, units))
    NU = len(blocks[0][2])

    def emit_S(bidx, k):
        h, qb, units = blocks[bidx]
        q0 = 128 + qb * 512
        j, tt, ci = units[k]
        g = bidx * NU + k
        bi = j % 3; sbk = (g % 2) * 2
        for cmp_ in range(2):
            S.op("pe", L("matmul", psum[:, sbk + cmp_, :], lhsT=kch[bi][cmp_ * 64:(cmp_ + 1) * 64, tt * 128:(tt + 1) * 128],
                         rhs=QaT[cmp_ * 64:(cmp_ + 1) * 64, h, q0:q0 + 512], start=True, stop=True),
                 reads=[Bkch[bi], BQ], writes=[PB[sbk + cmp_]])

    issue_load(0); issue_load(1)
    emit_S(0, 0)
    for bidx, (h, qb, units) in enumerate(blocks):
        for k in range(NU):
            j, tt, ci = units[k]
            g = bidx * NU + k
            if tt == 0:
                issue_load(j + 2)
            if k + 1 < NU:
                emit_S(bidx, k + 1)
            bi = j % 3; sbk = (g % 2) * 2; pi = g % 3
            S.op("act", L("activation", out=PT[pi], in_=pflat(sbk, 2), func=AF.Exp, scale=0.125),
                 reads=[PB[sbk], PB[sbk + 1]], writes=[BPT[pi]])
            first = (k == 0); last = (k == NU - 1)
            for cmp_ in range(2):
                S.op("pe", L("matmul", psum[:, 4 + cmp_, :], lhsT=vch[bi][:, tt, :], rhs=PT[pi][:, cmp_ * 512:(cmp_ + 1) * 512], start=first, stop=last),
                     reads=[Bvch[bi], BPT[pi]], writes=[PB[4 + cmp_]])
            for cmp_ in range(2):
                S.op("pe", L("matmul", psum[cmp_ * 32:(cmp_ + 1) * 32, 6, :], lhsT=ones_b[:, 0:32], rhs=PT[pi][:, cmp_ * 512:(cmp_ + 1) * 512],
                             start=first, stop=last, tile_position=(0, cmp_ * 32)),
                     reads=[BPT[pi], Bc], writes=[PB[6]])
        if bidx + 1 < len(blocks):
            emit_S(bidx + 1, 0)
        S.op("act", L("activation", out=o1sb, in_=psum[:, 4, :], func=AF.Copy), reads=[PB[4]], writes=[Bpp[8]])
        S.op("act", L("activation", out=o2sb, in_=psum[:, 5, :], func=AF.Copy), reads=[PB[5]], writes=[Bpp[9]])
        S.op("dve", L("tensor_copy", out=zsb, in_=psum[0:64, 6, :]), reads=[PB[6]], writes=[Bpp[7]])
        S.op("pe", L("matmul", psum[:, 7, :], lhsT=selA, rhs=zsb, start=True, stop=True), reads=[Bpp[7], Bsel], writes=[PB[7]])
        S.op("dve", L("reciprocal", out=r1, in_=psum[:, 7, :]), reads=[PB[7]], writes=[Bpp[0]])
        S.op("pe", L("matmul", psum[:, 7, :], lhsT=selB, rhs=zsb, start=True, stop=True), reads=[Bpp[7], Bsel], writes=[PB[7]])
        S.op("dve", L("reciprocal", out=r2, in_=psum[:, 7, :]), reads=[PB[7]], writes=[Bpp[1]])
        S.op("dve", L("tensor_tensor", out=t1, in0=o1sb, in1=r1, op=ALU.mult), reads=[Bpp[8], Bpp[0]], writes=[Bpp[2]])
        S.op("dve", L("tensor_tensor", out=t2, in0=o2sb, in1=r2, op=ALU.mult), reads=[Bpp[9], Bpp[1]], writes=[Bpp[3]])
        S.op("dve", L("scalar_tensor_tensor", out=ot, in0=t2, scalar=neglam, in1=t1, op0=ALU.mult, op1=ALU.add),
             reads=[Bpp[2], Bpp[3], Bvec], writes=[Bpp[4]])
        S.op("act", L("activation", out=osq, in_=ot, func=AF.Square), reads=[Bpp[4]], writes=[Bpp[5]])
        S.op("pe", L("matmul", psum[:, 7, :], lhsT=ones_f, rhs=osq, start=True, stop=True), reads=[Bpp[5], Bc], writes=[PB[7]])
        S.op("act", L("activation", out=rs, in_=psum[:, 7, :], func=AF.Sqrt, scale=1.0 / 128, bias=EPS), reads=[PB[7]], writes=[Bpp[6]])
        S.op("dve", L("reciprocal", out=rs, in_=rs), reads=[Bpp[6]], writes=[Bpp[6]])
        S.op("dve", L("scalar_tensor_tensor", out=yaT[:, h, qb * 512:(qb + 1) * 512], in0=ot, scalar=gsubp, in1=rs, op0=ALU.mult, op1=ALU.mult),
             reads=[Bpp[4], Bpp[6], Bvec], writes=[Bya])

    if stop_after == 2:
        return finish()
    NPW = 4
    PW = [alloc([128, 512], BF16) for _ in range(NPW)]; BPW = [Buf(f"PW{i}") for i in range(NPW)]
    zt = [alloc([128, 512]) for _ in range(2)]; Bzt = [Buf("zt0"), Buf("zt1")]
    WP = []
    for nb in range(16):
        for k, (kt, kind) in enumerate([(nb, "L"), (nb + 1, "C"), (nb + 2, "R"), (20, "X"), (21, "X")]):
            WP.append((nb, k, kt, kind))

    def emit_WS(p):
        nb, k, kt, kind = WP[p]
        kc0 = kt * 128; q0 = 128 + nb * 128
        for g in range(2):
            gs = slice(g * 64, (g + 1) * 64)
            sbk = 2 * (p % 2) + g
            S.op("pe", L("matmul", psum[:, sbk, :].rearrange("p (i q) -> p i q", q=128), lhsT=KbT[gs, kc0:kc0 + 128], rhs=QbT[gs, :, q0:q0 + 128],
                         start=True, stop=True), reads=[BKb, BQ], writes=[PB[sbk]])

    emit_WS(0); emit_WS(1)
    for p, (nb, k, kt, kind) in enumerate(WP):
        ob = 4 + 2 * (nb % 2)
        if kind == "L" and nb == 0:
            bias = wmask[:, 0:1]
        elif kind == "R" and nb == 15:
            bias = wmask[:, 1:2]
        else:
            bias = 0.0
        for g in range(2):
            sbk = 2 * (p % 2) + g; pi = 2 * (p % 2) + g
            S.op("act", L("activation", out=PW[pi], in_=psum[:, sbk, :], func=AF.Exp, scale=0.125, bias=bias),
                 reads=[PB[sbk], Bc], writes=[BPW[pi]])
            if kind in ("L", "R"):
                tri = triL if kind == "L" else triR
                pw3 = PW[pi].rearrange("p (i q) -> p i q", q=128)
                S.op("dve", L("tensor_tensor", out=pw3, in0=pw3, in1=tri.unsqueeze(1).to_broadcast([128, 4, 128]), op=ALU.mult),
                     reads=[BPW[pi], Bc], writes=[BPW[pi]])
        first = (k == 0); last = (k == 4)
        for g in range(2):
            gs = slice(g * 64, (g + 1) * 64); pi = 2 * (p % 2) + g
            S.op("pe", L("matmul", psum[gs, ob, :], lhsT=Vb[:, kt, gs], rhs=PW[pi], start=first, stop=last, tile_position=(0, g * 64)),
                 reads=[BVb, BPW[pi]], writes=[PB[ob]])
        for g in range(2):
            gs = slice(g * 64, (g + 1) * 64); pi = 2 * (p % 2) + g
            S.op("pe", L("matmul", psum[gs, ob + 1, :], lhsT=ones_b[:, 0:64], rhs=PW[pi], start=first, stop=last, tile_position=(0, g * 64)),
                 reads=[Bc, BPW[pi]], writes=[PB[ob + 1]])
        if p + 2 < len(WP):
            emit_WS(p + 2)
        if k == 4:
            zi = nb % 2
            z3 = zt[zi].rearrange("p (i q) -> p i q", q=128)
            S.op("dve", L("tensor_tensor", out=z3, in0=psum[:, ob + 1, :].rearrange("p (i q) -> p i q", q=128),
                          in1=sinkexp.unsqueeze(2).to_broadcast([128, 4, 128]), op=ALU.add), reads=[PB[ob + 1], Bvec], writes=[Bzt[zi]])
            S.op("dve", L("reciprocal", out=zt[zi], in_=zt[zi]), reads=[Bzt[zi]], writes=[Bzt[zi]])
            S.op("dve", L("tensor_tensor", out=ybT[:, :, nb * 128:(nb + 1) * 128], in0=psum[:, ob, :].rearrange("p (i q) -> p i q", q=128),
                          in1=z3, op=ALU.mult), reads=[PB[ob], Bzt[zi]], writes=[Byb])

    if dbg:
        S.dma("sp", dbg_kb, KbT, reads=[BKb])
        S.dma("sp", dbg_vb, Vb.rearrange("p a b -> p (a b)"), reads=[BVb])
        S.dma("sp", dbg_qb, QbT.rearrange("p a b -> p (a b)"), reads=[BQ])
        S.dma("sp", dbg_yb, ybT.rearrange("p a b -> p (a b)"), reads=[Byb])
    if stop_after == 3:
        return finish()
    S.barrier()
    off[0] = mark_att2
    wout = alloc([128, 8, D], BF16); Bwo = Buf("wout")
    wr_b = alloc([128, 8, NE], BF16); wr_f = alloc([128, 8, NE]); br_b = alloc([1, NE], BF16); br_f = alloc([1, NE])
    Bwr = Buf("wr")
    x4 = [alloc([128, D]) for _ in range(2)]; Bx4 = [Buf("x4a"), Buf("x4b")]
    tm4 = alloc([128, D]); Btm4 = Buf("tm4")
    x1t = [alloc([128, D]) for _ in range(2)]; Bx1 = [Buf("x1a"), Buf("x1b")]
    h2 = alloc([128, D]); Bh2 = Buf("h2")
    hx2all = alloc([128, 16, D], BF16); Bhx2t = [Buf(f"hx2_{i}") for i in range(16)]
    hx2T = alloc([128, 8, 128], BF16); Bhx2T = Buf("hx2T")
    s4 = [alloc([128, 4]) for _ in range(2)]; Bs4 = [Buf("s4a"), Buf("s4b")]
    lg = alloc([128, NE]); m8 = alloc([128, 8]); i8 = alloc([128, 8], U32); ekf = alloc([128, 8]); nm0 = alloc([128, 1])
    ge = alloc([128, 4]); gz = alloc([128, 1]); maskb = alloc([128, NE], BF16); destf = alloc([128, NE]); prod = alloc([128, NE])
    dk = alloc([128, 4]); Brr = Buf("rr")
    Bx1s = Buf("x1scr"); Bxes = [Buf(f"xes{i}") for i in range(64)]

    woutv = w_out.rearrange("(c p) n -> p c n", p=128)
    S.dma("pool", wout[:, 0:4, :], woutv[:, 0:4, :], writes=[Bwo])
    for gi in range(4):
        for g in range(2):
            r0 = 512 + g * 256 + gi * 64
            S.dma("pool", wout[g * 64:(g + 1) * 64, 4 + gi, :], w_out[r0:r0 + 64, :], writes=[Bwo])
    S.dma("sp", wr_f, w_r.rearrange("(c p) n -> p c n", p=128), writes=[Bwr])
    S.dma("sp", br_f, b_r.rearrange("(o n) -> o n", o=1), writes=[Bwr])
    S.op("dve", L("tensor_copy", out=wr_b, in_=wr_f), reads=[Bwr], writes=[Bwr])
    S.op("dve", L("tensor_copy", out=br_b, in_=br_f), reads=[Bwr], writes=[Bwr])

    lgs = [alloc([128, NE]) for _ in range(2)]; Blg = [Buf("lg0"), Buf("lg1")]

    def stageA(t):
        b = t % 2
        pb0 = 2 * b
        S.dma("sp", x4[b], xkv[128 + t * 128:128 + (t + 1) * 128, :], writes=[Bx4[b]])
        for hf in range(2):
            for c in range(8):
                lhs = yaT[:, c, t * 128:(t + 1) * 128] if c < 4 else ybT[:, c - 4, t * 128:(t + 1) * 128]
                S.op("pe", L("matmul", psum[:, pb0 + hf, :], lhsT=lhs, rhs=wout[:, c, hf * 512:(hf + 1) * 512],
                                                                             start=(c == 0), stop=(c == 7)),
                     reads=[Bya, Byb, Bwo], writes=[PB[pb0 + hf]])
        S.op("dve", L("tensor_tensor", out=tm4, in0=pflat(pb0, 2), in1=G1bc, op=ALU.mult), reads=[PB[pb0], PB[pb0 + 1], Bbc], writes=[Btm4])
        S.op("pool", L("tensor_tensor", out=x1t[b], in0=tm4, in1=x4[b], op=ALU.add), reads=[Btm4, Bx4[b]], writes=[Bx1[b]])
        S.dma("sp", x1scr[t * 128:(t + 1) * 128, :], x1t[b], reads=[Bx1[b]], writes=[Bx1s])
        if dbg:
            S.dma("sp", dbg_x1[t * 128:(t + 1) * 128, :], x1t[b], reads=[Bx1[b]])
        if not do_moe:
            return
        S.op("act", L("activation", out=junk, in_=x1t[b], func=AF.Square, accum_out=s4[b][:, 0:1]), reads=[Bx1[b]], writes=[Bjunk, Bs4[b]])
        S.op("act", L("activation", out=s4[b][:, 1:2], in_=s4[b][:, 0:1], func=AF.Sqrt, scale=1.0 / D, bias=EPS), reads=[Bs4[b]], writes=[Bs4[b]])
        S.op("dve", L("reciprocal", out=s4[b][:, 2:3], in_=s4[b][:, 1:2]), reads=[Bs4[b]], writes=[Bs4[b]])
        S.op("dve", L("scalar_tensor_tensor", out=h2, in0=x1t[b], scalar=s4[b][:, 2:3], in1=A2bc, op0=ALU.mult, op1=ALU.mult),
             reads=[Bx1[b], Bs4[b], Bbc], writes=[Bh2])
        S.op("pool", L("tensor_tensor", out=hx2all[:, t, :], in0=h2, in1=B2bc, op=ALU.add), reads=[Bh2, Bbc], writes=[Bhx2t[t]])
        for c in range(8):
            S.op("pe", L("transpose", out=pbf(4)[:, c, :], in_=hx2all[:, t, c * 128:(c + 1) * 128], identity=ident_b),
                 reads=[Bhx2t[t], Bc], writes=[PB[4]])
        S.op("act", L("activation", out=hx2T, in_=pbf(4), func=AF.Copy), reads=[PB[4]], writes=[Bhx2T])
        for c in range(8):
            S.op("pe", L("matmul", psum[:, 5, 0:NE], lhsT=hx2T[:, c, :], rhs=wr_b[:, c, :], start=(c == 0), stop=False),
                 reads=[Bhx2T, Bwr], writes=[PB[5]])
        S.op("pe", L("matmul", psum[:, 5, 0:NE], lhsT=ones_b[0:1, :], rhs=br_b, start=False, stop=True), reads=[Bwr, Bc], writes=[PB[5]])
        S.op("dve", L("tensor_copy", out=lgs[b], in_=psum[:, 5, 0:NE]), reads=[PB[5]], writes=[Blg[b]])

    def stageB(t):
        b = t % 2
        S.op("dve", L("max", out=m8, in_=lgs[b]), reads=[Brr, Blg[b]], writes=[Brr])
        S.op("dve", L("max_index", out=i8, in_max=m8, in_values=lgs[b]), reads=[Brr, Blg[b]], writes=[Brr])
        S.op("dve", L("tensor_copy", out=ek_all[:, t * 4:(t + 1) * 4], in_=i8[:, 0:4]), reads=[Brr], writes=[Brt])
        S.op("dve", L("tensor_scalar", out=nm0, in0=m8[:, 0:1], scalar1=-1.0, scalar2=None, op0=ALU.mult), reads=[Brr], writes=[Brr])
        S.op("act", L("activation", out=ge, in_=m8[:, 0:4], func=AF.Exp, bias=nm0, accum_out=gz), reads=[Brr], writes=[Brr])
        S.op("dve", L("reciprocal", out=gz, in_=gz), reads=[Brr], writes=[Brr])
        S.op("dve", L("tensor_scalar", out=gates[:, t * 4:(t + 1) * 4], in0=ge, scalar1=gz, scalar2=None, op0=ALU.mult), reads=[Brr], writes=[Brt])
        S.op("dve", L("tensor_scalar", out=maskb, in0=lgs[b], scalar1=m8[:, 3:4], scalar2=None, op0=ALU.is_ge), reads=[Brr, Blg[b]], writes=[Brr])
        S.op("pe", L("matmul", psum[:, 6, 0:NE], lhsT=U_b, rhs=maskb, start=True, stop=True), reads=[Brr, Bc], writes=[PB[6]])
        S.op("pe", L("matmul", psum[:, 7, 0:NE], lhsT=ones_b, rhs=maskb, start=True, stop=True), reads=[Brr, Bc], writes=[PB[7]])
        S.op("dve", L("tensor_tensor", out=destf, in0=psum[:, 6, 0:NE], in1=basecnt, op=ALU.add), reads=[PB[6], Brt], writes=[Brr])
        S.op("dve", L("tensor_tensor", out=basecnt, in0=psum[:, 7, 0:NE], in1=basecnt, op=ALU.add), reads=[PB[7], Brt], writes=[Brt])
        for k in range(4):
            col = t * 4 + k
            S.op("dve", L("scalar_tensor_tensor", out=prod, in0=iota_f, scalar=ek_all[:, col:col + 1], in1=destf, op0=ALU.is_equal, op1=ALU.mult,
                          accum_out=rk_all[:, col:col + 1]), reads=[Brr, Bc, Brt], writes=[Brr, Brt])

    stageA(0)
    for t in range(16):
        if t + 1 < 16:
            stageA(t + 1)
        if do_moe:
            stageB(t)

    if do_moe:
        cntcol = alloc([32, 1]); tmp32 = alloc([32, 32]); G32 = alloc([32, 32]); T32 = alloc([32, 32]); poscol = alloc([32, 1]); pos2 = alloc([32, 1])
        PmT = alloc([32, 32]); OFFpos = alloc([128, 32]); offk = alloc([128, 64]); dst64 = alloc([128, 64]); t1p = alloc([128, 32])
        carr = alloc([128, 8]); idxf = alloc([128, 32, 8])
        Bso = Buf("sort")
        S.op("dve", L("tensor_tensor", out=tmp32, in0=basecnt[0:32, :], in1=ident_f[0:32, 0:32], op=ALU.mult), reads=[Brt, Bc], writes=[Bso])
        S.op("dve", L("reduce_sum", out=cntcol, in_=tmp32, axis=AX.X), reads=[Bso], writes=[Bso])
        S.op("dve", L("tensor_scalar", out=G32, in0=basecnt[0:32, :], scalar1=cntcol, scalar2=None, op0=ALU.is_gt), reads=[Brt, Bso], writes=[Bso])
        S.op("dve", L("scalar_tensor_tensor", out=T32, in0=basecnt[0:32, :], scalar=cntcol, in1=Lmask, op0=ALU.is_equal, op1=ALU.mult),
             reads=[Brt, Bso, Bc], writes=[Bso])
        S.op("dve", L("tensor_tensor", out=G32, in0=G32, in1=T32, op=ALU.add), reads=[Bso], writes=[Bso])
        S.op("dve", L("reduce_sum", out=poscol, in_=G32, axis=AX.X), reads=[Bso], writes=[Bso])
        S.op("dve", L("tensor_scalar", out=Pm, in0=iota_f[0:32, :], scalar1=poscol, scalar2=None, op0=ALU.is_equal), reads=[Bso, Bc], writes=[Brt])
        S.op("pe", L("matmul", psum[0:32, 0, 0:32], lhsT=Pm, rhs=ident_f[0:32, 0:32], start=True, stop=True), reads=[Brt, Bc], writes=[PB[0]])
        S.op("dve", L("tensor_copy", out=PmT, in_=psum[0:32, 0, 0:32]), reads=[PB[0]], writes=[Bso])
        S.op("pe", L("matmul", psum[:, 1, 0:32], lhsT=OFFrep, rhs=PmT, start=True, stop=True), reads=[Bso, Bc], writes=[PB[1]])
        S.op("dve", L("tensor_scalar", out=OFFpos, in0=psum[:, 1, 0:32], scalar1=128.0, scalar2=None, op0=ALU.mult), reads=[PB[1]], writes=[Bso])
        S.op("pe", L("matmul", psum[:, 2, 0:32], lhsT=pidx[0:32, :], rhs=Pm, start=True, stop=True), reads=[Brt, Bc], writes=[PB[2]])
        S.op("dve", L("tensor_copy", out=permbc, in_=psum[:, 2, 0:32]), reads=[PB[2]], writes=[Brt])
        for col in range(64):
            S.op("dve", L("scalar_tensor_tensor", out=prod, in0=iota_f, scalar=ek_all[:, col:col + 1], in1=OFFpos, op0=ALU.is_equal, op1=ALU.mult,
                          accum_out=offk[:, col:col + 1]), reads=[Brt, Bc, Bso], writes=[Brr, Bso])
        S.op("dve", L("tensor_tensor", out=dst64, in0=offk, in1=rk_all, op=ALU.add), reads=[Bso, Brt], writes=[Bso])
        S.op("dve", L("tensor_copy", out=desti, in_=dst64), reads=[Bso], writes=[Brt])
        S.op("dve", L("scalar_tensor_tensor", out=t1p, in0=permbc, scalar=1024.0, in1=pidx[:, 0:32], op0=ALU.mult, op1=ALU.add), reads=[Brt, Bc], writes=[Bso])
        S.op("dve", L("tensor_scalar", out=carr, in0=iota_f[:, 0:8], scalar1=128.0, scalar2=None, op0=ALU.mult), reads=[Bc], writes=[Bso])
        S.op("dve", L("tensor_tensor", out=idxf, in0=t1p.unsqueeze(2).to_broadcast([128, 32, 8]), in1=carr.unsqueeze(1).to_broadcast([128, 32, 8]), op=ALU.add),
             reads=[Bso], writes=[Bso])
        S.op("dve", L("tensor_copy", out=idxg, in_=idxf.rearrange("p a b -> p (a b)")), reads=[Bso], writes=[Brt])
        S.op("dve", L("tensor_copy", out=bdn_idx, in_=permbc), reads=[Brt], writes=[Brt])
        for col in range(64):
            t = col // 4
            S.op("pool", L("indirect_dma_start", out=xe[:, :], out_offset=bass.IndirectOffsetOnAxis(ap=desti[:, col:col + 1], axis=0),
                           in_=hx2all[:, t, :], in_offset=None), reads=[Bhx2t[t], Brt] + Bxe0, writes=[Bxes[col]], dma=True)

    if not do_moe:
        lastd = [o for o in S.ops["sp"] if o.dma][-4:]
        S.op("sp", None, deps=[o for o in S.ops["sp"] if o.dma][-40:])
        S.emit(nc)
        return nc

    S.barrier()
    off[0] = mark_persist

    XC = 1024
    wgu = [alloc([128, 8, 2 * D], BF16) for _ in range(2)]; wdn = [alloc([128, 8, D], BF16) for _ in range(2)]
    Bwgu = [Buf("wgu0"), Buf("wgu1")]; Bwdn = [Buf("wdn0"), Buf("wdn1")]
    bdn1 = alloc([128, D]); Bbdn1 = Buf("bdn")
    bgu_f = alloc([NE, 2 * D]); biasT = alloc([128, 16, NE]); Bbias = Buf("bias")
    xet = [alloc([128, D], BF16) for _ in range(3)]; Bxet = [Buf(f"xet{i}") for i in range(3)]
    xeT = alloc([128, 8, XC], BF16); BxeT = Buf("xeT")
    aT = alloc([128, 8, XC], BF16); BaT = Buf("aT")
    g1 = [alloc([128, 512]) for _ in range(2)]; sgm = [alloc([128, 512]) for _ in range(2)]
    u1 = [alloc([128, 512]) for _ in range(2)]; gsx = [alloc([128, 512]) for _ in range(2)]
    Bg1 = [Buf("g1a"), Buf("g1b")]; Bsgm = [Buf("sga"), Buf("sgb")]; Bu1 = [Buf("u1a"), Buf("u1b")]; Bgsx = [Buf("gsa"), Buf("gsb")]
    yst = [alloc([128, D]) for _ in range(2)]; Byst = [Buf("yst0"), Buf("yst1")]

    S.dma("sp", bgu_f, b_gu, writes=[Bbias])
    for m in range(8):
        for two in range(2):
            j = m * 2 + two
            S.op("pe", L("matmul", psum[:, 0, j * NE:(j + 1) * NE], lhsT=bgu_f[:, 2 * m * 128 + two:2 * (m + 1) * 128:2],
                         rhs=Pm, start=True, stop=True), reads=[Bbias, Brt], writes=[PB[0]])
    S.op("dve", L("tensor_copy", out=biasT, in_=psum[:, 0, :].rearrange("p (j n) -> p j n", n=NE)), reads=[PB[0]], writes=[Bbias])
    S.op("dve", L("tensor_scalar", out=biasT[:, 1:16:2, :], in0=biasT[:, 1:16:2, :], scalar1=1.0, scalar2=None, op0=ALU.add), reads=[Bbias], writes=[Bbias])

    wgu_rows = w_gu.rearrange("e k n -> (e k) n"); wdn_rows = w_dn.rearrange("e k n -> (e k) n")
    IO = bass.IndirectOffsetOnAxis

    def load_w(i):
        b = i % 2
        for c in range(8):
            S.op("pool", L("indirect_dma_start", out=wgu[b][:, c, :], out_offset=None, in_=wgu_rows[:, :], in_offset=IO(ap=idxg[:, i * 8 + c:i * 8 + c + 1], axis=0)),
                 reads=[Brt], writes=[Bwgu[b]], dma=True)
        for c in range(8):
            S.op("pool", L("indirect_dma_start", out=wdn[b][:, c, :], out_offset=None, in_=wdn_rows[:, :], in_offset=IO(ap=idxg[:, i * 8 + c:i * 8 + c + 1], axis=0)),
                 reads=[Brt], writes=[Bwdn[b]], dma=True)

    def load_bdn(i):
        S.op("pool", L("indirect_dma_start", out=bdn1, out_offset=None, in_=b_dn[:, :], in_offset=IO(ap=bdn_idx[:, i:i + 1], axis=0)),
             reads=[Brt], writes=[Bbdn1], dma=True)

    work = []
    for i in range(NE):
        n = NT[i]; o = OFFT[i]; first = True
        while n > 0:
            k = min(XC // 128, n)
            work.append((i, o, k, first, n - k == 0)); o += k; n -= k; first = False

    load_w(0); load_bdn(0)
    pc5 = [0]; xc5 = [0]; yc5 = [0]

    def stage_x(w):
        i, o, k, first, lastw = w
        for s in range(k):
            xi = xc5[0] % 3; xc5[0] += 1
            tb_ = xi % 2
            S.dma("sp", xet[xi], xe[(o + s) * 128:(o + s + 1) * 128, :], reads=Bxes, writes=[Bxet[xi]])
            for c in range(8):
                S.op("pe", L("transpose", out=pbf(tb_)[:, c, :], in_=xet[xi][:, c * 128:(c + 1) * 128], identity=ident_b),
                     reads=[Bxet[xi], Bc], writes=[PB[tb_]])
            S.op("act", L("activation", out=xeT[:, :, s * 128:(s + 1) * 128], in_=pbf(tb_), func=AF.Copy), reads=[PB[tb_]], writes=[BxeT])

    stage_x(work[0])
    for wi, (i, o, k, first, lastw) in enumerate(work):
        b = i % 2
        if first and i + 1 < NE:
            load_w(i + 1)
        ncols = k * 128
        for m in range(8):
            for c0 in range(0, ncols, 512):
                nn = min(512, ncols - c0)
                ii = pc5[0] % 2; pc5[0] += 1
                bg_, bu_ = 2 + 2 * ii, 3 + 2 * ii
                cs = slice(c0, c0 + nn)
                for (bk, two) in ((bg_, 0), (bu_, 1)):
                    for c in range(8):
                        S.op("pe", L("matmul", psum[:, bk, 0:nn], lhsT=wgu[b][:, c, 2 * m * 128 + two:2 * (m + 1) * 128:2],
                                     rhs=xeT[:, c, cs], start=(c == 0), stop=(c == 7)),
                             reads=[Bwgu[b], BxeT], writes=[PB[bk]])
                S.op("dve", L("tensor_scalar", out=g1[ii][:, 0:nn], in0=psum[:, bg_, 0:nn], scalar1=biasT[:, 2 * m, i:i + 1], scalar2=7.0, op0=ALU.add, op1=ALU.min),
                     reads=[PB[bg_], Bbias], writes=[Bg1[ii]])
                S.op("act", L("activation", out=sgm[ii][:, 0:nn], in_=g1[ii][:, 0:nn], func=AF.Sigmoid, scale=1.702), reads=[Bg1[ii]], writes=[Bsgm[ii]])
                S.op("dve", L("tensor_scalar", out=u1[ii][:, 0:nn], in0=psum[:, bu_, 0:nn], scalar1=biasT[:, 2 * m + 1, i:i + 1], scalar2=8.0, op0=ALU.add, op1=ALU.min),
                     reads=[PB[bu_], Bbias], writes=[Bu1[ii]])
                S.op("pool", L("tensor_tensor", out=gsx[ii][:, 0:nn], in0=g1[ii][:, 0:nn], in1=sgm[ii][:, 0:nn], op=ALU.mult), reads=[Bg1[ii], Bsgm[ii]], writes=[Bgsx[ii]])
                S.op("dve", L("scalar_tensor_tensor", out=aT[:, m, cs], in0=u1[ii][:, 0:nn], scalar=-6.0, in1=gsx[ii][:, 0:nn], op0=ALU.max, op1=ALU.mult),
                     reads=[Bu1[ii], Bgsx[ii]], writes=[BaT])
        if wi + 1 < len(work):
            stage_x(work[wi + 1])
        for s in range(k):
            yb_ = yc5[0] % 2; yc5[0] += 1
            for hf in range(2):
                bk = 6 + hf
                for m in range(8):
                    S.op("pe", L("matmul", psum[:, bk, :], lhsT=aT[:, m, s * 128:(s + 1) * 128], rhs=wdn[b][:, m, hf * 512:(hf + 1) * 512],
                                 start=(m == 0), stop=(m == 7)),
                         reads=[BaT, Bwdn[b]], writes=[PB[bk]])
            S.op("dve", L("tensor_tensor", out=yst[yb_], in0=pflat(6, 2), in1=bdn1, op=ALU.add), reads=[PB[6], PB[7], Bbdn1], writes=[Byst[yb_]])
            S.dma("sp", ye[(o + s) * 128:(o + s + 1) * 128, :], yst[yb_], reads=[Byst[yb_]], writes=[Buf()])
        if lastw and i + 1 < NE:
            load_bdn(i + 1)

    S.barrier()
    off[0] = mark_persist

    yk = [alloc([128, D]) for _ in range(8)]; Byk = [Buf(f"yk{i}") for i in range(8)]
    x6 = [alloc([128, D]) for _ in range(2)]; Bx6 = [Buf("x6a"), Buf("x6b")]
    acc = [alloc([128, D]) for _ in range(2)]; Bacc = [Buf("acca"), Buf("accb")]
    acc2 = [alloc([128, D]) for _ in range(2)]; Bacc2 = [Buf("acc2a"), Buf("acc2b")]
    xo = [alloc([128, D]) for _ in range(2)]; Bxo = [Buf("xoa"), Buf("xob")]
    ot6 = [alloc([128, D]) for _ in range(2)]; Bot6 = [Buf("o6a"), Buf("o6b")]
    s6 = [alloc([128, 4]) for _ in range(2)]; Bs6 = [Buf("s6a"), Buf("s6b")]
    outs = []

    def loads6(t):
        b = t % 2
        S.dma("sp", x6[b], x1scr[t * 128:(t + 1) * 128, :], reads=[Bx1s], writes=[Bx6[b]])
        for k in range(4):
            col = t * 4 + k
            S.op("pool", L("indirect_dma_start", out=yk[4 * b + k], out_offset=None, in_=ye[:, :],
                           in_offset=bass.IndirectOffsetOnAxis(ap=desti[:, col:col + 1], axis=0)),
                 reads=[Brt], writes=[Byk[4 * b + k]], dma=True)

    loads6(0)
    for t in range(16):
        b = t % 2
        if t + 1 < 16:
            loads6(t + 1)
        S.op("act", L("activation", out=acc[b], in_=yk[4 * b], func=AF.Copy, scale=gates[:, t * 4:t * 4 + 1]), reads=[Byk[4 * b], Brt], writes=[Bacc[b]])
        for k in range(1, 4):
            S.op("dve", L("scalar_tensor_tensor", out=acc[b], in0=yk[4 * b + k], scalar=gates[:, t * 4 + k:t * 4 + k + 1], in1=acc[b], op0=ALU.mult, op1=ALU.add),
                 reads=[Byk[4 * b + k], Brt, Bacc[b]], writes=[Bacc[b]])
        S.op("dve", L("tensor_tensor", out=acc2[b], in0=acc[b], in1=G2bc, op=ALU.mult), reads=[Bacc[b], Bbc], writes=[Bacc2[b]])
        S.op("pool", L("tensor_tensor", out=xo[b], in0=acc2[b], in1=x6[b], op=ALU.add), reads=[Bacc2[b], Bx6[b]], writes=[Bxo[b]])
        S.op("act", L("activation", out=junk, in_=xo[b], func=AF.Square, accum_out=s6[b][:, 0:1]), reads=[Bxo[b]], writes=[Bjunk, Bs6[b]])
        S.op("act", L("activation", out=s6[b][:, 1:2], in_=s6[b][:, 0:1], func=AF.Sqrt, scale=1.0 / D, bias=EPS), reads=[Bs6[b]], writes=[Bs6[b]])
        S.op("dve", L("reciprocal", out=s6[b][:, 2:3], in_=s6[b][:, 1:2]), reads=[Bs6[b]], writes=[Bs6[b]])
        S.op("dve", L("scalar_tensor_tensor", out=ot6[b], in0=xo[b], scalar=s6[b][:, 2:3], in1=FGbc, op0=ALU.mult, op1=ALU.mult),
             reads=[Bxo[b], Bs6[b], Bbc], writes=[Bot6[b]])
        outs.append(S.dma("sp", outd[t * 128:(t + 1) * 128, :], ot6[b], reads=[Bot6[b]]))
    S.op("sp", None, deps=outs)
    S.emit(nc)
    return nc


def _rope_tables(pos):
    p = np.arange(128); d = p % 64; f = d % 16
    inv = (np.float32(10000.0) ** (-(np.arange(16, dtype=np.float32)) / np.float32(16))).astype(np.float32)
    row = (pos // 64).astype(np.float32); col = (pos % 64).astype(np.float32)
    coord = np.where((d < 32)[:, None], row[None, :], col[None, :]).astype(np.float32)
    ang = (coord * inv[f][:, None]).astype(np.float32)
    return np.cos(ang).astype(np.float32), np.sin(ang).astype(np.float32)


def _consts():
    c = np.zeros((128, 9, 128), np.float32)
    c[:, 0, :] = np.eye(128, dtype=np.float32)
    R = np.zeros((128, 128), np.float32)
    for m in range(128):
        if (m % 32) < 16: R[m, m + 16] = -1.0
        else: R[m, m - 16] = 1.0
    c[:, 1, :] = R.T
    j = np.arange(128)[:, None]; i = np.arange(128)[None, :]
    c[:, 2, :] = (j < i).astype(np.float32)
    c[:, 3, :] = (j >= i).astype(np.float32)
    c[:, 4, :] = (j <= i).astype(np.float32)
    c[:, 5, :] = np.arange(128, dtype=np.float32)[None, :]
    c[:, 6, :] = np.arange(128, dtype=np.float32)[:, None]
    c[0:NE, 7, :] = np.asarray(OFFT, np.float32)[:, None]
    c[:, 8, :] = (i < j).astype(np.float32)
    return c.reshape(128, 9 * 128)


def make_in_maps(inp, do_moe=True):
    x = np.asarray(inp["x"], np.float32); ctx = np.asarray(inp["ctx"], np.float32)
    c = np.asarray(inp["c"], np.float32); c_ctx = np.asarray(inp["c_ctx"], np.float32)
    consts = _consts()
    lam = np.stack([np.asarray(inp[k], np.float32)[0] for k in ("lam_q1", "lam_k1", "lam_q2", "lam_k2")], 0)
    shared = {
        "consts": consts, "w_mod": np.ascontiguousarray(inp["w_mod"][0]), "b_mod": np.ascontiguousarray(inp["b_mod"][0]),
        "norm1_g": np.ascontiguousarray(inp["norm1_g"][0]), "w_in": np.ascontiguousarray(inp["w_in"][0]), "lam": np.ascontiguousarray(lam),
        "subln_g": np.ascontiguousarray(inp["subln_g"][0]), "sink": np.ascontiguousarray(inp["sink"][0]),
        "w_out": np.ascontiguousarray(inp["w_out"][0]), "norm2_g": np.ascontiguousarray(inp["norm2_g"][0]),
        "w_router": np.ascontiguousarray(inp["w_router"][0]), "b_router": np.ascontiguousarray(inp["b_router"][0]),
        "final_g": np.ascontiguousarray(inp["final_g"]),
    }
    if do_moe:
        shared.update({"w_gate_up": np.ascontiguousarray(inp["w_gate_up"][0]), "b_gate_up": np.ascontiguousarray(inp["b_gate_up"][0]),
                       "w_down": np.ascontiguousarray(inp["w_down"][0]), "b_down": np.ascontiguousarray(inp["b_down"][0])})
    shared = {k: np.asarray(v, np.float32) for k, v in shared.items()}
    maps = []
    for core in range(8):
        b, j = core // 4, core % 4
        qs = j * NQ
        shift = qs - 128
        pos = (np.arange(S_LEN) + shift) % S_LEN
        cos, sin = _rope_tables(pos)
        cosT = np.concatenate([cos, np.ones((128, CTX), np.float32)], 1)
        sinT = np.concatenate([sin, np.zeros((128, CTX), np.float32)], 1)
        wmask = np.zeros((128, 2), np.float32)
        if j == 0: wmask[:, 0] = -1e30
        if j == 3: wmask[:, 1] = -1e30
        m = dict(shared)
        m.update({"xkv": np.ascontiguousarray(np.roll(x[b], -shift, axis=0)), "ctx": np.ascontiguousarray(ctx[b]),
                  "cvec": np.ascontiguousarray(np.stack([c[b], c_ctx], 0)), "cosT": cosT, "sinT": sinT, "wmask": wmask})
        maps.append(m)
    return maps


_NC_CACHE = {}


def kernel(**inputs):
    if "nc" not in _NC_CACHE:
        _NC_CACHE["nc"] = build_program(do_moe=True)
    nc = _NC_CACHE["nc"]
    maps = make_in_maps(inputs, do_moe=True)
    res = run_bass_kernel_spmd(nc, maps, core_ids=list(range(8)))
    out = np.empty((2, S_LEN, D), np.float32)
    for core in range(8):
        b, j = core // 4, core % 4
        out[b, j * NQ:(j + 1) * NQ, :] = res.results[core]["out"]
    return out
```

```python
import contextlib
import numpy as np
import concourse.bass as bass
import concourse.mybir as mybir
from concourse.bass_utils import run_bass_kernel_spmd

F32 = mybir.dt.float32; BF16 = mybir.dt.bfloat16; I32 = mybir.dt.int32; U8 = mybir.dt.uint8
U32 = mybir.dt.uint32
ALU = mybir.AluOpType; AF = mybir.ActivationFunctionType
AX = mybir.AxisListType
ENGS = ("pe", "act", "dve", "pool", "sp")
NDMASEM = 12

D = 1024; S_LEN = 8192; CTX = 256; NQ = 2048; NE = 32;
NT = [-(-min(2048, -(-8192 // (i + 1))) // 128) for i in range(NE)]
OFFT = [sum(NT[:i]) for i in range(NE)]
NSLOT = sum(NT) * 128; NSLOTP = NSLOT
QA, KA, VA, QB, KB, VB = 0, 512, 1024, 1536, 2048, 2176
NKT = 66
WROWS = 2560
EPS = 1e-6
LAM_INIT = 0.2


def L(method, *args, **kw):
    return lambda e: getattr(e, method)(*args, **kw)


class Buf:
    __slots__ = ("name", "w", "r", "excl")
    def __init__(self, name="", excl=False):
        self.name = name; self.w = None; self.r = []; self.excl = excl


class Op:
    __slots__ = ("eng", "fn", "deps", "dma", "signal", "sem", "val", "idx", "name")
    def __init__(self, eng, fn, dma, name):
        self.eng = eng; self.fn = fn; self.deps = []; self.dma = dma
        self.signal = False; self.sem = None; self.val = 0; self.idx = 0; self.name = name


class Sched:
    def __init__(self):
        self.ops = {e: [] for e in ENGS}
        self.all = []

    def op(self, eng, fn, reads=(), writes=(), dma=False, deps=(), name=""):
        o = Op(eng, fn, dma, name)
        ds = {}
        ex = [b for b in reads if b.excl]
        if ex:
            reads = [b for b in reads if not b.excl]; writes = list(writes) + ex
        for b in reads:
            if b.w is not None: ds[id(b.w)] = b.w
        for b in writes:
            if b.w is not None: ds[id(b.w)] = b.w
            lastr = {}
            for r in b.r:
                if r.dma: ds[id(r)] = r
                elif r.eng not in lastr or r.idx > lastr[r.eng].idx: lastr[r.eng] = r
            for r in lastr.values(): ds[id(r)] = r
        for d in deps: ds[id(d)] = d
        o.deps = list(ds.values())
        for b in reads: b.r.append(o)
        for b in writes:
            b.w = o; b.r = []
        o.idx = len(self.ops[eng]); self.ops[eng].append(o); self.all.append(o)
        return o

    def dma(self, eng, out, in_, reads=(), writes=(), name="", **kw):
        return self.op(eng, L("dma_start", out=out, in_=in_, **kw), reads, writes, dma=True, name=name)

    def barrier(self):
        lasts = []
        for e in ENGS:
            real = [o for o in self.ops[e] if o.fn is not None]
            comp = [o for o in real if not o.dma]
            if comp: lasts.append(comp[-1])
            lasts.extend([o for o in real if o.dma][-NDMASEM:])
        for e in ENGS:
            self.op(e, None, deps=lasts, name="barrier")

    def emit(self, nc):
        for o in self.all:
            for d in o.deps:
                if d.eng == "pe" and o.eng == "pe" and not d.dma:
                    continue
                d.signal = True
        with contextlib.ExitStack() as st:
            csem = {e: st.enter_context(nc.semaphore("c_" + e)) for e in ENGS}
            dsem = {e: [st.enter_context(nc.semaphore(f"d_{e}{i}")) for i in range(NDMASEM)]
                    for e in ("sp", "pool", "act")}
            for e in ENGS:
                cnt = 0; di = 0; dcnt = [0] * NDMASEM; dlast = [None] * NDMASEM
                for o in self.ops[e]:
                    if o.fn is None: continue
                    if o.dma:
                        o.signal = True
                        s = di % NDMASEM; di += 1
                        if dlast[s] is not None:
                            o.deps.append(dlast[s])
                        dcnt[s] += 16; o.sem = dsem[e][s]; o.val = dcnt[s]; dlast[s] = o
                    elif o.signal:
                        cnt += 1; o.sem = csem[e]; o.val = cnt
            blk = st.enter_context(nc.Block())
            engobj = {"pe": "tensor", "act": "scalar", "dve": "vector", "pool": "gpsimd", "sp": "sync"}

            def mk(e):
                def body(eng):
                    seen = {}
                    for o in self.ops[e]:
                        need = {}
                        for d in o.deps:
                            if d.fn is None: continue
                            if d.eng == "pe" and e == "pe" and not d.dma: continue
                            k = id(d.sem)
                            if seen.get(k, 0) >= d.val: continue
                            if k not in need or need[k][1] < d.val: need[k] = (d.sem, d.val)
                        for k, (s, v) in need.items():
                            eng.wait_ge(s, v); seen[k] = v
                        if o.fn is None: continue
                        inst = o.fn(eng)
                        if o.signal:
                            inst.then_inc(o.sem, 16 if o.dma else 1)
                return body
            for e in ENGS:
                if self.ops[e]:
                    getattr(blk, engobj[e])(mk(e))


def build_program(do_moe=True, dbg=False, stop_after=99):
    nc = bass.Bass("TRN2", target_bir_lowering=False)
    dt_ = nc.dram_tensor

    def din(name, shape, dt=F32):
        return dt_(name, list(shape), dt, kind="ExternalInput").ap()

    xkv = din("xkv", [S_LEN, D]); ctxd = din("ctx", [CTX, D]); cvec = din("cvec", [2, D])
    cosd = din("cosT", [128, S_LEN + CTX]); sind = din("sinT", [128, S_LEN + CTX])
    wmaskd = din("wmask", [128, 2]); constd = din("consts", [128, 9 * 128])
    w_mod = din("w_mod", [D, 6 * D]); b_mod = din("b_mod", [6 * D]); n1g = din("norm1_g", [D])
    w_in = din("w_in", [D, 2304]); lamd = din("lam", [4, 64]); sublnd = din("subln_g", [128])
    sinkd = din("sink", [8]); w_out = din("w_out", [D, D]); n2g = din("norm2_g", [D])
    w_r = din("w_router", [D, NE]); b_r = din("b_router", [NE]); fgd = din("final_g", [D])
    if do_moe:
        w_gu = din("w_gate_up", [NE, D, 2 * D]); b_gu = din("b_gate_up", [NE, 2 * D])
        w_dn = din("w_down", [NE, D, D]); b_dn = din("b_down", [NE, D])
    outd = dt_("out", [NQ, D], F32, kind="ExternalOutput").ap()
    kscr = dt_("kscr", [4, 9, 128, 1024], BF16).ap()
    vscr = dt_("vscr", [4, 9, 128, 8, 128], BF16).ap()
    x1scr = dt_("x1scr", [NQ, D], F32).ap()
    xe = dt_("xe", [NSLOTP, D], BF16).ap()
    ye = dt_("ye", [NSLOTP, D], F32).ap()
    if dbg:
        dbg_x1 = dt_("dbg_x1", [NQ, D], F32, kind="ExternalOutput").ap()
        dbg_yb = dt_("dbg_yb", [128, 4 * NQ], BF16, kind="ExternalOutput").ap()
        dbg_kb = dt_("dbg_kb", [128, WROWS + CTX], BF16, kind="ExternalOutput").ap()
        dbg_vb = dt_("dbg_vb", [128, 22 * 128], BF16, kind="ExternalOutput").ap()
        dbg_qb = dt_("dbg_qb", [128, 4 * WROWS], BF16, kind="ExternalOutput").ap()

    TOT = 206 * 1024
    arena = nc.alloc_sbuf_tensor("arena", [128, TOT], U8)
    psum = nc.alloc_psum_tensor("psum", [128, 8, 512], F32)
    off = [0]

    def alloc(shape, dt=F32):
        n = int(np.prod(shape[1:])) * (2 if dt == BF16 else 4)
        a = arena[0:shape[0], off[0]:off[0] + n].bitcast(dt)
        off[0] += (n + 63) // 64 * 64
        assert off[0] <= TOT, ("SBUF overflow", off[0])
        if len(shape) == 3:
            a = a.rearrange("p (a b) -> p a b", b=shape[2])
        elif len(shape) == 4:
            a = a.rearrange("p (a b c) -> p a b c", b=shape[2], c=shape[3])
        return a

    def pbank(b, n=1):
        return psum[:, b:b + n, :]

    def pflat(b, n=1):
        return psum[:, b:b + n, :].rearrange("p a b -> p (a b)")

    def pbf(b):
        return psum[:, b, :].bitcast(BF16).rearrange("p (a b) -> p a b", b=128)

    S = Sched()

    def finish():
        S.barrier()
        S.emit(nc)
        return nc
    PB = [Buf(f"psum{i}", excl=True) for i in range(8)]

    cst = alloc([128, 9, 128], F32)
    ident_f = cst[:, 0, :]; iota_f = cst[:, 5, 0:32]; pidx = cst[:, 6, :]; OFFrep = cst[0:32, 7, :]; Lmask = cst[0:32, 8, 0:32]
    cstb = alloc([128, 5, 128], BF16)
    ident_b = cstb[:, 0, :]; RT_b = cstb[:, 1, :]; U_b = cstb[:, 2, :]; triL = cstb[:, 3, :]; triR = cstb[:, 4, :]
    ones_b = alloc([128, 128], BF16); ones_f = alloc([128, 128], F32)
    A1T = alloc([128, 8]); B1T = alloc([128, 8]); A1cT = alloc([128, 8]); B1cT = alloc([128, 8])
    gsubp = alloc([128, 1]); neglam = alloc([128, 1]); sinkexp = alloc([128, 4]); wmask = alloc([128, 2])
    G1bc = alloc([128, D]); A2bc = alloc([128, D]); B2bc = alloc([128, D]); G2bc = alloc([128, D]); FGbc = alloc([128, D])
    desti = alloc([128, 64], I32); gates = alloc([128, 64]); basecnt = alloc([128, 32])
    ek_all = alloc([128, 64]); rk_all = alloc([128, 64]); Pm = alloc([32, 32]); permbc = alloc([128, 32])
    idxg = alloc([128, 32 * 8], I32); bdn_idx = alloc([128, 32], I32)
    junk = alloc([128, D], BF16); Bjunk = Buf("junk")
    Bc = Buf("consts"); Bvec = Buf("vecs"); Bbc = Buf("bcast"); Brt = Buf("route")

    S.dma("sp", cst.rearrange("p a b -> p (a b)"), constd, writes=[Bc])
    S.dma("sp", wmask, wmaskd, writes=[Bc])
    S.op("dve", L("tensor_copy", out=cstb, in_=cst[:, 0:5, :]), reads=[Bc], writes=[Bc])
    S.op("dve", L("memset", ones_b, 1.0), writes=[Bc])
    S.op("dve", L("memset", ones_f, 1.0), writes=[Bc])
    S.op("dve", L("memset", basecnt, 0.0), writes=[Brt])

    mark_persist = off[0]

    cT = alloc([128, 8, 2]); sg = alloc([128, 8, 2]); scT = alloc([128, 8, 2]); scT_b = alloc([128, 8, 2], BF16)
    screp = alloc([128, 8, 128], BF16)
    bmT = alloc([128, 16]); g1nT = alloc([128, 8]); modT = alloc([128, 16, 2])
    bmbc = alloc([128, 4 * D]); n2bc = alloc([128, D])
    lamt = alloc([1, 4, 64]); lamp = alloc([1, 2, 64]); lams = alloc([1, 2]); lamv = alloc([1, 2])
    wm = [alloc([128, 8, 1024], BF16) for _ in range(2)]
    Bwm = [Buf("wm0"), Buf("wm1")]; Bp0 = Buf("p0")

    for r_ in range(2):
        S.dma("sp", cT[:, :, r_], cvec[r_].rearrange("(c p) -> p c", p=128), writes=[Bp0], allow_slow_non_contiguous=True)
    S.dma("sp", bmT, b_mod[0:2048].rearrange("(c p) -> p c", p=128), writes=[Bp0], allow_slow_non_contiguous=True)
    S.dma("sp", g1nT, n1g.rearrange("(c p) -> p c", p=128), writes=[Bp0], allow_slow_non_contiguous=True)
    S.dma("sp", bmbc, b_mod[2048:6144].partition_broadcast(128), writes=[Bp0])
    S.dma("sp", n2bc, n2g.partition_broadcast(128), writes=[Bp0])
    S.dma("sp", FGbc, fgd.partition_broadcast(128), writes=[Bbc])
    S.dma("sp", gsubp, sublnd.rearrange("(p o) -> p o", o=1), writes=[Bvec])
    S.dma("sp", lamt.rearrange("p a b -> p (a b)"), lamd.rearrange("(o a) b -> o (a b)", o=1), writes=[Bp0])
    for g in range(2):
        S.dma("sp", sinkexp[g * 64:(g + 1) * 64, :], sinkd[g * 4:(g + 1) * 4].partition_broadcast(64), writes=[Bvec])
    wmv = w_mod.rearrange("(c p) n -> p c n", p=128)
    for i in range(2):
        S.dma("pool", wm[i], wmv[:, :, i * 1024:(i + 1) * 1024], writes=[Bwm[i]])

    S.op("act", L("activation", out=sg, in_=cT, func=AF.Sigmoid), reads=[Bp0], writes=[Bp0])
    S.op("dve", L("tensor_tensor", out=scT, in0=cT, in1=sg, op=ALU.mult), reads=[Bp0], writes=[Bp0])
    S.op("dve", L("tensor_copy", out=scT_b, in_=scT), reads=[Bp0], writes=[Bp0])
    S.op("dve", L("tensor_copy", out=screp, in_=scT[:, :, 0:1].to_broadcast([128, 8, 128])), reads=[Bp0], writes=[Bp0])
    S.op("dve", L("tensor_scalar", out=gsubp, in0=gsubp, scalar1=1.0 - LAM_INIT, scalar2=None, op0=ALU.mult),
         reads=[Bvec], writes=[Bvec])
    S.op("act", L("activation", out=sinkexp, in_=sinkexp, func=AF.Exp), reads=[Bvec], writes=[Bvec])
    S.op("dve", L("tensor_tensor", out=lamp, in0=lamt[:, 0:4:2, :], in1=lamt[:, 1:4:2, :], op=ALU.mult), reads=[Bp0], writes=[Bp0])
    S.op("dve", L("reduce_sum", out=lams, in_=lamp, axis=AX.X), reads=[Bp0], writes=[Bp0])
    S.op("act", L("activation", out=lamv, in_=lams, func=AF.Exp), reads=[Bp0], writes=[Bp0])
    S.op("dve", L("tensor_tensor", out=lamv[:, 0:1], in0=lamv[:, 1:2], in1=lamv[:, 0:1], op=ALU.subtract), reads=[Bp0], writes=[Bp0])
    S.op("dve", L("tensor_scalar", out=lamv[:, 0:1], in0=lamv[:, 0:1], scalar1=-LAM_INIT, scalar2=None, op0=ALU.add), reads=[Bp0], writes=[Bp0])
    S.op("pe", L("matmul", psum[:, 7, 0:1], lhsT=ones_f[0:1, :], rhs=lamv[:, 0:1], start=True, stop=True),
         reads=[Bp0, Bc], writes=[PB[7]])
    S.op("dve", L("tensor_copy", out=neglam, in_=psum[:, 7, 0:1]), reads=[PB[7]], writes=[Bvec])

    pm = psum[:, 0, 0:32].rearrange("p (a b) -> p a b", b=2)
    for blk in range(16):
        i = blk // 8
        for c in range(8):
            S.op("pe", L("matmul", pm[:, blk, :], lhsT=wm[i][:, c, (blk % 8) * 128:(blk % 8 + 1) * 128],
                                                              rhs=scT_b[:, c, :], start=(c == 0), stop=(c == 7)),
                 reads=[Bwm[i], Bp0], writes=[PB[0]])
    S.op("dve", L("tensor_tensor", out=modT, in0=pm, in1=bmT.unsqueeze(2).to_broadcast([128, 16, 2]), op=ALU.add),
         reads=[PB[0], Bp0], writes=[Bp0])
    for (AT, BT, r) in ((A1T, B1T, 0), (A1cT, B1cT, 1)):
        S.op("dve", L("scalar_tensor_tensor", out=AT, in0=modT[:, 8:16, r], scalar=1.0, in1=g1nT, op0=ALU.add, op1=ALU.mult),
             reads=[Bp0], writes=[Bvec])
        S.op("dve", L("tensor_copy", out=BT, in_=modT[:, 0:8, r]), reads=[Bp0], writes=[Bvec])
    dsts = [G1bc, B2bc, A2bc, G2bc]
    for q in range(4):
        i = q % 2
        S.dma("pool", wm[i], wmv[:, :, (2 + q) * 1024:(3 + q) * 1024], writes=[Bwm[i]])
        for hf in range(2):
            for c in range(8):
                S.op("pe", L("matmul", psum[:, 1 + hf, :], lhsT=screp[:, c, :], rhs=wm[i][:, c, hf * 512:(hf + 1) * 512],
                                                                start=(c == 0), stop=(c == 7)),
                     reads=[Bwm[i], Bp0], writes=[PB[1 + hf]])
        S.op("dve", L("tensor_tensor", out=dsts[q], in0=pflat(1, 2), in1=bmbc[:, q * 1024:(q + 1) * 1024], op=ALU.add),
             reads=[PB[1], PB[2], Bp0], writes=[Bbc])
    S.op("dve", L("scalar_tensor_tensor", out=A2bc, in0=A2bc, scalar=1.0, in1=n2bc, op0=ALU.add, op1=ALU.mult),
         reads=[Bbc, Bp0], writes=[Bbc])

    if stop_after == 0:
        return finish()
    S.barrier()
    off[0] = mark_persist

    QaT = alloc([128, 4, WROWS], BF16); QbT = alloc([128, 4, WROWS], BF16)
    KbT = alloc([128, WROWS + CTX], BF16); Vb = alloc([128, 22, 128], BF16)
    BQ = Buf("Q"); BKb = Buf("Kb"); BVb = Buf("Vb"); Bya = Buf("ya"); Byb = Buf("yb")
    Bks = Buf("kscr"); Bvs = Buf("vscr")
    mark_attn = off[0]

    win = alloc([128, 8, 2304], BF16); Bwin = Buf("win")
    xt = [alloc([128, D]) for _ in range(2)]; Bxt = [Buf("xt0"), Buf("xt1")]
    xn = [alloc([128, D], BF16) for _ in range(4)]; Bxn = [Buf(f"xn{i}") for i in range(4)]
    st4 = [alloc([128, 4]) for _ in range(4)]; Bst = [Buf(f"st{i}") for i in range(4)]
    evt = [alloc([128, 8, 128]) for _ in range(2)]; Bev = [Buf("ev0"), Buf("ev1")]
    hxT = [alloc([128, 8, 512], BF16) for _ in range(2)]; Bhx = [Buf("hx0"), Buf("hx1")]
    cosg = [alloc([128, 512]) for _ in range(2)]; sing = [alloc([128, 512]) for _ in range(2)]; Btab = [Buf("tab0"), Buf("tab1")]
    kbf = [alloc([128, 512], BF16) for _ in range(2)]; Bkbf = [Buf("kbf0"), Buf("kbf1")]
    rt1 = [alloc([128, 512]) for _ in range(2)]; rt2 = [alloc([128, 512]) for _ in range(2)]
    Brt1 = [Buf("rt1a"), Buf("rt1b")]; Brt2 = [Buf("rt2a"), Buf("rt2b")]
    kst = [alloc([128, 4, 512], BF16) for _ in range(2)]; Bkst = [Buf("kst0"), Buf("kst1")]
    vst = [alloc([128, 4, 4, 128], BF16) for _ in range(2)]; Bvst = [Buf("vst0"), Buf("vst1")]

    winv = w_in.rearrange("(c p) n -> p c n", p=128)
    S.dma("pool", win[:, :, 0:QB], winv[:, :, 0:QB], writes=[Bwin])
    S.dma("pool", win[:, :, KB:2304], winv[:, :, KB:2304], writes=[Bwin])
    for gi in range(4):
        for g in range(2):
            S.dma("pool", win[:, :, QB + gi * 128 + g * 64:QB + gi * 128 + (g + 1) * 64],
                  winv[:, :, QB + g * 256 + gi * 64:QB + g * 256 + (gi + 1) * 64], writes=[Bwin])
    import os
    rr = [0]

    pcyc = [0]

    def nextbank():
        b = 2 + pcyc[0] % 4; pcyc[0] += 1
        return b

    def projA(job, hb, tb):
        colbase, n, dst, wb = job
        i = rr[0] % 2; rr[0] += 1
        bk = nextbank()
        for c in range(8):
            S.op("pe", L("matmul", psum[:, bk, 0:n], lhsT=win[:, c, colbase:colbase + 128], rhs=hxT[hb][:, c, 0:n], start=(c == 0), stop=(c == 7)),
                 reads=[Bwin, Bhx[hb]], writes=[PB[bk]])
        S.op("act", L("activation", out=kbf[i][:, 0:n], in_=psum[:, bk, 0:n], func=AF.Copy), reads=[PB[bk]], writes=[Bkbf[i]])
        return (bk, i, n, dst, wb, tb)

    def ropeB(st):
        bk, i, n, dst, wb, tb = st
        S.op("pe", L("matmul", psum[:, 6 + i, 0:n], lhsT=RT_b, rhs=kbf[i][:, 0:n], start=True, stop=True), reads=[Bkbf[i], Bc], writes=[PB[6 + i]])
        S.op("dve", L("tensor_tensor", out=rt1[i][:, 0:n], in0=psum[:, bk, 0:n], in1=cosg[tb][:, 0:n], op=ALU.mult),
             reads=[PB[bk], Btab[tb]], writes=[Brt1[i]])
        S.op("dve", L("tensor_tensor", out=rt2[i][:, 0:n], in0=psum[:, 6 + i, 0:n], in1=sing[tb][:, 0:n], op=ALU.mult),
             reads=[PB[6 + i], Btab[tb]], writes=[Brt2[i]])
        S.op("pool", L("tensor_tensor", out=dst, in0=rt1[i][:, 0:n], in1=rt2[i][:, 0:n], op=ALU.add), reads=[Brt1[i], Brt2[i]], writes=wb)

    for G in range(17):
        ntile = 4 if G < 16 else 2
        n = ntile * 128
        hb = G % 2
        AT, BT = (A1T, B1T) if G < 16 else (A1cT, B1cT)
        tb = G % 2
        S.dma("sp", cosg[tb][:, 0:n], cosd[:, G * 512:G * 512 + n], writes=[Btab[tb]])
        S.dma("sp", sing[tb][:, 0:n], sind[:, G * 512:G * 512 + n], writes=[Btab[tb]])
        for t in range(ntile):
            gt = G * 4 + t
            xb = gt % 2; x4i = gt % 4
            src = xkv[gt * 128:(gt + 1) * 128, :] if G < 16 else ctxd[t * 128:(t + 1) * 128, :]
            S.dma("sp", xt[xb], src, writes=[Bxt[xb]])
            S.op("act", L("activation", out=junk, in_=xt[xb], func=AF.Square, accum_out=st4[x4i][:, 0:1]),
                 reads=[Bxt[xb]], writes=[Bjunk, Bst[x4i]])
            S.op("act", L("activation", out=st4[x4i][:, 1:2], in_=st4[x4i][:, 0:1], func=AF.Sqrt, scale=1.0 / D, bias=EPS),
                 reads=[Bst[x4i]], writes=[Bst[x4i]])
            S.op("dve", L("reciprocal", out=st4[x4i][:, 2:3], in_=st4[x4i][:, 1:2]), reads=[Bst[x4i]], writes=[Bst[x4i]])
            S.op("act", L("activation", out=xn[x4i], in_=xt[xb], func=AF.Copy, scale=st4[x4i][:, 2:3]),
                 reads=[Bxt[xb], Bst[x4i]], writes=[Bxn[x4i]])
            tbk = xb
            for c in range(8):
                S.op("pe", L("transpose", out=pbf(tbk)[:, c, :], in_=xn[x4i][:, c * 128:(c + 1) * 128], identity=ident_b),
                     reads=[Bxn[x4i], Bc], writes=[PB[tbk]])
            S.op("dve", L("tensor_tensor", out=evt[xb], in0=pbf(tbk), in1=AT.unsqueeze(2).to_broadcast([128, 8, 128]), op=ALU.mult),
                 reads=[PB[tbk], Bvec], writes=[Bev[xb]])
            S.op("pool", L("tensor_tensor", out=hxT[hb][:, :, t * 128:(t + 1) * 128], in0=evt[xb],
                           in1=BT.unsqueeze(2).to_broadcast([128, 8, 128]), op=ALU.add), reads=[Bev[xb], Bvec], writes=[Bhx[hb]])
        ch = G // 2 if G < 16 else 8
        ko = (G % 2) * 512 if G < 16 else 0
        sb = G % 2
        jobs = [(KA + h * 128, n, kst[sb][:, h, 0:n], [Bkst[sb]]) for h in range(4)]
        if G < 5 or G == 16:
            wo = G * 512 if G < 16 else WROWS
            jobs.append((KB, n, KbT[:, wo:wo + n], [BKb]))
        if G < 5:
            for (base, dstT) in ((QA, QaT), (QB, QbT)):
                for h in range(4):
                    jobs.append((base + h * 128, 512, dstT[:, h, G * 512:(G + 1) * 512], [BQ]))
        st_ = projA(jobs[0], hb, tb)
        for ji in range(len(jobs)):
            nxt = projA(jobs[ji + 1], hb, tb) if ji + 1 < len(jobs) else None
            ropeB(st_)
            st_ = nxt
            if ji == 3:
                for h in range(4):
                    S.dma("sp", kscr[h, ch, :, ko:ko + n], kst[sb][:, h, 0:n], reads=[Bkst[sb]], writes=[Buf()])
        for t in range(ntile):
            bk = nextbank()
            for c in range(8):
                S.op("pe", L("matmul", psum[:, bk, :], lhsT=hxT[hb][:, c, t * 128:(t + 1) * 128], rhs=win[:, c, VA:VA + 512],
                             start=(c == 0), stop=(c == 7)), reads=[Bwin, Bhx[hb]], writes=[PB[bk]])
            S.op("act", L("activation", out=vst[sb][:, :, t, :], in_=psum[:, bk, :].rearrange("p (h e) -> p h e", e=128), func=AF.Copy),
                 reads=[PB[bk]], writes=[Bvst[sb]])
        for h in range(4):
            tl0 = (G % 2) * 4 if G < 16 else 0
            S.dma("sp", vscr[h, ch, :, tl0:tl0 + ntile, :], vst[sb][:, h, 0:ntile, :], reads=[Bvst[sb]], writes=[Buf()])
        if G < 5 or G == 16:
            bk = nextbank()
            for t in range(ntile):
                for c in range(8):
                    S.op("pe", L("matmul", psum[:, bk, t * 128:(t + 1) * 128], lhsT=hxT[hb][:, c, t * 128:(t + 1) * 128],
                                 rhs=win[:, c, VB:VB + 128], start=(c == 0), stop=(c == 7)), reads=[Bwin, Bhx[hb]], writes=[PB[bk]])
            vt0 = G * 4 if G < 16 else 20
            S.op("act", L("activation", out=Vb[:, vt0:vt0 + ntile, :], in_=psum[:, bk, 0:n].rearrange("p (t e) -> p t e", e=128), func=AF.Copy),
                 reads=[PB[bk]], writes=[BVb])

    if stop_after == 1:
        return finish()
    S.barrier()
    off[0] = mark_attn
    yaT = alloc([128, 4, NQ], BF16); ybT = alloc([128, 4, NQ], BF16)
    mark_att2 = off[0]

    kch = [alloc([128, 1024], BF16) for _ in range(3)]; vch = [alloc([128, 8, 128], BF16) for _ in range(3)]
    Bkch = [Buf(f"kch{i}") for i in range(3)]; Bvch = [Buf(f"vch{i}") for i in range(3)]
    PT = [alloc([128, 1024], BF16) for _ in range(3)]; BPT = [Buf(f"PT{i}") for i in range(3)]
    r1 = alloc([128, 512]); r2 = alloc([128, 512]); t1 = alloc([128, 512]); t2 = alloc([128, 512])
    ot = alloc([128, 512]); osq = alloc([128, 512]); rs = alloc([128, 512]); zsb = alloc([64, 512])
    selA = alloc([64, 128]); selB = alloc([64, 128])
    o1sb = alloc([128, 512]); o2sb = alloc([128, 512])
    Bpp = [Buf(f"pp{i}") for i in range(10)]; Bsel = Buf("sel")
    S.op("dve", L("memset", selA, 0.0), writes=[Bsel]); S.op("dve", L("memset", selB, 0.0), writes=[Bsel])
    S.op("dve", L("memset", selA[0:32, :], 1.0 / 32), writes=[Bsel]); S.op("dve", L("memset", selB[32:64, :], 1.0 / 32), writes=[Bsel])
    if do_moe:
        zer = alloc([128, 4, D], BF16); Bzer = Buf("zer")
        S.op("pool", L("memset", zer, 0.0), writes=[Bzer])
        Bxe0 = []
        xev = xe[0:NSLOT, :].rearrange("(n p) d -> p n d", p=128)
        for i in range(NSLOT // 128 // 4):
            bz = Buf(); Bxe0.append(bz)
            S.dma("pool", xev[:, i * 4:(i + 1) * 4, :], zer, reads=[Bzer], writes=[bz])

    chunks = [(ci, 8) for ci in range(8)] + [(8, 2)]
    seq = [(h, qb, ci, nt) for h in range(4) for qb in range(4) for (ci, nt) in chunks]

    def issue_load(j):
        if j >= len(seq): return
        h, qb, ci, nt = seq[j]
        bi = j % 3
        S.dma("sp", kch[bi][:, 0:nt * 128], kscr[h, ci, :, 0:nt * 128], reads=[Bks], writes=[Bkch[bi]])
        S.dma("sp", vch[bi][:, 0:nt, :], vscr[h, ci, :, 0:nt, :], reads=[Bvs], writes=[Bvch[bi]])

    blocks = []
    for h in range(4):
        for qb in range(4):
            units = []
            for j0, (ci, nt) in enumerate(chunks):
                j = (h * 4 + qb) * 9 + j0
                for tt in range(nt):
                    units.append((j, tt, ci))
            blocks.append((h, qb, units))
    NU = len(blocks[0][2])

    def emit_S(bidx, k):
        h, qb, units = blocks[bidx]
        q0 = 128 + qb * 512
        j, tt, ci = units[k]
        g = bidx * NU + k
        bi = j % 3; sbk = (g % 2) * 2
        for cmp_ in range(2):
            S.op("pe", L("matmul", psum[:, sbk + cmp_, :], lhsT=kch[bi][cmp_ * 64:(cmp_ + 1) * 64, tt * 128:(tt + 1) * 128],
                         rhs=QaT[cmp_ * 64:(cmp_ + 1) * 64, h, q0:q0 + 512], start=True, stop=True),
                 reads=[Bkch[bi], BQ], writes=[PB[sbk + cmp_]])

    issue_load(0); issue_load(1)
    emit_S(0, 0)
    for bidx, (h, qb, units) in enumerate(blocks):
        for k in range(NU):
            j, tt, ci = units[k]
            g = bidx * NU + k
            if tt == 0:
                issue_load(j + 2)
            if k + 1 < NU:
                emit_S(bidx, k + 1)
            bi = j % 3; sbk = (g % 2) * 2; pi = g % 3
            S.op("act", L("activation", out=PT[pi], in_=pflat(sbk, 2), func=AF.Exp, scale=0.125),
                 reads=[PB[sbk], PB[sbk + 1]], writes=[BPT[pi]])
            first = (k == 0); last = (k == NU - 1)
            for cmp_ in range(2):
                S.op("pe", L("matmul", psum[:, 4 + cmp_, :], lhsT=vch[bi][:, tt, :], rhs=PT[pi][:, cmp_ * 512:(cmp_ + 1) * 512], start=first, stop=last),
                     reads=[Bvch[bi], BPT[pi]], writes=[PB[4 + cmp_]])
            for cmp_ in range(2):
                S.op("pe", L("matmul", psum[cmp_ * 32:(cmp_ + 1) * 32, 6, :], lhsT=ones_b[:, 0:32], rhs=PT[pi][:, cmp_ * 512:(cmp_ + 1) * 512],
                             start=first, stop=last, tile_position=(0, cmp_ * 32)),
                     reads=[BPT[pi], Bc], writes=[PB[6]])
        if bidx + 1 < len(blocks):
            emit_S(bidx + 1, 0)
        S.op("act", L("activation", out=o1sb, in_=psum[:, 4, :], func=AF.Copy), reads=[PB[4]], writes=[Bpp[8]])
        S.op("act", L("activation", out=o2sb, in_=psum[:, 5, :], func=AF.Copy), reads=[PB[5]], writes=[Bpp[9]])
        S.op("dve", L("tensor_copy", out=zsb, in_=psum[0:64, 6, :]), reads=[PB[6]], writes=[Bpp[7]])
        S.op("pe", L("matmul", psum[:, 7, :], lhsT=selA, rhs=zsb, start=True, stop=True), reads=[Bpp[7], Bsel], writes=[PB[7]])
        S.op("dve", L("reciprocal", out=r1, in_=psum[:, 7, :]), reads=[PB[7]], writes=[Bpp[0]])
        S.op("pe", L("matmul", psum[:, 7, :], lhsT=selB, rhs=zsb, start=True, stop=True), reads=[Bpp[7], Bsel], writes=[PB[7]])
        S.op("dve", L("reciprocal", out=r2, in_=psum[:, 7, :]), reads=[PB[7]], writes=[Bpp[1]])
        S.op("dve", L("tensor_tensor", out=t1, in0=o1sb, in1=r1, op=ALU.mult), reads=[Bpp[8], Bpp[0]], writes=[Bpp[2]])
        S.op("dve", L("tensor_tensor", out=t2, in0=o2sb, in1=r2, op=ALU.mult), reads=[Bpp[9], Bpp[1]], writes=[Bpp[3]])
        S.op("dve", L("scalar_tensor_tensor", out=ot, in0=t2, scalar=neglam, in1=t1, op0=ALU.mult, op1=ALU.add),
             reads=[Bpp[2], Bpp[3], Bvec], writes=[Bpp[4]])
        S.op("act", L("activation", out=osq, in_=ot, func=AF.Square), reads=[Bpp[4]], writes=[Bpp[5]])
        S.op("pe", L("matmul", psum[:, 7, :], lhsT=ones_f, rhs=osq, start=True, stop=True), reads=[Bpp[5], Bc], writes=[PB[7]])
        S.op("act", L("activation", out=rs, in_=psum[:, 7, :], func=AF.Sqrt, scale=1.0 / 128, bias=EPS), reads=[PB[7]], writes=[Bpp[6]])
        S.op("dve", L("reciprocal", out=rs, in_=rs), reads=[Bpp[6]], writes=[Bpp[6]])
        S.op("dve", L("scalar_tensor_tensor", out=yaT[:, h, qb * 512:(qb + 1) * 512], in0=ot, scalar=gsubp, in1=rs, op0=ALU.mult, op1=ALU.mult),
             reads=[Bpp[4], Bpp[6], Bvec], writes=[Bya])

    if stop_after == 2:
        return finish()
    NPW = 4
    PW = [alloc([128, 512], BF16) for _ in range(NPW)]; BPW = [Buf(f"PW{i}") for i in range(NPW)]
    zt = [alloc([128, 512]) for _ in range(2)]; Bzt = [Buf("zt0"), Buf("zt1")]
    WP = []
    for nb in range(16):
        for k, (kt, kind) in enumerate([(nb, "L"), (nb + 1, "C"), (nb + 2, "R"), (20, "X"), (21, "X")]):
            WP.append((nb, k, kt, kind))

    def emit_WS(p):
        nb, k, kt, kind = WP[p]
        kc0 = kt * 128; q0 = 128 + nb * 128
        for g in range(2):
            gs = slice(g * 64, (g + 1) * 64)
            sbk = 2 * (p % 2) + g
            S.op("pe", L("matmul", psum[:, sbk, :].rearrange("p (i q) -> p i q", q=128), lhsT=KbT[gs, kc0:kc0 + 128], rhs=QbT[gs, :, q0:q0 + 128],
                         start=True, stop=True), reads=[BKb, BQ], writes=[PB[sbk]])

    emit_WS(0); emit_WS(1)
    for p, (nb, k, kt, kind) in enumerate(WP):
        ob = 4 + 2 * (nb % 2)
        if kind == "L" and nb == 0:
            bias = wmask[:, 0:1]
        elif kind == "R" and nb == 15:
            bias = wmask[:, 1:2]
        else:
            bias = 0.0
        for g in range(2):
            sbk = 2 * (p % 2) + g; pi = 2 * (p % 2) + g
            S.op("act", L("activation", out=PW[pi], in_=psum[:, sbk, :], func=AF.Exp, scale=0.125, bias=bias),
                 reads=[PB[sbk], Bc], writes=[BPW[pi]])
            if kind in ("L", "R"):
                tri = triL if kind == "L" else triR
                pw3 = PW[pi].rearrange("p (i q) -> p i q", q=128)
                S.op("dve", L("tensor_tensor", out=pw3, in0=pw3, in1=tri.unsqueeze(1).to_broadcast([128, 4, 128]), op=ALU.mult),
                     reads=[BPW[pi], Bc], writes=[BPW[pi]])
        first = (k == 0); last = (k == 4)
        for g in range(2):
            gs = slice(g * 64, (g + 1) * 64); pi = 2 * (p % 2) + g
            S.op("pe", L("matmul", psum[gs, ob, :], lhsT=Vb[:, kt, gs], rhs=PW[pi], start=first, stop=last, tile_position=(0, g * 64)),
                 reads=[BVb, BPW[pi]], writes=[PB[ob]])
        for g in range(2):
            gs = slice(g * 64, (g + 1) * 64); pi = 2 * (p % 2) + g
            S.op("pe", L("matmul", psum[gs, ob + 1, :], lhsT=ones_b[:, 0:64], rhs=PW[pi], start=first, stop=last, tile_position=(0, g * 64)),
                 reads=[Bc, BPW[pi]], writes=[PB[ob + 1]])
        if p + 2 < len(WP):
            emit_WS(p + 2)
        if k == 4:
            zi = nb % 2
            z3 = zt[zi].rearrange("p (i q) -> p i q", q=128)
            S.op("dve", L("tensor_tensor", out=z3, in0=psum[:, ob + 1, :].rearrange("p (i q) -> p i q", q=128),
                          in1=sinkexp.unsqueeze(2).to_broadcast([128, 4, 128]), op=ALU.add), reads=[PB[ob + 1], Bvec], writes=[Bzt[zi]])
            S.op("dve", L("reciprocal", out=zt[zi], in_=zt[zi]), reads=[Bzt[zi]], writes=[Bzt[zi]])
            S.op("dve", L("tensor_tensor", out=ybT[:, :, nb * 128:(nb + 1) * 128], in0=psum[:, ob, :].rearrange("p (i q) -> p i q", q=128),
                          in1=z3, op=ALU.mult), reads=[PB[ob], Bzt[zi]], writes=[Byb])

    if dbg:
        S.dma("sp", dbg_kb, KbT, reads=[BKb])
        S.dma("sp", dbg_vb, Vb.rearrange("p a b -> p (a b)"), reads=[BVb])
        S.dma("sp", dbg_qb, QbT.rearrange("p a b -> p (a b)"), reads=[BQ])
        S.dma("sp", dbg_yb, ybT.rearrange("p a b -> p (a b)"), reads=[Byb])
    if stop_after == 3:
        return finish()
    S.barrier()
    off[0] = mark_att2
    wout = alloc([128, 8, D], BF16); Bwo = Buf("wout")
    wr_b = alloc([128, 8, NE], BF16); wr_f = alloc([128, 8, NE]); br_b = alloc([1, NE], BF16); br_f = alloc([1, NE])
    Bwr = Buf("wr")
    x4 = [alloc([128, D]) for _ in range(2)]; Bx4 = [Buf("x4a"), Buf("x4b")]
    tm4 = alloc([128, D]); Btm4 = Buf("tm4")
    x1t = [alloc([128, D]) for _ in range(2)]; Bx1 = [Buf("x1a"), Buf("x1b")]
    h2 = alloc([128, D]); Bh2 = Buf("h2")
    hx2all = alloc([128, 16, D], BF16); Bhx2t = [Buf(f"hx2_{i}") for i in range(16)]
    hx2T = alloc([128, 8, 128], BF16); Bhx2T = Buf("hx2T")
    s4 = [alloc([128, 4]) for _ in range(2)]; Bs4 = [Buf("s4a"), Buf("s4b")]
    lg = alloc([128, NE]); m8 = alloc([128, 8]); i8 = alloc([128, 8], U32); ekf = alloc([128, 8]); nm0 = alloc([128, 1])
    ge = alloc([128, 4]); gz = alloc([128, 1]); maskb = alloc([128, NE], BF16); destf = alloc([128, NE]); prod = alloc([128, NE])
    dk = alloc([128, 4]); Brr = Buf("rr")
    Bx1s = Buf("x1scr"); Bxes = [Buf(f"xes{i}") for i in range(64)]

    woutv = w_out.rearrange("(c p) n -> p c n", p=128)
    S.dma("pool", wout[:, 0:4, :], woutv[:, 0:4, :], writes=[Bwo])
    for gi in range(4):
        for g in range(2):
            r0 = 512 + g * 256 + gi * 64
            S.dma("pool", wout[g * 64:(g + 1) * 64, 4 + gi, :], w_out[r0:r0 + 64, :], writes=[Bwo])
    S.dma("sp", wr_f, w_r.rearrange("(c p) n -> p c n", p=128), writes=[Bwr])
    S.dma("sp", br_f, b_r.rearrange("(o n) -> o n", o=1), writes=[Bwr])
    S.op("dve", L("tensor_copy", out=wr_b, in_=wr_f), reads=[Bwr], writes=[Bwr])
    S.op("dve", L("tensor_copy", out=br_b, in_=br_f), reads=[Bwr], writes=[Bwr])

    lgs = [alloc([128, NE]) for _ in range(2)]; Blg = [Buf("lg0"), Buf("lg1")]

    def stageA(t):
        b = t % 2
        pb0 = 2 * b
        S.dma("sp", x4[b], xkv[128 + t * 128:128 + (t + 1) * 128, :], writes=[Bx4[b]])
        for hf in range(2):
            for c in range(8):
                lhs = yaT[:, c, t * 128:(t + 1) * 128] if c < 4 else ybT[:, c - 4, t * 128:(t + 1) * 128]
                S.op("pe", L("matmul", psum[:, pb0 + hf, :], lhsT=lhs, rhs=wout[:, c, hf * 512:(hf + 1) * 512],
                                                                             start=(c == 0), stop=(c == 7)),
                     reads=[Bya, Byb, Bwo], writes=[PB[pb0 + hf]])
        S.op("dve", L("tensor_tensor", out=tm4, in0=pflat(pb0, 2), in1=G1bc, op=ALU.mult), reads=[PB[pb0], PB[pb0 + 1], Bbc], writes=[Btm4])
        S.op("pool", L("tensor_tensor", out=x1t[b], in0=tm4, in1=x4[b], op=ALU.add), reads=[Btm4, Bx4[b]], writes=[Bx1[b]])
        S.dma("sp", x1scr[t * 128:(t + 1) * 128, :], x1t[b], reads=[Bx1[b]], writes=[Bx1s])
        if dbg:
            S.dma("sp", dbg_x1[t * 128:(t + 1) * 128, :], x1t[b], reads=[Bx1[b]])
        if not do_moe:
            return
        S.op("act", L("activation", out=junk, in_=x1t[b], func=AF.Square, accum_out=s4[b][:, 0:1]), reads=[Bx1[b]], writes=[Bjunk, Bs4[b]])
        S.op("act", L("activation", out=s4[b][:, 1:2], in_=s4[b][:, 0:1], func=AF.Sqrt, scale=1.0 / D, bias=EPS), reads=[Bs4[b]], writes=[Bs4[b]])
        S.op("dve", L("reciprocal", out=s4[b][:, 2:3], in_=s4[b][:, 1:2]), reads=[Bs4[b]], writes=[Bs4[b]])
        S.op("dve", L("scalar_tensor_tensor", out=h2, in0=x1t[b], scalar=s4[b][:, 2:3], in1=A2bc, op0=ALU.mult, op1=ALU.mult),
             reads=[Bx1[b], Bs4[b], Bbc], writes=[Bh2])
        S.op("pool", L("tensor_tensor", out=hx2all[:, t, :], in0=h2, in1=B2bc, op=ALU.add), reads=[Bh2, Bbc], writes=[Bhx2t[t]])
        for c in range(8):
            S.op("pe", L("transpose", out=pbf(4)[:, c, :], in_=hx2all[:, t, c * 128:(c + 1) * 128], identity=ident_b),
                 reads=[Bhx2t[t], Bc], writes=[PB[4]])
        S.op("act", L("activation", out=hx2T, in_=pbf(4), func=AF.Copy), reads=[PB[4]], writes=[Bhx2T])
        for c in range(8):
            S.op("pe", L("matmul", psum[:, 5, 0:NE], lhsT=hx2T[:, c, :], rhs=wr_b[:, c, :], start=(c == 0), stop=False),
                 reads=[Bhx2T, Bwr], writes=[PB[5]])
        S.op("pe", L("matmul", psum[:, 5, 0:NE], lhsT=ones_b[0:1, :], rhs=br_b, start=False, stop=True), reads=[Bwr, Bc], writes=[PB[5]])
        S.op("dve", L("tensor_copy", out=lgs[b], in_=psum[:, 5, 0:NE]), reads=[PB[5]], writes=[Blg[b]])

    def stageB(t):
        b = t % 2
        S.op("dve", L("max", out=m8, in_=lgs[b]), reads=[Brr, Blg[b]], writes=[Brr])
        S.op("dve", L("max_index", out=i8, in_max=m8, in_values=lgs[b]), reads=[Brr, Blg[b]], writes=[Brr])
        S.op("dve", L("tensor_copy", out=ek_all[:, t * 4:(t + 1) * 4], in_=i8[:, 0:4]), reads=[Brr], writes=[Brt])
        S.op("dve", L("tensor_scalar", out=nm0, in0=m8[:, 0:1], scalar1=-1.0, scalar2=None, op0=ALU.mult), reads=[Brr], writes=[Brr])
        S.op("act", L("activation", out=ge, in_=m8[:, 0:4], func=AF.Exp, bias=nm0, accum_out=gz), reads=[Brr], writes=[Brr])
        S.op("dve", L("reciprocal", out=gz, in_=gz), reads=[Brr], writes=[Brr])
        S.op("dve", L("tensor_scalar", out=gates[:, t * 4:(t + 1) * 4], in0=ge, scalar1=gz, scalar2=None, op0=ALU.mult), reads=[Brr], writes=[Brt])
        S.op("dve", L("tensor_scalar", out=maskb, in0=lgs[b], scalar1=m8[:, 3:4], scalar2=None, op0=ALU.is_ge), reads=[Brr, Blg[b]], writes=[Brr])
        S.op("pe", L("matmul", psum[:, 6, 0:NE], lhsT=U_b, rhs=maskb, start=True, stop=True), reads=[Brr, Bc], writes=[PB[6]])
        S.op("pe", L("matmul", psum[:, 7, 0:NE], lhsT=ones_b, rhs=maskb, start=True, stop=True), reads=[Brr, Bc], writes=[PB[7]])
        S.op("dve", L("tensor_tensor", out=destf, in0=psum[:, 6, 0:NE], in1=basecnt, op=ALU.add), reads=[PB[6], Brt], writes=[Brr])
        S.op("dve", L("tensor_tensor", out=basecnt, in0=psum[:, 7, 0:NE], in1=basecnt, op=ALU.add), reads=[PB[7], Brt], writes=[Brt])
        for k in range(4):
            col = t * 4 + k
            S.op("dve", L("scalar_tensor_tensor", out=prod, in0=iota_f, scalar=ek_all[:, col:col + 1], in1=destf, op0=ALU.is_equal, op1=ALU.mult,
                          accum_out=rk_all[:, col:col + 1]), reads=[Brr, Bc, Brt], writes=[Brr, Brt])

    stageA(0)
    for t in range(16):
        if t + 1 < 16:
            stageA(t + 1)
        if do_moe:
            stageB(t)

    if do_moe:
        cntcol = alloc([32, 1]); tmp32 = alloc([32, 32]); G32 = alloc([32, 32]); T32 = alloc([32, 32]); poscol = alloc([32, 1]); pos2 = alloc([32, 1])
        PmT = alloc([32, 32]); OFFpos = alloc([128, 32]); offk = alloc([128, 64]); dst64 = alloc([128, 64]); t1p = alloc([128, 32])
        carr = alloc([128, 8]); idxf = alloc([128, 32, 8])
        Bso = Buf("sort")
        S.op("dve", L("tensor_tensor", out=tmp32, in0=basecnt[0:32, :], in1=ident_f[0:32, 0:32], op=ALU.mult), reads=[Brt, Bc], writes=[Bso])
        S.op("dve", L("reduce_sum", out=cntcol, in_=tmp32, axis=AX.X), reads=[Bso], writes=[Bso])
        S.op("dve", L("tensor_scalar", out=G32, in0=basecnt[0:32, :], scalar1=cntcol, scalar2=None, op0=ALU.is_gt), reads=[Brt, Bso], writes=[Bso])
        S.op("dve", L("scalar_tensor_tensor", out=T32, in0=basecnt[0:32, :], scalar=cntcol, in1=Lmask, op0=ALU.is_equal, op1=ALU.mult),
             reads=[Brt, Bso, Bc], writes=[Bso])
        S.op("dve", L("tensor_tensor", out=G32, in0=G32, in1=T32, op=ALU.add), reads=[Bso], writes=[Bso])
        S.op("dve", L("reduce_sum", out=poscol, in_=G32, axis=AX.X), reads=[Bso], writes=[Bso])
        S.op("dve", L("tensor_scalar", out=Pm, in0=iota_f[0:32, :], scalar1=poscol, scalar2=None, op0=ALU.is_equal), reads=[Bso, Bc], writes=[Brt])
        S.op("pe", L("matmul", psum[0:32, 0, 0:32], lhsT=Pm, rhs=ident_f[0:32, 0:32], start=True, stop=True), reads=[Brt, Bc], writes=[PB[0]])
        S.op("dve", L("tensor_copy", out=PmT, in_=psum[0:32, 0, 0:32]), reads=[PB[0]], writes=[Bso])
        S.op("pe", L("matmul", psum[:, 1, 0:32], lhsT=OFFrep, rhs=PmT, start=True, stop=True), reads=[Bso, Bc], writes=[PB[1]])
        S.op("dve", L("tensor_scalar", out=OFFpos, in0=psum[:, 1, 0:32], scalar1=128.0, scalar2=None, op0=ALU.mult), reads=[PB[1]], writes=[Bso])
        S.op("pe", L("matmul", psum[:, 2, 0:32], lhsT=pidx[0:32, :], rhs=Pm, start=True, stop=True), reads=[Brt, Bc], writes=[PB[2]])
        S.op("dve", L("tensor_copy", out=permbc, in_=psum[:, 2, 0:32]), reads=[PB[2]], writes=[Brt])
        for col in range(64):
            S.op("dve", L("scalar_tensor_tensor", out=prod, in0=iota_f, scalar=ek_all[:, col:col + 1], in1=OFFpos, op0=ALU.is_equal, op1=ALU.mult,
                          accum_out=offk[:, col:col + 1]), reads=[Brt, Bc, Bso], writes=[Brr, Bso])
        S.op("dve", L("tensor_tensor", out=dst64, in0=offk, in1=rk_all, op=ALU.add), reads=[Bso, Brt], writes=[Bso])
        S.op("dve", L("tensor_copy", out=desti, in_=dst64), reads=[Bso], writes=[Brt])
        S.op("dve", L("scalar_tensor_tensor", out=t1p, in0=permbc, scalar=1024.0, in1=pidx[:, 0:32], op0=ALU.mult, op1=ALU.add), reads=[Brt, Bc], writes=[Bso])
        S.op("dve", L("tensor_scalar", out=carr, in0=iota_f[:, 0:8], scalar1=128.0, scalar2=None, op0=ALU.mult), reads=[Bc], writes=[Bso])
        S.op("dve", L("tensor_tensor", out=idxf, in0=t1p.unsqueeze(2).to_broadcast([128, 32, 8]), in1=carr.unsqueeze(1).to_broadcast([128, 32, 8]), op=ALU.add),
             reads=[Bso], writes=[Bso])
        S.op("dve", L("tensor_copy", out=idxg, in_=idxf.rearrange("p a b -> p (a b)")), reads=[Bso], writes=[Brt])
        S.op("dve", L("tensor_copy", out=bdn_idx, in_=permbc), reads=[Brt], writes=[Brt])
        for col in range(64):
            t = col // 4
            S.op("pool", L("indirect_dma_start", out=xe[:, :], out_offset=bass.IndirectOffsetOnAxis(ap=desti[:, col:col + 1], axis=0),
                           in_=hx2all[:, t, :], in_offset=None), reads=[Bhx2t[t], Brt] + Bxe0, writes=[Bxes[col]], dma=True)

    if not do_moe:
        lastd = [o for o in S.ops["sp"] if o.dma][-4:]
        S.op("sp", None, deps=[o for o in S.ops["sp"] if o.dma][-40:])
        S.emit(nc)
        return nc

    S.barrier()
    off[0] = mark_persist

    XC = 1024
    wgu = [alloc([128, 8, 2 * D], BF16) for _ in range(2)]; wdn = [alloc([128, 8, D], BF16) for _ in range(2)]
    Bwgu = [[Buf(f"wgu{b}_{c}") for c in range(8)] for b in range(2)]; Bwdn = [[Buf(f"wdn{b}_{c}") for c in range(8)] for b in range(2)]
    bdn1 = alloc([128, D]); Bbdn1 = Buf("bdn")
    bgu_f = alloc([NE, 2 * D]); biasT = alloc([128, 16, NE]); Bbias = Buf("bias")
    xet = [alloc([128, D], BF16) for _ in range(3)]; Bxet = [Buf(f"xet{i}") for i in range(3)]
    xeT = alloc([128, 8, XC], BF16); BxeT = Buf("xeT")
    aT = alloc([128, 8, XC], BF16); BaT = Buf("aT")
    g1 = [alloc([128, 512]) for _ in range(2)]; sgm = [alloc([128, 512]) for _ in range(2)]
    u1 = [alloc([128, 512]) for _ in range(2)]; gsx = [alloc([128, 512]) for _ in range(2)]
    Bg1 = [Buf("g1a"), Buf("g1b")]; Bsgm = [Buf("sga"), Buf("sgb")]; Bu1 = [Buf("u1a"), Buf("u1b")]; Bgsx = [Buf("gsa"), Buf("gsb")]
    yst = [alloc([128, D]) for _ in range(2)]; Byst = [Buf("yst0"), Buf("yst1")]

    S.dma("sp", bgu_f, b_gu, writes=[Bbias])
    for m in range(8):
        for two in range(2):
            j = m * 2 + two
            S.op("pe", L("matmul", psum[:, 0, j * NE:(j + 1) * NE], lhsT=bgu_f[:, 2 * m * 128 + two:2 * (m + 1) * 128:2],
                         rhs=Pm, start=True, stop=True), reads=[Bbias, Brt], writes=[PB[0]])
    S.op("dve", L("tensor_copy", out=biasT, in_=psum[:, 0, :].rearrange("p (j n) -> p j n", n=NE)), reads=[PB[0]], writes=[Bbias])
    S.op("dve", L("tensor_scalar", out=biasT[:, 1:16:2, :], in0=biasT[:, 1:16:2, :], scalar1=1.0, scalar2=None, op0=ALU.add), reads=[Bbias], writes=[Bbias])

    wgu_rows = w_gu.rearrange("e k n -> (e k) n"); wdn_rows = w_dn.rearrange("e k n -> (e k) n")
    IO = bass.IndirectOffsetOnAxis

    def load_w(i):
        b = i % 2
        for c in range(8):
            S.op("pool", L("indirect_dma_start", out=wgu[b][:, c, :], out_offset=None, in_=wgu_rows[:, :], in_offset=IO(ap=idxg[:, i * 8 + c:i * 8 + c + 1], axis=0)),
                 reads=[Brt], writes=[Bwgu[b][c]], dma=True)
        for c in range(8):
            S.op("pool", L("indirect_dma_start", out=wdn[b][:, c, :], out_offset=None, in_=wdn_rows[:, :], in_offset=IO(ap=idxg[:, i * 8 + c:i * 8 + c + 1], axis=0)),
                 reads=[Brt], writes=[Bwdn[b][c]], dma=True)

    def load_bdn(i):
        S.op("pool", L("indirect_dma_start", out=bdn1, out_offset=None, in_=b_dn[:, :], in_offset=IO(ap=bdn_idx[:, i:i + 1], axis=0)),
             reads=[Brt], writes=[Bbdn1], dma=True)

    work = []
    for i in range(NE):
        n = NT[i]; o = OFFT[i]; first = True
        while n > 0:
            k = min(XC // 128, n)
            work.append((i, o, k, first, n - k == 0)); o += k; n -= k; first = False

    load_w(0); load_bdn(0)
    pc5 = [0]; xc5 = [0]; yc5 = [0]

    def stage_x(w):
        i, o, k, first, lastw = w
        for s in range(k):
            xi = xc5[0] % 3; xc5[0] += 1
            tb_ = xi % 2
            S.dma("sp", xet[xi], xe[(o + s) * 128:(o + s + 1) * 128, :], reads=Bxes, writes=[Bxet[xi]])
            for c in range(8):
                S.op("pe", L("transpose", out=pbf(tb_)[:, c, :], in_=xet[xi][:, c * 128:(c + 1) * 128], identity=ident_b),
                     reads=[Bxet[xi], Bc], writes=[PB[tb_]])
            S.op("act", L("activation", out=xeT[:, :, s * 128:(s + 1) * 128], in_=pbf(tb_), func=AF.Copy), reads=[PB[tb_]], writes=[BxeT])

    stage_x(work[0])
    for wi, (i, o, k, first, lastw) in enumerate(work):
        b = i % 2
        if first and i + 1 < NE:
            load_w(i + 1)
        ncols = k * 128
        for m in range(8):
            for c0 in range(0, ncols, 512):
                nn = min(512, ncols - c0)
                ii = pc5[0] % 2; pc5[0] += 1
                bg_, bu_ = 2 + 2 * ii, 3 + 2 * ii
                cs = slice(c0, c0 + nn)
                for (bk, two) in ((bg_, 0), (bu_, 1)):
                    for c in range(8):
                        S.op("pe", L("matmul", psum[:, bk, 0:nn], lhsT=wgu[b][:, c, 2 * m * 128 + two:2 * (m + 1) * 128:2],
                                     rhs=xeT[:, c, cs], start=(c == 0), stop=(c == 7)),
                             reads=[Bwgu[b][c], BxeT], writes=[PB[bk]])
                S.op("dve", L("tensor_scalar", out=g1[ii][:, 0:nn], in0=psum[:, bg_, 0:nn], scalar1=biasT[:, 2 * m, i:i + 1], scalar2=7.0, op0=ALU.add, op1=ALU.min),
                     reads=[PB[bg_], Bbias], writes=[Bg1[ii]])
                S.op("act", L("activation", out=sgm[ii][:, 0:nn], in_=g1[ii][:, 0:nn], func=AF.Sigmoid, scale=1.702), reads=[Bg1[ii]], writes=[Bsgm[ii]])
                S.op("dve", L("tensor_scalar", out=u1[ii][:, 0:nn], in0=psum[:, bu_, 0:nn], scalar1=biasT[:, 2 * m + 1, i:i + 1], scalar2=8.0, op0=ALU.add, op1=ALU.min),
                     reads=[PB[bu_], Bbias], writes=[Bu1[ii]])
                S.op("pool", L("tensor_tensor", out=gsx[ii][:, 0:nn], in0=g1[ii][:, 0:nn], in1=sgm[ii][:, 0:nn], op=ALU.mult), reads=[Bg1[ii], Bsgm[ii]], writes=[Bgsx[ii]])
                S.op("dve", L("scalar_tensor_tensor", out=aT[:, m, cs], in0=u1[ii][:, 0:nn], scalar=-6.0, in1=gsx[ii][:, 0:nn], op0=ALU.max, op1=ALU.mult),
                     reads=[Bu1[ii], Bgsx[ii]], writes=[BaT])
        if wi + 1 < len(work):
            stage_x(work[wi + 1])
        for s in range(k):
            yb_ = yc5[0] % 2; yc5[0] += 1
            for hf in range(2):
                bk = 6 + hf
                for m in range(8):
                    S.op("pe", L("matmul", psum[:, bk, :], lhsT=aT[:, m, s * 128:(s + 1) * 128], rhs=wdn[b][:, m, hf * 512:(hf + 1) * 512],
                                 start=(m == 0), stop=(m == 7)),
                         reads=[BaT, Bwdn[b][m]], writes=[PB[bk]])
            S.op("dve", L("tensor_tensor", out=yst[yb_], in0=pflat(6, 2), in1=bdn1, op=ALU.add), reads=[PB[6], PB[7], Bbdn1], writes=[Byst[yb_]])
            S.dma("sp", ye[(o + s) * 128:(o + s + 1) * 128, :], yst[yb_], reads=[Byst[yb_]], writes=[Buf()])
        if lastw and i + 1 < NE:
            load_bdn(i + 1)

    S.barrier()
    off[0] = mark_persist

    yk = [alloc([128, D]) for _ in range(8)]; Byk = [Buf(f"yk{i}") for i in range(8)]
    x6 = [alloc([128, D]) for _ in range(2)]; Bx6 = [Buf("x6a"), Buf("x6b")]
    acc = [alloc([128, D]) for _ in range(2)]; Bacc = [Buf("acca"), Buf("accb")]
    acc2 = [alloc([128, D]) for _ in range(2)]; Bacc2 = [Buf("acc2a"), Buf("acc2b")]
    xo = [alloc([128, D]) for _ in range(2)]; Bxo = [Buf("xoa"), Buf("xob")]
    ot6 = [alloc([128, D]) for _ in range(2)]; Bot6 = [Buf("o6a"), Buf("o6b")]
    s6 = [alloc([128, 4]) for _ in range(2)]; Bs6 = [Buf("s6a"), Buf("s6b")]
    outs = []

    def loads6(t):
        b = t % 2
        S.dma("sp", x6[b], x1scr[t * 128:(t + 1) * 128, :], reads=[Bx1s], writes=[Bx6[b]])
        for k in range(4):
            col = t * 4 + k
            S.op("pool", L("indirect_dma_start", out=yk[4 * b + k], out_offset=None, in_=ye[:, :],
                           in_offset=bass.IndirectOffsetOnAxis(ap=desti[:, col:col + 1], axis=0)),
                 reads=[Brt], writes=[Byk[4 * b + k]], dma=True)

    loads6(0)
    for t in range(16):
        b = t % 2
        if t + 1 < 16:
            loads6(t + 1)
        S.op("act", L("activation", out=acc[b], in_=yk[4 * b], func=AF.Copy, scale=gates[:, t * 4:t * 4 + 1]), reads=[Byk[4 * b], Brt], writes=[Bacc[b]])
        for k in range(1, 4):
            S.op("dve", L("scalar_tensor_tensor", out=acc[b], in0=yk[4 * b + k], scalar=gates[:, t * 4 + k:t * 4 + k + 1], in1=acc[b], op0=ALU.mult, op1=ALU.add),
                 reads=[Byk[4 * b + k], Brt, Bacc[b]], writes=[Bacc[b]])
        S.op("dve", L("tensor_tensor", out=acc2[b], in0=acc[b], in1=G2bc, op=ALU.mult), reads=[Bacc[b], Bbc], writes=[Bacc2[b]])
        S.op("pool", L("tensor_tensor", out=xo[b], in0=acc2[b], in1=x6[b], op=ALU.add), reads=[Bacc2[b], Bx6[b]], writes=[Bxo[b]])
        S.op("act", L("activation", out=junk, in_=xo[b], func=AF.Square, accum_out=s6[b][:, 0:1]), reads=[Bxo[b]], writes=[Bjunk, Bs6[b]])
        S.op("act", L("activation", out=s6[b][:, 1:2], in_=s6[b][:, 0:1], func=AF.Sqrt, scale=1.0 / D, bias=EPS), reads=[Bs6[b]], writes=[Bs6[b]])
        S.op("dve", L("reciprocal", out=s6[b][:, 2:3], in_=s6[b][:, 1:2]), reads=[Bs6[b]], writes=[Bs6[b]])
        S.op("dve", L("scalar_tensor_tensor", out=ot6[b], in0=xo[b], scalar=s6[b][:, 2:3], in1=FGbc, op0=ALU.mult, op1=ALU.mult),
             reads=[Bxo[b], Bs6[b], Bbc], writes=[Bot6[b]])
        outs.append(S.dma("sp", outd[t * 128:(t + 1) * 128, :], ot6[b], reads=[Bot6[b]]))
    S.op("sp", None, deps=outs)
    S.emit(nc)
    return nc


def _rope_tables(pos):
    p = np.arange(128); d = p % 64; f = d % 16
    inv = (np.float32(10000.0) ** (-(np.arange(16, dtype=np.float32)) / np.float32(16))).astype(np.float32)
    row = (pos // 64).astype(np.float32); col = (pos % 64).astype(np.float32)
    coord = np.where((d < 32)[:, None], row[None, :], col[None, :]).astype(np.float32)
    ang = (coord * inv[f][:, None]).astype(np.float32)
    return np.cos(ang).astype(np.float32), np.sin(ang).astype(np.float32)


def _consts():
    c = np.zeros((128, 9, 128), np.float32)
    c[:, 0, :] = np.eye(128, dtype=np.float32)
    R = np.zeros((128, 128), np.float32)
    for m in range(128):
        if (m % 32) < 16: R[m, m + 16] = -1.0
        else: R[m, m - 16] = 1.0
    c[:, 1, :] = R.T
    j = np.arange(128)[:, None]; i = np.arange(128)[None, :]
    c[:, 2, :] = (j < i).astype(np.float32)
    c[:, 3, :] = (j >= i).astype(np.float32)
    c[:, 4, :] = (j <= i).astype(np.float32)
    c[:, 5, :] = np.arange(128, dtype=np.float32)[None, :]
    c[:, 6, :] = np.arange(128, dtype=np.float32)[:, None]
    c[0:NE, 7, :] = np.asarray(OFFT, np.float32)[:, None]
    c[:, 8, :] = (i < j).astype(np.float32)
    return c.reshape(128, 9 * 128)


def make_in_maps(inp, do_moe=True):
    x = np.asarray(inp["x"], np.float32); ctx = np.asarray(inp["ctx"], np.float32)
    c = np.asarray(inp["c"], np.float32); c_ctx = np.asarray(inp["c_ctx"], np.float32)
    consts = _consts()
    lam = np.stack([np.asarray(inp[k], np.float32)[0] for k in ("lam_q1", "lam_k1", "lam_q2", "lam_k2")], 0)
    shared = {
        "consts": consts, "w_mod": np.ascontiguousarray(inp["w_mod"][0]), "b_mod": np.ascontiguousarray(inp["b_mod"][0]),
        "norm1_g": np.ascontiguousarray(inp["norm1_g"][0]), "w_in": np.ascontiguousarray(inp["w_in"][0]), "lam": np.ascontiguousarray(lam),
        "subln_g": np.ascontiguousarray(inp["subln_g"][0]), "sink": np.ascontiguousarray(inp["sink"][0]),
        "w_out": np.ascontiguousarray(inp["w_out"][0]), "norm2_g": np.ascontiguousarray(inp["norm2_g"][0]),
        "w_router": np.ascontiguousarray(inp["w_router"][0]), "b_router": np.ascontiguousarray(inp["b_router"][0]),
        "final_g": np.ascontiguousarray(inp["final_g"]),
    }
    if do_moe:
        shared.update({"w_gate_up": np.ascontiguousarray(inp["w_gate_up"][0]), "b_gate_up": np.ascontiguousarray(inp["b_gate_up"][0]),
                       "w_down": np.ascontiguousarray(inp["w_down"][0]), "b_down": np.ascontiguousarray(inp["b_down"][0])})
    shared = {k: np.asarray(v, np.float32) for k, v in shared.items()}
    maps = []
    for core in range(8):
        b, j = core // 4, core % 4
        qs = j * NQ
        shift = qs - 128
        pos = (np.arange(S_LEN) + shift) % S_LEN
        cos, sin = _rope_tables(pos)
        cosT = np.concatenate([cos, np.ones((128, CTX), np.float32)], 1)
        sinT = np.concatenate([sin, np.zeros((128, CTX), np.float32)], 1)
        wmask = np.zeros((128, 2), np.float32)
        if j == 0: wmask[:, 0] = -1e30
        if j == 3: wmask[:, 1] = -1e30
        m = dict(shared)
        m.update({"xkv": np.ascontiguousarray(np.roll(x[b], -shift, axis=0)), "ctx": np.ascontiguousarray(ctx[b]),
                  "cvec": np.ascontiguousarray(np.stack([c[b], c_ctx], 0)), "cosT": cosT, "sinT": sinT, "wmask": wmask})
        maps.append(m)
    return maps


_NC_CACHE = {}


def kernel(**inputs):
    if "nc" not in _NC_CACHE:
        _NC_CACHE["nc"] = build_program(do_moe=True)
    nc = _NC_CACHE["nc"]
    maps = make_in_maps(inputs, do_moe=True)
    res = run_bass_kernel_spmd(nc, maps, core_ids=list(range(8)))
    out = np.empty((2, S_LEN, D), np.float32)
    for core in range(8):
        b, j = core // 4, core % 4
        out[b, j * NQ:(j + 1) * NQ, :] = res.results[core]["out"]
    return out
```

```python
import contextlib
import numpy as np
import concourse.bass as bass
import concourse.mybir as mybir
from concourse.bass_utils import run_bass_kernel_spmd

F32 = mybir.dt.float32; BF16 = mybir.dt.bfloat16; I32 = mybir.dt.int32; U8 = mybir.dt.uint8
U32 = mybir.dt.uint32
ALU = mybir.AluOpType; AF = mybir.ActivationFunctionType
AX = mybir.AxisListType
ENGS = ("pe", "act", "dve", "pool", "sp")
NDMASEM = 12

D = 1024; S_LEN = 8192; CTX = 256; NQ = 2048; NE = 32;
NT = [-(-min(2048, -(-8192 // (i + 1))) // 128) for i in range(NE)]
OFFT = [sum(NT[:i]) for i in range(NE)]
NSLOT = sum(NT) * 128; NSLOTP = NSLOT
QA, KA, VA, QB, KB, VB = 0, 512, 1024, 1536, 2048, 2176
NKT = 66
WROWS = 2560
EPS = 1e-6
LAM_INIT = 0.2


def L(method, *args, **kw):
    return lambda e: getattr(e, method)(*args, **kw)


class Buf:
    __slots__ = ("name", "w", "r", "excl")
    def __init__(self, name="", excl=False):
        self.name = name; self.w = None; self.r = []; self.excl = excl


class Op:
    __slots__ = ("eng", "fn", "deps", "dma", "signal", "sem", "val", "idx", "name")
    def __init__(self, eng, fn, dma, name):
        self.eng = eng; self.fn = fn; self.deps = []; self.dma = dma
        self.signal = False; self.sem = None; self.val = 0; self.idx = 0; self.name = name


class Sched:
    def __init__(self):
        self.ops = {e: [] for e in ENGS}
        self.all = []

    def op(self, eng, fn, reads=(), writes=(), dma=False, deps=(), name=""):
        o = Op(eng, fn, dma, name)
        ds = {}
        ex = [b for b in reads if b.excl]
        if ex:
            reads = [b for b in reads if not b.excl]; writes = list(writes) + ex
        for b in reads:
            if b.w is not None: ds[id(b.w)] = b.w
        for b in writes:
            if b.w is not None: ds[id(b.w)] = b.w
            lastr = {}
            for r in b.r:
                if r.dma: ds[id(r)] = r
                elif r.eng not in lastr or r.idx > lastr[r.eng].idx: lastr[r.eng] = r
            for r in lastr.values(): ds[id(r)] = r
        for d in deps: ds[id(d)] = d
        o.deps = list(ds.values())
        for b in reads: b.r.append(o)
        for b in writes:
            b.w = o; b.r = []
        o.idx = len(self.ops[eng]); self.ops[eng].append(o); self.all.append(o)
        return o

    def dma(self, eng, out, in_, reads=(), writes=(), name="", **kw):
        return self.op(eng, L("dma_start", out=out, in_=in_, **kw), reads, writes, dma=True, name=name)

    def barrier(self):
        lasts = []
        for e in ENGS:
            real = [o for o in self.ops[e] if o.fn is not None]
            comp = [o for o in real if not o.dma]
            if comp: lasts.append(comp[-1])
            lasts.extend([o for o in real if o.dma][-NDMASEM:])
        for e in ENGS:
            self.op(e, None, deps=lasts, name="barrier")

    def emit(self, nc):
        for o in self.all:
            for d in o.deps:
                if d.eng == "pe" and o.eng == "pe" and not d.dma:
                    continue
                d.signal = True
        with contextlib.ExitStack() as st:
            csem = {e: st.enter_context(nc.semaphore("c_" + e)) for e in ENGS}
            dsem = {e: [st.enter_context(nc.semaphore(f"d_{e}{i}")) for i in range(NDMASEM)]
                    for e in ("sp", "pool", "act")}
            for e in ENGS:
                cnt = 0; di = 0; dcnt = [0] * NDMASEM; dlast = [None] * NDMASEM
                for o in self.ops[e]:
                    if o.fn is None: continue
                    if o.dma:
                        o.signal = True
                        s = di % NDMASEM; di += 1
                        if dlast[s] is not None:
                            o.deps.append(dlast[s])
                        dcnt[s] += 16; o.sem = dsem[e][s]; o.val = dcnt[s]; dlast[s] = o
                    elif o.signal:
                        cnt += 1; o.sem = csem[e]; o.val = cnt
            blk = st.enter_context(nc.Block())
            engobj = {"pe": "tensor", "act": "scalar", "dve": "vector", "pool": "gpsimd", "sp": "sync"}

            def mk(e):
                def body(eng):
                    seen = {}
                    for o in self.ops[e]:
                        need = {}
                        for d in o.deps:
                            if d.fn is None: continue
                            if d.eng == "pe" and e == "pe" and not d.dma: continue
                            k = id(d.sem)
                            if seen.get(k, 0) >= d.val: continue
                            if k not in need or need[k][1] < d.val: need[k] = (d.sem, d.val)
                        for k, (s, v) in need.items():
                            eng.wait_ge(s, v); seen[k] = v
                        if o.fn is None: continue
                        inst = o.fn(eng)
                        if o.signal:
                            inst.then_inc(o.sem, 16 if o.dma else 1)
                return body
            for e in ENGS:
                if self.ops[e]:
                    getattr(blk, engobj[e])(mk(e))


def build_program(do_moe=True, dbg=False, stop_after=99):
    nc = bass.Bass("TRN2", target_bir_lowering=False)
    dt_ = nc.dram_tensor

    def din(name, shape, dt=F32):
        return dt_(name, list(shape), dt, kind="ExternalInput").ap()

    xkv = din("xkv", [S_LEN, D]); ctxd = din("ctx", [CTX, D]); cvec = din("cvec", [2, D])
    cosd = din("cosT", [128, S_LEN + CTX]); sind = din("sinT", [128, S_LEN + CTX])
    wmaskd = din("wmask", [128, 2]); constd = din("consts", [128, 9 * 128])
    w_mod = din("w_mod", [D, 6 * D]); b_mod = din("b_mod", [6 * D]); n1g = din("norm1_g", [D])
    w_in = din("w_in", [D, 2304]); lamd = din("lam", [4, 64]); sublnd = din("subln_g", [128])
    sinkd = din("sink", [8]); w_out = din("w_out", [D, D]); n2g = din("norm2_g", [D])
    w_r = din("w_router", [D, NE]); b_r = din("b_router", [NE]); fgd = din("final_g", [D])
    if do_moe:
        w_gu = din("w_gate_up", [NE, D, 2 * D]); b_gu = din("b_gate_up", [NE, 2 * D])
        w_dn = din("w_down", [NE, D, D]); b_dn = din("b_down", [NE, D])
    outd = dt_("out", [NQ, D], F32, kind="ExternalOutput").ap()
    kscr = dt_("kscr", [4, 9, 128, 1024], BF16).ap()
    vscr = dt_("vscr", [4, 9, 128, 8, 128], BF16).ap()
    x1scr = dt_("x1scr", [NQ, D], F32).ap()
    xe = dt_("xe", [NSLOTP, D], BF16).ap()
    ye = dt_("ye", [NSLOTP, D], F32).ap()
    if dbg:
        dbg_x1 = dt_("dbg_x1", [NQ, D], F32, kind="ExternalOutput").ap()
        dbg_yb = dt_("dbg_yb", [128, 4 * NQ], BF16, kind="ExternalOutput").ap()
        dbg_kb = dt_("dbg_kb", [128, WROWS + CTX], BF16, kind="ExternalOutput").ap()
        dbg_vb = dt_("dbg_vb", [128, 22 * 128], BF16, kind="ExternalOutput").ap()
        dbg_qb = dt_("dbg_qb", [128, 4 * WROWS], BF16, kind="ExternalOutput").ap()

    TOT = 206 * 1024
    arena = nc.alloc_sbuf_tensor("arena", [128, TOT], U8)
    psum = nc.alloc_psum_tensor("psum", [128, 8, 512], F32)
    off = [0]

    def alloc(shape, dt=F32):
        n = int(np.prod(shape[1:])) * (2 if dt == BF16 else 4)
        a = arena[0:shape[0], off[0]:off[0] + n].bitcast(dt)
        off[0] += (n + 63) // 64 * 64
        assert off[0] <= TOT, ("SBUF overflow", off[0])
        if len(shape) == 3:
            a = a.rearrange("p (a b) -> p a b", b=shape[2])
        elif len(shape) == 4:
            a = a.rearrange("p (a b c) -> p a b c", b=shape[2], c=shape[3])
        return a

    def pbank(b, n=1):
        return psum[:, b:b + n, :]

    def pflat(b, n=1):
        return psum[:, b:b + n, :].rearrange("p a b -> p (a b)")

    def pbf(b):
        return psum[:, b, :].bitcast(BF16).rearrange("p (a b) -> p a b", b=128)

    S = Sched()

    def finish():
        S.barrier()
        S.emit(nc)
        return nc
    PB = [Buf(f"psum{i}", excl=True) for i in range(8)]

    cst = alloc([128, 9, 128], F32)
    ident_f = cst[:, 0, :]; iota_f = cst[:, 5, 0:32]; pidx = cst[:, 6, :]; OFFrep = cst[0:32, 7, :]; Lmask = cst[0:32, 8, 0:32]
    cstb = alloc([128, 5, 128], BF16)
    ident_b = cstb[:, 0, :]; RT_b = cstb[:, 1, :]; U_b = cstb[:, 2, :]; triL = cstb[:, 3, :]; triR = cstb[:, 4, :]
    ones_b = alloc([128, 128], BF16); ones_f = alloc([128, 128], F32)
    A1T = alloc([128, 8]); B1T = alloc([128, 8]); A1cT = alloc([128, 8]); B1cT = alloc([128, 8])
    gsubp = alloc([128, 1]); neglam = alloc([128, 1]); sinkexp = alloc([128, 4]); wmask = alloc([128, 2])
    G1bc = alloc([128, D]); A2bc = alloc([128, D]); B2bc = alloc([128, D]); G2bc = alloc([128, D]); FGbc = alloc([128, D])
    desti = alloc([128, 64], I32); gates = alloc([128, 64]); basecnt = alloc([128, 32])
    ek_all = alloc([128, 64]); rk_all = alloc([128, 64]); Pm = alloc([32, 32]); permbc = alloc([128, 32])
    idxg = alloc([128, 32 * 8], I32); bdn_idx = alloc([128, 32], I32)
    junk = alloc([128, D], BF16); Bjunk = Buf("junk")
    Bc = Buf("consts"); Bvec = Buf("vecs"); Bbc = Buf("bcast"); Brt = Buf("route")

    S.dma("sp", cst.rearrange("p a b -> p (a b)"), constd, writes=[Bc])
    S.dma("sp", wmask, wmaskd, writes=[Bc])
    S.op("dve", L("tensor_copy", out=cstb, in_=cst[:, 0:5, :]), reads=[Bc], writes=[Bc])
    S.op("dve", L("memset", ones_b, 1.0), writes=[Bc])
    S.op("dve", L("memset", ones_f, 1.0), writes=[Bc])
    S.op("dve", L("memset", basecnt, 0.0), writes=[Brt])

    mark_persist = off[0]

    cT = alloc([128, 8, 2]); sg = alloc([128, 8, 2]); scT = alloc([128, 8, 2]); scT_b = alloc([128, 8, 2], BF16)
    screp = alloc([128, 8, 128], BF16)
    bmT = alloc([128, 16]); g1nT = alloc([128, 8]); modT = alloc([128, 16, 2])
    bmbc = alloc([128, 4 * D]); n2bc = alloc([128, D])
    lamt = alloc([1, 4, 64]); lamp = alloc([1, 2, 64]); lams = alloc([1, 2]); lamv = alloc([1, 2])
    wm = [alloc([128, 8, 1024], BF16) for _ in range(2)]
    Bwm = [Buf("wm0"), Buf("wm1")]; Bp0 = Buf("p0")

    for r_ in range(2):
        S.dma("sp", cT[:, :, r_], cvec[r_].rearrange("(c p) -> p c", p=128), writes=[Bp0], allow_slow_non_contiguous=True)
    S.dma("sp", bmT, b_mod[0:2048].rearrange("(c p) -> p c", p=128), writes=[Bp0], allow_slow_non_contiguous=True)
    S.dma("sp", g1nT, n1g.rearrange("(c p) -> p c", p=128), writes=[Bp0], allow_slow_non_contiguous=True)
    S.dma("sp", bmbc, b_mod[2048:6144].partition_broadcast(128), writes=[Bp0])
    S.dma("sp", n2bc, n2g.partition_broadcast(128), writes=[Bp0])
    S.dma("sp", FGbc, fgd.partition_broadcast(128), writes=[Bbc])
    S.dma("sp", gsubp, sublnd.rearrange("(p o) -> p o", o=1), writes=[Bvec])
    S.dma("sp", lamt.rearrange("p a b -> p (a b)"), lamd.rearrange("(o a) b -> o (a b)", o=1), writes=[Bp0])
    for g in range(2):
        S.dma("sp", sinkexp[g * 64:(g + 1) * 64, :], sinkd[g * 4:(g + 1) * 4].partition_broadcast(64), writes=[Bvec])
    wmv = w_mod.rearrange("(c p) n -> p c n", p=128)
    for i in range(2):
        S.dma("pool", wm[i], wmv[:, :, i * 1024:(i + 1) * 1024], writes=[Bwm[i]])

    S.op("act", L("activation", out=sg, in_=cT, func=AF.Sigmoid), reads=[Bp0], writes=[Bp0])
    S.op("dve", L("tensor_tensor", out=scT, in0=cT, in1=sg, op=ALU.mult), reads=[Bp0], writes=[Bp0])
    S.op("dve", L("tensor_copy", out=scT_b, in_=scT), reads=[Bp0], writes=[Bp0])
    S.op("dve", L("tensor_copy", out=screp, in_=scT[:, :, 0:1].to_broadcast([128, 8, 128])), reads=[Bp0], writes=[Bp0])
    S.op("dve", L("tensor_scalar", out=gsubp, in0=gsubp, scalar1=1.0 - LAM_INIT, scalar2=None, op0=ALU.mult),
         reads=[Bvec], writes=[Bvec])
    S.op("act", L("activation", out=sinkexp, in_=sinkexp, func=AF.Exp), reads=[Bvec], writes=[Bvec])
    S.op("dve", L("tensor_tensor", out=lamp, in0=lamt[:, 0:4:2, :], in1=lamt[:, 1:4:2, :], op=ALU.mult), reads=[Bp0], writes=[Bp0])
    S.op("dve", L("reduce_sum", out=lams, in_=lamp, axis=AX.X), reads=[Bp0], writes=[Bp0])
    S.op("act", L("activation", out=lamv, in_=lams, func=AF.Exp), reads=[Bp0], writes=[Bp0])
    S.op("dve", L("tensor_tensor", out=lamv[:, 0:1], in0=lamv[:, 1:2], in1=lamv[:, 0:1], op=ALU.subtract), reads=[Bp0], writes=[Bp0])
    S.op("dve", L("tensor_scalar", out=lamv[:, 0:1], in0=lamv[:, 0:1], scalar1=-LAM_INIT, scalar2=None, op0=ALU.add), reads=[Bp0], writes=[Bp0])
    S.op("pe", L("matmul", psum[:, 7, 0:1], lhsT=ones_f[0:1, :], rhs=lamv[:, 0:1], start=True, stop=True),
         reads=[Bp0, Bc], writes=[PB[7]])
    S.op("dve", L("tensor_copy", out=neglam, in_=psum[:, 7, 0:1]), reads=[PB[7]], writes=[Bvec])

    pm = psum[:, 0, 0:32].rearrange("p (a b) -> p a b", b=2)
    for blk in range(16):
        i = blk // 8
        for c in range(8):
            S.op("pe", L("matmul", pm[:, blk, :], lhsT=wm[i][:, c, (blk % 8) * 128:(blk % 8 + 1) * 128],
                                                              rhs=scT_b[:, c, :], start=(c == 0), stop=(c == 7)),
                 reads=[Bwm[i], Bp0], writes=[PB[0]])
    S.op("dve", L("tensor_tensor", out=modT, in0=pm, in1=bmT.unsqueeze(2).to_broadcast([128, 16, 2]), op=ALU.add),
         reads=[PB[0], Bp0], writes=[Bp0])
    for (AT, BT, r) in ((A1T, B1T, 0), (A1cT, B1cT, 1)):
        S.op("dve", L("scalar_tensor_tensor", out=AT, in0=modT[:, 8:16, r], scalar=1.0, in1=g1nT, op0=ALU.add, op1=ALU.mult),
             reads=[Bp0], writes=[Bvec])
        S.op("dve", L("tensor_copy", out=BT, in_=modT[:, 0:8, r]), reads=[Bp0], writes=[Bvec])
    dsts = [G1bc, B2bc, A2bc, G2bc]
    for q in range(4):
        i = q % 2
        S.dma("pool", wm[i], wmv[:, :, (2 + q) * 1024:(3 + q) * 1024], writes=[Bwm[i]])
        for hf in range(2):
            for c in range(8):
                S.op("pe", L("matmul", psum[:, 1 + hf, :], lhsT=screp[:, c, :], rhs=wm[i][:, c, hf * 512:(hf + 1) * 512],
                                                                start=(c == 0), stop=(c == 7)),
                     reads=[Bwm[i], Bp0], writes=[PB[1 + hf]])
        S.op("dve", L("tensor_tensor", out=dsts[q], in0=pflat(1, 2), in1=bmbc[:, q * 1024:(q + 1) * 1024], op=ALU.add),
             reads=[PB[1], PB[2], Bp0], writes=[Bbc])
    S.op("dve", L("scalar_tensor_tensor", out=A2bc, in0=A2bc, scalar=1.0, in1=n2bc, op0=ALU.add, op1=ALU.mult),
         reads=[Bbc, Bp0], writes=[Bbc])

    if stop_after == 0:
        return finish()
    S.barrier()
    off[0] = mark_persist

    QaT = alloc([128, 4, WROWS], BF16); QbT = alloc([128, 4, WROWS], BF16)
    KbT = alloc([128, WROWS + CTX], BF16); Vb = alloc([128, 22, 128], BF16)
    BQ = Buf("Q"); BKb = Buf("Kb"); BVb = Buf("Vb"); Bya = Buf("ya"); Byb = Buf("yb")
    Bks = Buf("kscr"); Bvs = Buf("vscr")
    mark_attn = off[0]

    win = alloc([128, 8, 2304], BF16); Bwin = Buf("win")
    xt = [alloc([128, D]) for _ in range(2)]; Bxt = [Buf("xt0"), Buf("xt1")]
    xn = [alloc([128, D], BF16) for _ in range(4)]; Bxn = [Buf(f"xn{i}") for i in range(4)]
    st4 = [alloc([128, 4]) for _ in range(4)]; Bst = [Buf(f"st{i}") for i in range(4)]
    evt = [alloc([128, 8, 128]) for _ in range(2)]; Bev = [Buf("ev0"), Buf("ev1")]
    hxT = [alloc([128, 8, 512], BF16) for _ in range(2)]; Bhx = [Buf("hx0"), Buf("hx1")]
    cosg = [alloc([128, 512]) for _ in range(2)]; sing = [alloc([128, 512]) for _ in range(2)]; Btab = [Buf("tab0"), Buf("tab1")]
    kbf = [alloc([128, 512], BF16) for _ in range(2)]; Bkbf = [Buf("kbf0"), Buf("kbf1")]
    rt1 = [alloc([128, 512]) for _ in range(2)]; rt2 = [alloc([128, 512]) for _ in range(2)]
    Brt1 = [Buf("rt1a"), Buf("rt1b")]; Brt2 = [Buf("rt2a"), Buf("rt2b")]
    kst = [alloc([128, 4, 512], BF16) for _ in range(2)]; Bkst = [Buf("kst0"), Buf("kst1")]
    vst = [alloc([128, 4, 4, 128], BF16) for _ in range(2)]; Bvst = [Buf("vst0"), Buf("vst1")]

    winv = w_in.rearrange("(c p) n -> p c n", p=128)
    S.dma("pool", win[:, :, 0:QB], winv[:, :, 0:QB], writes=[Bwin])
    S.dma("pool", win[:, :, KB:2304], winv[:, :, KB:2304], writes=[Bwin])
    for gi in range(4):
        for g in range(2):
            S.dma("pool", win[:, :, QB + gi * 128 + g * 64:QB + gi * 128 + (g + 1) * 64],
                  winv[:, :, QB + g * 256 + gi * 64:QB + g * 256 + (gi + 1) * 64], writes=[Bwin])
    import os
    rr = [0]

    pcyc = [0]

    def nextbank():
        b = 2 + pcyc[0] % 4; pcyc[0] += 1
        return b

    def projA(job, hb, tb):
        colbase, n, dst, wb = job
        i = rr[0] % 2; rr[0] += 1
        bk = nextbank()
        for c in range(8):
            S.op("pe", L("matmul", psum[:, bk, 0:n], lhsT=win[:, c, colbase:colbase + 128], rhs=hxT[hb][:, c, 0:n], start=(c == 0), stop=(c == 7)),
                 reads=[Bwin, Bhx[hb]], writes=[PB[bk]])
        S.op("act", L("activation", out=kbf[i][:, 0:n], in_=psum[:, bk, 0:n], func=AF.Copy), reads=[PB[bk]], writes=[Bkbf[i]])
        return (bk, i, n, dst, wb, tb)

    def ropeB(st):
        bk, i, n, dst, wb, tb = st
        S.op("pe", L("matmul", psum[:, 6 + i, 0:n], lhsT=RT_b, rhs=kbf[i][:, 0:n], start=True, stop=True), reads=[Bkbf[i], Bc], writes=[PB[6 + i]])
        S.op("dve", L("tensor_tensor", out=rt1[i][:, 0:n], in0=psum[:, bk, 0:n], in1=cosg[tb][:, 0:n], op=ALU.mult),
             reads=[PB[bk], Btab[tb]], writes=[Brt1[i]])
        S.op("dve", L("tensor_tensor", out=rt2[i][:, 0:n], in0=psum[:, 6 + i, 0:n], in1=sing[tb][:, 0:n], op=ALU.mult),
             reads=[PB[6 + i], Btab[tb]], writes=[Brt2[i]])
        S.op("pool", L("tensor_tensor", out=dst, in0=rt1[i][:, 0:n], in1=rt2[i][:, 0:n], op=ALU.add), reads=[Brt1[i], Brt2[i]], writes=wb)

    for G in range(17):
        ntile = 4 if G < 16 else 2
        n = ntile * 128
        hb = G % 2
        AT, BT = (A1T, B1T) if G < 16 else (A1cT, B1cT)
        tb = G % 2
        S.dma("sp", cosg[tb][:, 0:n], cosd[:, G * 512:G * 512 + n], writes=[Btab[tb]])
        S.dma("sp", sing[tb][:, 0:n], sind[:, G * 512:G * 512 + n], writes=[Btab[tb]])
        for t in range(ntile):
            gt = G * 4 + t
            xb = gt % 2; x4i = gt % 4
            src = xkv[gt * 128:(gt + 1) * 128, :] if G < 16 else ctxd[t * 128:(t + 1) * 128, :]
            S.dma("sp", xt[xb], src, writes=[Bxt[xb]])
            S.op("act", L("activation", out=junk, in_=xt[xb], func=AF.Square, accum_out=st4[x4i][:, 0:1]),
                 reads=[Bxt[xb]], writes=[Bjunk, Bst[x4i]])
            S.op("act", L("activation", out=st4[x4i][:, 1:2], in_=st4[x4i][:, 0:1], func=AF.Sqrt, scale=1.0 / D, bias=EPS),
                 reads=[Bst[x4i]], writes=[Bst[x4i]])
            S.op("dve", L("reciprocal", out=st4[x4i][:, 2:3], in_=st4[x4i][:, 1:2]), reads=[Bst[x4i]], writes=[Bst[x4i]])
            S.op("act", L("activation", out=xn[x4i], in_=xt[xb], func=AF.Copy, scale=st4[x4i][:, 2:3]),
                 reads=[Bxt[xb], Bst[x4i]], writes=[Bxn[x4i]])
            tbk = xb
            for c in range(8):
                S.op("pe", L("transpose", out=pbf(tbk)[:, c, :], in_=xn[x4i][:, c * 128:(c + 1) * 128], identity=ident_b),
                     reads=[Bxn[x4i], Bc], writes=[PB[tbk]])
            S.op("dve", L("tensor_tensor", out=evt[xb], in0=pbf(tbk), in1=AT.unsqueeze(2).to_broadcast([128, 8, 128]), op=ALU.mult),
                 reads=[PB[tbk], Bvec], writes=[Bev[xb]])
            S.op("pool", L("tensor_tensor", out=hxT[hb][:, :, t * 128:(t + 1) * 128], in0=evt[xb],
                           in1=BT.unsqueeze(2).to_broadcast([128, 8, 128]), op=ALU.add), reads=[Bev[xb], Bvec], writes=[Bhx[hb]])
        ch = G // 2 if G < 16 else 8
        ko = (G % 2) * 512 if G < 16 else 0
        sb = G % 2
        jobs = [(KA + h * 128, n, kst[sb][:, h, 0:n], [Bkst[sb]]) for h in range(4)]
        if G < 5 or G == 16:
            wo = G * 512 if G < 16 else WROWS
            jobs.append((KB, n, KbT[:, wo:wo + n], [BKb]))
        if G < 5:
            for (base, dstT) in ((QA, QaT), (QB, QbT)):
                for h in range(4):
                    jobs.append((base + h * 128, 512, dstT[:, h, G * 512:(G + 1) * 512], [BQ]))
        st_ = projA(jobs[0], hb, tb)
        for ji in range(len(jobs)):
            nxt = projA(jobs[ji + 1], hb, tb) if ji + 1 < len(jobs) else None
            ropeB(st_)
            st_ = nxt
            if ji == 3:
                for h in range(4):
                    S.dma("sp", kscr[h, ch, :, ko:ko + n], kst[sb][:, h, 0:n], reads=[Bkst[sb]], writes=[Buf()])
        for t in range(ntile):
            bk = nextbank()
            for c in range(8):
                S.op("pe", L("matmul", psum[:, bk, :], lhsT=hxT[hb][:, c, t * 128:(t + 1) * 128], rhs=win[:, c, VA:VA + 512],
                             start=(c == 0), stop=(c == 7)), reads=[Bwin, Bhx[hb]], writes=[PB[bk]])
            S.op("act", L("activation", out=vst[sb][:, :, t, :], in_=psum[:, bk, :].rearrange("p (h e) -> p h e", e=128), func=AF.Copy),
                 reads=[PB[bk]], writes=[Bvst[sb]])
        for h in range(4):
            tl0 = (G % 2) * 4 if G < 16 else 0
            S.dma("sp", vscr[h, ch, :, tl0:tl0 + ntile, :], vst[sb][:, h, 0:ntile, :], reads=[Bvst[sb]], writes=[Buf()])
        if G < 5 or G == 16:
            bk = nextbank()
            for t in range(ntile):
                for c in range(8):
                    S.op("pe", L("matmul", psum[:, bk, t * 128:(t + 1) * 128], lhsT=hxT[hb][:, c, t * 128:(t + 1) * 128],
                                 rhs=win[:, c, VB:VB + 128], start=(c == 0), stop=(c == 7)), reads=[Bwin, Bhx[hb]], writes=[PB[bk]])
            vt0 = G * 4 if G < 16 else 20
            S.op("act", L("activation", out=Vb[:, vt0:vt0 + ntile, :], in_=psum[:, bk, 0:n].rearrange("p (t e) -> p t e", e=128), func=AF.Copy),
                 reads=[PB[bk]], writes=[BVb])

    if stop_after == 1:
        return finish()
    S.barrier()
    off[0] = mark_attn
    yaT = alloc([128, 4, NQ], BF16); ybT = alloc([128, 4, NQ], BF16)
    mark_att2 = off[0]

    kch = [alloc([128, 1024], BF16) for _ in range(3)]; vch = [alloc([128, 8, 128], BF16) for _ in range(3)]
    Bkch = [Buf(f"kch{i}") for i in range(3)]; Bvch = [Buf(f"vch{i}") for i in range(3)]
    PT = [alloc([128, 1024], BF16) for _ in range(3)]; BPT = [Buf(f"PT{i}") for i in range(3)]
    r1 = alloc([128, 512]); r2 = alloc([128, 512]); t1 = alloc([128, 512]); t2 = alloc([128, 512])
    ot = alloc([128, 512]); osq = alloc([128, 512]); rs = alloc([128, 512]); zsb = alloc([64, 512])
    selA = alloc([64, 128]); selB = alloc([64, 128])
    o1sb = alloc([128, 512]); o2sb = alloc([128, 512])
    Bpp = [Buf(f"pp{i}") for i in range(10)]; Bsel = Buf("sel")
    S.op("dve", L("memset", selA, 0.0), writes=[Bsel]); S.op("dve", L("memset", selB, 0.0), writes=[Bsel])
    S.op("dve", L("memset", selA[0:32, :], 1.0 / 32), writes=[Bsel]); S.op("dve", L("memset", selB[32:64, :], 1.0 / 32), writes=[Bsel])
    if do_moe:
        zer = alloc([128, 4, D], BF16); Bzer = Buf("zer")
        S.op("pool", L("memset", zer, 0.0), writes=[Bzer])
        Bxe0 = []
        xev = xe[0:NSLOT, :].rearrange("(n p) d -> p n d", p=128)
        for i in range(NSLOT // 128 // 4):
            bz = Buf(); Bxe0.append(bz)
            S.dma("pool", xev[:, i * 4:(i + 1) * 4, :], zer, reads=[Bzer], writes=[bz])

    chunks = [(ci, 8) for ci in range(8)] + [(8, 2)]
    seq = [(h, qb, ci, nt) for h in range(4) for qb in range(4) for (ci, nt) in chunks]

    def issue_load(j):
        if j >= len(seq): return
        h, qb, ci, nt = seq[j]
        bi = j % 3
        S.dma("sp", kch[bi][:, 0:nt * 128], kscr[h, ci, :, 0:nt * 128], reads=[Bks], writes=[Bkch[bi]])
        S.dma("sp", vch[bi][:, 0:nt, :], vscr[h, ci, :, 0:nt, :], reads=[Bvs], writes=[Bvch[bi]])

    blocks = []
    for h in range(4):
        for qb in range(4):
            units = []
            for j0, (ci, nt) in enumerate(chunks):
                j = (h * 4 + qb) * 9 + j0
                for tt in range(nt):
                    units.append((j, tt, ci))
            blocks.append((h, qb, units))
    NU = len(blocks[0][2])

    def emit_S(bidx, k):
        h, qb, units = blocks[bidx]
        q0 = 128 + qb * 512
        j, tt, ci = units[k]
        g = bidx * NU + k
        bi = j % 3; sbk = (g % 2) * 2
        for cmp_ in range(2):
            S.op("pe", L("matmul", psum[:, sbk + cmp_, :], lhsT=kch[bi][cmp_ * 64:(cmp_ + 1) * 64, tt * 128:(tt + 1) * 128],
                         rhs=QaT[cmp_ * 64:(cmp_ + 1) * 64, h, q0:q0 + 512], start=True, stop=True),
                 reads=[Bkch[bi], BQ], writes=[PB[sbk + cmp_]])

    def post_stages(h, qb):
        def s1():
            S.op("pe", L("matmul", psum[:, 7, :], lhsT=selA, rhs=zsb, start=True, stop=True), reads=[Bpp[7], Bsel], writes=[PB[7]])
        def s2():
            S.op("dve", L("reciprocal", out=r1, in_=psum[:, 7, :]), reads=[PB[7]], writes=[Bpp[0]])
        def s3():
            S.op("pe", L("matmul", psum[:, 7, :], lhsT=selB, rhs=zsb, start=True, stop=True), reads=[Bpp[7], Bsel], writes=[PB[7]])
        def s4():
            S.op("dve", L("reciprocal", out=r2, in_=psum[:, 7, :]), reads=[PB[7]], writes=[Bpp[1]])
            S.op("dve", L("tensor_tensor", out=t1, in0=o1sb, in1=r1, op=ALU.mult), reads=[Bpp[8], Bpp[0]], writes=[Bpp[2]])
            S.op("dve", L("tensor_tensor", out=t2, in0=o2sb, in1=r2, op=ALU.mult), reads=[Bpp[9], Bpp[1]], writes=[Bpp[3]])
            S.op("dve", L("scalar_tensor_tensor", out=ot, in0=t2, scalar=neglam, in1=t1, op0=ALU.mult, op1=ALU.add),
                 reads=[Bpp[2], Bpp[3], Bvec], writes=[Bpp[4]])
        def s5():
            S.op("act", L("activation", out=osq, in_=ot, func=AF.Square), reads=[Bpp[4]], writes=[Bpp[5]])
        def s6():
            S.op("pe", L("matmul", psum[:, 7, :], lhsT=ones_f, rhs=osq, start=True, stop=True), reads=[Bpp[5], Bc], writes=[PB[7]])
        def s7():
            S.op("act", L("activation", out=rs, in_=psum[:, 7, :], func=AF.Sqrt, scale=1.0 / 128, bias=EPS), reads=[PB[7]], writes=[Bpp[6]])
        def s8():
            S.op("dve", L("reciprocal", out=rs, in_=rs), reads=[Bpp[6]], writes=[Bpp[6]])
            S.op("dve", L("scalar_tensor_tensor", out=yaT[:, h, qb * 512:(qb + 1) * 512], in0=ot, scalar=gsubp, in1=rs, op0=ALU.mult, op1=ALU.mult),
                 reads=[Bpp[4], Bpp[6], Bvec], writes=[Bya])
        return [s1, s2, s3, s4, s5, s6, s7, s8]

    STAGE_AT = {3: 0, 7: 1, 11: 2, 15: 3, 22: 4, 26: 5, 31: 6, 35: 7}
    issue_load(0); issue_load(1)
    emit_S(0, 0)
    pending = None
    for bidx, (h, qb, units) in enumerate(blocks):
        for k in range(NU):
            j, tt, ci = units[k]
            g = bidx * NU + k
            if tt == 0:
                issue_load(j + 2)
            if k + 1 < NU:
                emit_S(bidx, k + 1)
            bi = j % 3; sbk = (g % 2) * 2; pi = g % 3
            S.op("act", L("activation", out=PT[pi], in_=pflat(sbk, 2), func=AF.Exp, scale=0.125),
                 reads=[PB[sbk], PB[sbk + 1]], writes=[BPT[pi]])
            first = (k == 0); last = (k == NU - 1)
            for cmp_ in range(2):
                S.op("pe", L("matmul", psum[:, 4 + cmp_, :], lhsT=vch[bi][:, tt, :], rhs=PT[pi][:, cmp_ * 512:(cmp_ + 1) * 512], start=first, stop=last),
                     reads=[Bvch[bi], BPT[pi]], writes=[PB[4 + cmp_]])
            for cmp_ in range(2):
                S.op("pe", L("matmul", psum[cmp_ * 32:(cmp_ + 1) * 32, 6, :], lhsT=ones_b[:, 0:32], rhs=PT[pi][:, cmp_ * 512:(cmp_ + 1) * 512],
                             start=first, stop=last, tile_position=(0, cmp_ * 32)),
                     reads=[BPT[pi], Bc], writes=[PB[6]])
            if pending is not None and k in STAGE_AT:
                pending[STAGE_AT[k]]()
        if bidx + 1 < len(blocks):
            emit_S(bidx + 1, 0)
        S.op("act", L("activation", out=o1sb, in_=psum[:, 4, :], func=AF.Copy), reads=[PB[4]], writes=[Bpp[8]])
        S.op("act", L("activation", out=o2sb, in_=psum[:, 5, :], func=AF.Copy), reads=[PB[5]], writes=[Bpp[9]])
        S.op("dve", L("tensor_copy", out=zsb, in_=psum[0:64, 6, :]), reads=[PB[6]], writes=[Bpp[7]])
        pending = post_stages(h, qb)
    for st_fn in pending:
        st_fn()

    if stop_after == 2:
        return finish()
    NPW = 4
    PW = [alloc([128, 512], BF16) for _ in range(NPW)]; BPW = [Buf(f"PW{i}") for i in range(NPW)]
    zt = [alloc([128, 512]) for _ in range(2)]; Bzt = [Buf("zt0"), Buf("zt1")]
    WP = []
    for nb in range(16):
        for k, (kt, kind) in enumerate([(nb, "L"), (nb + 1, "C"), (nb + 2, "R"), (20, "X"), (21, "X")]):
            WP.append((nb, k, kt, kind))

    def emit_WS(p):
        nb, k, kt, kind = WP[p]
        kc0 = kt * 128; q0 = 128 + nb * 128
        for g in range(2):
            gs = slice(g * 64, (g + 1) * 64)
            sbk = 2 * (p % 2) + g
            S.op("pe", L("matmul", psum[:, sbk, :].rearrange("p (i q) -> p i q", q=128), lhsT=KbT[gs, kc0:kc0 + 128], rhs=QbT[gs, :, q0:q0 + 128],
                         start=True, stop=True), reads=[BKb, BQ], writes=[PB[sbk]])

    emit_WS(0); emit_WS(1)
    for p, (nb, k, kt, kind) in enumerate(WP):
        ob = 4 + 2 * (nb % 2)
        if kind == "L" and nb == 0:
            bias = wmask[:, 0:1]
        elif kind == "R" and nb == 15:
            bias = wmask[:, 1:2]
        else:
            bias = 0.0
        for g in range(2):
            sbk = 2 * (p % 2) + g; pi = 2 * (p % 2) + g
            S.op("act", L("activation", out=PW[pi], in_=psum[:, sbk, :], func=AF.Exp, scale=0.125, bias=bias),
                 reads=[PB[sbk], Bc], writes=[BPW[pi]])
            if kind in ("L", "R"):
                tri = triL if kind == "L" else triR
                pw3 = PW[pi].rearrange("p (i q) -> p i q", q=128)
                S.op("dve", L("tensor_tensor", out=pw3, in0=pw3, in1=tri.unsqueeze(1).to_broadcast([128, 4, 128]), op=ALU.mult),
                     reads=[BPW[pi], Bc], writes=[BPW[pi]])
        first = (k == 0); last = (k == 4)
        for g in range(2):
            gs = slice(g * 64, (g + 1) * 64); pi = 2 * (p % 2) + g
            S.op("pe", L("matmul", psum[gs, ob, :], lhsT=Vb[:, kt, gs], rhs=PW[pi], start=first, stop=last, tile_position=(0, g * 64)),
                 reads=[BVb, BPW[pi]], writes=[PB[ob]])
        for g in range(2):
            gs = slice(g * 64, (g + 1) * 64); pi = 2 * (p % 2) + g
            S.op("pe", L("matmul", psum[gs, ob + 1, :], lhsT=ones_b[:, 0:64], rhs=PW[pi], start=first, stop=last, tile_position=(0, g * 64)),
                 reads=[Bc, BPW[pi]], writes=[PB[ob + 1]])
        if p + 2 < len(WP):
            emit_WS(p + 2)
        if k == 4:
            zi = nb % 2
            z3 = zt[zi].rearrange("p (i q) -> p i q", q=128)
            S.op("dve", L("tensor_tensor", out=z3, in0=psum[:, ob + 1, :].rearrange("p (i q) -> p i q", q=128),
                          in1=sinkexp.unsqueeze(2).to_broadcast([128, 4, 128]), op=ALU.add), reads=[PB[ob + 1], Bvec], writes=[Bzt[zi]])
            S.op("dve", L("reciprocal", out=zt[zi], in_=zt[zi]), reads=[Bzt[zi]], writes=[Bzt[zi]])
            S.op("dve", L("tensor_tensor", out=ybT[:, :, nb * 128:(nb + 1) * 128], in0=psum[:, ob, :].rearrange("p (i q) -> p i q", q=128),
                          in1=z3, op=ALU.mult), reads=[PB[ob], Bzt[zi]], writes=[Byb])

    if dbg:
        S.dma("sp", dbg_kb, KbT, reads=[BKb])
        S.dma("sp", dbg_vb, Vb.rearrange("p a b -> p (a b)"), reads=[BVb])
        S.dma("sp", dbg_qb, QbT.rearrange("p a b -> p (a b)"), reads=[BQ])
        S.dma("sp", dbg_yb, ybT.rearrange("p a b -> p (a b)"), reads=[Byb])
    if stop_after == 3:
        return finish()
    S.barrier()
    off[0] = mark_att2
    wout = alloc([128, 8, D], BF16); Bwo = Buf("wout")
    wr_b = alloc([128, 8, NE], BF16); wr_f = alloc([128, 8, NE]); br_b = alloc([1, NE], BF16); br_f = alloc([1, NE])
    Bwr = Buf("wr")
    x4 = [alloc([128, D]) for _ in range(2)]; Bx4 = [Buf("x4a"), Buf("x4b")]
    tm4 = alloc([128, D]); Btm4 = Buf("tm4")
    x1t = [alloc([128, D]) for _ in range(2)]; Bx1 = [Buf("x1a"), Buf("x1b")]
    h2 = alloc([128, D]); Bh2 = Buf("h2")
    hx2all = alloc([128, 16, D], BF16); Bhx2t = [Buf(f"hx2_{i}") for i in range(16)]
    hx2T = alloc([128, 8, 128], BF16); Bhx2T = Buf("hx2T")
    s4 = [alloc([128, 4]) for _ in range(2)]; Bs4 = [Buf("s4a"), Buf("s4b")]
    lg = alloc([128, NE]); m8 = alloc([128, 8]); i8 = alloc([128, 8], U32); ekf = alloc([128, 8]); nm0 = alloc([128, 1])
    ge = alloc([128, 4]); gz = alloc([128, 1]); maskb = alloc([128, NE], BF16); destf = alloc([128, NE]); prod = alloc([128, NE])
    dk = alloc([128, 4]); Brr = Buf("rr")
    Bx1s = Buf("x1scr"); Bxes = [Buf(f"xes{i}") for i in range(64)]

    woutv = w_out.rearrange("(c p) n -> p c n", p=128)
    S.dma("pool", wout[:, 0:4, :], woutv[:, 0:4, :], writes=[Bwo])
    for gi in range(4):
        for g in range(2):
            r0 = 512 + g * 256 + gi * 64
            S.dma("pool", wout[g * 64:(g + 1) * 64, 4 + gi, :], w_out[r0:r0 + 64, :], writes=[Bwo])
    S.dma("sp", wr_f, w_r.rearrange("(c p) n -> p c n", p=128), writes=[Bwr])
    S.dma("sp", br_f, b_r.rearrange("(o n) -> o n", o=1), writes=[Bwr])
    S.op("dve", L("tensor_copy", out=wr_b, in_=wr_f), reads=[Bwr], writes=[Bwr])
    S.op("dve", L("tensor_copy", out=br_b, in_=br_f), reads=[Bwr], writes=[Bwr])

    lgs = [alloc([128, NE]) for _ in range(2)]; Blg = [Buf("lg0"), Buf("lg1")]

    def stageA(t):
        b = t % 2
        pb0 = 2 * b
        S.dma("sp", x4[b], xkv[128 + t * 128:128 + (t + 1) * 128, :], writes=[Bx4[b]])
        for hf in range(2):
            for c in range(8):
                lhs = yaT[:, c, t * 128:(t + 1) * 128] if c < 4 else ybT[:, c - 4, t * 128:(t + 1) * 128]
                S.op("pe", L("matmul", psum[:, pb0 + hf, :], lhsT=lhs, rhs=wout[:, c, hf * 512:(hf + 1) * 512],
                                                                             start=(c == 0), stop=(c == 7)),
                     reads=[Bya, Byb, Bwo], writes=[PB[pb0 + hf]])
        S.op("dve", L("tensor_tensor", out=tm4, in0=pflat(pb0, 2), in1=G1bc, op=ALU.mult), reads=[PB[pb0], PB[pb0 + 1], Bbc], writes=[Btm4])
        S.op("pool", L("tensor_tensor", out=x1t[b], in0=tm4, in1=x4[b], op=ALU.add), reads=[Btm4, Bx4[b]], writes=[Bx1[b]])
        S.dma("sp", x1scr[t * 128:(t + 1) * 128, :], x1t[b], reads=[Bx1[b]], writes=[Bx1s])
        if dbg:
            S.dma("sp", dbg_x1[t * 128:(t + 1) * 128, :], x1t[b], reads=[Bx1[b]])
        if not do_moe:
            return
        S.op("act", L("activation", out=junk, in_=x1t[b], func=AF.Square, accum_out=s4[b][:, 0:1]), reads=[Bx1[b]], writes=[Bjunk, Bs4[b]])
        S.op("act", L("activation", out=s4[b][:, 1:2], in_=s4[b][:, 0:1], func=AF.Sqrt, scale=1.0 / D, bias=EPS), reads=[Bs4[b]], writes=[Bs4[b]])
        S.op("dve", L("reciprocal", out=s4[b][:, 2:3], in_=s4[b][:, 1:2]), reads=[Bs4[b]], writes=[Bs4[b]])
        S.op("dve", L("scalar_tensor_tensor", out=h2, in0=x1t[b], scalar=s4[b][:, 2:3], in1=A2bc, op0=ALU.mult, op1=ALU.mult),
             reads=[Bx1[b], Bs4[b], Bbc], writes=[Bh2])
        S.op("pool", L("tensor_tensor", out=hx2all[:, t, :], in0=h2, in1=B2bc, op=ALU.add), reads=[Bh2, Bbc], writes=[Bhx2t[t]])
        for c in range(8):
            S.op("pe", L("transpose", out=pbf(4)[:, c, :], in_=hx2all[:, t, c * 128:(c + 1) * 128], identity=ident_b),
                 reads=[Bhx2t[t], Bc], writes=[PB[4]])
        S.op("act", L("activation", out=hx2T, in_=pbf(4), func=AF.Copy), reads=[PB[4]], writes=[Bhx2T])
        for c in range(8):
            S.op("pe", L("matmul", psum[:, 5, 0:NE], lhsT=hx2T[:, c, :], rhs=wr_b[:, c, :], start=(c == 0), stop=False),
                 reads=[Bhx2T, Bwr], writes=[PB[5]])
        S.op("pe", L("matmul", psum[:, 5, 0:NE], lhsT=ones_b[0:1, :], rhs=br_b, start=False, stop=True), reads=[Bwr, Bc], writes=[PB[5]])
        S.op("dve", L("tensor_copy", out=lgs[b], in_=psum[:, 5, 0:NE]), reads=[PB[5]], writes=[Blg[b]])

    def stageB(t):
        b = t % 2
        S.op("dve", L("max", out=m8, in_=lgs[b]), reads=[Brr, Blg[b]], writes=[Brr])
        S.op("dve", L("max_index", out=i8, in_max=m8, in_values=lgs[b]), reads=[Brr, Blg[b]], writes=[Brr])
        S.op("dve", L("tensor_copy", out=ek_all[:, t * 4:(t + 1) * 4], in_=i8[:, 0:4]), reads=[Brr], writes=[Brt])
        S.op("dve", L("tensor_scalar", out=nm0, in0=m8[:, 0:1], scalar1=-1.0, scalar2=None, op0=ALU.mult), reads=[Brr], writes=[Brr])
        S.op("act", L("activation", out=ge, in_=m8[:, 0:4], func=AF.Exp, bias=nm0, accum_out=gz), reads=[Brr], writes=[Brr])
        S.op("dve", L("reciprocal", out=gz, in_=gz), reads=[Brr], writes=[Brr])
        S.op("dve", L("tensor_scalar", out=gates[:, t * 4:(t + 1) * 4], in0=ge, scalar1=gz, scalar2=None, op0=ALU.mult), reads=[Brr], writes=[Brt])
        S.op("dve", L("tensor_scalar", out=maskb, in0=lgs[b], scalar1=m8[:, 3:4], scalar2=None, op0=ALU.is_ge), reads=[Brr, Blg[b]], writes=[Brr])
        S.op("pe", L("matmul", psum[:, 6, 0:NE], lhsT=U_b, rhs=maskb, start=True, stop=True), reads=[Brr, Bc], writes=[PB[6]])
        S.op("pe", L("matmul", psum[:, 7, 0:NE], lhsT=ones_b, rhs=maskb, start=True, stop=True), reads=[Brr, Bc], writes=[PB[7]])
        S.op("dve", L("tensor_tensor", out=destf, in0=psum[:, 6, 0:NE], in1=basecnt, op=ALU.add), reads=[PB[6], Brt], writes=[Brr])
        S.op("dve", L("tensor_tensor", out=basecnt, in0=psum[:, 7, 0:NE], in1=basecnt, op=ALU.add), reads=[PB[7], Brt], writes=[Brt])
        for k in range(4):
            col = t * 4 + k
            S.op("dve", L("scalar_tensor_tensor", out=prod, in0=iota_f, scalar=ek_all[:, col:col + 1], in1=destf, op0=ALU.is_equal, op1=ALU.mult,
                          accum_out=rk_all[:, col:col + 1]), reads=[Brr, Bc, Brt], writes=[Brr, Brt])

    stageA(0)
    for t in range(16):
        if t + 1 < 16:
            stageA(t + 1)
        if do_moe:
            stageB(t)

    if do_moe:
        cntcol = alloc([32, 1]); tmp32 = alloc([32, 32]); G32 = alloc([32, 32]); T32 = alloc([32, 32]); poscol = alloc([32, 1]); pos2 = alloc([32, 1])
        PmT = alloc([32, 32]); OFFpos = alloc([128, 32]); offk = alloc([128, 64]); dst64 = alloc([128, 64]); t1p = alloc([128, 32])
        carr = alloc([128, 8]); idxf = alloc([128, 32, 8])
        Bso = Buf("sort")
        S.op("dve", L("tensor_tensor", out=tmp32, in0=basecnt[0:32, :], in1=ident_f[0:32, 0:32], op=ALU.mult), reads=[Brt, Bc], writes=[Bso])
        S.op("dve", L("reduce_sum", out=cntcol, in_=tmp32, axis=AX.X), reads=[Bso], writes=[Bso])
        S.op("dve", L("tensor_scalar", out=G32, in0=basecnt[0:32, :], scalar1=cntcol, scalar2=None, op0=ALU.is_gt), reads=[Brt, Bso], writes=[Bso])
        S.op("dve", L("scalar_tensor_tensor", out=T32, in0=basecnt[0:32, :], scalar=cntcol, in1=Lmask, op0=ALU.is_equal, op1=ALU.mult),
             reads=[Brt, Bso, Bc], writes=[Bso])
        S.op("dve", L("tensor_tensor", out=G32, in0=G32, in1=T32, op=ALU.add), reads=[Bso], writes=[Bso])
        S.op("dve", L("reduce_sum", out=poscol, in_=G32, axis=AX.X), reads=[Bso], writes=[Bso])
        S.op("dve", L("tensor_scalar", out=Pm, in0=iota_f[0:32, :], scalar1=poscol, scalar2=None, op0=ALU.is_equal), reads=[Bso, Bc], writes=[Brt])
        S.op("pe", L("matmul", psum[0:32, 0, 0:32], lhsT=Pm, rhs=ident_f[0:32, 0:32], start=True, stop=True), reads=[Brt, Bc], writes=[PB[0]])
        S.op("dve", L("tensor_copy", out=PmT, in_=psum[0:32, 0, 0:32]), reads=[PB[0]], writes=[Bso])
        S.op("pe", L("matmul", psum[:, 1, 0:32], lhsT=OFFrep, rhs=PmT, start=True, stop=True), reads=[Bso, Bc], writes=[PB[1]])
        S.op("dve", L("tensor_scalar", out=OFFpos, in0=psum[:, 1, 0:32], scalar1=128.0, scalar2=None, op0=ALU.mult), reads=[PB[1]], writes=[Bso])
        S.op("pe", L("matmul", psum[:, 2, 0:32], lhsT=pidx[0:32, :], rhs=Pm, start=True, stop=True), reads=[Brt, Bc], writes=[PB[2]])
        S.op("dve", L("tensor_copy", out=permbc, in_=psum[:, 2, 0:32]), reads=[PB[2]], writes=[Brt])
        for col in range(64):
            S.op("dve", L("scalar_tensor_tensor", out=prod, in0=iota_f, scalar=ek_all[:, col:col + 1], in1=OFFpos, op0=ALU.is_equal, op1=ALU.mult,
                          accum_out=offk[:, col:col + 1]), reads=[Brt, Bc, Bso], writes=[Brr, Bso])
        S.op("dve", L("tensor_tensor", out=dst64, in0=offk, in1=rk_all, op=ALU.add), reads=[Bso, Brt], writes=[Bso])
        S.op("dve", L("tensor_copy", out=desti, in_=dst64), reads=[Bso], writes=[Brt])
        S.op("dve", L("scalar_tensor_tensor", out=t1p, in0=permbc, scalar=1024.0, in1=pidx[:, 0:32], op0=ALU.mult, op1=ALU.add), reads=[Brt, Bc], writes=[Bso])
        S.op("dve", L("tensor_scalar", out=carr, in0=iota_f[:, 0:8], scalar1=128.0, scalar2=None, op0=ALU.mult), reads=[Bc], writes=[Bso])
        S.op("dve", L("tensor_tensor", out=idxf, in0=t1p.unsqueeze(2).to_broadcast([128, 32, 8]), in1=carr.unsqueeze(1).to_broadcast([128, 32, 8]), op=ALU.add),
             reads=[Bso], writes=[Bso])
        S.op("dve", L("tensor_copy", out=idxg, in_=idxf.rearrange("p a b -> p (a b)")), reads=[Bso], writes=[Brt])
        S.op("dve", L("tensor_copy", out=bdn_idx, in_=permbc), reads=[Brt], writes=[Brt])
        for col in range(64):
            t = col // 4
            S.op("pool", L("indirect_dma_start", out=xe[:, :], out_offset=bass.IndirectOffsetOnAxis(ap=desti[:, col:col + 1], axis=0),
                           in_=hx2all[:, t, :], in_offset=None), reads=[Bhx2t[t], Brt] + Bxe0, writes=[Bxes[col]], dma=True)

    if not do_moe:
        lastd = [o for o in S.ops["sp"] if o.dma][-4:]
        S.op("sp", None, deps=[o for o in S.ops["sp"] if o.dma][-40:])
        S.emit(nc)
        return nc

    S.barrier()
    off[0] = mark_persist

    XC = 1024
    wgu = [alloc([128, 8, 2 * D], BF16) for _ in range(2)]; wdn = [alloc([128, 8, D], BF16) for _ in range(2)]
    Bwgu = [[Buf(f"wgu{b}_{c}") for c in range(8)] for b in range(2)]; Bwdn = [[Buf(f"wdn{b}_{c}") for c in range(8)] for b in range(2)]
    bdn1 = alloc([128, D]); Bbdn1 = Buf("bdn")
    bgu_f = alloc([NE, 2 * D]); biasT = alloc([128, 16, NE]); Bbias = Buf("bias")
    xet = [alloc([128, D], BF16) for _ in range(3)]; Bxet = [Buf(f"xet{i}") for i in range(3)]
    xeT = alloc([128, 8, XC], BF16); BxeT = Buf("xeT")
    aT = alloc([128, 8, XC], BF16); BaT = Buf("aT")
    g1 = [alloc([128, 512]) for _ in range(2)]; sgm = [alloc([128, 512]) for _ in range(2)]
    u1 = [alloc([128, 512]) for _ in range(2)]; gsx = [alloc([128, 512]) for _ in range(2)]
    Bg1 = [Buf("g1a"), Buf("g1b")]; Bsgm = [Buf("sga"), Buf("sgb")]; Bu1 = [Buf("u1a"), Buf("u1b")]; Bgsx = [Buf("gsa"), Buf("gsb")]
    yst = [alloc([128, D]) for _ in range(2)]; Byst = [Buf("yst0"), Buf("yst1")]

    S.dma("sp", bgu_f, b_gu, writes=[Bbias])
    for m in range(8):
        for two in range(2):
            j = m * 2 + two
            S.op("pe", L("matmul", psum[:, 0, j * NE:(j + 1) * NE], lhsT=bgu_f[:, 2 * m * 128 + two:2 * (m + 1) * 128:2],
                         rhs=Pm, start=True, stop=True), reads=[Bbias, Brt], writes=[PB[0]])
    S.op("dve", L("tensor_copy", out=biasT, in_=psum[:, 0, :].rearrange("p (j n) -> p j n", n=NE)), reads=[PB[0]], writes=[Bbias])
    S.op("dve", L("tensor_scalar", out=biasT[:, 1:16:2, :], in0=biasT[:, 1:16:2, :], scalar1=1.0, scalar2=None, op0=ALU.add), reads=[Bbias], writes=[Bbias])

    wgu_rows = w_gu.rearrange("e k n -> (e k) n"); wdn_rows = w_dn.rearrange("e k n -> (e k) n")
    IO = bass.IndirectOffsetOnAxis

    def load_w(i):
        b = i % 2
        for c in range(8):
            S.op("pool", L("indirect_dma_start", out=wgu[b][:, c, :], out_offset=None, in_=wgu_rows[:, :], in_offset=IO(ap=idxg[:, i * 8 + c:i * 8 + c + 1], axis=0)),
                 reads=[Brt], writes=[Bwgu[b][c]], dma=True)
        for c in range(8):
            S.op("pool", L("indirect_dma_start", out=wdn[b][:, c, :], out_offset=None, in_=wdn_rows[:, :], in_offset=IO(ap=idxg[:, i * 8 + c:i * 8 + c + 1], axis=0)),
                 reads=[Brt], writes=[Bwdn[b][c]], dma=True)

    def load_bdn(i):
        S.op("pool", L("indirect_dma_start", out=bdn1, out_offset=None, in_=b_dn[:, :], in_offset=IO(ap=bdn_idx[:, i:i + 1], axis=0)),
             reads=[Brt], writes=[Bbdn1], dma=True)

    work = []
    for i in range(NE):
        n = NT[i]; o = OFFT[i]
        nitem = -(-n // (XC // 128))
        sizes = [n // nitem + (1 if q < n % nitem else 0) for q in range(nitem)]
        for q, k in enumerate(sizes):
            work.append((i, o, k, q == 0, q == nitem - 1)); o += k

    load_w(0); load_bdn(0)
    pc5 = [0]; xc5 = [0]; yc5 = [0]

    def stage_x(w):
        i, o, k, first, lastw = w
        for s in range(k):
            xi = xc5[0] % 3; xc5[0] += 1
            tb_ = xi % 2
            S.dma("sp", xet[xi], xe[(o + s) * 128:(o + s + 1) * 128, :], reads=Bxes, writes=[Bxet[xi]])
            for c in range(8):
                S.op("pe", L("transpose", out=pbf(tb_)[:, c, :], in_=xet[xi][:, c * 128:(c + 1) * 128], identity=ident_b),
                     reads=[Bxet[xi], Bc], writes=[PB[tb_]])
            S.op("act", L("activation", out=xeT[:, :, s * 128:(s + 1) * 128], in_=pbf(tb_), func=AF.Copy), reads=[PB[tb_]], writes=[BxeT])

    stage_x(work[0])
    for wi, (i, o, k, first, lastw) in enumerate(work):
        b = i % 2
        if first and i + 1 < NE:
            load_w(i + 1)
        ncols = k * 128
        for m in range(8):
            nch = -(-ncols // 512); cw = ncols // nch
            for c0 in range(0, ncols, cw):
                nn = cw
                ii = pc5[0] % 2; pc5[0] += 1
                bg_, bu_ = 2 + 2 * ii, 3 + 2 * ii
                cs = slice(c0, c0 + nn)
                for (bk, two) in ((bg_, 0), (bu_, 1)):
                    for c in range(8):
                        S.op("pe", L("matmul", psum[:, bk, 0:nn], lhsT=wgu[b][:, c, 2 * m * 128 + two:2 * (m + 1) * 128:2],
                                     rhs=xeT[:, c, cs], start=(c == 0), stop=(c == 7)),
                             reads=[Bwgu[b][c], BxeT], writes=[PB[bk]])
                S.op("dve", L("tensor_scalar", out=g1[ii][:, 0:nn], in0=psum[:, bg_, 0:nn], scalar1=biasT[:, 2 * m, i:i + 1], scalar2=7.0, op0=ALU.add, op1=ALU.min),
                     reads=[PB[bg_], Bbias], writes=[Bg1[ii]])
                S.op("act", L("activation", out=sgm[ii][:, 0:nn], in_=g1[ii][:, 0:nn], func=AF.Sigmoid, scale=1.702), reads=[Bg1[ii]], writes=[Bsgm[ii]])
                S.op("dve", L("tensor_scalar", out=u1[ii][:, 0:nn], in0=psum[:, bu_, 0:nn], scalar1=biasT[:, 2 * m + 1, i:i + 1], scalar2=8.0, op0=ALU.add, op1=ALU.min),
                     reads=[PB[bu_], Bbias], writes=[Bu1[ii]])
                S.op("pool", L("tensor_tensor", out=gsx[ii][:, 0:nn], in0=g1[ii][:, 0:nn], in1=sgm[ii][:, 0:nn], op=ALU.mult), reads=[Bg1[ii], Bsgm[ii]], writes=[Bgsx[ii]])
                S.op("dve", L("scalar_tensor_tensor", out=aT[:, m, cs], in0=u1[ii][:, 0:nn], scalar=-6.0, in1=gsx[ii][:, 0:nn], op0=ALU.max, op1=ALU.mult),
                     reads=[Bu1[ii], Bgsx[ii]], writes=[BaT])
        if wi + 1 < len(work):
            stage_x(work[wi + 1])
        for s in range(k):
            yb_ = yc5[0] % 2; yc5[0] += 1
            for hf in range(2):
                bk = 6 + hf
                for m in range(8):
                    S.op("pe", L("matmul", psum[:, bk, :], lhsT=aT[:, m, s * 128:(s + 1) * 128], rhs=wdn[b][:, m, hf * 512:(hf + 1) * 512],
                                 start=(m == 0), stop=(m == 7)),
                         reads=[BaT, Bwdn[b][m]], writes=[PB[bk]])
            S.op("dve", L("tensor_tensor", out=yst[yb_], in0=pflat(6, 2), in1=bdn1, op=ALU.add), reads=[PB[6], PB[7], Bbdn1], writes=[Byst[yb_]])
            S.dma("sp", ye[(o + s) * 128:(o + s + 1) * 128, :], yst[yb_], reads=[Byst[yb_]], writes=[Buf()])
        if lastw and i + 1 < NE:
            load_bdn(i + 1)

    S.barrier()
    off[0] = mark_persist

    yk = [alloc([128, D]) for _ in range(8)]; Byk = [Buf(f"yk{i}") for i in range(8)]
    x6 = [alloc([128, D]) for _ in range(2)]; Bx6 = [Buf("x6a"), Buf("x6b")]
    acc = [alloc([128, D]) for _ in range(2)]; Bacc = [Buf("acca"), Buf("accb")]
    acc2 = [alloc([128, D]) for _ in range(2)]; Bacc2 = [Buf("acc2a"), Buf("acc2b")]
    xo = [alloc([128, D]) for _ in range(2)]; Bxo = [Buf("xoa"), Buf("xob")]
    ot6 = [alloc([128, D]) for _ in range(2)]; Bot6 = [Buf("o6a"), Buf("o6b")]
    s6 = [alloc([128, 4]) for _ in range(2)]; Bs6 = [Buf("s6a"), Buf("s6b")]
    outs = []

    def loads6(t):
        b = t % 2
        S.dma("sp", x6[b], x1scr[t * 128:(t + 1) * 128, :], reads=[Bx1s], writes=[Bx6[b]])
        for k in range(4):
            col = t * 4 + k
            S.op("pool", L("indirect_dma_start", out=yk[4 * b + k], out_offset=None, in_=ye[:, :],
                           in_offset=bass.IndirectOffsetOnAxis(ap=desti[:, col:col + 1], axis=0)),
                 reads=[Brt], writes=[Byk[4 * b + k]], dma=True)

    loads6(0)
    for t in range(16):
        b = t % 2
        if t + 1 < 16:
            loads6(t + 1)
        S.op("act", L("activation", out=acc[b], in_=yk[4 * b], func=AF.Copy, scale=gates[:, t * 4:t * 4 + 1]), reads=[Byk[4 * b], Brt], writes=[Bacc[b]])
        for k in range(1, 4):
            S.op("dve", L("scalar_tensor_tensor", out=acc[b], in0=yk[4 * b + k], scalar=gates[:, t * 4 + k:t * 4 + k + 1], in1=acc[b], op0=ALU.mult, op1=ALU.add),
                 reads=[Byk[4 * b + k], Brt, Bacc[b]], writes=[Bacc[b]])
        S.op("dve", L("tensor_tensor", out=acc2[b], in0=acc[b], in1=G2bc, op=ALU.mult), reads=[Bacc[b], Bbc], writes=[Bacc2[b]])
        S.op("pool", L("tensor_tensor", out=xo[b], in0=acc2[b], in1=x6[b], op=ALU.add), reads=[Bacc2[b], Bx6[b]], writes=[Bxo[b]])
        S.op("act", L("activation", out=junk, in_=xo[b], func=AF.Square, accum_out=s6[b][:, 0:1]), reads=[Bxo[b]], writes=[Bjunk, Bs6[b]])
        S.op("act", L("activation", out=s6[b][:, 1:2], in_=s6[b][:, 0:1], func=AF.Sqrt, scale=1.0 / D, bias=EPS), reads=[Bs6[b]], writes=[Bs6[b]])
        S.op("dve", L("reciprocal", out=s6[b][:, 2:3], in_=s6[b][:, 1:2]), reads=[Bs6[b]], writes=[Bs6[b]])
        S.op("dve", L("scalar_tensor_tensor", out=ot6[b], in0=xo[b], scalar=s6[b][:, 2:3], in1=FGbc, op0=ALU.mult, op1=ALU.mult),
             reads=[Bxo[b], Bs6[b], Bbc], writes=[Bot6[b]])
        outs.append(S.dma("sp", outd[t * 128:(t + 1) * 128, :], ot6[b], reads=[Bot6[b]]))
    S.op("sp", None, deps=outs)
    S.emit(nc)
    return nc


def _rope_tables(pos):
    p = np.arange(128); d = p % 64; f = d % 16
    inv = (np.float32(10000.0) ** (-(np.arange(16, dtype=np.float32)) / np.float32(16))).astype(np.float32)
    row = (pos // 64).astype(np.float32); col = (pos % 64).astype(np.float32)
    coord = np.where((d < 32)[:, None], row[None, :], col[None, :]).astype(np.float32)
    ang = (coord * inv[f][:, None]).astype(np.float32)
    return np.cos(ang).astype(np.float32), np.sin(ang).astype(np.float32)


def _consts():
    c = np.zeros((128, 9, 128), np.float32)
    c[:, 0, :] = np.eye(128, dtype=np.float32)
    R = np.zeros((128, 128), np.float32)
    for m in range(128):
        if (m % 32) < 16: R[m, m + 16] = -1.0
        else: R[m, m - 16] = 1.0
    c[:, 1, :] = R.T
    j = np.arange(128)[:, None]; i = np.arange(128)[None, :]
    c[:, 2, :] = (j < i).astype(np.float32)
    c[:, 3, :] = (j >= i).astype(np.float32)
    c[:, 4, :] = (j <= i).astype(np.float32)
    c[:, 5, :] = np.arange(128, dtype=np.float32)[None, :]
    c[:, 6, :] = np.arange(128, dtype=np.float32)[:, None]
    c[0:NE, 7, :] = np.asarray(OFFT, np.float32)[:, None]
    c[:, 8, :] = (i < j).astype(np.float32)
    return c.reshape(128, 9 * 128)


def make_in_maps(inp, do_moe=True):
    x = np.asarray(inp["x"], np.float32); ctx = np.asarray(inp["ctx"], np.float32)
    c = np.asarray(inp["c"], np.float32); c_ctx = np.asarray(inp["c_ctx"], np.float32)
    consts = _consts()
    lam = np.stack([np.asarray(inp[k], np.float32)[0] for k in ("lam_q1", "lam_k1", "lam_q2", "lam_k2")], 0)
    shared = {
        "consts": consts, "w_mod": np.ascontiguousarray(inp["w_mod"][0]), "b_mod": np.ascontiguousarray(inp["b_mod"][0]),
        "norm1_g": np.ascontiguousarray(inp["norm1_g"][0]), "w_in": np.ascontiguousarray(inp["w_in"][0]), "lam": np.ascontiguousarray(lam),
        "subln_g": np.ascontiguousarray(inp["subln_g"][0]), "sink": np.ascontiguousarray(inp["sink"][0]),
        "w_out": np.ascontiguousarray(inp["w_out"][0]), "norm2_g": np.ascontiguousarray(inp["norm2_g"][0]),
        "w_router": np.ascontiguousarray(inp["w_router"][0]), "b_router": np.ascontiguousarray(inp["b_router"][0]),
        "final_g": np.ascontiguousarray(inp["final_g"]),
    }
    if do_moe:
        shared.update({"w_gate_up": np.ascontiguousarray(inp["w_gate_up"][0]), "b_gate_up": np.ascontiguousarray(inp["b_gate_up"][0]),
                       "w_down": np.ascontiguousarray(inp["w_down"][0]), "b_down": np.ascontiguousarray(inp["b_down"][0])})
    shared = {k: np.asarray(v, np.float32) for k, v in shared.items()}
    maps = []
    for core in range(8):
        b, j = core // 4, core % 4
        qs = j * NQ
        shift = qs - 128
        pos = (np.arange(S_LEN) + shift) % S_LEN
        cos, sin = _rope_tables(pos)
        cosT = np.concatenate([cos, np.ones((128, CTX), np.float32)], 1)
        sinT = np.concatenate([sin, np.zeros((128, CTX), np.float32)], 1)
        wmask = np.zeros((128, 2), np.float32)
        if j == 0: wmask[:, 0] = -1e30
        if j == 3: wmask[:, 1] = -1e30
        m = dict(shared)
        m.update({"xkv": np.ascontiguousarray(np.roll(x[b], -shift, axis=0)), "ctx": np.ascontiguousarray(ctx[b]),
                  "cvec": np.ascontiguousarray(np.stack([c[b], c_ctx], 0)), "cosT": cosT, "sinT": sinT, "wmask": wmask})
        maps.append(m)
    return maps


_NC_CACHE = {}


def kernel(**inputs):
    if "nc" not in _NC_CACHE:
        _NC_CACHE["nc"] = build_program(do_moe=True)
    nc = _NC_CACHE["nc"]
    maps = make_in_maps(inputs, do_moe=True)
    res = run_bass_kernel_spmd(nc, maps, core_ids=list(range(8)))
    out = np.empty((2, S_LEN, D), np.float32)
    for core in range(8):
        b, j = core // 4, core % 4
        out[b, j * NQ:(j + 1) * NQ, :] = res.results[core]["out"]
    return out
```

```python
import contextlib
import numpy as np
import concourse.bass as bass
import concourse.mybir as mybir
from concourse.bass_utils import run_bass_kernel_spmd

F32 = mybir.dt.float32; BF16 = mybir.dt.bfloat16; I32 = mybir.dt.int32; U8 = mybir.dt.uint8
U32 = mybir.dt.uint32
ALU = mybir.AluOpType; AF = mybir.ActivationFunctionType
AX = mybir.AxisListType
ENGS = ("pe", "act", "dve", "pool", "sp")
NDMASEM = 12

D = 1024; S_LEN = 8192; CTX = 256; NQ = 2048; NE = 32;
NT = [-(-min(2048, -(-8192 // (i + 1))) // 128) for i in range(NE)]
OFFT = [sum(NT[:i]) for i in range(NE)]
NSLOT = sum(NT) * 128; NSLOTP = NSLOT
QA, KA, VA, QB, KB, VB = 0, 512, 1024, 1536, 2048, 2176
NKT = 66
WROWS = 2560
EPS = 1e-6
LAM_INIT = 0.2


def L(method, *args, **kw):
    return lambda e: getattr(e, method)(*args, **kw)


class Buf:
    __slots__ = ("name", "w", "r", "excl")
    def __init__(self, name="", excl=False):
        self.name = name; self.w = None; self.r = []; self.excl = excl


class Op:
    __slots__ = ("eng", "fn", "deps", "dma", "signal", "sem", "val", "idx", "name")
    def __init__(self, eng, fn, dma, name):
        self.eng = eng; self.fn = fn; self.deps = []; self.dma = dma
        self.signal = False; self.sem = None; self.val = 0; self.idx = 0; self.name = name


class Sched:
    def __init__(self):
        self.ops = {e: [] for e in ENGS}
        self.all = []

    def op(self, eng, fn, reads=(), writes=(), dma=False, deps=(), name=""):
        o = Op(eng, fn, dma, name)
        ds = {}
        ex = [b for b in reads if b.excl]
        if ex:
            reads = [b for b in reads if not b.excl]; writes = list(writes) + ex
        for b in reads:
            if b.w is not None: ds[id(b.w)] = b.w
        for b in writes:
            if b.w is not None: ds[id(b.w)] = b.w
            lastr = {}
            for r in b.r:
                if r.dma: ds[id(r)] = r
                elif r.eng not in lastr or r.idx > lastr[r.eng].idx: lastr[r.eng] = r
            for r in lastr.values(): ds[id(r)] = r
        for d in deps: ds[id(d)] = d
        o.deps = list(ds.values())
        for b in reads: b.r.append(o)
        for b in writes:
            b.w = o; b.r = []
        o.idx = len(self.ops[eng]); self.ops[eng].append(o); self.all.append(o)
        return o

    def dma(self, eng, out, in_, reads=(), writes=(), name="", **kw):
        return self.op(eng, L("dma_start", out=out, in_=in_, **kw), reads, writes, dma=True, name=name)

    def barrier(self):
        lasts = []
        for e in ENGS:
            real = [o for o in self.ops[e] if o.fn is not None]
            comp = [o for o in real if not o.dma]
            if comp: lasts.append(comp[-1])
            lasts.extend([o for o in real if o.dma][-NDMASEM:])
        for e in ENGS:
            self.op(e, None, deps=lasts, name="barrier")

    def emit(self, nc):
        for o in self.all:
            for d in o.deps:
                if d.eng == "pe" and o.eng == "pe" and not d.dma:
                    continue
                d.signal = True
        with contextlib.ExitStack() as st:
            csem = {e: st.enter_context(nc.semaphore("c_" + e)) for e in ENGS}
            dsem = {e: [st.enter_context(nc.semaphore(f"d_{e}{i}")) for i in range(NDMASEM)]
                    for e in ("sp", "pool", "act")}
            for e in ENGS:
                cnt = 0; di = 0; dcnt = [0] * NDMASEM; dlast = [None] * NDMASEM
                for o in self.ops[e]:
                    if o.fn is None: continue
                    if o.dma:
                        o.signal = True
                        s = di % NDMASEM; di += 1
                        if dlast[s] is not None:
                            o.deps.append(dlast[s])
                        dcnt[s] += 16; o.sem = dsem[e][s]; o.val = dcnt[s]; dlast[s] = o
                    elif o.signal:
                        cnt += 1; o.sem = csem[e]; o.val = cnt
            blk = st.enter_context(nc.Block())
            engobj = {"pe": "tensor", "act": "scalar", "dve": "vector", "pool": "gpsimd", "sp": "sync"}

            def mk(e):
                def body(eng):
                    seen = {}
                    for o in self.ops[e]:
                        need = {}
                        for d in o.deps:
                            if d.fn is None: continue
                            if d.eng == "pe" and e == "pe" and not d.dma: continue
                            k = id(d.sem)
                            if seen.get(k, 0) >= d.val: continue
                            if k not in need or need[k][1] < d.val: need[k] = (d.sem, d.val)
                        for k, (s, v) in need.items():
                            eng.wait_ge(s, v); seen[k] = v
                        if o.fn is None: continue
                        inst = o.fn(eng)
                        if o.signal:
                            inst.then_inc(o.sem, 16 if o.dma else 1)
                return body
            for e in ENGS:
                if self.ops[e]:
                    getattr(blk, engobj[e])(mk(e))


def build_program(do_moe=True, dbg=False, stop_after=99):
    nc = bass.Bass("TRN2", target_bir_lowering=False)
    dt_ = nc.dram_tensor

    def din(name, shape, dt=F32):
        return dt_(name, list(shape), dt, kind="ExternalInput").ap()

    xkv = din("xkv", [S_LEN, D]); ctxd = din("ctx", [CTX, D]); cvec = din("cvec", [2, D])
    cosd = din("cosT", [128, S_LEN + CTX]); sind = din("sinT", [128, S_LEN + CTX])
    wmaskd = din("wmask", [128, 2]); constd = din("consts", [128, 9 * 128])
    w_mod = din("w_mod", [D, 6 * D]); b_mod = din("b_mod", [6 * D]); n1g = din("norm1_g", [D])
    w_in = din("w_in", [D, 2304]); lamd = din("lam", [4, 64]); sublnd = din("subln_g", [128])
    sinkd = din("sink", [8]); w_out = din("w_out", [D, D]); n2g = din("norm2_g", [D])
    w_r = din("w_router", [D, NE]); b_r = din("b_router", [NE]); fgd = din("final_g", [D])
    if do_moe:
        w_gu = din("w_gate_up", [NE, D, 2 * D]); b_gu = din("b_gate_up", [NE, 2 * D])
        w_dn = din("w_down", [NE, D, D]); b_dn = din("b_down", [NE, D])
    outd = dt_("out", [NQ, D], F32, kind="ExternalOutput").ap()
    kscr = dt_("kscr", [4, 9, 128, 1024], BF16).ap()
    vscr = dt_("vscr", [4, 9, 128, 8, 128], BF16).ap()
    x1scr = dt_("x1scr", [NQ, D], F32).ap()
    xe = dt_("xe", [NSLOTP, D], BF16).ap()
    ye = dt_("ye", [NSLOTP, D], F32).ap()
    if dbg:
        dbg_x1 = dt_("dbg_x1", [NQ, D], F32, kind="ExternalOutput").ap()
        dbg_yb = dt_("dbg_yb", [128, 4 * NQ], BF16, kind="ExternalOutput").ap()
        dbg_kb = dt_("dbg_kb", [128, WROWS + CTX], BF16, kind="ExternalOutput").ap()
        dbg_vb = dt_("dbg_vb", [128, 22 * 128], BF16, kind="ExternalOutput").ap()
        dbg_qb = dt_("dbg_qb", [128, 4 * WROWS], BF16, kind="ExternalOutput").ap()

    TOT = 206 * 1024
    arena = nc.alloc_sbuf_tensor("arena", [128, TOT], U8)
    psum = nc.alloc_psum_tensor("psum", [128, 8, 512], F32)
    off = [0]

    def alloc(shape, dt=F32):
        n = int(np.prod(shape[1:])) * (2 if dt == BF16 else 4)
        a = arena[0:shape[0], off[0]:off[0] + n].bitcast(dt)
        off[0] += (n + 63) // 64 * 64
        assert off[0] <= TOT, ("SBUF overflow", off[0])
        if len(shape) == 3:
            a = a.rearrange("p (a b) -> p a b", b=shape[2])
        elif len(shape) == 4:
            a = a.rearrange("p (a b c) -> p a b c", b=shape[2], c=shape[3])
        return a

    def pbank(b, n=1):
        return psum[:, b:b + n, :]

    def pflat(b, n=1):
        return psum[:, b:b + n, :].rearrange("p a b -> p (a b)")

    def pbf(b):
        return psum[:, b, :].bitcast(BF16).rearrange("p (a b) -> p a b", b=128)

    S = Sched()

    def finish():
        S.barrier()
        S.emit(nc)
        return nc
    PB = [Buf(f"psum{i}", excl=True) for i in range(8)]

    cst = alloc([128, 9, 128], F32)
    ident_f = cst[:, 0, :]; iota_f = cst[:, 5, 0:32]; pidx = cst[:, 6, :]; OFFrep = cst[0:32, 7, :]; Lmask = cst[0:32, 8, 0:32]
    cstb = alloc([128, 5, 128], BF16)
    ident_b = cstb[:, 0, :]; RT_b = cstb[:, 1, :]; U_b = cstb[:, 2, :]; triL = cstb[:, 3, :]; triR = cstb[:, 4, :]
    ones_b = alloc([128, 128], BF16); ones_f = alloc([128, 128], F32)
    A1T = alloc([128, 8]); B1T = alloc([128, 8]); A1cT = alloc([128, 8]); B1cT = alloc([128, 8])
    gsubp = alloc([128, 1]); neglam = alloc([128, 1]); sinkexp = alloc([128, 4]); wmask = alloc([128, 2])
    G1bc = alloc([128, D]); A2bc = alloc([128, D]); B2bc = alloc([128, D]); G2bc = alloc([128, D]); FGbc = alloc([128, D])
    desti = alloc([128, 64], I32); gates = alloc([128, 64]); basecnt = alloc([128, 32])
    ek_all = alloc([128, 64]); rk_all = alloc([128, 64]); Pm = alloc([32, 32]); permbc = alloc([128, 32])
    idxg = alloc([128, 32 * 8], I32); bdn_idx = alloc([128, 32], I32)
    junk = alloc([128, D], BF16); Bjunk = Buf("junk")
    Bc = Buf("consts"); Bvec = Buf("vecs"); Bbc = Buf("bcast"); Brt = Buf("route")

    S.dma("sp", cst.rearrange("p a b -> p (a b)"), constd, writes=[Bc])
    S.dma("sp", wmask, wmaskd, writes=[Bc])
    S.op("dve", L("tensor_copy", out=cstb, in_=cst[:, 0:5, :]), reads=[Bc], writes=[Bc])
    S.op("dve", L("memset", ones_b, 1.0), writes=[Bc])
    S.op("dve", L("memset", ones_f, 1.0), writes=[Bc])
    S.op("dve", L("memset", basecnt, 0.0), writes=[Brt])

    mark_persist = off[0]

    cT = alloc([128, 8, 2]); sg = alloc([128, 8, 2]); scT = alloc([128, 8, 2]); scT_b = alloc([128, 8, 2], BF16)
    screp = alloc([128, 8, 128], BF16)
    bmT = alloc([128, 16]); g1nT = alloc([128, 8]); modT = alloc([128, 16, 2])
    bmbc = alloc([128, 4 * D]); n2bc = alloc([128, D])
    lamt = alloc([1, 4, 64]); lamp = alloc([1, 2, 64]); lams = alloc([1, 2]); lamv = alloc([1, 2])
    wm = [alloc([128, 8, 1024], BF16) for _ in range(2)]
    Bwm = [Buf("wm0"), Buf("wm1")]; Bp0 = Buf("p0")

    for r_ in range(2):
        S.dma("sp", cT[:, :, r_], cvec[r_].rearrange("(c p) -> p c", p=128), writes=[Bp0], allow_slow_non_contiguous=True)
    S.dma("sp", bmT, b_mod[0:2048].rearrange("(c p) -> p c", p=128), writes=[Bp0], allow_slow_non_contiguous=True)
    S.dma("sp", g1nT, n1g.rearrange("(c p) -> p c", p=128), writes=[Bp0], allow_slow_non_contiguous=True)
    S.dma("sp", bmbc, b_mod[2048:6144].partition_broadcast(128), writes=[Bp0])
    S.dma("sp", n2bc, n2g.partition_broadcast(128), writes=[Bp0])
    S.dma("sp", FGbc, fgd.partition_broadcast(128), writes=[Bbc])
    S.dma("sp", gsubp, sublnd.rearrange("(p o) -> p o", o=1), writes=[Bvec])
    S.dma("sp", lamt.rearrange("p a b -> p (a b)"), lamd.rearrange("(o a) b -> o (a b)", o=1), writes=[Bp0])
    for g in range(2):
        S.dma("sp", sinkexp[g * 64:(g + 1) * 64, :], sinkd[g * 4:(g + 1) * 4].partition_broadcast(64), writes=[Bvec])
    wmv = w_mod.rearrange("(c p) n -> p c n", p=128)
    for i in range(2):
        S.dma("pool", wm[i], wmv[:, :, i * 1024:(i + 1) * 1024], writes=[Bwm[i]])

    S.op("act", L("activation", out=sg, in_=cT, func=AF.Sigmoid), reads=[Bp0], writes=[Bp0])
    S.op("dve", L("tensor_tensor", out=scT, in0=cT, in1=sg, op=ALU.mult), reads=[Bp0], writes=[Bp0])
    S.op("dve", L("tensor_copy", out=scT_b, in_=scT), reads=[Bp0], writes=[Bp0])
    S.op("dve", L("tensor_copy", out=screp, in_=scT[:, :, 0:1].to_broadcast([128, 8, 128])), reads=[Bp0], writes=[Bp0])
    S.op("dve", L("tensor_scalar", out=gsubp, in0=gsubp, scalar1=1.0 - LAM_INIT, scalar2=None, op0=ALU.mult),
         reads=[Bvec], writes=[Bvec])
    S.op("act", L("activation", out=sinkexp, in_=sinkexp, func=AF.Exp), reads=[Bvec], writes=[Bvec])
    S.op("dve", L("tensor_tensor", out=lamp, in0=lamt[:, 0:4:2, :], in1=lamt[:, 1:4:2, :], op=ALU.mult), reads=[Bp0], writes=[Bp0])
    S.op("dve", L("reduce_sum", out=lams, in_=lamp, axis=AX.X), reads=[Bp0], writes=[Bp0])
    S.op("act", L("activation", out=lamv, in_=lams, func=AF.Exp), reads=[Bp0], writes=[Bp0])
    S.op("dve", L("tensor_tensor", out=lamv[:, 0:1], in0=lamv[:, 1:2], in1=lamv[:, 0:1], op=ALU.subtract), reads=[Bp0], writes=[Bp0])
    S.op("dve", L("tensor_scalar", out=lamv[:, 0:1], in0=lamv[:, 0:1], scalar1=-LAM_INIT, scalar2=None, op0=ALU.add), reads=[Bp0], writes=[Bp0])
    S.op("pe", L("matmul", psum[:, 7, 0:1], lhsT=ones_f[0:1, :], rhs=lamv[:, 0:1], start=True, stop=True),
         reads=[Bp0, Bc], writes=[PB[7]])
    S.op("dve", L("tensor_copy", out=neglam, in_=psum[:, 7, 0:1]), reads=[PB[7]], writes=[Bvec])

    pm = psum[:, 0, 0:32].rearrange("p (a b) -> p a b", b=2)
    for blk in range(16):
        i = blk // 8
        for c in range(8):
            S.op("pe", L("matmul", pm[:, blk, :], lhsT=wm[i][:, c, (blk % 8) * 128:(blk % 8 + 1) * 128],
                                                              rhs=scT_b[:, c, :], start=(c == 0), stop=(c == 7)),
                 reads=[Bwm[i], Bp0], writes=[PB[0]])
    S.op("dve", L("tensor_tensor", out=modT, in0=pm, in1=bmT.unsqueeze(2).to_broadcast([128, 16, 2]), op=ALU.add),
         reads=[PB[0], Bp0], writes=[Bp0])
    for (AT, BT, r) in ((A1T, B1T, 0), (A1cT, B1cT, 1)):
        S.op("dve", L("scalar_tensor_tensor", out=AT, in0=modT[:, 8:16, r], scalar=1.0, in1=g1nT, op0=ALU.add, op1=ALU.mult),
             reads=[Bp0], writes=[Bvec])
        S.op("dve", L("tensor_copy", out=BT, in_=modT[:, 0:8, r]), reads=[Bp0], writes=[Bvec])
    dsts = [G1bc, B2bc, A2bc, G2bc]
    for q in range(4):
        i = q % 2
        S.dma("pool", wm[i], wmv[:, :, (2 + q) * 1024:(3 + q) * 1024], writes=[Bwm[i]])
        for hf in range(2):
            for c in range(8):
                S.op("pe", L("matmul", psum[:, 1 + hf, :], lhsT=screp[:, c, :], rhs=wm[i][:, c, hf * 512:(hf + 1) * 512],
                                                                start=(c == 0), stop=(c == 7)),
                     reads=[Bwm[i], Bp0], writes=[PB[1 + hf]])
        S.op("dve", L("tensor_tensor", out=dsts[q], in0=pflat(1, 2), in1=bmbc[:, q * 1024:(q + 1) * 1024], op=ALU.add),
             reads=[PB[1], PB[2], Bp0], writes=[Bbc])
    S.op("dve", L("scalar_tensor_tensor", out=A2bc, in0=A2bc, scalar=1.0, in1=n2bc, op0=ALU.add, op1=ALU.mult),
         reads=[Bbc, Bp0], writes=[Bbc])

    if stop_after == 0:
        return finish()
    S.barrier()
    off[0] = mark_persist

    QaT = alloc([128, 4, WROWS], BF16); QbT = alloc([128, 4, WROWS], BF16)
    KbT = alloc([128, WROWS + CTX], BF16); Vb = alloc([128, 22, 128], BF16)
    BQ = Buf("Q"); BKb = Buf("Kb"); BVb = Buf("Vb"); Bya = Buf("ya"); Byb = Buf("yb")
    Bks = Buf("kscr"); Bvs = Buf("vscr")
    mark_attn = off[0]

    win = alloc([128, 8, 2304], BF16); Bwin = Buf("win")
    xt = [alloc([128, D]) for _ in range(2)]; Bxt = [Buf("xt0"), Buf("xt1")]
    xn = [alloc([128, D], BF16) for _ in range(4)]; Bxn = [Buf(f"xn{i}") for i in range(4)]
    st4 = [alloc([128, 4]) for _ in range(4)]; Bst = [Buf(f"st{i}") for i in range(4)]
    evt = [alloc([128, 8, 128]) for _ in range(2)]; Bev = [Buf("ev0"), Buf("ev1")]
    hxT = [alloc([128, 8, 512], BF16) for _ in range(2)]; Bhx = [Buf("hx0"), Buf("hx1")]
    cosg = [alloc([128, 512]) for _ in range(2)]; sing = [alloc([128, 512]) for _ in range(2)]; Btab = [Buf("tab0"), Buf("tab1")]
    kbf = [alloc([128, 512], BF16) for _ in range(2)]; Bkbf = [Buf("kbf0"), Buf("kbf1")]
    rt1 = [alloc([128, 512]) for _ in range(2)]; rt2 = [alloc([128, 512]) for _ in range(2)]
    Brt1 = [Buf("rt1a"), Buf("rt1b")]; Brt2 = [Buf("rt2a"), Buf("rt2b")]
    kst = [alloc([128, 4, 512], BF16) for _ in range(2)]; Bkst = [Buf("kst0"), Buf("kst1")]
    vst = [alloc([128, 4, 4, 128], BF16) for _ in range(2)]; Bvst = [Buf("vst0"), Buf("vst1")]

    winv = w_in.rearrange("(c p) n -> p c n", p=128)
    S.dma("pool", win[:, :, 0:QB], winv[:, :, 0:QB], writes=[Bwin])
    S.dma("pool", win[:, :, KB:2304], winv[:, :, KB:2304], writes=[Bwin])
    for gi in range(4):
        for g in range(2):
            S.dma("pool", win[:, :, QB + gi * 128 + g * 64:QB + gi * 128 + (g + 1) * 64],
                  winv[:, :, QB + g * 256 + gi * 64:QB + g * 256 + (gi + 1) * 64], writes=[Bwin])
    import os
    rr = [0]

    pcyc = [0]

    def nextbank():
        b = 2 + pcyc[0] % 4; pcyc[0] += 1
        return b

    def projA(job, hb, tb):
        colbase, n, dst, wb = job
        i = rr[0] % 2; rr[0] += 1
        bk = nextbank()
        for c in range(8):
            S.op("pe", L("matmul", psum[:, bk, 0:n], lhsT=win[:, c, colbase:colbase + 128], rhs=hxT[hb][:, c, 0:n], start=(c == 0), stop=(c == 7)),
                 reads=[Bwin, Bhx[hb]], writes=[PB[bk]])
        S.op("act", L("activation", out=kbf[i][:, 0:n], in_=psum[:, bk, 0:n], func=AF.Copy), reads=[PB[bk]], writes=[Bkbf[i]])
        return (bk, i, n, dst, wb, tb)

    def ropeB(st):
        bk, i, n, dst, wb, tb = st
        S.op("pe", L("matmul", psum[:, 6 + i, 0:n], lhsT=RT_b, rhs=kbf[i][:, 0:n], start=True, stop=True), reads=[Bkbf[i], Bc], writes=[PB[6 + i]])
        S.op("dve", L("tensor_tensor", out=rt1[i][:, 0:n], in0=psum[:, bk, 0:n], in1=cosg[tb][:, 0:n], op=ALU.mult),
             reads=[PB[bk], Btab[tb]], writes=[Brt1[i]])
        S.op("dve", L("tensor_tensor", out=rt2[i][:, 0:n], in0=psum[:, 6 + i, 0:n], in1=sing[tb][:, 0:n], op=ALU.mult),
             reads=[PB[6 + i], Btab[tb]], writes=[Brt2[i]])
        S.op("pool", L("tensor_tensor", out=dst, in0=rt1[i][:, 0:n], in1=rt2[i][:, 0:n], op=ALU.add), reads=[Brt1[i], Brt2[i]], writes=wb)

    for G in range(17):
        ntile = 4 if G < 16 else 2
        n = ntile * 128
        hb = G % 2
        AT, BT = (A1T, B1T) if G < 16 else (A1cT, B1cT)
        tb = G % 2
        S.dma("sp", cosg[tb][:, 0:n], cosd[:, G * 512:G * 512 + n], writes=[Btab[tb]])
        S.dma("sp", sing[tb][:, 0:n], sind[:, G * 512:G * 512 + n], writes=[Btab[tb]])
        for t in range(ntile):
            gt = G * 4 + t
            xb = gt % 2; x4i = gt % 4
            src = xkv[gt * 128:(gt + 1) * 128, :] if G < 16 else ctxd[t * 128:(t + 1) * 128, :]
            S.dma("sp", xt[xb], src, writes=[Bxt[xb]])
            S.op("act", L("activation", out=junk, in_=xt[xb], func=AF.Square, accum_out=st4[x4i][:, 0:1]),
                 reads=[Bxt[xb]], writes=[Bjunk, Bst[x4i]])
            S.op("act", L("activation", out=st4[x4i][:, 1:2], in_=st4[x4i][:, 0:1], func=AF.Sqrt, scale=1.0 / D, bias=EPS),
                 reads=[Bst[x4i]], writes=[Bst[x4i]])
            S.op("dve", L("reciprocal", out=st4[x4i][:, 2:3], in_=st4[x4i][:, 1:2]), reads=[Bst[x4i]], writes=[Bst[x4i]])
            S.op("act", L("activation", out=xn[x4i], in_=xt[xb], func=AF.Copy, scale=st4[x4i][:, 2:3]),
                 reads=[Bxt[xb], Bst[x4i]], writes=[Bxn[x4i]])
            tbk = xb
            for c in range(8):
                S.op("pe", L("transpose", out=pbf(tbk)[:, c, :], in_=xn[x4i][:, c * 128:(c + 1) * 128], identity=ident_b),
                     reads=[Bxn[x4i], Bc], writes=[PB[tbk]])
            S.op("dve", L("tensor_tensor", out=evt[xb], in0=pbf(tbk), in1=AT.unsqueeze(2).to_broadcast([128, 8, 128]), op=ALU.mult),
                 reads=[PB[tbk], Bvec], writes=[Bev[xb]])
            S.op("pool", L("tensor_tensor", out=hxT[hb][:, :, t * 128:(t + 1) * 128], in0=evt[xb],
                           in1=BT.unsqueeze(2).to_broadcast([128, 8, 128]), op=ALU.add), reads=[Bev[xb], Bvec], writes=[Bhx[hb]])
        ch = G // 2 if G < 16 else 8
        ko = (G % 2) * 512 if G < 16 else 0
        sb = G % 2
        jobs = [(KA + h * 128, n, kst[sb][:, h, 0:n], [Bkst[sb]]) for h in range(4)]
        if G < 5 or G == 16:
            wo = G * 512 if G < 16 else WROWS
            jobs.append((KB, n, KbT[:, wo:wo + n], [BKb]))
        if G < 5:
            for (base, dstT) in ((QA, QaT), (QB, QbT)):
                for h in range(4):
                    jobs.append((base + h * 128, 512, dstT[:, h, G * 512:(G + 1) * 512], [BQ]))
        st_ = projA(jobs[0], hb, tb)
        for ji in range(len(jobs)):
            nxt = projA(jobs[ji + 1], hb, tb) if ji + 1 < len(jobs) else None
            ropeB(st_)
            st_ = nxt
            if ji == 3:
                for h in range(4):
                    S.dma("sp", kscr[h, ch, :, ko:ko + n], kst[sb][:, h, 0:n], reads=[Bkst[sb]], writes=[Buf()])
        for t in range(ntile):
            bk = nextbank()
            for c in range(8):
                S.op("pe", L("matmul", psum[:, bk, :], lhsT=hxT[hb][:, c, t * 128:(t + 1) * 128], rhs=win[:, c, VA:VA + 512],
                             start=(c == 0), stop=(c == 7)), reads=[Bwin, Bhx[hb]], writes=[PB[bk]])
            S.op("act", L("activation", out=vst[sb][:, :, t, :], in_=psum[:, bk, :].rearrange("p (h e) -> p h e", e=128), func=AF.Copy),
                 reads=[PB[bk]], writes=[Bvst[sb]])
        for h in range(4):
            tl0 = (G % 2) * 4 if G < 16 else 0
            S.dma("sp", vscr[h, ch, :, tl0:tl0 + ntile, :], vst[sb][:, h, 0:ntile, :], reads=[Bvst[sb]], writes=[Buf()])
        if G < 5 or G == 16:
            bk = nextbank()
            for t in range(ntile):
                for c in range(8):
                    S.op("pe", L("matmul", psum[:, bk, t * 128:(t + 1) * 128], lhsT=hxT[hb][:, c, t * 128:(t + 1) * 128],
                                 rhs=win[:, c, VB:VB + 128], start=(c == 0), stop=(c == 7)), reads=[Bwin, Bhx[hb]], writes=[PB[bk]])
            vt0 = G * 4 if G < 16 else 20
            S.op("act", L("activation", out=Vb[:, vt0:vt0 + ntile, :], in_=psum[:, bk, 0:n].rearrange("p (t e) -> p t e", e=128), func=AF.Copy),
                 reads=[PB[bk]], writes=[BVb])

    if stop_after == 1:
        return finish()
    S.barrier()
    off[0] = mark_attn
    yaT = alloc([128, 4, NQ], BF16); ybT = alloc([128, 4, NQ], BF16)
    mark_att2 = off[0]

    kch = [alloc([128, 1024], BF16) for _ in range(3)]; vch = [alloc([128, 8, 128], BF16) for _ in range(3)]
    Bkch = [Buf(f"kch{i}") for i in range(3)]; Bvch = [Buf(f"vch{i}") for i in range(3)]
    PT = [alloc([128, 1024], BF16) for _ in range(3)]; BPT = [Buf(f"PT{i}") for i in range(3)]
    r1 = alloc([128, 512]); r2 = alloc([128, 512]); t1 = alloc([128, 512]); t2 = alloc([128, 512])
    ot = alloc([128, 512]); osq = alloc([128, 512]); rs = alloc([128, 512]); zsb = alloc([64, 512])
    selA = alloc([64, 128]); selB = alloc([64, 128])
    o1sb = alloc([128, 512]); o2sb = alloc([128, 512])
    Bpp = [Buf(f"pp{i}") for i in range(10)]; Bsel = Buf("sel")
    S.op("dve", L("memset", selA, 0.0), writes=[Bsel]); S.op("dve", L("memset", selB, 0.0), writes=[Bsel])
    S.op("dve", L("memset", selA[0:32, :], 1.0 / 32), writes=[Bsel]); S.op("dve", L("memset", selB[32:64, :], 1.0 / 32), writes=[Bsel])
    if do_moe:
        zer = alloc([128, 4, D], BF16); Bzer = Buf("zer")
        S.op("pool", L("memset", zer, 0.0), writes=[Bzer])
        Bxe0 = []
        xev = xe[0:NSLOT, :].rearrange("(n p) d -> p n d", p=128)
        for i in range(NSLOT // 128 // 4):
            bz = Buf(); Bxe0.append(bz)
            S.dma("pool", xev[:, i * 4:(i + 1) * 4, :], zer, reads=[Bzer], writes=[bz])

    chunks = [(ci, 8) for ci in range(8)] + [(8, 2)]
    seq = [(h, qb, ci, nt) for h in range(4) for qb in range(4) for (ci, nt) in chunks]

    def issue_load(j):
        if j >= len(seq): return
        h, qb, ci, nt = seq[j]
        bi = j % 3
        S.dma("sp", kch[bi][:, 0:nt * 128], kscr[h, ci, :, 0:nt * 128], reads=[Bks], writes=[Bkch[bi]])
        S.dma("sp", vch[bi][:, 0:nt, :], vscr[h, ci, :, 0:nt, :], reads=[Bvs], writes=[Bvch[bi]])

    blocks = []
    for h in range(4):
        for qb in range(4):
            units = []
            for j0, (ci, nt) in enumerate(chunks):
                j = (h * 4 + qb) * 9 + j0
                for tt in range(nt):
                    units.append((j, tt, ci))
            blocks.append((h, qb, units))
    NU = len(blocks[0][2])

    def emit_S(bidx, k):
        h, qb, units = blocks[bidx]
        q0 = 128 + qb * 512
        j, tt, ci = units[k]
        g = bidx * NU + k
        bi = j % 3; sbk = (g % 2) * 2
        for cmp_ in range(2):
            S.op("pe", L("matmul", psum[:, sbk + cmp_, :], lhsT=kch[bi][cmp_ * 64:(cmp_ + 1) * 64, tt * 128:(tt + 1) * 128],
                         rhs=QaT[cmp_ * 64:(cmp_ + 1) * 64, h, q0:q0 + 512], start=True, stop=True),
                 reads=[Bkch[bi], BQ], writes=[PB[sbk + cmp_]])

    def post_stages(h, qb):
        def s1():
            S.op("pe", L("matmul", psum[:, 7, :], lhsT=selA, rhs=zsb, start=True, stop=True), reads=[Bpp[7], Bsel], writes=[PB[7]])
        def s2():
            S.op("dve", L("reciprocal", out=r1, in_=psum[:, 7, :]), reads=[PB[7]], writes=[Bpp[0]])
        def s3():
            S.op("pe", L("matmul", psum[:, 7, :], lhsT=selB, rhs=zsb, start=True, stop=True), reads=[Bpp[7], Bsel], writes=[PB[7]])
        def s4():
            S.op("dve", L("reciprocal", out=r2, in_=psum[:, 7, :]), reads=[PB[7]], writes=[Bpp[1]])
            S.op("dve", L("tensor_tensor", out=t1, in0=o1sb, in1=r1, op=ALU.mult), reads=[Bpp[8], Bpp[0]], writes=[Bpp[2]])
            S.op("dve", L("tensor_tensor", out=t2, in0=o2sb, in1=r2, op=ALU.mult), reads=[Bpp[9], Bpp[1]], writes=[Bpp[3]])
            S.op("dve", L("scalar_tensor_tensor", out=ot, in0=t2, scalar=neglam, in1=t1, op0=ALU.mult, op1=ALU.add),
                 reads=[Bpp[2], Bpp[3], Bvec], writes=[Bpp[4]])
        def s5():
            S.op("act", L("activation", out=osq, in_=ot, func=AF.Square), reads=[Bpp[4]], writes=[Bpp[5]])
        def s6():
            S.op("pe", L("matmul", psum[:, 7, :], lhsT=ones_f, rhs=osq, start=True, stop=True), reads=[Bpp[5], Bc], writes=[PB[7]])
        def s7():
            S.op("act", L("activation", out=rs, in_=psum[:, 7, :], func=AF.Sqrt, scale=1.0 / 128, bias=EPS), reads=[PB[7]], writes=[Bpp[6]])
        def s8():
            S.op("dve", L("reciprocal", out=rs, in_=rs), reads=[Bpp[6]], writes=[Bpp[6]])
            S.op("dve", L("scalar_tensor_tensor", out=yaT[:, h, qb * 512:(qb + 1) * 512], in0=ot, scalar=gsubp, in1=rs, op0=ALU.mult, op1=ALU.mult),
                 reads=[Bpp[4], Bpp[6], Bvec], writes=[Bya])
        return [s1, s2, s3, s4, s5, s6, s7, s8]

    STAGE_AT = {3: 0, 7: 1, 11: 2, 15: 3, 22: 4, 26: 5, 31: 6, 35: 7}
    def emit_Sg(g):
        if g < len(blocks) * NU:
            emit_S(g // NU, g % NU)

    issue_load(0); issue_load(1)
    emit_Sg(0); emit_Sg(1)
    pending = None
    for bidx, (h, qb, units) in enumerate(blocks):
        for k in range(NU):
            j, tt, ci = units[k]
            g = bidx * NU + k
            if tt == 0:
                issue_load(j + 2)
            bi = j % 3; sbk = (g % 2) * 2; pi = g % 3
            S.op("act", L("activation", out=PT[pi], in_=pflat(sbk, 2), func=AF.Exp, scale=0.125),
                 reads=[PB[sbk], PB[sbk + 1]], writes=[BPT[pi]])
            emit_Sg(g + 2)
            first = (k == 0); last = (k == NU - 1)
            for cmp_ in range(2):
                S.op("pe", L("matmul", psum[:, 4 + cmp_, :], lhsT=vch[bi][:, tt, :], rhs=PT[pi][:, cmp_ * 512:(cmp_ + 1) * 512], start=first, stop=last),
                     reads=[Bvch[bi], BPT[pi]], writes=[PB[4 + cmp_]])
            for cmp_ in range(2):
                S.op("pe", L("matmul", psum[cmp_ * 32:(cmp_ + 1) * 32, 6, :], lhsT=ones_b[:, 0:32], rhs=PT[pi][:, cmp_ * 512:(cmp_ + 1) * 512],
                             start=first, stop=last, tile_position=(0, cmp_ * 32)),
                     reads=[BPT[pi], Bc], writes=[PB[6]])
            if pending is not None and k in STAGE_AT:
                pending[STAGE_AT[k]]()
        S.op("act", L("activation", out=o1sb, in_=psum[:, 4, :], func=AF.Copy), reads=[PB[4]], writes=[Bpp[8]])
        S.op("act", L("activation", out=o2sb, in_=psum[:, 5, :], func=AF.Copy), reads=[PB[5]], writes=[Bpp[9]])
        S.op("dve", L("tensor_copy", out=zsb, in_=psum[0:64, 6, :]), reads=[PB[6]], writes=[Bpp[7]])
        pending = post_stages(h, qb)
    for st_fn in pending:
        st_fn()

    if stop_after == 2:
        return finish()
    NPW = 4
    PW = [alloc([128, 512], BF16) for _ in range(NPW)]; BPW = [Buf(f"PW{i}") for i in range(NPW)]
    zt = [alloc([128, 512]) for _ in range(2)]; Bzt = [Buf("zt0"), Buf("zt1")]
    WP = []
    for nb in range(16):
        for k, (kt, kind) in enumerate([(nb, "L"), (nb + 1, "C"), (nb + 2, "R"), (20, "X"), (21, "X")]):
            WP.append((nb, k, kt, kind))

    def emit_WS(p):
        nb, k, kt, kind = WP[p]
        kc0 = kt * 128; q0 = 128 + nb * 128
        for g in range(2):
            gs = slice(g * 64, (g + 1) * 64)
            sbk = 2 * (p % 2) + g
            S.op("pe", L("matmul", psum[:, sbk, :].rearrange("p (i q) -> p i q", q=128), lhsT=KbT[gs, kc0:kc0 + 128], rhs=QbT[gs, :, q0:q0 + 128],
                         start=True, stop=True), reads=[BKb, BQ], writes=[PB[sbk]])

    emit_WS(0); emit_WS(1)
    for p, (nb, k, kt, kind) in enumerate(WP):
        ob = 4 + 2 * (nb % 2)
        if kind == "L" and nb == 0:
            bias = wmask[:, 0:1]
        elif kind == "R" and nb == 15:
            bias = wmask[:, 1:2]
        else:
            bias = 0.0
        for g in range(2):
            sbk = 2 * (p % 2) + g; pi = 2 * (p % 2) + g
            S.op("act", L("activation", out=PW[pi], in_=psum[:, sbk, :], func=AF.Exp, scale=0.125, bias=bias),
                 reads=[PB[sbk], Bc], writes=[BPW[pi]])
            if kind in ("L", "R"):
                tri = triL if kind == "L" else triR
                pw3 = PW[pi].rearrange("p (i q) -> p i q", q=128)
                S.op("dve", L("tensor_tensor", out=pw3, in0=pw3, in1=tri.unsqueeze(1).to_broadcast([128, 4, 128]), op=ALU.mult),
                     reads=[BPW[pi], Bc], writes=[BPW[pi]])
        first = (k == 0); last = (k == 4)
        for g in range(2):
            gs = slice(g * 64, (g + 1) * 64); pi = 2 * (p % 2) + g
            S.op("pe", L("matmul", psum[gs, ob, :], lhsT=Vb[:, kt, gs], rhs=PW[pi], start=first, stop=last, tile_position=(0, g * 64)),
                 reads=[BVb, BPW[pi]], writes=[PB[ob]])
        for g in range(2):
            gs = slice(g * 64, (g + 1) * 64); pi = 2 * (p % 2) + g
            S.op("pe", L("matmul", psum[gs, ob + 1, :], lhsT=ones_b[:, 0:64], rhs=PW[pi], start=first, stop=last, tile_position=(0, g * 64)),
                 reads=[Bc, BPW[pi]], writes=[PB[ob + 1]])
        if p + 2 < len(WP):
            emit_WS(p + 2)
        if k == 4:
            zi = nb % 2
            z3 = zt[zi].rearrange("p (i q) -> p i q", q=128)
            S.op("dve", L("tensor_tensor", out=z3, in0=psum[:, ob + 1, :].rearrange("p (i q) -> p i q", q=128),
                          in1=sinkexp.unsqueeze(2).to_broadcast([128, 4, 128]), op=ALU.add), reads=[PB[ob + 1], Bvec], writes=[Bzt[zi]])
            S.op("dve", L("reciprocal", out=zt[zi], in_=zt[zi]), reads=[Bzt[zi]], writes=[Bzt[zi]])
            S.op("dve", L("tensor_tensor", out=ybT[:, :, nb * 128:(nb + 1) * 128], in0=psum[:, ob, :].rearrange("p (i q) -> p i q", q=128),
                          in1=z3, op=ALU.mult), reads=[PB[ob], Bzt[zi]], writes=[Byb])

    if dbg:
        S.dma("sp", dbg_kb, KbT, reads=[BKb])
        S.dma("sp", dbg_vb, Vb.rearrange("p a b -> p (a b)"), reads=[BVb])
        S.dma("sp", dbg_qb, QbT.rearrange("p a b -> p (a b)"), reads=[BQ])
        S.dma("sp", dbg_yb, ybT.rearrange("p a b -> p (a b)"), reads=[Byb])
    if stop_after == 3:
        return finish()
    S.barrier()
    off[0] = mark_att2
    wout = alloc([128, 8, D], BF16); Bwo = Buf("wout")
    wr_b = alloc([128, 8, NE], BF16); wr_f = alloc([128, 8, NE]); br_b = alloc([1, NE], BF16); br_f = alloc([1, NE])
    Bwr = Buf("wr")
    x4 = [alloc([128, D]) for _ in range(2)]; Bx4 = [Buf("x4a"), Buf("x4b")]
    tm4 = alloc([128, D]); Btm4 = Buf("tm4")
    x1t = [alloc([128, D]) for _ in range(2)]; Bx1 = [Buf("x1a"), Buf("x1b")]
    h2 = alloc([128, D]); Bh2 = Buf("h2")
    hx2all = alloc([128, 16, D], BF16); Bhx2t = [Buf(f"hx2_{i}") for i in range(16)]
    hx2T = alloc([128, 8, 128], BF16); Bhx2T = Buf("hx2T")
    s4 = [alloc([128, 4]) for _ in range(2)]; Bs4 = [Buf("s4a"), Buf("s4b")]
    lg = alloc([128, NE]); m8 = alloc([128, 8]); i8 = alloc([128, 8], U32); ekf = alloc([128, 8]); nm0 = alloc([128, 1])
    ge = alloc([128, 4]); gz = alloc([128, 1]); maskb = alloc([128, NE], BF16); destf = alloc([128, NE]); prod = alloc([128, NE])
    dk = alloc([128, 4]); Brr = Buf("rr")
    Bx1s = Buf("x1scr"); Bxes = [Buf(f"xes{i}") for i in range(64)]

    woutv = w_out.rearrange("(c p) n -> p c n", p=128)
    S.dma("pool", wout[:, 0:4, :], woutv[:, 0:4, :], writes=[Bwo])
    for gi in range(4):
        for g in range(2):
            r0 = 512 + g * 256 + gi * 64
            S.dma("pool", wout[g * 64:(g + 1) * 64, 4 + gi, :], w_out[r0:r0 + 64, :], writes=[Bwo])
    S.dma("sp", wr_f, w_r.rearrange("(c p) n -> p c n", p=128), writes=[Bwr])
    S.dma("sp", br_f, b_r.rearrange("(o n) -> o n", o=1), writes=[Bwr])
    S.op("dve", L("tensor_copy", out=wr_b, in_=wr_f), reads=[Bwr], writes=[Bwr])
    S.op("dve", L("tensor_copy", out=br_b, in_=br_f), reads=[Bwr], writes=[Bwr])

    lgs = [alloc([128, NE]) for _ in range(2)]; Blg = [Buf("lg0"), Buf("lg1")]

    def stageA(t):
        b = t % 2
        pb0 = 2 * b
        S.dma("sp", x4[b], xkv[128 + t * 128:128 + (t + 1) * 128, :], writes=[Bx4[b]])
        for hf in range(2):
            for c in range(8):
                lhs = yaT[:, c, t * 128:(t + 1) * 128] if c < 4 else ybT[:, c - 4, t * 128:(t + 1) * 128]
                S.op("pe", L("matmul", psum[:, pb0 + hf, :], lhsT=lhs, rhs=wout[:, c, hf * 512:(hf + 1) * 512],
                                                                             start=(c == 0), stop=(c == 7)),
                     reads=[Bya, Byb, Bwo], writes=[PB[pb0 + hf]])
        S.op("dve", L("tensor_tensor", out=tm4, in0=pflat(pb0, 2), in1=G1bc, op=ALU.mult), reads=[PB[pb0], PB[pb0 + 1], Bbc], writes=[Btm4])
        S.op("pool", L("tensor_tensor", out=x1t[b], in0=tm4, in1=x4[b], op=ALU.add), reads=[Btm4, Bx4[b]], writes=[Bx1[b]])
        S.dma("sp", x1scr[t * 128:(t + 1) * 128, :], x1t[b], reads=[Bx1[b]], writes=[Bx1s])
        if dbg:
            S.dma("sp", dbg_x1[t * 128:(t + 1) * 128, :], x1t[b], reads=[Bx1[b]])
        if not do_moe:
            return
        S.op("act", L("activation", out=junk, in_=x1t[b], func=AF.Square, accum_out=s4[b][:, 0:1]), reads=[Bx1[b]], writes=[Bjunk, Bs4[b]])
        S.op("act", L("activation", out=s4[b][:, 1:2], in_=s4[b][:, 0:1], func=AF.Sqrt, scale=1.0 / D, bias=EPS), reads=[Bs4[b]], writes=[Bs4[b]])
        S.op("dve", L("reciprocal", out=s4[b][:, 2:3], in_=s4[b][:, 1:2]), reads=[Bs4[b]], writes=[Bs4[b]])
        S.op("dve", L("scalar_tensor_tensor", out=h2, in0=x1t[b], scalar=s4[b][:, 2:3], in1=A2bc, op0=ALU.mult, op1=ALU.mult),
             reads=[Bx1[b], Bs4[b], Bbc], writes=[Bh2])
        S.op("pool", L("tensor_tensor", out=hx2all[:, t, :], in0=h2, in1=B2bc, op=ALU.add), reads=[Bh2, Bbc], writes=[Bhx2t[t]])
        for c in range(8):
            S.op("pe", L("transpose", out=pbf(4)[:, c, :], in_=hx2all[:, t, c * 128:(c + 1) * 128], identity=ident_b),
                 reads=[Bhx2t[t], Bc], writes=[PB[4]])
        S.op("act", L("activation", out=hx2T, in_=pbf(4), func=AF.Copy), reads=[PB[4]], writes=[Bhx2T])
        for c in range(8):
            S.op("pe", L("matmul", psum[:, 5, 0:NE], lhsT=hx2T[:, c, :], rhs=wr_b[:, c, :], start=(c == 0), stop=False),
                 reads=[Bhx2T, Bwr], writes=[PB[5]])
        S.op("pe", L("matmul", psum[:, 5, 0:NE], lhsT=ones_b[0:1, :], rhs=br_b, start=False, stop=True), reads=[Bwr, Bc], writes=[PB[5]])
        S.op("dve", L("tensor_copy", out=lgs[b], in_=psum[:, 5, 0:NE]), reads=[PB[5]], writes=[Blg[b]])

    def stageB(t):
        b = t % 2
        S.op("dve", L("max", out=m8, in_=lgs[b]), reads=[Brr, Blg[b]], writes=[Brr])
        S.op("dve", L("max_index", out=i8, in_max=m8, in_values=lgs[b]), reads=[Brr, Blg[b]], writes=[Brr])
        S.op("dve", L("tensor_copy", out=ek_all[:, t * 4:(t + 1) * 4], in_=i8[:, 0:4]), reads=[Brr], writes=[Brt])
        S.op("dve", L("tensor_scalar", out=nm0, in0=m8[:, 0:1], scalar1=-1.0, scalar2=None, op0=ALU.mult), reads=[Brr], writes=[Brr])
        S.op("act", L("activation", out=ge, in_=m8[:, 0:4], func=AF.Exp, bias=nm0, accum_out=gz), reads=[Brr], writes=[Brr])
        S.op("dve", L("reciprocal", out=gz, in_=gz), reads=[Brr], writes=[Brr])
        S.op("dve", L("tensor_scalar", out=gates[:, t * 4:(t + 1) * 4], in0=ge, scalar1=gz, scalar2=None, op0=ALU.mult), reads=[Brr], writes=[Brt])
        S.op("dve", L("tensor_scalar", out=maskb, in0=lgs[b], scalar1=m8[:, 3:4], scalar2=None, op0=ALU.is_ge), reads=[Brr, Blg[b]], writes=[Brr])
        S.op("pe", L("matmul", psum[:, 6, 0:NE], lhsT=U_b, rhs=maskb, start=True, stop=True), reads=[Brr, Bc], writes=[PB[6]])
        S.op("pe", L("matmul", psum[:, 7, 0:NE], lhsT=ones_b, rhs=maskb, start=True, stop=True), reads=[Brr, Bc], writes=[PB[7]])
        S.op("dve", L("tensor_tensor", out=destf, in0=psum[:, 6, 0:NE], in1=basecnt, op=ALU.add), reads=[PB[6], Brt], writes=[Brr])
        S.op("dve", L("tensor_tensor", out=basecnt, in0=psum[:, 7, 0:NE], in1=basecnt, op=ALU.add), reads=[PB[7], Brt], writes=[Brt])
        for k in range(4):
            col = t * 4 + k
            S.op("dve", L("scalar_tensor_tensor", out=prod, in0=iota_f, scalar=ek_all[:, col:col + 1], in1=destf, op0=ALU.is_equal, op1=ALU.mult,
                          accum_out=rk_all[:, col:col + 1]), reads=[Brr, Bc, Brt], writes=[Brr, Brt])

    stageA(0)
    for t in range(16):
        if t + 1 < 16:
            stageA(t + 1)
        if do_moe:
            stageB(t)

    if do_moe:
        cntcol = alloc([32, 1]); tmp32 = alloc([32, 32]); G32 = alloc([32, 32]); T32 = alloc([32, 32]); poscol = alloc([32, 1]); pos2 = alloc([32, 1])
        PmT = alloc([32, 32]); OFFpos = alloc([128, 32]); offk = alloc([128, 64]); dst64 = alloc([128, 64]); t1p = alloc([128, 32])
        carr = alloc([128, 8]); idxf = alloc([128, 32, 8])
        Bso = Buf("sort")
        S.op("dve", L("tensor_tensor", out=tmp32, in0=basecnt[0:32, :], in1=ident_f[0:32, 0:32], op=ALU.mult), reads=[Brt, Bc], writes=[Bso])
        S.op("dve", L("reduce_sum", out=cntcol, in_=tmp32, axis=AX.X), reads=[Bso], writes=[Bso])
        S.op("dve", L("tensor_scalar", out=G32, in0=basecnt[0:32, :], scalar1=cntcol, scalar2=None, op0=ALU.is_gt), reads=[Brt, Bso], writes=[Bso])
        S.op("dve", L("scalar_tensor_tensor", out=T32, in0=basecnt[0:32, :], scalar=cntcol, in1=Lmask, op0=ALU.is_equal, op1=ALU.mult),
             reads=[Brt, Bso, Bc], writes=[Bso])
        S.op("dve", L("tensor_tensor", out=G32, in0=G32, in1=T32, op=ALU.add), reads=[Bso], writes=[Bso])
        S.op("dve", L("reduce_sum", out=poscol, in_=G32, axis=AX.X), reads=[Bso], writes=[Bso])
        S.op("dve", L("tensor_scalar", out=Pm, in0=iota_f[0:32, :], scalar1=poscol, scalar2=None, op0=ALU.is_equal), reads=[Bso, Bc], writes=[Brt])
        S.op("pe", L("matmul", psum[0:32, 0, 0:32], lhsT=Pm, rhs=ident_f[0:32, 0:32], start=True, stop=True), reads=[Brt, Bc], writes=[PB[0]])
        S.op("dve", L("tensor_copy", out=PmT, in_=psum[0:32, 0, 0:32]), reads=[PB[0]], writes=[Bso])
        S.op("pe", L("matmul", psum[:, 1, 0:32], lhsT=OFFrep, rhs=PmT, start=True, stop=True), reads=[Bso, Bc], writes=[PB[1]])
        S.op("dve", L("tensor_scalar", out=OFFpos, in0=psum[:, 1, 0:32], scalar1=128.0, scalar2=None, op0=ALU.mult), reads=[PB[1]], writes=[Bso])
        S.op("pe", L("matmul", psum[:, 2, 0:32], lhsT=pidx[0:32, :], rhs=Pm, start=True, stop=True), reads=[Brt, Bc], writes=[PB[2]])
        S.op("dve", L("tensor_copy", out=permbc, in_=psum[:, 2, 0:32]), reads=[PB[2]], writes=[Brt])
        for col in range(64):
            S.op("dve", L("scalar_tensor_tensor", out=prod, in0=iota_f, scalar=ek_all[:, col:col + 1], in1=OFFpos, op0=ALU.is_equal, op1=ALU.mult,
                          accum_out=offk[:, col:col + 1]), reads=[Brt, Bc, Bso], writes=[Brr, Bso])
        S.op("dve", L("tensor_tensor", out=dst64, in0=offk, in1=rk_all, op=ALU.add), reads=[Bso, Brt], writes=[Bso])
        S.op("dve", L("tensor_copy", out=desti, in_=dst64), reads=[Bso], writes=[Brt])
        S.op("dve", L("scalar_tensor_tensor", out=t1p, in0=permbc, scalar=1024.0, in1=pidx[:, 0:32], op0=ALU.mult, op1=ALU.add), reads=[Brt, Bc], writes=[Bso])
        S.op("dve", L("tensor_scalar", out=carr, in0=iota_f[:, 0:8], scalar1=128.0, scalar2=None, op0=ALU.mult), reads=[Bc], writes=[Bso])
        S.op("dve", L("tensor_tensor", out=idxf, in0=t1p.unsqueeze(2).to_broadcast([128, 32, 8]), in1=carr.unsqueeze(1).to_broadcast([128, 32, 8]), op=ALU.add),
             reads=[Bso], writes=[Bso])
        S.op("dve", L("tensor_copy", out=idxg, in_=idxf.rearrange("p a b -> p (a b)")), reads=[Bso], writes=[Brt])
        S.op("dve", L("tensor_copy", out=bdn_idx, in_=permbc), reads=[Brt], writes=[Brt])
        for col in range(64):
            t = col // 4
            S.op("pool", L("indirect_dma_start", out=xe[:, :], out_offset=bass.IndirectOffsetOnAxis(ap=desti[:, col:col + 1], axis=0),
                           in_=hx2all[:, t, :], in_offset=None), reads=[Bhx2t[t], Brt] + Bxe0, writes=[Bxes[col]], dma=True)

    if not do_moe:
        lastd = [o for o in S.ops["sp"] if o.dma][-4:]
        S.op("sp", None, deps=[o for o in S.ops["sp"] if o.dma][-40:])
        S.emit(nc)
        return nc

    S.barrier()
    off[0] = mark_persist

    XC = 1024
    wgu = [alloc([128, 8, 2 * D], BF16) for _ in range(2)]; wdn = [alloc([128, 8, D], BF16) for _ in range(2)]
    Bwgu = [[Buf(f"wgu{b}_{c}") for c in range(8)] for b in range(2)]; Bwdn = [[Buf(f"wdn{b}_{c}") for c in range(8)] for b in range(2)]
    bdn1 = alloc([128, D]); Bbdn1 = Buf("bdn")
    bgu_f = alloc([NE, 2 * D]); biasT = alloc([128, 16, NE]); Bbias = Buf("bias")
    xet = [alloc([128, D], BF16) for _ in range(3)]; Bxet = [Buf(f"xet{i}") for i in range(3)]
    xeT = alloc([128, 8, XC], BF16); BxeT = Buf("xeT")
    aT = alloc([128, 8, XC], BF16); BaT = Buf("aT")
    g1 = [alloc([128, 512]) for _ in range(2)]; sgm = [alloc([128, 512]) for _ in range(2)]
    u1 = [alloc([128, 512]) for _ in range(2)]; gsx = [alloc([128, 512]) for _ in range(2)]
    Bg1 = [Buf("g1a"), Buf("g1b")]; Bsgm = [Buf("sga"), Buf("sgb")]; Bu1 = [Buf("u1a"), Buf("u1b")]; Bgsx = [Buf("gsa"), Buf("gsb")]
    yst = [alloc([128, D]) for _ in range(2)]; Byst = [Buf("yst0"), Buf("yst1")]

    S.dma("sp", bgu_f, b_gu, writes=[Bbias])
    for m in range(8):
        for two in range(2):
            j = m * 2 + two
            S.op("pe", L("matmul", psum[:, 0, j * NE:(j + 1) * NE], lhsT=bgu_f[:, 2 * m * 128 + two:2 * (m + 1) * 128:2],
                         rhs=Pm, start=True, stop=True), reads=[Bbias, Brt], writes=[PB[0]])
    S.op("dve", L("tensor_copy", out=biasT, in_=psum[:, 0, :].rearrange("p (j n) -> p j n", n=NE)), reads=[PB[0]], writes=[Bbias])
    S.op("dve", L("tensor_scalar", out=biasT[:, 1:16:2, :], in0=biasT[:, 1:16:2, :], scalar1=1.0, scalar2=None, op0=ALU.add), reads=[Bbias], writes=[Bbias])

    wgu_rows = w_gu.rearrange("e k n -> (e k) n"); wdn_rows = w_dn.rearrange("e k n -> (e k) n")
    IO = bass.IndirectOffsetOnAxis

    def load_w(i):
        b = i % 2
        for c in range(8):
            S.op("pool", L("indirect_dma_start", out=wgu[b][:, c, :], out_offset=None, in_=wgu_rows[:, :], in_offset=IO(ap=idxg[:, i * 8 + c:i * 8 + c + 1], axis=0)),
                 reads=[Brt], writes=[Bwgu[b][c]], dma=True)
        for c in range(8):
            S.op("pool", L("indirect_dma_start", out=wdn[b][:, c, :], out_offset=None, in_=wdn_rows[:, :], in_offset=IO(ap=idxg[:, i * 8 + c:i * 8 + c + 1], axis=0)),
                 reads=[Brt], writes=[Bwdn[b][c]], dma=True)

    def load_bdn(i):
        S.op("pool", L("indirect_dma_start", out=bdn1, out_offset=None, in_=b_dn[:, :], in_offset=IO(ap=bdn_idx[:, i:i + 1], axis=0)),
             reads=[Brt], writes=[Bbdn1], dma=True)

    work = []
    for i in range(NE):
        n = NT[i]; o = OFFT[i]
        nitem = -(-n // (XC // 128))
        sizes = [n // nitem + (1 if q < n % nitem else 0) for q in range(nitem)]
        for q, k in enumerate(sizes):
            work.append((i, o, k, q == 0, q == nitem - 1)); o += k

    load_w(0); load_bdn(0)
    pc5 = [0]; xc5 = [0]; yc5 = [0]

    def stage_x(w):
        i, o, k, first, lastw = w
        for s in range(k):
            xi = xc5[0] % 3; xc5[0] += 1
            tb_ = xi % 2
            S.dma("sp", xet[xi], xe[(o + s) * 128:(o + s + 1) * 128, :], reads=Bxes, writes=[Bxet[xi]])
            for c in range(8):
                S.op("pe", L("transpose", out=pbf(tb_)[:, c, :], in_=xet[xi][:, c * 128:(c + 1) * 128], identity=ident_b),
                     reads=[Bxet[xi], Bc], writes=[PB[tb_]])
            S.op("act", L("activation", out=xeT[:, :, s * 128:(s + 1) * 128], in_=pbf(tb_), func=AF.Copy), reads=[PB[tb_]], writes=[BxeT])

    stage_x(work[0])
    for wi, (i, o, k, first, lastw) in enumerate(work):
        b = i % 2
        if first and i + 1 < NE:
            load_w(i + 1)
        ncols = k * 128
        for m in range(8):
            nch = -(-ncols // 512); cw = ncols // nch
            for c0 in range(0, ncols, cw):
                nn = cw
                ii = pc5[0] % 2; pc5[0] += 1
                bg_, bu_ = 2 + 2 * ii, 3 + 2 * ii
                cs = slice(c0, c0 + nn)
                for (bk, two) in ((bg_, 0), (bu_, 1)):
                    for c in range(8):
                        S.op("pe", L("matmul", psum[:, bk, 0:nn], lhsT=wgu[b][:, c, 2 * m * 128 + two:2 * (m + 1) * 128:2],
                                     rhs=xeT[:, c, cs], start=(c == 0), stop=(c == 7)),
                             reads=[Bwgu[b][c], BxeT], writes=[PB[bk]])
                S.op("dve", L("tensor_scalar", out=g1[ii][:, 0:nn], in0=psum[:, bg_, 0:nn], scalar1=biasT[:, 2 * m, i:i + 1], scalar2=7.0, op0=ALU.add, op1=ALU.min),
                     reads=[PB[bg_], Bbias], writes=[Bg1[ii]])
                S.op("act", L("activation", out=sgm[ii][:, 0:nn], in_=g1[ii][:, 0:nn], func=AF.Sigmoid, scale=1.702), reads=[Bg1[ii]], writes=[Bsgm[ii]])
                S.op("dve", L("tensor_scalar", out=u1[ii][:, 0:nn], in0=psum[:, bu_, 0:nn], scalar1=biasT[:, 2 * m + 1, i:i + 1], scalar2=8.0, op0=ALU.add, op1=ALU.min),
                     reads=[PB[bu_], Bbias], writes=[Bu1[ii]])
                S.op("dve", L("tensor_tensor", out=gsx[ii][:, 0:nn], in0=g1[ii][:, 0:nn], in1=sgm[ii][:, 0:nn], op=ALU.mult), reads=[Bg1[ii], Bsgm[ii]], writes=[Bgsx[ii]])
                S.op("dve", L("scalar_tensor_tensor", out=aT[:, m, cs], in0=u1[ii][:, 0:nn], scalar=-6.0, in1=gsx[ii][:, 0:nn], op0=ALU.max, op1=ALU.mult),
                     reads=[Bu1[ii], Bgsx[ii]], writes=[BaT])
        if wi + 1 < len(work):
            stage_x(work[wi + 1])
        for s in range(k):
            yb_ = yc5[0] % 2; yc5[0] += 1
            for hf in range(2):
                bk = 6 + hf
                for m in range(8):
                    S.op("pe", L("matmul", psum[:, bk, :], lhsT=aT[:, m, s * 128:(s + 1) * 128], rhs=wdn[b][:, m, hf * 512:(hf + 1) * 512],
                                 start=(m == 0), stop=(m == 7)),
                         reads=[BaT, Bwdn[b][m]], writes=[PB[bk]])
            S.op("dve", L("tensor_tensor", out=yst[yb_], in0=pflat(6, 2), in1=bdn1, op=ALU.add), reads=[PB[6], PB[7], Bbdn1], writes=[Byst[yb_]])
            S.dma("sp", ye[(o + s) * 128:(o + s + 1) * 128, :], yst[yb_], reads=[Byst[yb_]], writes=[Buf()])
        if lastw and i + 1 < NE:
            load_bdn(i + 1)

    S.barrier()
    off[0] = mark_persist

    yk = [alloc([128, D]) for _ in range(8)]; Byk = [Buf(f"yk{i}") for i in range(8)]
    x6 = [alloc([128, D]) for _ in range(2)]; Bx6 = [Buf("x6a"), Buf("x6b")]
    acc = [alloc([128, D]) for _ in range(2)]; Bacc = [Buf("acca"), Buf("accb")]
    acc2 = [alloc([128, D]) for _ in range(2)]; Bacc2 = [Buf("acc2a"), Buf("acc2b")]
    xo = [alloc([128, D]) for _ in range(2)]; Bxo = [Buf("xoa"), Buf("xob")]
    ot6 = [alloc([128, D]) for _ in range(2)]; Bot6 = [Buf("o6a"), Buf("o6b")]
    s6 = [alloc([128, 4]) for _ in range(2)]; Bs6 = [Buf("s6a"), Buf("s6b")]
    outs = []

    def loads6(t):
        b = t % 2
        S.dma("sp", x6[b], x1scr[t * 128:(t + 1) * 128, :], reads=[Bx1s], writes=[Bx6[b]])
        for k in range(4):
            col = t * 4 + k
            S.op("pool", L("indirect_dma_start", out=yk[4 * b + k], out_offset=None, in_=ye[:, :],
                           in_offset=bass.IndirectOffsetOnAxis(ap=desti[:, col:col + 1], axis=0)),
                 reads=[Brt], writes=[Byk[4 * b + k]], dma=True)

    loads6(0)
    for t in range(16):
        b = t % 2
        if t + 1 < 16:
            loads6(t + 1)
        S.op("act", L("activation", out=acc[b], in_=yk[4 * b], func=AF.Copy, scale=gates[:, t * 4:t * 4 + 1]), reads=[Byk[4 * b], Brt], writes=[Bacc[b]])
        for k in range(1, 4):
            S.op("dve", L("scalar_tensor_tensor", out=acc[b], in0=yk[4 * b + k], scalar=gates[:, t * 4 + k:t * 4 + k + 1], in1=acc[b], op0=ALU.mult, op1=ALU.add),
                 reads=[Byk[4 * b + k], Brt, Bacc[b]], writes=[Bacc[b]])
        S.op("dve", L("tensor_tensor", out=acc2[b], in0=acc[b], in1=G2bc, op=ALU.mult), reads=[Bacc[b], Bbc], writes=[Bacc2[b]])
        S.op("pool", L("tensor_tensor", out=xo[b], in0=acc2[b], in1=x6[b], op=ALU.add), reads=[Bacc2[b], Bx6[b]], writes=[Bxo[b]])
        S.op("act", L("activation", out=junk, in_=xo[b], func=AF.Square, accum_out=s6[b][:, 0:1]), reads=[Bxo[b]], writes=[Bjunk, Bs6[b]])
        S.op("act", L("activation", out=s6[b][:, 1:2], in_=s6[b][:, 0:1], func=AF.Sqrt, scale=1.0 / D, bias=EPS), reads=[Bs6[b]], writes=[Bs6[b]])
        S.op("dve", L("reciprocal", out=s6[b][:, 2:3], in_=s6[b][:, 1:2]), reads=[Bs6[b]], writes=[Bs6[b]])
        S.op("dve", L("scalar_tensor_tensor", out=ot6[b], in0=xo[b], scalar=s6[b][:, 2:3], in1=FGbc, op0=ALU.mult, op1=ALU.mult),
             reads=[Bxo[b], Bs6[b], Bbc], writes=[Bot6[b]])
        outs.append(S.dma("sp", outd[t * 128:(t + 1) * 128, :], ot6[b], reads=[Bot6[b]]))
    S.op("sp", None, deps=outs)
    S.emit(nc)
    return nc


def _rope_tables(pos):
    p = np.arange(128); d = p % 64; f = d % 16
    inv = (np.float32(10000.0) ** (-(np.arange(16, dtype=np.float32)) / np.float32(16))).astype(np.float32)
    row = (pos // 64).astype(np.float32); col = (pos % 64).astype(np.float32)
    coord = np.where((d < 32)[:, None], row[None, :], col[None, :]).astype(np.float32)
    ang = (coord * inv[f][:, None]).astype(np.float32)
    return np.cos(ang).astype(np.float32), np.sin(ang).astype(np.float32)


def _consts():
    c = np.zeros((128, 9, 128), np.float32)
    c[:, 0, :] = np.eye(128, dtype=np.float32)
    R = np.zeros((128, 128), np.float32)
    for m in range(128):
        if (m % 32) < 16: R[m, m + 16] = -1.0
        else: R[m, m - 16] = 1.0
    c[:, 1, :] = R.T
    j = np.arange(128)[:, None]; i = np.arange(128)[None, :]
    c[:, 2, :] = (j < i).astype(np.float32)
    c[:, 3, :] = (j >= i).astype(np.float32)
    c[:, 4, :] = (j <= i).astype(np.float32)
    c[:, 5, :] = np.arange(128, dtype=np.float32)[None, :]
    c[:, 6, :] = np.arange(128, dtype=np.float32)[:, None]
    c[0:NE, 7, :] = np.asarray(OFFT, np.float32)[:, None]
    c[:, 8, :] = (i < j).astype(np.float32)
    return c.reshape(128, 9 * 128)


def make_in_maps(inp, do_moe=True):
    x = np.asarray(inp["x"], np.float32); ctx = np.asarray(inp["ctx"], np.float32)
    c = np.asarray(inp["c"], np.float32); c_ctx = np.asarray(inp["c_ctx"], np.float32)
    consts = _consts()
    lam = np.stack([np.asarray(inp[k], np.float32)[0] for k in ("lam_q1", "lam_k1", "lam_q2", "lam_k2")], 0)
    shared = {
        "consts": consts, "w_mod": np.ascontiguousarray(inp["w_mod"][0]), "b_mod": np.ascontiguousarray(inp["b_mod"][0]),
        "norm1_g": np.ascontiguousarray(inp["norm1_g"][0]), "w_in": np.ascontiguousarray(inp["w_in"][0]), "lam": np.ascontiguousarray(lam),
        "subln_g": np.ascontiguousarray(inp["subln_g"][0]), "sink": np.ascontiguousarray(inp["sink"][0]),
        "w_out": np.ascontiguousarray(inp["w_out"][0]), "norm2_g": np.ascontiguousarray(inp["norm2_g"][0]),
        "w_router": np.ascontiguousarray(inp["w_router"][0]), "b_router": np.ascontiguousarray(inp["b_router"][0]),
        "final_g": np.ascontiguousarray(inp["final_g"]),
    }
    if do_moe:
        shared.update({"w_gate_up": np.ascontiguousarray(inp["w_gate_up"][0]), "b_gate_up": np.ascontiguousarray(inp["b_gate_up"][0]),
                       "w_down": np.ascontiguousarray(inp["w_down"][0]), "b_down": np.ascontiguousarray(inp["b_down"][0])})
    shared = {k: np.asarray(v, np.float32) for k, v in shared.items()}
    maps = []
    for core in range(8):
        b, j = core // 4, core % 4
        qs = j * NQ
        shift = qs - 128
        pos = (np.arange(S_LEN) + shift) % S_LEN
        cos, sin = _rope_tables(pos)
        cosT = np.concatenate([cos, np.ones((128, CTX), np.float32)], 1)
        sinT = np.concatenate([sin, np.zeros((128, CTX), np.float32)], 1)
        wmask = np.zeros((128, 2), np.float32)
        if j == 0: wmask[:, 0] = -1e30
        if j == 3: wmask[:, 1] = -1e30
        m = dict(shared)
        m.update({"xkv": np.ascontiguousarray(np.roll(x[b], -shift, axis=0)), "ctx": np.ascontiguousarray(ctx[b]),
                  "cvec": np.ascontiguousarray(np.stack([c[b], c_ctx], 0)), "cosT": cosT, "sinT": sinT, "wmask": wmask})
        maps.append(m)
    return maps


_NC_CACHE = {}


def kernel(**inputs):
    if "nc" not in _NC_CACHE:
        _NC_CACHE["nc"] = build_program(do_moe=True)
    nc = _NC_CACHE["nc"]
    maps = make_in_maps(inputs, do_moe=True)
    res = run_bass_kernel_spmd(nc, maps, core_ids=list(range(8)))
    out = np.empty((2, S_LEN, D), np.float32)
    for core in range(8):
        b, j = core // 4, core % 4
        out[b, j * NQ:(j + 1) * NQ, :] = res.results[core]["out"]
    return out
```

```python
import contextlib
import numpy as np
import concourse.bass as bass
import concourse.mybir as mybir
from concourse.bass_utils import run_bass_kernel_spmd

F32 = mybir.dt.float32; BF16 = mybir.dt.bfloat16; I32 = mybir.dt.int32; U8 = mybir.dt.uint8
U32 = mybir.dt.uint32
ALU = mybir.AluOpType; AF = mybir.ActivationFunctionType
AX = mybir.AxisListType
ENGS = ("pe", "act", "dve", "pool", "sp")
NDMASEM = 12

D = 1024; S_LEN = 8192; CTX = 256; NQ = 2048; NE = 32;
NT = [-(-min(2048, -(-8192 // (i + 1))) // 128) for i in range(NE)]
OFFT = [sum(NT[:i]) for i in range(NE)]
NSLOT = sum(NT) * 128; NSLOTP = NSLOT
QA, KA, VA, QB, KB, VB = 0, 512, 1024, 1536, 2048, 2176
NKT = 66
WROWS = 2560
EPS = 1e-6
LAM_INIT = 0.2


def L(method, *args, **kw):
    return lambda e: getattr(e, method)(*args, **kw)


class Buf:
    __slots__ = ("name", "w", "r", "excl")
    def __init__(self, name="", excl=False):
        self.name = name; self.w = None; self.r = []; self.excl = excl


class Op:
    __slots__ = ("eng", "fn", "deps", "dma", "signal", "sem", "val", "idx", "name")
    def __init__(self, eng, fn, dma, name):
        self.eng = eng; self.fn = fn; self.deps = []; self.dma = dma
        self.signal = False; self.sem = None; self.val = 0; self.idx = 0; self.name = name


class Sched:
    def __init__(self):
        self.ops = {e: [] for e in ENGS}
        self.all = []

    def op(self, eng, fn, reads=(), writes=(), dma=False, deps=(), name=""):
        o = Op(eng, fn, dma, name)
        ds = {}
        ex = [b for b in reads if b.excl]
        if ex:
            reads = [b for b in reads if not b.excl]; writes = list(writes) + ex
        for b in reads:
            if b.w is not None: ds[id(b.w)] = b.w
        for b in writes:
            if b.w is not None: ds[id(b.w)] = b.w
            lastr = {}
            for r in b.r:
                if r.dma: ds[id(r)] = r
                elif r.eng not in lastr or r.idx > lastr[r.eng].idx: lastr[r.eng] = r
            for r in lastr.values(): ds[id(r)] = r
        for d in deps: ds[id(d)] = d
        o.deps = list(ds.values())
        for b in reads: b.r.append(o)
        for b in writes:
            b.w = o; b.r = []
        o.idx = len(self.ops[eng]); self.ops[eng].append(o); self.all.append(o)
        return o

    def dma(self, eng, out, in_, reads=(), writes=(), name="", **kw):
        return self.op(eng, L("dma_start", out=out, in_=in_, **kw), reads, writes, dma=True, name=name)

    def barrier(self):
        lasts = []
        for e in ENGS:
            real = [o for o in self.ops[e] if o.fn is not None]
            comp = [o for o in real if not o.dma]
            if comp: lasts.append(comp[-1])
            lasts.extend([o for o in real if o.dma][-NDMASEM:])
        for e in ENGS:
            self.op(e, None, deps=lasts, name="barrier")

    def emit(self, nc):
        for o in self.all:
            for d in o.deps:
                if d.eng == "pe" and o.eng == "pe" and not d.dma:
                    continue
                d.signal = True
        with contextlib.ExitStack() as st:
            csem = {e: st.enter_context(nc.semaphore("c_" + e)) for e in ENGS}
            dsem = {e: [st.enter_context(nc.semaphore(f"d_{e}{i}")) for i in range(NDMASEM)]
                    for e in ("sp", "pool", "act")}
            for e in ENGS:
                cnt = 0; di = 0; dcnt = [0] * NDMASEM; dlast = [None] * NDMASEM
                for o in self.ops[e]:
                    if o.fn is None: continue
                    if o.dma:
                        o.signal = True
                        s = di % NDMASEM; di += 1
                        if dlast[s] is not None:
                            o.deps.append(dlast[s])
                        dcnt[s] += 16; o.sem = dsem[e][s]; o.val = dcnt[s]; dlast[s] = o
                    elif o.signal:
                        cnt += 1; o.sem = csem[e]; o.val = cnt
            blk = st.enter_context(nc.Block())
            engobj = {"pe": "tensor", "act": "scalar", "dve": "vector", "pool": "gpsimd", "sp": "sync"}

            def mk(e):
                def body(eng):
                    seen = {}
                    for o in self.ops[e]:
                        need = {}
                        for d in o.deps:
                            if d.fn is None: continue
                            if d.eng == "pe" and e == "pe" and not d.dma: continue
                            k = id(d.sem)
                            if seen.get(k, 0) >= d.val: continue
                            if k not in need or need[k][1] < d.val: need[k] = (d.sem, d.val)
                        for k, (s, v) in need.items():
                            eng.wait_ge(s, v); seen[k] = v
                        if o.fn is None: continue
                        inst = o.fn(eng)
                        if o.signal:
                            inst.then_inc(o.sem, 16 if o.dma else 1)
                return body
            for e in ENGS:
                if self.ops[e]:
                    getattr(blk, engobj[e])(mk(e))


def build_program(do_moe=True, dbg=False, stop_after=99):
    nc = bass.Bass("TRN2", target_bir_lowering=False)
    dt_ = nc.dram_tensor

    def din(name, shape, dt=F32):
        return dt_(name, list(shape), dt, kind="ExternalInput").ap()

    xkv = din("xkv", [S_LEN, D]); ctxd = din("ctx", [CTX, D]); cvec = din("cvec", [2, D])
    cosd = din("cosT", [128, S_LEN + CTX]); sind = din("sinT", [128, S_LEN + CTX])
    wmaskd = din("wmask", [128, 2]); constd = din("consts", [128, 9 * 128])
    w_mod = din("w_mod", [D, 6 * D]); b_mod = din("b_mod", [6 * D]); n1g = din("norm1_g", [D])
    w_in = din("w_in", [D, 2304]); lamd = din("lam", [4, 64]); sublnd = din("subln_g", [128])
    sinkd = din("sink", [8]); w_out = din("w_out", [D, D]); n2g = din("norm2_g", [D])
    w_r = din("w_router", [D, NE]); b_r = din("b_router", [NE]); fgd = din("final_g", [D])
    if do_moe:
        w_gu = din("w_gate_up", [NE, D, 2 * D]); b_gu = din("b_gate_up", [NE, 2 * D])
        w_dn = din("w_down", [NE, D, D]); b_dn = din("b_down", [NE, D])
    outd = dt_("out", [NQ, D], F32, kind="ExternalOutput").ap()
    kscr = dt_("kscr", [4, 9, 128, 1024], BF16).ap()
    vscr = dt_("vscr", [4, 9, 128, 8, 128], BF16).ap()
    x1scr = dt_("x1scr", [NQ, D], F32).ap()
    xe = dt_("xe", [NSLOTP, D], BF16).ap()
    ye = dt_("ye", [NSLOTP, D], F32).ap()
    if dbg:
        dbg_x1 = dt_("dbg_x1", [NQ, D], F32, kind="ExternalOutput").ap()
        dbg_yb = dt_("dbg_yb", [128, 4 * NQ], BF16, kind="ExternalOutput").ap()
        dbg_kb = dt_("dbg_kb", [128, WROWS + CTX], BF16, kind="ExternalOutput").ap()
        dbg_vb = dt_("dbg_vb", [128, 22 * 128], BF16, kind="ExternalOutput").ap()
        dbg_qb = dt_("dbg_qb", [128, 4 * WROWS], BF16, kind="ExternalOutput").ap()

    TOT = 206 * 1024
    arena = nc.alloc_sbuf_tensor("arena", [128, TOT], U8)
    psum = nc.alloc_psum_tensor("psum", [128, 8, 512], F32)
    off = [0]

    def alloc(shape, dt=F32):
        n = int(np.prod(shape[1:])) * (2 if dt == BF16 else 4)
        a = arena[0:shape[0], off[0]:off[0] + n].bitcast(dt)
        off[0] += (n + 63) // 64 * 64
        assert off[0] <= TOT, ("SBUF overflow", off[0])
        if len(shape) == 3:
            a = a.rearrange("p (a b) -> p a b", b=shape[2])
        elif len(shape) == 4:
            a = a.rearrange("p (a b c) -> p a b c", b=shape[2], c=shape[3])
        return a

    def pbank(b, n=1):
        return psum[:, b:b + n, :]

    def pflat(b, n=1):
        return psum[:, b:b + n, :].rearrange("p a b -> p (a b)")

    def pbf(b):
        return psum[:, b, :].bitcast(BF16).rearrange("p (a b) -> p a b", b=128)

    S = Sched()

    def finish():
        S.barrier()
        S.emit(nc)
        return nc
    PB = [Buf(f"psum{i}", excl=True) for i in range(8)]

    cst = alloc([128, 9, 128], F32)
    ident_f = cst[:, 0, :]; iota_f = cst[:, 5, 0:32]; pidx = cst[:, 6, :]; OFFrep = cst[0:32, 7, :]; Lmask = cst[0:32, 8, 0:32]
    cstb = alloc([128, 5, 128], BF16)
    ident_b = cstb[:, 0, :]; RT_b = cstb[:, 1, :]; U_b = cstb[:, 2, :]; triL = cstb[:, 3, :]; triR = cstb[:, 4, :]
    ones_b = alloc([128, 128], BF16); ones_f = alloc([128, 128], F32)
    A1T = alloc([128, 8]); B1T = alloc([128, 8]); A1cT = alloc([128, 8]); B1cT = alloc([128, 8])
    gsubp = alloc([128, 1]); neglam = alloc([128, 1]); sinkexp = alloc([128, 4]); wmask = alloc([128, 2])
    G1bc = alloc([128, D]); A2bc = alloc([128, D]); B2bc = alloc([128, D]); G2bc = alloc([128, D]); FGbc = alloc([128, D])
    desti = alloc([128, 64], I32); gates = alloc([128, 64]); basecnt = alloc([128, 32])
    ek_all = alloc([128, 64]); rk_all = alloc([128, 64]); Pm = alloc([32, 32]); permbc = alloc([128, 32])
    idxg = alloc([128, 32 * 8], I32); bdn_idx = alloc([128, 32], I32)
    junk = alloc([128, D], BF16); Bjunk = Buf("junk")
    Bc = Buf("consts"); Bvec = Buf("vecs"); Bbc = Buf("bcast"); Brt = Buf("route")

    S.dma("sp", cst.rearrange("p a b -> p (a b)"), constd, writes=[Bc])
    S.dma("sp", wmask, wmaskd, writes=[Bc])
    S.op("dve", L("tensor_copy", out=cstb, in_=cst[:, 0:5, :]), reads=[Bc], writes=[Bc])
    S.op("dve", L("memset", ones_b, 1.0), writes=[Bc])
    S.op("dve", L("memset", ones_f, 1.0), writes=[Bc])
    S.op("dve", L("memset", basecnt, 0.0), writes=[Brt])

    mark_persist = off[0]

    cT = alloc([128, 8, 2]); sg = alloc([128, 8, 2]); scT = alloc([128, 8, 2]); scT_b = alloc([128, 8, 2], BF16)
    screp = alloc([128, 8, 128], BF16)
    bmT = alloc([128, 16]); g1nT = alloc([128, 8]); modT = alloc([128, 16, 2])
    bmbc = alloc([128, 4 * D]); n2bc = alloc([128, D])
    lamt = alloc([1, 4, 64]); lamp = alloc([1, 2, 64]); lams = alloc([1, 2]); lamv = alloc([1, 2])
    wm = [alloc([128, 8, 1024], BF16) for _ in range(2)]
    Bwm = [Buf("wm0"), Buf("wm1")]; Bp0 = Buf("p0")

    for r_ in range(2):
        S.dma("sp", cT[:, :, r_], cvec[r_].rearrange("(c p) -> p c", p=128), writes=[Bp0], allow_slow_non_contiguous=True)
    S.dma("sp", bmT, b_mod[0:2048].rearrange("(c p) -> p c", p=128), writes=[Bp0], allow_slow_non_contiguous=True)
    S.dma("sp", g1nT, n1g.rearrange("(c p) -> p c", p=128), writes=[Bp0], allow_slow_non_contiguous=True)
    S.dma("sp", bmbc, b_mod[2048:6144].partition_broadcast(128), writes=[Bp0])
    S.dma("sp", n2bc, n2g.partition_broadcast(128), writes=[Bp0])
    S.dma("sp", FGbc, fgd.partition_broadcast(128), writes=[Bbc])
    S.dma("sp", gsubp, sublnd.rearrange("(p o) -> p o", o=1), writes=[Bvec])
    S.dma("sp", lamt.rearrange("p a b -> p (a b)"), lamd.rearrange("(o a) b -> o (a b)", o=1), writes=[Bp0])
    for g in range(2):
        S.dma("sp", sinkexp[g * 64:(g + 1) * 64, :], sinkd[g * 4:(g + 1) * 4].partition_broadcast(64), writes=[Bvec])
    wmv = w_mod.rearrange("(c p) n -> p c n", p=128)
    for i in range(2):
        S.dma("pool", wm[i], wmv[:, :, i * 1024:(i + 1) * 1024], writes=[Bwm[i]])

    S.op("act", L("activation", out=sg, in_=cT, func=AF.Sigmoid), reads=[Bp0], writes=[Bp0])
    S.op("dve", L("tensor_tensor", out=scT, in0=cT, in1=sg, op=ALU.mult), reads=[Bp0], writes=[Bp0])
    S.op("dve", L("tensor_copy", out=scT_b, in_=scT), reads=[Bp0], writes=[Bp0])
    S.op("dve", L("tensor_copy", out=screp, in_=scT[:, :, 0:1].to_broadcast([128, 8, 128])), reads=[Bp0], writes=[Bp0])
    S.op("dve", L("tensor_scalar", out=gsubp, in0=gsubp, scalar1=1.0 - LAM_INIT, scalar2=None, op0=ALU.mult),
         reads=[Bvec], writes=[Bvec])
    S.op("act", L("activation", out=sinkexp, in_=sinkexp, func=AF.Exp), reads=[Bvec], writes=[Bvec])
    S.op("dve", L("tensor_tensor", out=lamp, in0=lamt[:, 0:4:2, :], in1=lamt[:, 1:4:2, :], op=ALU.mult), reads=[Bp0], writes=[Bp0])
    S.op("dve", L("reduce_sum", out=lams, in_=lamp, axis=AX.X), reads=[Bp0], writes=[Bp0])
    S.op("act", L("activation", out=lamv, in_=lams, func=AF.Exp), reads=[Bp0], writes=[Bp0])
    S.op("dve", L("tensor_tensor", out=lamv[:, 0:1], in0=lamv[:, 1:2], in1=lamv[:, 0:1], op=ALU.subtract), reads=[Bp0], writes=[Bp0])
    S.op("dve", L("tensor_scalar", out=lamv[:, 0:1], in0=lamv[:, 0:1], scalar1=-LAM_INIT, scalar2=None, op0=ALU.add), reads=[Bp0], writes=[Bp0])
    S.op("pe", L("matmul", psum[:, 7, 0:1], lhsT=ones_f[0:1, :], rhs=lamv[:, 0:1], start=True, stop=True),
         reads=[Bp0, Bc], writes=[PB[7]])
    S.op("dve", L("tensor_copy", out=neglam, in_=psum[:, 7, 0:1]), reads=[PB[7]], writes=[Bvec])

    pm = psum[:, 0, 0:32].rearrange("p (a b) -> p a b", b=2)
    for blk in range(16):
        i = blk // 8
        for c in range(8):
            S.op("pe", L("matmul", pm[:, blk, :], lhsT=wm[i][:, c, (blk % 8) * 128:(blk % 8 + 1) * 128],
                                                              rhs=scT_b[:, c, :], start=(c == 0), stop=(c == 7)),
                 reads=[Bwm[i], Bp0], writes=[PB[0]])
    S.op("dve", L("tensor_tensor", out=modT, in0=pm, in1=bmT.unsqueeze(2).to_broadcast([128, 16, 2]), op=ALU.add),
         reads=[PB[0], Bp0], writes=[Bp0])
    for (AT, BT, r) in ((A1T, B1T, 0), (A1cT, B1cT, 1)):
        S.op("dve", L("scalar_tensor_tensor", out=AT, in0=modT[:, 8:16, r], scalar=1.0, in1=g1nT, op0=ALU.add, op1=ALU.mult),
             reads=[Bp0], writes=[Bvec])
        S.op("dve", L("tensor_copy", out=BT, in_=modT[:, 0:8, r]), reads=[Bp0], writes=[Bvec])
    dsts = [G1bc, B2bc, A2bc, G2bc]
    for q in range(4):
        i = q % 2
        S.dma("pool", wm[i], wmv[:, :, (2 + q) * 1024:(3 + q) * 1024], writes=[Bwm[i]])
        for hf in range(2):
            for c in range(8):
                S.op("pe", L("matmul", psum[:, 1 + hf, :], lhsT=screp[:, c, :], rhs=wm[i][:, c, hf * 512:(hf + 1) * 512],
                                                                start=(c == 0), stop=(c == 7)),
                     reads=[Bwm[i], Bp0], writes=[PB[1 + hf]])
        S.op("dve", L("tensor_tensor", out=dsts[q], in0=pflat(1, 2), in1=bmbc[:, q * 1024:(q + 1) * 1024], op=ALU.add),
             reads=[PB[1], PB[2], Bp0], writes=[Bbc])
    S.op("dve", L("scalar_tensor_tensor", out=A2bc, in0=A2bc, scalar=1.0, in1=n2bc, op0=ALU.add, op1=ALU.mult),
         reads=[Bbc, Bp0], writes=[Bbc])

    if stop_after == 0:
        return finish()
    S.barrier()
    off[0] = mark_persist

    QaT = alloc([128, 4, WROWS], BF16); QbT = alloc([128, 4, WROWS], BF16)
    KbT = alloc([128, WROWS + CTX], BF16); Vb = alloc([128, 22, 128], BF16)
    BQ = Buf("Q"); BKb = Buf("Kb"); BVb = Buf("Vb"); Bya = Buf("ya"); Byb = Buf("yb")
    Bks = Buf("kscr"); Bvs = Buf("vscr")
    mark_attn = off[0]

    win = alloc([128, 8, 2304], BF16); Bwin = Buf("win")
    xt = [alloc([128, D]) for _ in range(2)]; Bxt = [Buf("xt0"), Buf("xt1")]
    xn = [alloc([128, D], BF16) for _ in range(4)]; Bxn = [Buf(f"xn{i}") for i in range(4)]
    st4 = [alloc([128, 4]) for _ in range(4)]; Bst = [Buf(f"st{i}") for i in range(4)]
    evt = [alloc([128, 8, 128]) for _ in range(2)]; Bev = [Buf("ev0"), Buf("ev1")]
    hxT = [alloc([128, 8, 512], BF16) for _ in range(2)]; Bhx = [Buf("hx0"), Buf("hx1")]
    cosg = [alloc([128, 512]) for _ in range(2)]; sing = [alloc([128, 512]) for _ in range(2)]; Btab = [Buf("tab0"), Buf("tab1")]
    kbf = [alloc([128, 512], BF16) for _ in range(2)]; Bkbf = [Buf("kbf0"), Buf("kbf1")]
    rt1 = [alloc([128, 512]) for _ in range(2)]; rt2 = [alloc([128, 512]) for _ in range(2)]
    Brt1 = [Buf("rt1a"), Buf("rt1b")]; Brt2 = [Buf("rt2a"), Buf("rt2b")]
    kst = [alloc([128, 4, 512], BF16) for _ in range(2)]; Bkst = [Buf("kst0"), Buf("kst1")]
    vst = [alloc([128, 4, 4, 128], BF16) for _ in range(2)]; Bvst = [Buf("vst0"), Buf("vst1")]

    winv = w_in.rearrange("(c p) n -> p c n", p=128)
    S.dma("pool", win[:, :, 0:QB], winv[:, :, 0:QB], writes=[Bwin])
    S.dma("pool", win[:, :, KB:2304], winv[:, :, KB:2304], writes=[Bwin])
    for gi in range(4):
        for g in range(2):
            S.dma("pool", win[:, :, QB + gi * 128 + g * 64:QB + gi * 128 + (g + 1) * 64],
                  winv[:, :, QB + g * 256 + gi * 64:QB + g * 256 + (gi + 1) * 64], writes=[Bwin])
    import os
    rr = [0]

    pcyc = [0]

    def nextbank():
        b = 2 + pcyc[0] % 4; pcyc[0] += 1
        return b

    def projA(job, hb, tb):
        colbase, n, dst, wb = job
        i = rr[0] % 2; rr[0] += 1
        bk = nextbank()
        for c in range(8):
            S.op("pe", L("matmul", psum[:, bk, 0:n], lhsT=win[:, c, colbase:colbase + 128], rhs=hxT[hb][:, c, 0:n], start=(c == 0), stop=(c == 7)),
                 reads=[Bwin, Bhx[hb]], writes=[PB[bk]])
        S.op("act", L("activation", out=kbf[i][:, 0:n], in_=psum[:, bk, 0:n], func=AF.Copy), reads=[PB[bk]], writes=[Bkbf[i]])
        return (bk, i, n, dst, wb, tb)

    def ropeB(st):
        bk, i, n, dst, wb, tb = st
        S.op("pe", L("matmul", psum[:, 6 + i, 0:n], lhsT=RT_b, rhs=kbf[i][:, 0:n], start=True, stop=True), reads=[Bkbf[i], Bc], writes=[PB[6 + i]])
        S.op("dve", L("tensor_tensor", out=rt1[i][:, 0:n], in0=psum[:, bk, 0:n], in1=cosg[tb][:, 0:n], op=ALU.mult),
             reads=[PB[bk], Btab[tb]], writes=[Brt1[i]])
        S.op("dve", L("tensor_tensor", out=rt2[i][:, 0:n], in0=psum[:, 6 + i, 0:n], in1=sing[tb][:, 0:n], op=ALU.mult),
             reads=[PB[6 + i], Btab[tb]], writes=[Brt2[i]])
        S.op("pool", L("tensor_tensor", out=dst, in0=rt1[i][:, 0:n], in1=rt2[i][:, 0:n], op=ALU.add), reads=[Brt1[i], Brt2[i]], writes=wb)

    def norm_group(G):
        ntile = 4 if G < 16 else 2
        for t in range(ntile):
            gt = G * 4 + t
            xb = gt % 2; x4i = gt % 4
            src = xkv[gt * 128:(gt + 1) * 128, :] if G < 16 else ctxd[t * 128:(t + 1) * 128, :]
            S.dma("sp", xt[xb], src, writes=[Bxt[xb]])
            S.op("act", L("activation", out=junk, in_=xt[xb], func=AF.Square, accum_out=st4[x4i][:, 0:1]),
                 reads=[Bxt[xb]], writes=[Bjunk, Bst[x4i]])
            S.op("act", L("activation", out=st4[x4i][:, 1:2], in_=st4[x4i][:, 0:1], func=AF.Sqrt, scale=1.0 / D, bias=EPS),
                 reads=[Bst[x4i]], writes=[Bst[x4i]])
            S.op("dve", L("reciprocal", out=st4[x4i][:, 2:3], in_=st4[x4i][:, 1:2]), reads=[Bst[x4i]], writes=[Bst[x4i]])
            S.op("act", L("activation", out=xn[x4i], in_=xt[xb], func=AF.Copy, scale=st4[x4i][:, 2:3]),
                 reads=[Bxt[xb], Bst[x4i]], writes=[Bxn[x4i]])

    norm_group(0)
    for G in range(17):
        ntile = 4 if G < 16 else 2
        n = ntile * 128
        hb = G % 2
        AT, BT = (A1T, B1T) if G < 16 else (A1cT, B1cT)
        tb = G % 2
        S.dma("sp", cosg[tb][:, 0:n], cosd[:, G * 512:G * 512 + n], writes=[Btab[tb]])
        S.dma("sp", sing[tb][:, 0:n], sind[:, G * 512:G * 512 + n], writes=[Btab[tb]])
        for t in range(ntile):
            gt = G * 4 + t
            xb = gt % 2; x4i = gt % 4
            tbk = xb
            for c in range(8):
                S.op("pe", L("transpose", out=pbf(tbk)[:, c, :], in_=xn[x4i][:, c * 128:(c + 1) * 128], identity=ident_b),
                     reads=[Bxn[x4i], Bc], writes=[PB[tbk]])
            S.op("dve", L("tensor_tensor", out=evt[xb], in0=pbf(tbk), in1=AT.unsqueeze(2).to_broadcast([128, 8, 128]), op=ALU.mult),
                 reads=[PB[tbk], Bvec], writes=[Bev[xb]])
            S.op("pool", L("tensor_tensor", out=hxT[hb][:, :, t * 128:(t + 1) * 128], in0=evt[xb],
                           in1=BT.unsqueeze(2).to_broadcast([128, 8, 128]), op=ALU.add), reads=[Bev[xb], Bvec], writes=[Bhx[hb]])
        if G + 1 < 17:
            norm_group(G + 1)
        ch = G // 2 if G < 16 else 8
        ko = (G % 2) * 512 if G < 16 else 0
        sb = G % 2
        jobs = [(KA + h * 128, n, kst[sb][:, h, 0:n], [Bkst[sb]]) for h in range(4)]
        if G < 5 or G == 16:
            wo = G * 512 if G < 16 else WROWS
            jobs.append((KB, n, KbT[:, wo:wo + n], [BKb]))
        if G < 5:
            for (base, dstT) in ((QA, QaT), (QB, QbT)):
                for h in range(4):
                    jobs.append((base + h * 128, 512, dstT[:, h, G * 512:(G + 1) * 512], [BQ]))
        st_ = projA(jobs[0], hb, tb)
        for ji in range(len(jobs)):
            nxt = projA(jobs[ji + 1], hb, tb) if ji + 1 < len(jobs) else None
            ropeB(st_)
            st_ = nxt
            if ji == 3:
                for h in range(4):
                    S.dma("sp", kscr[h, ch, :, ko:ko + n], kst[sb][:, h, 0:n], reads=[Bkst[sb]], writes=[Buf()])
        for t in range(ntile):
            bk = nextbank()
            for c in range(8):
                S.op("pe", L("matmul", psum[:, bk, :], lhsT=hxT[hb][:, c, t * 128:(t + 1) * 128], rhs=win[:, c, VA:VA + 512],
                             start=(c == 0), stop=(c == 7)), reads=[Bwin, Bhx[hb]], writes=[PB[bk]])
            S.op("act", L("activation", out=vst[sb][:, :, t, :], in_=psum[:, bk, :].rearrange("p (h e) -> p h e", e=128), func=AF.Copy),
                 reads=[PB[bk]], writes=[Bvst[sb]])
        for h in range(4):
            tl0 = (G % 2) * 4 if G < 16 else 0
            S.dma("sp", vscr[h, ch, :, tl0:tl0 + ntile, :], vst[sb][:, h, 0:ntile, :], reads=[Bvst[sb]], writes=[Buf()])
        if G < 5 or G == 16:
            bk = nextbank()
            for t in range(ntile):
                for c in range(8):
                    S.op("pe", L("matmul", psum[:, bk, t * 128:(t + 1) * 128], lhsT=hxT[hb][:, c, t * 128:(t + 1) * 128],
                                 rhs=win[:, c, VB:VB + 128], start=(c == 0), stop=(c == 7)), reads=[Bwin, Bhx[hb]], writes=[PB[bk]])
            vt0 = G * 4 if G < 16 else 20
            S.op("act", L("activation", out=Vb[:, vt0:vt0 + ntile, :], in_=psum[:, bk, 0:n].rearrange("p (t e) -> p t e", e=128), func=AF.Copy),
                 reads=[PB[bk]], writes=[BVb])

    if stop_after == 1:
        return finish()
    S.barrier()
    off[0] = mark_attn
    yaT = alloc([128, 4, NQ], BF16); ybT = alloc([128, 4, NQ], BF16)
    mark_att2 = off[0]

    kch = [alloc([128, 1024], BF16) for _ in range(3)]; vch = [alloc([128, 8, 128], BF16) for _ in range(3)]
    Bkch = [Buf(f"kch{i}") for i in range(3)]; Bvch = [Buf(f"vch{i}") for i in range(3)]
    PT = [alloc([128, 1024], BF16) for _ in range(3)]; BPT = [Buf(f"PT{i}") for i in range(3)]
    r1 = alloc([128, 512]); r2 = alloc([128, 512]); t1 = alloc([128, 512]); t2 = alloc([128, 512])
    ot = alloc([128, 512]); osq = alloc([128, 512]); rs = alloc([128, 512]); zsb = alloc([64, 512])
    selA = alloc([64, 128]); selB = alloc([64, 128])
    o1sb = alloc([128, 512]); o2sb = alloc([128, 512])
    Bpp = [Buf(f"pp{i}") for i in range(10)]; Bsel = Buf("sel")
    S.op("dve", L("memset", selA, 0.0), writes=[Bsel]); S.op("dve", L("memset", selB, 0.0), writes=[Bsel])
    S.op("dve", L("memset", selA[0:32, :], 1.0 / 32), writes=[Bsel]); S.op("dve", L("memset", selB[32:64, :], 1.0 / 32), writes=[Bsel])
    if do_moe:
        zer = alloc([128, 4, D], BF16); Bzer = Buf("zer")
        S.op("pool", L("memset", zer, 0.0), writes=[Bzer])
        Bxe0 = []
        xev = xe[0:NSLOT, :].rearrange("(n p) d -> p n d", p=128)
        for i in range(NSLOT // 128 // 4):
            bz = Buf(); Bxe0.append(bz)
            S.dma("pool", xev[:, i * 4:(i + 1) * 4, :], zer, reads=[Bzer], writes=[bz])

    chunks = [(ci, 8) for ci in range(8)] + [(8, 2)]
    seq = [(h, qb, ci, nt) for h in range(4) for qb in range(4) for (ci, nt) in chunks]

    def issue_load(j):
        if j >= len(seq): return
        h, qb, ci, nt = seq[j]
        bi = j % 3
        S.dma("sp", kch[bi][:, 0:nt * 128], kscr[h, ci, :, 0:nt * 128], reads=[Bks], writes=[Bkch[bi]])
        S.dma("sp", vch[bi][:, 0:nt, :], vscr[h, ci, :, 0:nt, :], reads=[Bvs], writes=[Bvch[bi]])

    blocks = []
    for h in range(4):
        for qb in range(4):
            units = []
            for j0, (ci, nt) in enumerate(chunks):
                j = (h * 4 + qb) * 9 + j0
                for tt in range(nt):
                    units.append((j, tt, ci))
            blocks.append((h, qb, units))
    NU = len(blocks[0][2])

    def emit_S(bidx, k):
        h, qb, units = blocks[bidx]
        q0 = 128 + qb * 512
        j, tt, ci = units[k]
        g = bidx * NU + k
        bi = j % 3; sbk = (g % 2) * 2
        for cmp_ in range(2):
            S.op("pe", L("matmul", psum[:, sbk + cmp_, :], lhsT=kch[bi][cmp_ * 64:(cmp_ + 1) * 64, tt * 128:(tt + 1) * 128],
                         rhs=QaT[cmp_ * 64:(cmp_ + 1) * 64, h, q0:q0 + 512], start=True, stop=True),
                 reads=[Bkch[bi], BQ], writes=[PB[sbk + cmp_]])

    def post_stages(h, qb):
        def s1():
            S.op("pe", L("matmul", psum[:, 7, :], lhsT=selA, rhs=zsb, start=True, stop=True), reads=[Bpp[7], Bsel], writes=[PB[7]])
        def s2():
            S.op("dve", L("reciprocal", out=r1, in_=psum[:, 7, :]), reads=[PB[7]], writes=[Bpp[0]])
        def s3():
            S.op("pe", L("matmul", psum[:, 7, :], lhsT=selB, rhs=zsb, start=True, stop=True), reads=[Bpp[7], Bsel], writes=[PB[7]])
        def s4():
            S.op("dve", L("reciprocal", out=r2, in_=psum[:, 7, :]), reads=[PB[7]], writes=[Bpp[1]])
            S.op("dve", L("tensor_tensor", out=t1, in0=o1sb, in1=r1, op=ALU.mult), reads=[Bpp[8], Bpp[0]], writes=[Bpp[2]])
            S.op("dve", L("tensor_tensor", out=t2, in0=o2sb, in1=r2, op=ALU.mult), reads=[Bpp[9], Bpp[1]], writes=[Bpp[3]])
            S.op("dve", L("scalar_tensor_tensor", out=ot, in0=t2, scalar=neglam, in1=t1, op0=ALU.mult, op1=ALU.add),
                 reads=[Bpp[2], Bpp[3], Bvec], writes=[Bpp[4]])
        def s5():
            S.op("act", L("activation", out=osq, in_=ot, func=AF.Square), reads=[Bpp[4]], writes=[Bpp[5]])
        def s6():
            S.op("pe", L("matmul", psum[:, 7, :], lhsT=ones_f, rhs=osq, start=True, stop=True), reads=[Bpp[5], Bc], writes=[PB[7]])
        def s7():
            S.op("act", L("activation", out=rs, in_=psum[:, 7, :], func=AF.Sqrt, scale=1.0 / 128, bias=EPS), reads=[PB[7]], writes=[Bpp[6]])
        def s8():
            S.op("dve", L("reciprocal", out=rs, in_=rs), reads=[Bpp[6]], writes=[Bpp[6]])
            S.op("dve", L("scalar_tensor_tensor", out=yaT[:, h, qb * 512:(qb + 1) * 512], in0=ot, scalar=gsubp, in1=rs, op0=ALU.mult, op1=ALU.mult),
                 reads=[Bpp[4], Bpp[6], Bvec], writes=[Bya])
        return [s1, s2, s3, s4, s5, s6, s7, s8]

    STAGE_AT = {3: 0, 7: 1, 11: 2, 15: 3, 22: 4, 26: 5, 31: 6, 35: 7}
    def emit_Sg(g):
        if g < len(blocks) * NU:
            emit_S(g // NU, g % NU)

    issue_load(0); issue_load(1)
    emit_Sg(0); emit_Sg(1)
    pending = None
    for bidx, (h, qb, units) in enumerate(blocks):
        for k in range(NU):
            j, tt, ci = units[k]
            g = bidx * NU + k
            if tt == 0:
                issue_load(j + 2)
            bi = j % 3; sbk = (g % 2) * 2; pi = g % 3
            S.op("act", L("activation", out=PT[pi], in_=pflat(sbk, 2), func=AF.Exp, scale=0.125),
                 reads=[PB[sbk], PB[sbk + 1]], writes=[BPT[pi]])
            emit_Sg(g + 2)
            first = (k == 0); last = (k == NU - 1)
            for cmp_ in range(2):
                S.op("pe", L("matmul", psum[:, 4 + cmp_, :], lhsT=vch[bi][:, tt, :], rhs=PT[pi][:, cmp_ * 512:(cmp_ + 1) * 512], start=first, stop=last),
                     reads=[Bvch[bi], BPT[pi]], writes=[PB[4 + cmp_]])
            for cmp_ in range(2):
                S.op("pe", L("matmul", psum[cmp_ * 32:(cmp_ + 1) * 32, 6, :], lhsT=ones_b[:, 0:32], rhs=PT[pi][:, cmp_ * 512:(cmp_ + 1) * 512],
                             start=first, stop=last, tile_position=(0, cmp_ * 32)),
                     reads=[BPT[pi], Bc], writes=[PB[6]])
            if pending is not None and k in STAGE_AT:
                pending[STAGE_AT[k]]()
        S.op("act", L("activation", out=o1sb, in_=psum[:, 4, :], func=AF.Copy), reads=[PB[4]], writes=[Bpp[8]])
        S.op("act", L("activation", out=o2sb, in_=psum[:, 5, :], func=AF.Copy), reads=[PB[5]], writes=[Bpp[9]])
        S.op("dve", L("tensor_copy", out=zsb, in_=psum[0:64, 6, :]), reads=[PB[6]], writes=[Bpp[7]])
        pending = post_stages(h, qb)
    for st_fn in pending:
        st_fn()

    if stop_after == 2:
        return finish()
    NPW = 4
    PW = [alloc([128, 512], BF16) for _ in range(NPW)]; BPW = [Buf(f"PW{i}") for i in range(NPW)]
    zt = [alloc([128, 512]) for _ in range(2)]; Bzt = [Buf("zt0"), Buf("zt1")]
    WP = []
    for nb in range(16):
        for k, (kt, kind) in enumerate([(nb, "L"), (nb + 1, "C"), (nb + 2, "R"), (20, "X"), (21, "X")]):
            WP.append((nb, k, kt, kind))

    def emit_WS(p):
        nb, k, kt, kind = WP[p]
        kc0 = kt * 128; q0 = 128 + nb * 128
        for g in range(2):
            gs = slice(g * 64, (g + 1) * 64)
            sbk = 2 * (p % 2) + g
            S.op("pe", L("matmul", psum[:, sbk, :].rearrange("p (i q) -> p i q", q=128), lhsT=KbT[gs, kc0:kc0 + 128], rhs=QbT[gs, :, q0:q0 + 128],
                         start=True, stop=True), reads=[BKb, BQ], writes=[PB[sbk]])

    emit_WS(0); emit_WS(1)
    for p, (nb, k, kt, kind) in enumerate(WP):
        ob = 4 + 2 * (nb % 2)
        if kind == "L" and nb == 0:
            bias = wmask[:, 0:1]
        elif kind == "R" and nb == 15:
            bias = wmask[:, 1:2]
        else:
            bias = 0.0
        for g in range(2):
            sbk = 2 * (p % 2) + g; pi = 2 * (p % 2) + g
            S.op("act", L("activation", out=PW[pi], in_=psum[:, sbk, :], func=AF.Exp, scale=0.125, bias=bias),
                 reads=[PB[sbk], Bc], writes=[BPW[pi]])
            if kind in ("L", "R"):
                tri = triL if kind == "L" else triR
                pw3 = PW[pi].rearrange("p (i q) -> p i q", q=128)
                S.op("dve", L("tensor_tensor", out=pw3, in0=pw3, in1=tri.unsqueeze(1).to_broadcast([128, 4, 128]), op=ALU.mult),
                     reads=[BPW[pi], Bc], writes=[BPW[pi]])
        first = (k == 0); last = (k == 4)
        for g in range(2):
            gs = slice(g * 64, (g + 1) * 64); pi = 2 * (p % 2) + g
            S.op("pe", L("matmul", psum[gs, ob, :], lhsT=Vb[:, kt, gs], rhs=PW[pi], start=first, stop=last, tile_position=(0, g * 64)),
                 reads=[BVb, BPW[pi]], writes=[PB[ob]])
        for g in range(2):
            gs = slice(g * 64, (g + 1) * 64); pi = 2 * (p % 2) + g
            S.op("pe", L("matmul", psum[gs, ob + 1, :], lhsT=ones_b[:, 0:64], rhs=PW[pi], start=first, stop=last, tile_position=(0, g * 64)),
                 reads=[Bc, BPW[pi]], writes=[PB[ob + 1]])
        if p + 2 < len(WP):
            emit_WS(p + 2)
        if k == 4:
            zi = nb % 2
            z3 = zt[zi].rearrange("p (i q) -> p i q", q=128)
            S.op("dve", L("tensor_tensor", out=z3, in0=psum[:, ob + 1, :].rearrange("p (i q) -> p i q", q=128),
                          in1=sinkexp.unsqueeze(2).to_broadcast([128, 4, 128]), op=ALU.add), reads=[PB[ob + 1], Bvec], writes=[Bzt[zi]])
            S.op("dve", L("reciprocal", out=zt[zi], in_=zt[zi]), reads=[Bzt[zi]], writes=[Bzt[zi]])
            S.op("dve", L("tensor_tensor", out=ybT[:, :, nb * 128:(nb + 1) * 128], in0=psum[:, ob, :].rearrange("p (i q) -> p i q", q=128),
                          in1=z3, op=ALU.mult), reads=[PB[ob], Bzt[zi]], writes=[Byb])

    if dbg:
        S.dma("sp", dbg_kb, KbT, reads=[BKb])
        S.dma("sp", dbg_vb, Vb.rearrange("p a b -> p (a b)"), reads=[BVb])
        S.dma("sp", dbg_qb, QbT.rearrange("p a b -> p (a b)"), reads=[BQ])
        S.dma("sp", dbg_yb, ybT.rearrange("p a b -> p (a b)"), reads=[Byb])
    if stop_after == 3:
        return finish()
    S.barrier()
    off[0] = mark_att2
    wout = alloc([128, 8, D], BF16); Bwo = Buf("wout")
    wr_b = alloc([128, 8, NE], BF16); wr_f = alloc([128, 8, NE]); br_b = alloc([1, NE], BF16); br_f = alloc([1, NE])
    Bwr = Buf("wr")
    x4 = [alloc([128, D]) for _ in range(2)]; Bx4 = [Buf("x4a"), Buf("x4b")]
    tm4 = alloc([128, D]); Btm4 = Buf("tm4")
    x1t = [alloc([128, D]) for _ in range(2)]; Bx1 = [Buf("x1a"), Buf("x1b")]
    h2 = alloc([128, D]); Bh2 = Buf("h2")
    hx2all = alloc([128, 16, D], BF16); Bhx2t = [Buf(f"hx2_{i}") for i in range(16)]
    hx2T = alloc([128, 8, 128], BF16); Bhx2T = Buf("hx2T")
    s4 = [alloc([128, 4]) for _ in range(2)]; Bs4 = [Buf("s4a"), Buf("s4b")]
    lg = alloc([128, NE]); m8 = alloc([128, 8]); i8 = alloc([128, 8], U32); ekf = alloc([128, 8]); nm0 = alloc([128, 1])
    ge = alloc([128, 4]); gz = alloc([128, 1]); maskb = alloc([128, NE], BF16); destf = alloc([128, NE]); prod = alloc([128, NE])
    dk = alloc([128, 4]); Brr = Buf("rr")
    Bx1s = Buf("x1scr"); Bxes = [Buf(f"xes{i}") for i in range(64)]

    woutv = w_out.rearrange("(c p) n -> p c n", p=128)
    S.dma("pool", wout[:, 0:4, :], woutv[:, 0:4, :], writes=[Bwo])
    for gi in range(4):
        for g in range(2):
            r0 = 512 + g * 256 + gi * 64
            S.dma("pool", wout[g * 64:(g + 1) * 64, 4 + gi, :], w_out[r0:r0 + 64, :], writes=[Bwo])
    S.dma("sp", wr_f, w_r.rearrange("(c p) n -> p c n", p=128), writes=[Bwr])
    S.dma("sp", br_f, b_r.rearrange("(o n) -> o n", o=1), writes=[Bwr])
    S.op("dve", L("tensor_copy", out=wr_b, in_=wr_f), reads=[Bwr], writes=[Bwr])
    S.op("dve", L("tensor_copy", out=br_b, in_=br_f), reads=[Bwr], writes=[Bwr])

    lgs = [alloc([128, NE]) for _ in range(2)]; Blg = [Buf("lg0"), Buf("lg1")]

    def stageA(t):
        b = t % 2
        pb0 = 2 * b
        S.dma("sp", x4[b], xkv[128 + t * 128:128 + (t + 1) * 128, :], writes=[Bx4[b]])
        for hf in range(2):
            for c in range(8):
                lhs = yaT[:, c, t * 128:(t + 1) * 128] if c < 4 else ybT[:, c - 4, t * 128:(t + 1) * 128]
                S.op("pe", L("matmul", psum[:, pb0 + hf, :], lhsT=lhs, rhs=wout[:, c, hf * 512:(hf + 1) * 512],
                                                                             start=(c == 0), stop=(c == 7)),
                     reads=[Bya, Byb, Bwo], writes=[PB[pb0 + hf]])
        S.op("dve", L("tensor_tensor", out=tm4, in0=pflat(pb0, 2), in1=G1bc, op=ALU.mult), reads=[PB[pb0], PB[pb0 + 1], Bbc], writes=[Btm4])
        S.op("pool", L("tensor_tensor", out=x1t[b], in0=tm4, in1=x4[b], op=ALU.add), reads=[Btm4, Bx4[b]], writes=[Bx1[b]])
        S.dma("sp", x1scr[t * 128:(t + 1) * 128, :], x1t[b], reads=[Bx1[b]], writes=[Bx1s])
        if dbg:
            S.dma("sp", dbg_x1[t * 128:(t + 1) * 128, :], x1t[b], reads=[Bx1[b]])
        if not do_moe:
            return
        S.op("act", L("activation", out=junk, in_=x1t[b], func=AF.Square, accum_out=s4[b][:, 0:1]), reads=[Bx1[b]], writes=[Bjunk, Bs4[b]])
        S.op("act", L("activation", out=s4[b][:, 1:2], in_=s4[b][:, 0:1], func=AF.Sqrt, scale=1.0 / D, bias=EPS), reads=[Bs4[b]], writes=[Bs4[b]])
        S.op("dve", L("reciprocal", out=s4[b][:, 2:3], in_=s4[b][:, 1:2]), reads=[Bs4[b]], writes=[Bs4[b]])
        S.op("dve", L("scalar_tensor_tensor", out=h2, in0=x1t[b], scalar=s4[b][:, 2:3], in1=A2bc, op0=ALU.mult, op1=ALU.mult),
             reads=[Bx1[b], Bs4[b], Bbc], writes=[Bh2])
        S.op("pool", L("tensor_tensor", out=hx2all[:, t, :], in0=h2, in1=B2bc, op=ALU.add), reads=[Bh2, Bbc], writes=[Bhx2t[t]])
        for c in range(8):
            S.op("pe", L("transpose", out=pbf(4)[:, c, :], in_=hx2all[:, t, c * 128:(c + 1) * 128], identity=ident_b),
                 reads=[Bhx2t[t], Bc], writes=[PB[4]])
        S.op("act", L("activation", out=hx2T, in_=pbf(4), func=AF.Copy), reads=[PB[4]], writes=[Bhx2T])
        for c in range(8):
            S.op("pe", L("matmul", psum[:, 5, 0:NE], lhsT=hx2T[:, c, :], rhs=wr_b[:, c, :], start=(c == 0), stop=False),
                 reads=[Bhx2T, Bwr], writes=[PB[5]])
        S.op("pe", L("matmul", psum[:, 5, 0:NE], lhsT=ones_b[0:1, :], rhs=br_b, start=False, stop=True), reads=[Bwr, Bc], writes=[PB[5]])
        S.op("dve", L("tensor_copy", out=lgs[b], in_=psum[:, 5, 0:NE]), reads=[PB[5]], writes=[Blg[b]])

    def stageB(t):
        b = t % 2
        S.op("dve", L("max", out=m8, in_=lgs[b]), reads=[Brr, Blg[b]], writes=[Brr])
        S.op("dve", L("max_index", out=i8, in_max=m8, in_values=lgs[b]), reads=[Brr, Blg[b]], writes=[Brr])
        S.op("dve", L("tensor_copy", out=ek_all[:, t * 4:(t + 1) * 4], in_=i8[:, 0:4]), reads=[Brr], writes=[Brt])
        S.op("dve", L("tensor_scalar", out=nm0, in0=m8[:, 0:1], scalar1=-1.0, scalar2=None, op0=ALU.mult), reads=[Brr], writes=[Brr])
        S.op("act", L("activation", out=ge, in_=m8[:, 0:4], func=AF.Exp, bias=nm0, accum_out=gz), reads=[Brr], writes=[Brr])
        S.op("dve", L("reciprocal", out=gz, in_=gz), reads=[Brr], writes=[Brr])
        S.op("dve", L("tensor_scalar", out=gates[:, t * 4:(t + 1) * 4], in0=ge, scalar1=gz, scalar2=None, op0=ALU.mult), reads=[Brr], writes=[Brt])
        S.op("dve", L("tensor_scalar", out=maskb, in0=lgs[b], scalar1=m8[:, 3:4], scalar2=None, op0=ALU.is_ge), reads=[Brr, Blg[b]], writes=[Brr])
        S.op("pe", L("matmul", psum[:, 6, 0:NE], lhsT=U_b, rhs=maskb, start=True, stop=True), reads=[Brr, Bc], writes=[PB[6]])
        S.op("pe", L("matmul", psum[:, 7, 0:NE], lhsT=ones_b, rhs=maskb, start=True, stop=True), reads=[Brr, Bc], writes=[PB[7]])
        S.op("dve", L("tensor_tensor", out=destf, in0=psum[:, 6, 0:NE], in1=basecnt, op=ALU.add), reads=[PB[6], Brt], writes=[Brr])
        S.op("dve", L("tensor_tensor", out=basecnt, in0=psum[:, 7, 0:NE], in1=basecnt, op=ALU.add), reads=[PB[7], Brt], writes=[Brt])
        for k in range(4):
            col = t * 4 + k
            S.op("dve", L("scalar_tensor_tensor", out=prod, in0=iota_f, scalar=ek_all[:, col:col + 1], in1=destf, op0=ALU.is_equal, op1=ALU.mult,
                          accum_out=rk_all[:, col:col + 1]), reads=[Brr, Bc, Brt], writes=[Brr, Brt])

    stageA(0)
    for t in range(16):
        if t + 1 < 16:
            stageA(t + 1)
        if do_moe:
            stageB(t)

    if do_moe:
        cntcol = alloc([32, 1]); tmp32 = alloc([32, 32]); G32 = alloc([32, 32]); T32 = alloc([32, 32]); poscol = alloc([32, 1]); pos2 = alloc([32, 1])
        PmT = alloc([32, 32]); OFFpos = alloc([128, 32]); offk = alloc([128, 64]); dst64 = alloc([128, 64]); t1p = alloc([128, 32])
        carr = alloc([128, 8]); idxf = alloc([128, 32, 8])
        Bso = Buf("sort")
        S.op("dve", L("tensor_tensor", out=tmp32, in0=basecnt[0:32, :], in1=ident_f[0:32, 0:32], op=ALU.mult), reads=[Brt, Bc], writes=[Bso])
        S.op("dve", L("reduce_sum", out=cntcol, in_=tmp32, axis=AX.X), reads=[Bso], writes=[Bso])
        S.op("dve", L("tensor_scalar", out=G32, in0=basecnt[0:32, :], scalar1=cntcol, scalar2=None, op0=ALU.is_gt), reads=[Brt, Bso], writes=[Bso])
        S.op("dve", L("scalar_tensor_tensor", out=T32, in0=basecnt[0:32, :], scalar=cntcol, in1=Lmask, op0=ALU.is_equal, op1=ALU.mult),
             reads=[Brt, Bso, Bc], writes=[Bso])
        S.op("dve", L("tensor_tensor", out=G32, in0=G32, in1=T32, op=ALU.add), reads=[Bso], writes=[Bso])
        S.op("dve", L("reduce_sum", out=poscol, in_=G32, axis=AX.X), reads=[Bso], writes=[Bso])
        S.op("dve", L("tensor_scalar", out=Pm, in0=iota_f[0:32, :], scalar1=poscol, scalar2=None, op0=ALU.is_equal), reads=[Bso, Bc], writes=[Brt])
        S.op("pe", L("matmul", psum[0:32, 0, 0:32], lhsT=Pm, rhs=ident_f[0:32, 0:32], start=True, stop=True), reads=[Brt, Bc], writes=[PB[0]])
        S.op("dve", L("tensor_copy", out=PmT, in_=psum[0:32, 0, 0:32]), reads=[PB[0]], writes=[Bso])
        S.op("pe", L("matmul", psum[:, 1, 0:32], lhsT=OFFrep, rhs=PmT, start=True, stop=True), reads=[Bso, Bc], writes=[PB[1]])
        S.op("dve", L("tensor_scalar", out=OFFpos, in0=psum[:, 1, 0:32], scalar1=128.0, scalar2=None, op0=ALU.mult), reads=[PB[1]], writes=[Bso])
        S.op("pe", L("matmul", psum[:, 2, 0:32], lhsT=pidx[0:32, :], rhs=Pm, start=True, stop=True), reads=[Brt, Bc], writes=[PB[2]])
        S.op("dve", L("tensor_copy", out=permbc, in_=psum[:, 2, 0:32]), reads=[PB[2]], writes=[Brt])
        for col in range(64):
            S.op("dve", L("scalar_tensor_tensor", out=prod, in0=iota_f, scalar=ek_all[:, col:col + 1], in1=OFFpos, op0=ALU.is_equal, op1=ALU.mult,
                          accum_out=offk[:, col:col + 1]), reads=[Brt, Bc, Bso], writes=[Brr, Bso])
        S.op("dve", L("tensor_tensor", out=dst64, in0=offk, in1=rk_all, op=ALU.add), reads=[Bso, Brt], writes=[Bso])
        S.op("dve", L("tensor_copy", out=desti, in_=dst64), reads=[Bso], writes=[Brt])
        S.op("dve", L("scalar_tensor_tensor", out=t1p, in0=permbc, scalar=1024.0, in1=pidx[:, 0:32], op0=ALU.mult, op1=ALU.add), reads=[Brt, Bc], writes=[Bso])
        S.op("dve", L("tensor_scalar", out=carr, in0=iota_f[:, 0:8], scalar1=128.0, scalar2=None, op0=ALU.mult), reads=[Bc], writes=[Bso])
        S.op("dve", L("tensor_tensor", out=idxf, in0=t1p.unsqueeze(2).to_broadcast([128, 32, 8]), in1=carr.unsqueeze(1).to_broadcast([128, 32, 8]), op=ALU.add),
             reads=[Bso], writes=[Bso])
        S.op("dve", L("tensor_copy", out=idxg, in_=idxf.rearrange("p a b -> p (a b)")), reads=[Bso], writes=[Brt])
        S.op("dve", L("tensor_copy", out=bdn_idx, in_=permbc), reads=[Brt], writes=[Brt])
        for col in range(64):
            t = col // 4
            S.op("pool", L("indirect_dma_start", out=xe[:, :], out_offset=bass.IndirectOffsetOnAxis(ap=desti[:, col:col + 1], axis=0),
                           in_=hx2all[:, t, :], in_offset=None), reads=[Bhx2t[t], Brt] + Bxe0, writes=[Bxes[col]], dma=True)

    if not do_moe:
        lastd = [o for o in S.ops["sp"] if o.dma][-4:]
        S.op("sp", None, deps=[o for o in S.ops["sp"] if o.dma][-40:])
        S.emit(nc)
        return nc

    S.barrier()
    off[0] = mark_persist

    XC = 1024
    wgu = [alloc([128, 8, 2 * D], BF16) for _ in range(2)]; wdn = [alloc([128, 8, D], BF16) for _ in range(2)]
    Bwgu = [[Buf(f"wgu{b}_{c}") for c in range(8)] for b in range(2)]; Bwdn = [[Buf(f"wdn{b}_{c}") for c in range(8)] for b in range(2)]
    bdn1 = alloc([128, D]); Bbdn1 = Buf("bdn")
    bgu_f = alloc([NE, 2 * D]); biasT = alloc([128, 16, NE]); Bbias = Buf("bias")
    xet = [alloc([128, D], BF16) for _ in range(3)]; Bxet = [Buf(f"xet{i}") for i in range(3)]
    xeT = alloc([128, 8, XC], BF16); BxeT = Buf("xeT")
    aT = alloc([128, 8, XC], BF16); BaT = Buf("aT")
    g1 = [alloc([128, 512]) for _ in range(2)]; sgm = [alloc([128, 512]) for _ in range(2)]
    u1 = [alloc([128, 512]) for _ in range(2)]; gsx = [alloc([128, 512]) for _ in range(2)]
    Bg1 = [Buf("g1a"), Buf("g1b")]; Bsgm = [Buf("sga"), Buf("sgb")]; Bu1 = [Buf("u1a"), Buf("u1b")]; Bgsx = [Buf("gsa"), Buf("gsb")]
    yst = [alloc([128, D]) for _ in range(2)]; Byst = [Buf("yst0"), Buf("yst1")]

    S.dma("sp", bgu_f, b_gu, writes=[Bbias])
    for m in range(8):
        for two in range(2):
            j = m * 2 + two
            S.op("pe", L("matmul", psum[:, 0, j * NE:(j + 1) * NE], lhsT=bgu_f[:, 2 * m * 128 + two:2 * (m + 1) * 128:2],
                         rhs=Pm, start=True, stop=True), reads=[Bbias, Brt], writes=[PB[0]])
    S.op("dve", L("tensor_copy", out=biasT, in_=psum[:, 0, :].rearrange("p (j n) -> p j n", n=NE)), reads=[PB[0]], writes=[Bbias])
    S.op("dve", L("tensor_scalar", out=biasT[:, 1:16:2, :], in0=biasT[:, 1:16:2, :], scalar1=1.0, scalar2=None, op0=ALU.add), reads=[Bbias], writes=[Bbias])

    wgu_rows = w_gu.rearrange("e k n -> (e k) n"); wdn_rows = w_dn.rearrange("e k n -> (e k) n")
    IO = bass.IndirectOffsetOnAxis

    def load_w(i):
        b = i % 2
        for c in range(8):
            S.op("pool", L("indirect_dma_start", out=wgu[b][:, c, :], out_offset=None, in_=wgu_rows[:, :], in_offset=IO(ap=idxg[:, i * 8 + c:i * 8 + c + 1], axis=0)),
                 reads=[Brt], writes=[Bwgu[b][c]], dma=True)
        for c in range(8):
            S.op("pool", L("indirect_dma_start", out=wdn[b][:, c, :], out_offset=None, in_=wdn_rows[:, :], in_offset=IO(ap=idxg[:, i * 8 + c:i * 8 + c + 1], axis=0)),
                 reads=[Brt], writes=[Bwdn[b][c]], dma=True)

    def load_bdn(i):
        S.op("pool", L("indirect_dma_start", out=bdn1, out_offset=None, in_=b_dn[:, :], in_offset=IO(ap=bdn_idx[:, i:i + 1], axis=0)),
             reads=[Brt], writes=[Bbdn1], dma=True)

    work = []
    for i in range(NE):
        n = NT[i]; o = OFFT[i]
        nitem = -(-n // (XC // 128))
        sizes = [n // nitem + (1 if q < n % nitem else 0) for q in range(nitem)]
        for q, k in enumerate(sizes):
            work.append((i, o, k, q == 0, q == nitem - 1)); o += k

    load_w(0); load_bdn(0)
    pc5 = [0]; xc5 = [0]; yc5 = [0]

    def stage_x(w):
        i, o, k, first, lastw = w
        for s in range(k):
            xi = xc5[0] % 3; xc5[0] += 1
            tb_ = xi % 2
            S.dma("sp", xet[xi], xe[(o + s) * 128:(o + s + 1) * 128, :], reads=Bxes, writes=[Bxet[xi]])
            for c in range(8):
                S.op("pe", L("transpose", out=pbf(tb_)[:, c, :], in_=xet[xi][:, c * 128:(c + 1) * 128], identity=ident_b),
                     reads=[Bxet[xi], Bc], writes=[PB[tb_]])
            S.op("act", L("activation", out=xeT[:, :, s * 128:(s + 1) * 128], in_=pbf(tb_), func=AF.Copy), reads=[PB[tb_]], writes=[BxeT])

    stage_x(work[0])
    for wi, (i, o, k, first, lastw) in enumerate(work):
        b = i % 2
        if first and i + 1 < NE:
            load_w(i + 1)
        ncols = k * 128
        for m in range(8):
            nch = -(-ncols // 512); cw = ncols // nch
            for c0 in range(0, ncols, cw):
                nn = cw
                ii = pc5[0] % 2; pc5[0] += 1
                bg_, bu_ = 2 + 2 * ii, 3 + 2 * ii
                cs = slice(c0, c0 + nn)
                for (bk, two) in ((bg_, 0), (bu_, 1)):
                    for c in range(8):
                        S.op("pe", L("matmul", psum[:, bk, 0:nn], lhsT=wgu[b][:, c, 2 * m * 128 + two:2 * (m + 1) * 128:2],
                                     rhs=xeT[:, c, cs], start=(c == 0), stop=(c == 7)),
                             reads=[Bwgu[b][c], BxeT], writes=[PB[bk]])
                S.op("dve", L("tensor_scalar", out=g1[ii][:, 0:nn], in0=psum[:, bg_, 0:nn], scalar1=biasT[:, 2 * m, i:i + 1], scalar2=7.0, op0=ALU.add, op1=ALU.min),
                     reads=[PB[bg_], Bbias], writes=[Bg1[ii]])
                S.op("act", L("activation", out=sgm[ii][:, 0:nn], in_=g1[ii][:, 0:nn], func=AF.Sigmoid, scale=1.702), reads=[Bg1[ii]], writes=[Bsgm[ii]])
                S.op("dve", L("tensor_scalar", out=u1[ii][:, 0:nn], in0=psum[:, bu_, 0:nn], scalar1=biasT[:, 2 * m + 1, i:i + 1], scalar2=8.0, op0=ALU.add, op1=ALU.min),
                     reads=[PB[bu_], Bbias], writes=[Bu1[ii]])
                S.op("dve", L("tensor_tensor", out=gsx[ii][:, 0:nn], in0=g1[ii][:, 0:nn], in1=sgm[ii][:, 0:nn], op=ALU.mult), reads=[Bg1[ii], Bsgm[ii]], writes=[Bgsx[ii]])
                S.op("dve", L("scalar_tensor_tensor", out=aT[:, m, cs], in0=u1[ii][:, 0:nn], scalar=-6.0, in1=gsx[ii][:, 0:nn], op0=ALU.max, op1=ALU.mult),
                     reads=[Bu1[ii], Bgsx[ii]], writes=[BaT])
        if wi + 1 < len(work):
            stage_x(work[wi + 1])
        for s in range(k):
            yb_ = yc5[0] % 2; yc5[0] += 1
            for hf in range(2):
                bk = 6 + hf
                for m in range(8):
                    S.op("pe", L("matmul", psum[:, bk, :], lhsT=aT[:, m, s * 128:(s + 1) * 128], rhs=wdn[b][:, m, hf * 512:(hf + 1) * 512],
                                 start=(m == 0), stop=(m == 7)),
                         reads=[BaT, Bwdn[b][m]], writes=[PB[bk]])
                S.op("dve", L("tensor_tensor", out=yst[yb_][:, hf * 512:(hf + 1) * 512], in0=psum[:, bk, :], in1=bdn1[:, hf * 512:(hf + 1) * 512], op=ALU.add),
                     reads=[PB[bk], Bbdn1], writes=[Byst[yb_]])
            S.dma("sp", ye[(o + s) * 128:(o + s + 1) * 128, :], yst[yb_], reads=[Byst[yb_]], writes=[Buf()])
        if lastw and i + 1 < NE:
            load_bdn(i + 1)

    S.barrier()
    off[0] = mark_persist

    yk = [alloc([128, D]) for _ in range(8)]; Byk = [Buf(f"yk{i}") for i in range(8)]
    x6 = [alloc([128, D]) for _ in range(2)]; Bx6 = [Buf("x6a"), Buf("x6b")]
    acc = [alloc([128, D]) for _ in range(2)]; Bacc = [Buf("acca"), Buf("accb")]
    acc2 = [alloc([128, D]) for _ in range(2)]; Bacc2 = [Buf("acc2a"), Buf("acc2b")]
    xo = [alloc([128, D]) for _ in range(2)]; Bxo = [Buf("xoa"), Buf("xob")]
    ot6 = [alloc([128, D]) for _ in range(2)]; Bot6 = [Buf("o6a"), Buf("o6b")]
    s6 = [alloc([128, 4]) for _ in range(2)]; Bs6 = [Buf("s6a"), Buf("s6b")]
    outs = []

    def loads6(t):
        b = t % 2
        S.dma("sp", x6[b], x1scr[t * 128:(t + 1) * 128, :], reads=[Bx1s], writes=[Bx6[b]])
        for k in range(4):
            col = t * 4 + k
            S.op("pool", L("indirect_dma_start", out=yk[4 * b + k], out_offset=None, in_=ye[:, :],
                           in_offset=bass.IndirectOffsetOnAxis(ap=desti[:, col:col + 1], axis=0)),
                 reads=[Brt], writes=[Byk[4 * b + k]], dma=True)

    loads6(0)
    for t in range(16):
        b = t % 2
        if t + 1 < 16:
            loads6(t + 1)
        S.op("act", L("activation", out=acc[b], in_=yk[4 * b], func=AF.Copy, scale=gates[:, t * 4:t * 4 + 1]), reads=[Byk[4 * b], Brt], writes=[Bacc[b]])
        for k in range(1, 4):
            S.op("dve", L("scalar_tensor_tensor", out=acc[b], in0=yk[4 * b + k], scalar=gates[:, t * 4 + k:t * 4 + k + 1], in1=acc[b], op0=ALU.mult, op1=ALU.add),
                 reads=[Byk[4 * b + k], Brt, Bacc[b]], writes=[Bacc[b]])
        S.op("dve", L("tensor_tensor", out=acc2[b], in0=acc[b], in1=G2bc, op=ALU.mult), reads=[Bacc[b], Bbc], writes=[Bacc2[b]])
        S.op("pool", L("tensor_tensor", out=xo[b], in0=acc2[b], in1=x6[b], op=ALU.add), reads=[Bacc2[b], Bx6[b]], writes=[Bxo[b]])
        S.op("act", L("activation", out=junk, in_=xo[b], func=AF.Square, accum_out=s6[b][:, 0:1]), reads=[Bxo[b]], writes=[Bjunk, Bs6[b]])
        S.op("act", L("activation", out=s6[b][:, 1:2], in_=s6[b][:, 0:1], func=AF.Sqrt, scale=1.0 / D, bias=EPS), reads=[Bs6[b]], writes=[Bs6[b]])
        S.op("dve", L("reciprocal", out=s6[b][:, 2:3], in_=s6[b][:, 1:2]), reads=[Bs6[b]], writes=[Bs6[b]])
        S.op("dve", L("scalar_tensor_tensor", out=ot6[b], in0=xo[b], scalar=s6[b][:, 2:3], in1=FGbc, op0=ALU.mult, op1=ALU.mult),
             reads=[Bxo[b], Bs6[b], Bbc], writes=[Bot6[b]])
        outs.append(S.dma("sp", outd[t * 128:(t + 1) * 128, :], ot6[b], reads=[Bot6[b]]))
    S.op("sp", None, deps=outs)
    S.emit(nc)
    return nc


def _rope_tables(pos):
    p = np.arange(128); d = p % 64; f = d % 16
    inv = (np.float32(10000.0) ** (-(np.arange(16, dtype=np.float32)) / np.float32(16))).astype(np.float32)
    row = (pos // 64).astype(np.float32); col = (pos % 64).astype(np.float32)
    coord = np.where((d < 32)[:, None], row[None, :], col[None, :]).astype(np.float32)
    ang = (coord * inv[f][:, None]).astype(np.float32)
    return np.cos(ang).astype(np.float32), np.sin(ang).astype(np.float32)


def _consts():
    c = np.zeros((128, 9, 128), np.float32)
    c[:, 0, :] = np.eye(128, dtype=np.float32)
    R = np.zeros((128, 128), np.float32)
    for m in range(128):
        if (m % 32) < 16: R[m, m + 16] = -1.0
        else: R[m, m - 16] = 1.0
    c[:, 1, :] = R.T
    j = np.arange(128)[:, None]; i = np.arange(128)[None, :]
    c[:, 2, :] = (j < i).astype(np.float32)
    c[:, 3, :] = (j >= i).astype(np.float32)
    c[:, 4, :] = (j <= i).astype(np.float32)
    c[:, 5, :] = np.arange(128, dtype=np.float32)[None, :]
    c[:, 6, :] = np.arange(128, dtype=np.float32)[:, None]
    c[0:NE, 7, :] = np.asarray(OFFT, np.float32)[:, None]
    c[:, 8, :] = (i < j).astype(np.float32)
    return c.reshape(128, 9 * 128)


def make_in_maps(inp, do_moe=True):
    x = np.asarray(inp["x"], np.float32); ctx = np.asarray(inp["ctx"], np.float32)
    c = np.asarray(inp["c"], np.float32); c_ctx = np.asarray(inp["c_ctx"], np.float32)
    consts = _consts()
    lam = np.stack([np.asarray(inp[k], np.float32)[0] for k in ("lam_q1", "lam_k1", "lam_q2", "lam_k2")], 0)
    shared = {
        "consts": consts, "w_mod": np.ascontiguousarray(inp["w_mod"][0]), "b_mod": np.ascontiguousarray(inp["b_mod"][0]),
        "norm1_g": np.ascontiguousarray(inp["norm1_g"][0]), "w_in": np.ascontiguousarray(inp["w_in"][0]), "lam": np.ascontiguousarray(lam),
        "subln_g": np.ascontiguousarray(inp["subln_g"][0]), "sink": np.ascontiguousarray(inp["sink"][0]),
        "w_out": np.ascontiguousarray(inp["w_out"][0]), "norm2_g": np.ascontiguousarray(inp["norm2_g"][0]),
        "w_router": np.ascontiguousarray(inp["w_router"][0]), "b_router": np.ascontiguousarray(inp["b_router"][0]),
        "final_g": np.ascontiguousarray(inp["final_g"]),
    }
    if do_moe:
        shared.update({"w_gate_up": np.ascontiguousarray(inp["w_gate_up"][0]), "b_gate_up": np.ascontiguousarray(inp["b_gate_up"][0]),
                       "w_down": np.ascontiguousarray(inp["w_down"][0]), "b_down": np.ascontiguousarray(inp["b_down"][0])})
    shared = {k: np.asarray(v, np.float32) for k, v in shared.items()}
    maps = []
    for core in range(8):
        b, j = core // 4, core % 4
        qs = j * NQ
        shift = qs - 128
        pos = (np.arange(S_LEN) + shift) % S_LEN
        cos, sin = _rope_tables(pos)
        cosT = np.concatenate([cos, np.ones((128, CTX), np.float32)], 1)
        sinT = np.concatenate([sin, np.zeros((128, CTX), np.float32)], 1)
        wmask = np.zeros((128, 2), np.float32)
        if j == 0: wmask[:, 0] = -1e30
        if j == 3: wmask[:, 1] = -1e30
        m = dict(shared)
        m.update({"xkv": np.ascontiguousarray(np.roll(x[b], -shift, axis=0)), "ctx": np.ascontiguousarray(ctx[b]),
                  "cvec": np.ascontiguousarray(np.stack([c[b], c_ctx], 0)), "cosT": cosT, "sinT": sinT, "wmask": wmask})
        maps.append(m)
    return maps


_NC_CACHE = {}


def kernel(**inputs):
    if "nc" not in _NC_CACHE:
        _NC_CACHE["nc"] = build_program(do_moe=True)
    nc = _NC_CACHE["nc"]
    maps = make_in_maps(inputs, do_moe=True)
    res = run_bass_kernel_spmd(nc, maps, core_ids=list(range(8)))
    out = np.empty((2, S_LEN, D), np.float32)
    for core in range(8):
        b, j = core // 4, core % 4
        out[b, j * NQ:(j + 1) * NQ, :] = res.results[core]["out"]
    return out
```

```python
import contextlib
import numpy as np
import concourse.bass as bass
import concourse.mybir as mybir
from concourse.bass_utils import run_bass_kernel_spmd

F32 = mybir.dt.float32; BF16 = mybir.dt.bfloat16; I32 = mybir.dt.int32; U8 = mybir.dt.uint8
U32 = mybir.dt.uint32
ALU = mybir.AluOpType; AF = mybir.ActivationFunctionType
AX = mybir.AxisListType
ENGS = ("pe", "act", "dve", "pool", "sp")
NDMASEM = 12

D = 1024; S_LEN = 8192; CTX = 256; NQ = 2048; NE = 32;
NT = [-(-min(2048, -(-8192 // (i + 1))) // 128) for i in range(NE)]
OFFT = [sum(NT[:i]) for i in range(NE)]
NSLOT = sum(NT) * 128; NSLOTP = NSLOT
QA, KA, VA, QB, KB, VB = 0, 512, 1024, 1536, 2048, 2176
NKT = 66
WROWS = 2560
EPS = 1e-6
LAM_INIT = 0.2


def L(method, *args, **kw):
    return lambda e: getattr(e, method)(*args, **kw)


class Buf:
    __slots__ = ("name", "w", "r", "excl")
    def __init__(self, name="", excl=False):
        self.name = name; self.w = None; self.r = []; self.excl = excl


class Op:
    __slots__ = ("eng", "fn", "deps", "dma", "signal", "sem", "val", "idx", "name")
    def __init__(self, eng, fn, dma, name):
        self.eng = eng; self.fn = fn; self.deps = []; self.dma = dma
        self.signal = False; self.sem = None; self.val = 0; self.idx = 0; self.name = name


class Sched:
    def __init__(self):
        self.ops = {e: [] for e in ENGS}
        self.all = []

    def op(self, eng, fn, reads=(), writes=(), dma=False, deps=(), name=""):
        o = Op(eng, fn, dma, name)
        ds = {}
        ex = [b for b in reads if b.excl]
        if ex:
            reads = [b for b in reads if not b.excl]; writes = list(writes) + ex
        for b in reads:
            if b.w is not None: ds[id(b.w)] = b.w
        for b in writes:
            if b.w is not None: ds[id(b.w)] = b.w
            lastr = {}
            for r in b.r:
                if r.dma: ds[id(r)] = r
                elif r.eng not in lastr or r.idx > lastr[r.eng].idx: lastr[r.eng] = r
            for r in lastr.values(): ds[id(r)] = r
        for d in deps: ds[id(d)] = d
        o.deps = list(ds.values())
        for b in reads: b.r.append(o)
        for b in writes:
            b.w = o; b.r = []
        o.idx = len(self.ops[eng]); self.ops[eng].append(o); self.all.append(o)
        return o

    def dma(self, eng, out, in_, reads=(), writes=(), name="", **kw):
        return self.op(eng, L("dma_start", out=out, in_=in_, **kw), reads, writes, dma=True, name=name)

    def barrier(self):
        lasts = []
        for e in ENGS:
            real = [o for o in self.ops[e] if o.fn is not None]
            comp = [o for o in real if not o.dma]
            if comp: lasts.append(comp[-1])
            lasts.extend([o for o in real if o.dma][-NDMASEM:])
        for e in ENGS:
            self.op(e, None, deps=lasts, name="barrier")

    def emit(self, nc):
        for o in self.all:
            for d in o.deps:
                if d.eng == "pe" and o.eng == "pe" and not d.dma:
                    continue
                d.signal = True
        with contextlib.ExitStack() as st:
            csem = {e: st.enter_context(nc.semaphore("c_" + e)) for e in ENGS}
            dsem = {e: [st.enter_context(nc.semaphore(f"d_{e}{i}")) for i in range(NDMASEM)]
                    for e in ("sp", "pool", "act")}
            for e in ENGS:
                cnt = 0; di = 0; dcnt = [0] * NDMASEM; dlast = [None] * NDMASEM
                for o in self.ops[e]:
                    if o.fn is None: continue
                    if o.dma:
                        o.signal = True
                        s = di % NDMASEM; di += 1
                        if dlast[s] is not None:
                            o.deps.append(dlast[s])
                        dcnt[s] += 16; o.sem = dsem[e][s]; o.val = dcnt[s]; dlast[s] = o
                    elif o.signal:
                        cnt += 1; o.sem = csem[e]; o.val = cnt
            blk = st.enter_context(nc.Block())
            engobj = {"pe": "tensor", "act": "scalar", "dve": "vector", "pool": "gpsimd", "sp": "sync"}

            def mk(e):
                def body(eng):
                    seen = {}
                    for o in self.ops[e]:
                        need = {}
                        for d in o.deps:
                            if d.fn is None: continue
                            if d.eng == "pe" and e == "pe" and not d.dma: continue
                            k = id(d.sem)
                            if seen.get(k, 0) >= d.val: continue
                            if k not in need or need[k][1] < d.val: need[k] = (d.sem, d.val)
                        for k, (s, v) in need.items():
                            eng.wait_ge(s, v); seen[k] = v
                        if o.fn is None: continue
                        inst = o.fn(eng)
                        if o.signal:
                            inst.then_inc(o.sem, 16 if o.dma else 1)
                return body
            for e in ENGS:
                if self.ops[e]:
                    getattr(blk, engobj[e])(mk(e))


def build_program(do_moe=True, dbg=False, stop_after=99):
    nc = bass.Bass("TRN2", target_bir_lowering=False)
    dt_ = nc.dram_tensor

    def din(name, shape, dt=F32):
        return dt_(name, list(shape), dt, kind="ExternalInput").ap()

    xkv = din("xkv", [S_LEN, D]); ctxd = din("ctx", [CTX, D]); cvec = din("cvec", [2, D])
    cosd = din("cosT", [128, S_LEN + CTX]); sind = din("sinT", [128, S_LEN + CTX])
    wmaskd = din("wmask", [128, 2]); constd = din("consts", [128, 9 * 128])
    w_mod = din("w_mod", [D, 6 * D]); b_mod = din("b_mod", [6 * D]); n1g = din("norm1_g", [D])
    w_in = din("w_in", [D, 2304]); lamd = din("lam", [4, 64]); sublnd = din("subln_g", [128])
    sinkd = din("sink", [8]); w_out = din("w_out", [D, D]); n2g = din("norm2_g", [D])
    w_r = din("w_router", [D, NE]); b_r = din("b_router", [NE]); fgd = din("final_g", [D])
    if do_moe:
        w_gu = din("w_gate_up", [NE, D, 2 * D]); b_gu = din("b_gate_up", [NE, 2 * D])
        w_dn = din("w_down", [NE, D, D]); b_dn = din("b_down", [NE, D])
    outd = dt_("out", [NQ, D], F32, kind="ExternalOutput").ap()
    kscr = dt_("kscr", [4, 9, 128, 1024], BF16).ap()
    vscr = dt_("vscr", [4, 9, 128, 8, 128], BF16).ap()
    x1scr = dt_("x1scr", [NQ, D], F32).ap()
    xe = dt_("xe", [NSLOTP, D], BF16).ap()
    ye = dt_("ye", [NSLOTP, D], F32).ap()
    if dbg:
        dbg_x1 = dt_("dbg_x1", [NQ, D], F32, kind="ExternalOutput").ap()
        dbg_yb = dt_("dbg_yb", [128, 4 * NQ], BF16, kind="ExternalOutput").ap()
        dbg_kb = dt_("dbg_kb", [128, WROWS + CTX], BF16, kind="ExternalOutput").ap()
        dbg_vb = dt_("dbg_vb", [128, 22 * 128], BF16, kind="ExternalOutput").ap()
        dbg_qb = dt_("dbg_qb", [128, 4 * WROWS], BF16, kind="ExternalOutput").ap()

    TOT = 206 * 1024
    arena = nc.alloc_sbuf_tensor("arena", [128, TOT], U8)
    psum = nc.alloc_psum_tensor("psum", [128, 8, 512], F32)
    off = [0]

    def alloc(shape, dt=F32):
        n = int(np.prod(shape[1:])) * (2 if dt == BF16 else 4)
        a = arena[0:shape[0], off[0]:off[0] + n].bitcast(dt)
        off[0] += (n + 63) // 64 * 64
        assert off[0] <= TOT, ("SBUF overflow", off[0])
        if len(shape) == 3:
            a = a.rearrange("p (a b) -> p a b", b=shape[2])
        elif len(shape) == 4:
            a = a.rearrange("p (a b c) -> p a b c", b=shape[2], c=shape[3])
        return a

    def pbank(b, n=1):
        return psum[:, b:b + n, :]

    def pflat(b, n=1):
        return psum[:, b:b + n, :].rearrange("p a b -> p (a b)")

    def pbf(b):
        return psum[:, b, :].bitcast(BF16).rearrange("p (a b) -> p a b", b=128)

    S = Sched()

    def finish():
        S.barrier()
        S.emit(nc)
        return nc
    PB = [Buf(f"psum{i}", excl=True) for i in range(8)]

    cst = alloc([128, 9, 128], F32)
    ident_f = cst[:, 0, :]; iota_f = cst[:, 5, 0:32]; pidx = cst[:, 6, :]; OFFrep = cst[0:32, 7, :]; Lmask = cst[0:32, 8, 0:32]
    cstb = alloc([128, 5, 128], BF16)
    ident_b = cstb[:, 0, :]; RT_b = cstb[:, 1, :]; U_b = cstb[:, 2, :]; triL = cstb[:, 3, :]; triR = cstb[:, 4, :]
    ones_b = alloc([128, 128], BF16); ones_f = alloc([128, 128], F32)
    A1T = alloc([128, 8]); B1T = alloc([128, 8]); A1cT = alloc([128, 8]); B1cT = alloc([128, 8])
    gsubp = alloc([128, 1]); neglam = alloc([128, 1]); sinkexp = alloc([128, 4]); wmask = alloc([128, 2])
    G1bc = alloc([128, D]); A2bc = alloc([128, D]); B2bc = alloc([128, D]); G2bc = alloc([128, D]); FGbc = alloc([128, D])
    desti = alloc([128, 64], I32); gates = alloc([128, 64]); basecnt = alloc([128, 32])
    ek_all = alloc([128, 64]); rk_all = alloc([128, 64]); Pm = alloc([32, 32]); permbc = alloc([128, 32])
    idxg = alloc([128, 32 * 8], I32); bdn_idx = alloc([128, 32], I32)
    junk = alloc([128, D], BF16); Bjunk = Buf("junk")
    Bc = Buf("consts"); Bvec = Buf("vecs"); Bbc = Buf("bcast"); Brt = Buf("route")

    S.dma("sp", cst.rearrange("p a b -> p (a b)"), constd, writes=[Bc])
    S.dma("sp", wmask, wmaskd, writes=[Bc])
    S.op("dve", L("tensor_copy", out=cstb, in_=cst[:, 0:5, :]), reads=[Bc], writes=[Bc])
    S.op("dve", L("memset", ones_b, 1.0), writes=[Bc])
    S.op("dve", L("memset", ones_f, 1.0), writes=[Bc])
    S.op("dve", L("memset", basecnt, 0.0), writes=[Brt])

    mark_persist = off[0]

    cT = alloc([128, 8, 2]); sg = alloc([128, 8, 2]); scT = alloc([128, 8, 2]); scT_b = alloc([128, 8, 2], BF16)
    screp = alloc([128, 8, 128], BF16)
    bmT = alloc([128, 16]); g1nT = alloc([128, 8]); modT = alloc([128, 16, 2])
    bmbc = alloc([128, 4 * D]); n2bc = alloc([128, D])
    lamt = alloc([1, 4, 64]); lamp = alloc([1, 2, 64]); lams = alloc([1, 2]); lamv = alloc([1, 2])
    wm = [alloc([128, 8, 1024], BF16) for _ in range(2)]
    Bwm = [Buf("wm0"), Buf("wm1")]; Bp0 = Buf("p0")

    for r_ in range(2):
        S.dma("sp", cT[:, :, r_], cvec[r_].rearrange("(c p) -> p c", p=128), writes=[Bp0], allow_slow_non_contiguous=True)
    S.dma("sp", bmT, b_mod[0:2048].rearrange("(c p) -> p c", p=128), writes=[Bp0], allow_slow_non_contiguous=True)
    S.dma("sp", g1nT, n1g.rearrange("(c p) -> p c", p=128), writes=[Bp0], allow_slow_non_contiguous=True)
    S.dma("sp", bmbc, b_mod[2048:6144].partition_broadcast(128), writes=[Bp0])
    S.dma("sp", n2bc, n2g.partition_broadcast(128), writes=[Bp0])
    S.dma("sp", FGbc, fgd.partition_broadcast(128), writes=[Bbc])
    S.dma("sp", gsubp, sublnd.rearrange("(p o) -> p o", o=1), writes=[Bvec])
    S.dma("sp", lamt.rearrange("p a b -> p (a b)"), lamd.rearrange("(o a) b -> o (a b)", o=1), writes=[Bp0])
    for g in range(2):
        S.dma("sp", sinkexp[g * 64:(g + 1) * 64, :], sinkd[g * 4:(g + 1) * 4].partition_broadcast(64), writes=[Bvec])
    wmv = w_mod.rearrange("(c p) n -> p c n", p=128)
    for i in range(2):
        S.dma("pool", wm[i], wmv[:, :, i * 1024:(i + 1) * 1024], writes=[Bwm[i]])

    S.op("act", L("activation", out=sg, in_=cT, func=AF.Sigmoid), reads=[Bp0], writes=[Bp0])
    S.op("dve", L("tensor_tensor", out=scT, in0=cT, in1=sg, op=ALU.mult), reads=[Bp0], writes=[Bp0])
    S.op("dve", L("tensor_copy", out=scT_b, in_=scT), reads=[Bp0], writes=[Bp0])
    S.op("dve", L("tensor_copy", out=screp, in_=scT[:, :, 0:1].to_broadcast([128, 8, 128])), reads=[Bp0], writes=[Bp0])
    S.op("dve", L("tensor_scalar", out=gsubp, in0=gsubp, scalar1=1.0 - LAM_INIT, scalar2=None, op0=ALU.mult),
         reads=[Bvec], writes=[Bvec])
    S.op("act", L("activation", out=sinkexp, in_=sinkexp, func=AF.Exp), reads=[Bvec], writes=[Bvec])
    S.op("dve", L("tensor_tensor", out=lamp, in0=lamt[:, 0:4:2, :], in1=lamt[:, 1:4:2, :], op=ALU.mult), reads=[Bp0], writes=[Bp0])
    S.op("dve", L("reduce_sum", out=lams, in_=lamp, axis=AX.X), reads=[Bp0], writes=[Bp0])
    S.op("act", L("activation", out=lamv, in_=lams, func=AF.Exp), reads=[Bp0], writes=[Bp0])
    S.op("dve", L("tensor_tensor", out=lamv[:, 0:1], in0=lamv[:, 1:2], in1=lamv[:, 0:1], op=ALU.subtract), reads=[Bp0], writes=[Bp0])
    S.op("dve", L("tensor_scalar", out=lamv[:, 0:1], in0=lamv[:, 0:1], scalar1=-LAM_INIT, scalar2=None, op0=ALU.add), reads=[Bp0], writes=[Bp0])
    S.op("pe", L("matmul", psum[:, 7, 0:1], lhsT=ones_f[0:1, :], rhs=lamv[:, 0:1], start=True, stop=True),
         reads=[Bp0, Bc], writes=[PB[7]])
    S.op("dve", L("tensor_copy", out=neglam, in_=psum[:, 7, 0:1]), reads=[PB[7]], writes=[Bvec])

    pm = psum[:, 0, 0:32].rearrange("p (a b) -> p a b", b=2)
    for blk in range(16):
        i = blk // 8
        for c in range(8):
            S.op("pe", L("matmul", pm[:, blk, :], lhsT=wm[i][:, c, (blk % 8) * 128:(blk % 8 + 1) * 128],
                                                              rhs=scT_b[:, c, :], start=(c == 0), stop=(c == 7)),
                 reads=[Bwm[i], Bp0], writes=[PB[0]])
    S.op("dve", L("tensor_tensor", out=modT, in0=pm, in1=bmT.unsqueeze(2).to_broadcast([128, 16, 2]), op=ALU.add),
         reads=[PB[0], Bp0], writes=[Bp0])
    for (AT, BT, r) in ((A1T, B1T, 0), (A1cT, B1cT, 1)):
        S.op("dve", L("scalar_tensor_tensor", out=AT, in0=modT[:, 8:16, r], scalar=1.0, in1=g1nT, op0=ALU.add, op1=ALU.mult),
             reads=[Bp0], writes=[Bvec])
        S.op("dve", L("tensor_copy", out=BT, in_=modT[:, 0:8, r]), reads=[Bp0], writes=[Bvec])
    dsts = [G1bc, B2bc, A2bc, G2bc]
    for q in range(4):
        i = q % 2
        S.dma("pool", wm[i], wmv[:, :, (2 + q) * 1024:(3 + q) * 1024], writes=[Bwm[i]])
        for hf in range(2):
            for c in range(8):
                S.op("pe", L("matmul", psum[:, 1 + hf, :], lhsT=screp[:, c, :], rhs=wm[i][:, c, hf * 512:(hf + 1) * 512],
                                                                start=(c == 0), stop=(c == 7)),
                     reads=[Bwm[i], Bp0], writes=[PB[1 + hf]])
        S.op("dve", L("tensor_tensor", out=dsts[q], in0=pflat(1, 2), in1=bmbc[:, q * 1024:(q + 1) * 1024], op=ALU.add),
             reads=[PB[1], PB[2], Bp0], writes=[Bbc])
    S.op("dve", L("scalar_tensor_tensor", out=A2bc, in0=A2bc, scalar=1.0, in1=n2bc, op0=ALU.add, op1=ALU.mult),
         reads=[Bbc, Bp0], writes=[Bbc])

    if stop_after == 0:
        return finish()
    S.barrier()
    off[0] = mark_persist

    QaT = alloc([128, 4, WROWS], BF16); QbT = alloc([128, 4, WROWS], BF16)
    KbT = alloc([128, WROWS + CTX], BF16); Vb = alloc([128, 22, 128], BF16)
    BQ = Buf("Q"); BKb = Buf("Kb"); BVb = Buf("Vb"); Bya = Buf("ya"); Byb = Buf("yb")
    Bks = Buf("kscr"); Bvs = Buf("vscr")
    mark_attn = off[0]

    win = alloc([128, 8, 2304], BF16); Bwin = Buf("win")
    xt = [alloc([128, D]) for _ in range(2)]; Bxt = [Buf("xt0"), Buf("xt1")]
    xn = [alloc([128, D], BF16) for _ in range(4)]; Bxn = [Buf(f"xn{i}") for i in range(4)]
    st4 = [alloc([128, 4]) for _ in range(4)]; Bst = [Buf(f"st{i}") for i in range(4)]
    evt = [alloc([128, 8, 128]) for _ in range(2)]; Bev = [Buf("ev0"), Buf("ev1")]
    hxT = [alloc([128, 8, 512], BF16) for _ in range(2)]; Bhx = [Buf("hx0"), Buf("hx1")]
    cosg = [alloc([128, 512]) for _ in range(2)]; sing = [alloc([128, 512]) for _ in range(2)]; Btab = [Buf("tab0"), Buf("tab1")]
    kbf = [alloc([128, 512], BF16) for _ in range(2)]; Bkbf = [Buf("kbf0"), Buf("kbf1")]
    rt1 = [alloc([128, 512]) for _ in range(2)]; rt2 = [alloc([128, 512]) for _ in range(2)]
    Brt1 = [Buf("rt1a"), Buf("rt1b")]; Brt2 = [Buf("rt2a"), Buf("rt2b")]
    kst = [alloc([128, 4, 512], BF16) for _ in range(2)]; Bkst = [Buf("kst0"), Buf("kst1")]
    vst = [alloc([128, 4, 4, 128], BF16) for _ in range(2)]; Bvst = [Buf("vst0"), Buf("vst1")]

    winv = w_in.rearrange("(c p) n -> p c n", p=128)
    S.dma("pool", win[:, :, 0:QB], winv[:, :, 0:QB], writes=[Bwin])
    S.dma("pool", win[:, :, KB:2304], winv[:, :, KB:2304], writes=[Bwin])
    for gi in range(4):
        for g in range(2):
            S.dma("pool", win[:, :, QB + gi * 128 + g * 64:QB + gi * 128 + (g + 1) * 64],
                  winv[:, :, QB + g * 256 + gi * 64:QB + g * 256 + (gi + 1) * 64], writes=[Bwin])
    import os
    rr = [0]

    pcyc = [0]

    def nextbank():
        b = 2 + pcyc[0] % 4; pcyc[0] += 1
        return b

    def projA(job, hb, tb):
        colbase, n, dst, wb = job
        i = rr[0] % 2; rr[0] += 1
        bk = nextbank()
        for c in range(8):
            S.op("pe", L("matmul", psum[:, bk, 0:n], lhsT=win[:, c, colbase:colbase + 128], rhs=hxT[hb][:, c, 0:n], start=(c == 0), stop=(c == 7)),
                 reads=[Bwin, Bhx[hb]], writes=[PB[bk]])
        S.op("act", L("activation", out=kbf[i][:, 0:n], in_=psum[:, bk, 0:n], func=AF.Copy), reads=[PB[bk]], writes=[Bkbf[i]])
        return (bk, i, n, dst, wb, tb)

    def ropeB(st):
        bk, i, n, dst, wb, tb = st
        S.op("pe", L("matmul", psum[:, 6 + i, 0:n], lhsT=RT_b, rhs=kbf[i][:, 0:n], start=True, stop=True), reads=[Bkbf[i], Bc], writes=[PB[6 + i]])
        S.op("dve", L("tensor_tensor", out=rt1[i][:, 0:n], in0=psum[:, bk, 0:n], in1=cosg[tb][:, 0:n], op=ALU.mult),
             reads=[PB[bk], Btab[tb]], writes=[Brt1[i]])
        S.op("dve", L("tensor_tensor", out=rt2[i][:, 0:n], in0=psum[:, 6 + i, 0:n], in1=sing[tb][:, 0:n], op=ALU.mult),
             reads=[PB[6 + i], Btab[tb]], writes=[Brt2[i]])
        S.op("pool", L("tensor_tensor", out=dst, in0=rt1[i][:, 0:n], in1=rt2[i][:, 0:n], op=ALU.add), reads=[Brt1[i], Brt2[i]], writes=wb)

    def norm_group(G):
        ntile = 4 if G < 16 else 2
        for t in range(ntile):
            gt = G * 4 + t
            xb = gt % 2; x4i = gt % 4
            src = xkv[gt * 128:(gt + 1) * 128, :] if G < 16 else ctxd[t * 128:(t + 1) * 128, :]
            S.dma("sp", xt[xb], src, writes=[Bxt[xb]])
            S.op("act", L("activation", out=junk, in_=xt[xb], func=AF.Square, accum_out=st4[x4i][:, 0:1]),
                 reads=[Bxt[xb]], writes=[Bjunk, Bst[x4i]])
            S.op("act", L("activation", out=st4[x4i][:, 1:2], in_=st4[x4i][:, 0:1], func=AF.Sqrt, scale=1.0 / D, bias=EPS),
                 reads=[Bst[x4i]], writes=[Bst[x4i]])
            S.op("dve", L("reciprocal", out=st4[x4i][:, 2:3], in_=st4[x4i][:, 1:2]), reads=[Bst[x4i]], writes=[Bst[x4i]])
            S.op("act", L("activation", out=xn[x4i], in_=xt[xb], func=AF.Copy, scale=st4[x4i][:, 2:3]),
                 reads=[Bxt[xb], Bst[x4i]], writes=[Bxn[x4i]])

    norm_group(0)
    for G in range(17):
        ntile = 4 if G < 16 else 2
        n = ntile * 128
        hb = G % 2
        AT, BT = (A1T, B1T) if G < 16 else (A1cT, B1cT)
        tb = G % 2
        S.dma("sp", cosg[tb][:, 0:n], cosd[:, G * 512:G * 512 + n], writes=[Btab[tb]])
        S.dma("sp", sing[tb][:, 0:n], sind[:, G * 512:G * 512 + n], writes=[Btab[tb]])
        for t in range(ntile):
            gt = G * 4 + t
            xb = gt % 2; x4i = gt % 4
            tbk = xb
            for c in range(8):
                S.op("pe", L("transpose", out=pbf(tbk)[:, c, :], in_=xn[x4i][:, c * 128:(c + 1) * 128], identity=ident_b),
                     reads=[Bxn[x4i], Bc], writes=[PB[tbk]])
            S.op("dve", L("tensor_tensor", out=evt[xb], in0=pbf(tbk), in1=AT.unsqueeze(2).to_broadcast([128, 8, 128]), op=ALU.mult),
                 reads=[PB[tbk], Bvec], writes=[Bev[xb]])
            S.op("pool", L("tensor_tensor", out=hxT[hb][:, :, t * 128:(t + 1) * 128], in0=evt[xb],
                           in1=BT.unsqueeze(2).to_broadcast([128, 8, 128]), op=ALU.add), reads=[Bev[xb], Bvec], writes=[Bhx[hb]])
        if G + 1 < 17:
            norm_group(G + 1)
        ch = G // 2 if G < 16 else 8
        ko = (G % 2) * 512 if G < 16 else 0
        sb = G % 2
        jobs = [(KA + h * 128, n, kst[sb][:, h, 0:n], [Bkst[sb]]) for h in range(4)]
        if G < 5 or G == 16:
            wo = G * 512 if G < 16 else WROWS
            jobs.append((KB, n, KbT[:, wo:wo + n], [BKb]))
        if G < 5:
            for (base, dstT) in ((QA, QaT), (QB, QbT)):
                for h in range(4):
                    jobs.append((base + h * 128, 512, dstT[:, h, G * 512:(G + 1) * 512], [BQ]))
        st_ = projA(jobs[0], hb, tb)
        for ji in range(len(jobs)):
            nxt = projA(jobs[ji + 1], hb, tb) if ji + 1 < len(jobs) else None
            ropeB(st_)
            st_ = nxt
            if ji == 3:
                for h in range(4):
                    S.dma("sp", kscr[h, ch, :, ko:ko + n], kst[sb][:, h, 0:n], reads=[Bkst[sb]], writes=[Buf()])
        for t in range(ntile):
            bk = nextbank()
            for c in range(8):
                S.op("pe", L("matmul", psum[:, bk, :], lhsT=hxT[hb][:, c, t * 128:(t + 1) * 128], rhs=win[:, c, VA:VA + 512],
                             start=(c == 0), stop=(c == 7)), reads=[Bwin, Bhx[hb]], writes=[PB[bk]])
            S.op("act", L("activation", out=vst[sb][:, :, t, :], in_=psum[:, bk, :].rearrange("p (h e) -> p h e", e=128), func=AF.Copy),
                 reads=[PB[bk]], writes=[Bvst[sb]])
        for h in range(4):
            tl0 = (G % 2) * 4 if G < 16 else 0
            S.dma("sp", vscr[h, ch, :, tl0:tl0 + ntile, :], vst[sb][:, h, 0:ntile, :], reads=[Bvst[sb]], writes=[Buf()])
        if G < 5 or G == 16:
            bk = nextbank()
            for t in range(ntile):
                for c in range(8):
                    S.op("pe", L("matmul", psum[:, bk, t * 128:(t + 1) * 128], lhsT=hxT[hb][:, c, t * 128:(t + 1) * 128],
                                 rhs=win[:, c, VB:VB + 128], start=(c == 0), stop=(c == 7)), reads=[Bwin, Bhx[hb]], writes=[PB[bk]])
            vt0 = G * 4 if G < 16 else 20
            S.op("act", L("activation", out=Vb[:, vt0:vt0 + ntile, :], in_=psum[:, bk, 0:n].rearrange("p (t e) -> p t e", e=128), func=AF.Copy),
                 reads=[PB[bk]], writes=[BVb])

    if stop_after == 1:
        return finish()
    S.barrier()
    off[0] = mark_attn
    yaT = alloc([128, 4, NQ], BF16); ybT = alloc([128, 4, NQ], BF16)
    mark_att2 = off[0]

    kch = [alloc([128, 1024], BF16) for _ in range(3)]; vch = [alloc([128, 8, 128], BF16) for _ in range(3)]
    Bkch = [Buf(f"kch{i}") for i in range(3)]; Bvch = [Buf(f"vch{i}") for i in range(3)]
    PT = [alloc([128, 1024], BF16) for _ in range(3)]; BPT = [Buf(f"PT{i}") for i in range(3)]
    r1 = alloc([128, 512]); r2 = alloc([128, 512]); t1 = alloc([128, 512]); t2 = alloc([128, 512])
    ot = alloc([128, 512]); osq = alloc([128, 512]); rs = alloc([128, 512]); zsb = alloc([64, 512])
    selA = alloc([64, 128]); selB = alloc([64, 128])
    o1sb = alloc([128, 512]); o2sb = alloc([128, 512])
    Bpp = [Buf(f"pp{i}") for i in range(10)]; Bsel = Buf("sel")
    S.op("dve", L("memset", selA, 0.0), writes=[Bsel]); S.op("dve", L("memset", selB, 0.0), writes=[Bsel])
    S.op("dve", L("memset", selA[0:32, :], 1.0 / 32), writes=[Bsel]); S.op("dve", L("memset", selB[32:64, :], 1.0 / 32), writes=[Bsel])
    if do_moe:
        zer = alloc([128, 4, D], BF16); Bzer = Buf("zer")
        S.op("pool", L("memset", zer, 0.0), writes=[Bzer])
        Bxe0 = []
        xev = xe[0:NSLOT, :].rearrange("(n p) d -> p n d", p=128)
        for i in range(NSLOT // 128 // 4):
            bz = Buf(); Bxe0.append(bz)
            S.dma("pool", xev[:, i * 4:(i + 1) * 4, :], zer, reads=[Bzer], writes=[bz])

    chunks = [(ci, 8) for ci in range(8)] + [(8, 2)]
    seq = [(h, qb, ci, nt) for h in range(4) for qb in range(4) for (ci, nt) in chunks]

    def issue_load(j):
        if j >= len(seq): return
        h, qb, ci, nt = seq[j]
        bi = j % 3
        S.dma("sp", kch[bi][:, 0:nt * 128], kscr[h, ci, :, 0:nt * 128], reads=[Bks], writes=[Bkch[bi]])
        S.dma("sp", vch[bi][:, 0:nt, :], vscr[h, ci, :, 0:nt, :], reads=[Bvs], writes=[Bvch[bi]])

    blocks = []
    for h in range(4):
        for qb in range(4):
            units = []
            for j0, (ci, nt) in enumerate(chunks):
                j = (h * 4 + qb) * 9 + j0
                for tt in range(nt):
                    units.append((j, tt, ci))
            blocks.append((h, qb, units))
    NU = len(blocks[0][2])

    def emit_S(bidx, k):
        h, qb, units = blocks[bidx]
        q0 = 128 + qb * 512
        j, tt, ci = units[k]
        g = bidx * NU + k
        bi = j % 3; sbk = (g % 2) * 2
        for cmp_ in range(2):
            S.op("pe", L("matmul", psum[:, sbk + cmp_, :], lhsT=kch[bi][cmp_ * 64:(cmp_ + 1) * 64, tt * 128:(tt + 1) * 128],
                         rhs=QaT[cmp_ * 64:(cmp_ + 1) * 64, h, q0:q0 + 512], start=True, stop=True),
                 reads=[Bkch[bi], BQ], writes=[PB[sbk + cmp_]])

    def post_stages(h, qb):
        def s1():
            S.op("pe", L("matmul", psum[:, 7, :], lhsT=selA, rhs=zsb, start=True, stop=True), reads=[Bpp[7], Bsel], writes=[PB[7]])
        def s2():
            S.op("dve", L("reciprocal", out=r1, in_=psum[:, 7, :]), reads=[PB[7]], writes=[Bpp[0]])
        def s3():
            S.op("pe", L("matmul", psum[:, 7, :], lhsT=selB, rhs=zsb, start=True, stop=True), reads=[Bpp[7], Bsel], writes=[PB[7]])
        def s4():
            S.op("dve", L("reciprocal", out=r2, in_=psum[:, 7, :]), reads=[PB[7]], writes=[Bpp[1]])
            S.op("dve", L("tensor_tensor", out=t1, in0=o1sb, in1=r1, op=ALU.mult), reads=[Bpp[8], Bpp[0]], writes=[Bpp[2]])
            S.op("dve", L("tensor_tensor", out=t2, in0=o2sb, in1=r2, op=ALU.mult), reads=[Bpp[9], Bpp[1]], writes=[Bpp[3]])
            S.op("dve", L("scalar_tensor_tensor", out=ot, in0=t2, scalar=neglam, in1=t1, op0=ALU.mult, op1=ALU.add),
                 reads=[Bpp[2], Bpp[3], Bvec], writes=[Bpp[4]])
        def s5():
            S.op("act", L("activation", out=osq, in_=ot, func=AF.Square), reads=[Bpp[4]], writes=[Bpp[5]])
        def s6():
            S.op("pe", L("matmul", psum[:, 7, :], lhsT=ones_f, rhs=osq, start=True, stop=True), reads=[Bpp[5], Bc], writes=[PB[7]])
        def s7():
            S.op("act", L("activation", out=rs, in_=psum[:, 7, :], func=AF.Sqrt, scale=1.0 / 128, bias=EPS), reads=[PB[7]], writes=[Bpp[6]])
        def s8():
            S.op("dve", L("reciprocal", out=rs, in_=rs), reads=[Bpp[6]], writes=[Bpp[6]])
            S.op("dve", L("scalar_tensor_tensor", out=yaT[:, h, qb * 512:(qb + 1) * 512], in0=ot, scalar=gsubp, in1=rs, op0=ALU.mult, op1=ALU.mult),
                 reads=[Bpp[4], Bpp[6], Bvec], writes=[Bya])
        return [s1, s2, s3, s4, s5, s6, s7, s8]

    STAGE_AT = {3: 0, 7: 1, 11: 2, 15: 3, 22: 4, 26: 5, 31: 6, 35: 7}
    def emit_Sg(g):
        if g < len(blocks) * NU:
            emit_S(g // NU, g % NU)

    issue_load(0); issue_load(1)
    emit_Sg(0); emit_Sg(1)
    pending = None
    for bidx, (h, qb, units) in enumerate(blocks):
        for k in range(NU):
            j, tt, ci = units[k]
            g = bidx * NU + k
            if tt == 0:
                issue_load(j + 2)
            bi = j % 3; sbk = (g % 2) * 2; pi = g % 3
            S.op("act", L("activation", out=PT[pi], in_=pflat(sbk, 2), func=AF.Exp, scale=0.125),
                 reads=[PB[sbk], PB[sbk + 1]], writes=[BPT[pi]])
            emit_Sg(g + 2)
            first = (k == 0); last = (k == NU - 1)
            for cmp_ in range(2):
                S.op("pe", L("matmul", psum[:, 4 + cmp_, :], lhsT=vch[bi][:, tt, :], rhs=PT[pi][:, cmp_ * 512:(cmp_ + 1) * 512], start=first, stop=last),
                     reads=[Bvch[bi], BPT[pi]], writes=[PB[4 + cmp_]])
            for cmp_ in range(2):
                S.op("pe", L("matmul", psum[cmp_ * 32:(cmp_ + 1) * 32, 6, :], lhsT=ones_b[:, 0:32], rhs=PT[pi][:, cmp_ * 512:(cmp_ + 1) * 512],
                             start=first, stop=last, tile_position=(0, cmp_ * 32)),
                     reads=[BPT[pi], Bc], writes=[PB[6]])
            if pending is not None and k in STAGE_AT:
                pending[STAGE_AT[k]]()
        S.op("act", L("activation", out=o1sb, in_=psum[:, 4, :], func=AF.Copy), reads=[PB[4]], writes=[Bpp[8]])
        S.op("act", L("activation", out=o2sb, in_=psum[:, 5, :], func=AF.Copy), reads=[PB[5]], writes=[Bpp[9]])
        S.op("dve", L("tensor_copy", out=zsb, in_=psum[0:64, 6, :]), reads=[PB[6]], writes=[Bpp[7]])
        pending = post_stages(h, qb)
    for st_fn in pending:
        st_fn()

    if stop_after == 2:
        return finish()
    NPW = 4
    PW = [alloc([128, 512], BF16) for _ in range(NPW)]; BPW = [Buf(f"PW{i}") for i in range(NPW)]
    zt = [alloc([128, 512]) for _ in range(2)]; Bzt = [Buf("zt0"), Buf("zt1")]
    WP = []
    for nb in range(16):
        for k, (kt, kind) in enumerate([(nb, "L"), (nb + 1, "C"), (nb + 2, "R"), (20, "X"), (21, "X")]):
            WP.append((nb, k, kt, kind))

    def emit_WS(p):
        nb, k, kt, kind = WP[p]
        kc0 = kt * 128; q0 = 128 + nb * 128
        for g in range(2):
            gs = slice(g * 64, (g + 1) * 64)
            sbk = 2 * (p % 2) + g
            S.op("pe", L("matmul", psum[:, sbk, :].rearrange("p (i q) -> p i q", q=128), lhsT=KbT[gs, kc0:kc0 + 128], rhs=QbT[gs, :, q0:q0 + 128],
                         start=True, stop=True), reads=[BKb, BQ], writes=[PB[sbk]])

    emit_WS(0); emit_WS(1)
    for p, (nb, k, kt, kind) in enumerate(WP):
        ob = 4 + 2 * (nb % 2)
        if kind == "L" and nb == 0:
            bias = wmask[:, 0:1]
        elif kind == "R" and nb == 15:
            bias = wmask[:, 1:2]
        else:
            bias = 0.0
        for g in range(2):
            sbk = 2 * (p % 2) + g; pi = 2 * (p % 2) + g
            S.op("act", L("activation", out=PW[pi], in_=psum[:, sbk, :], func=AF.Exp, scale=0.125, bias=bias),
                 reads=[PB[sbk], Bc], writes=[BPW[pi]])
            if kind in ("L", "R"):
                tri = triL if kind == "L" else triR
                pw3 = PW[pi].rearrange("p (i q) -> p i q", q=128)
                S.op("dve", L("tensor_tensor", out=pw3, in0=pw3, in1=tri.unsqueeze(1).to_broadcast([128, 4, 128]), op=ALU.mult),
                     reads=[BPW[pi], Bc], writes=[BPW[pi]])
        first = (k == 0); last = (k == 4)
        for g in range(2):
            gs = slice(g * 64, (g + 1) * 64); pi = 2 * (p % 2) + g
            S.op("pe", L("matmul", psum[gs, ob, :], lhsT=Vb[:, kt, gs], rhs=PW[pi], start=first, stop=last, tile_position=(0, g * 64)),
                 reads=[BVb, BPW[pi]], writes=[PB[ob]])
        for g in range(2):
            gs = slice(g * 64, (g + 1) * 64); pi = 2 * (p % 2) + g
            S.op("pe", L("matmul", psum[gs, ob + 1, :], lhsT=ones_b[:, 0:64], rhs=PW[pi], start=first, stop=last, tile_position=(0, g * 64)),
                 reads=[Bc, BPW[pi]], writes=[PB[ob + 1]])
        if p + 2 < len(WP):
            emit_WS(p + 2)
        if k == 4:
            zi = nb % 2
            z3 = zt[zi].rearrange("p (i q) -> p i q", q=128)
            S.op("dve", L("tensor_tensor", out=z3, in0=psum[:, ob + 1, :].rearrange("p (i q) -> p i q", q=128),
                          in1=sinkexp.unsqueeze(2).to_broadcast([128, 4, 128]), op=ALU.add), reads=[PB[ob + 1], Bvec], writes=[Bzt[zi]])
            S.op("dve", L("reciprocal", out=zt[zi], in_=zt[zi]), reads=[Bzt[zi]], writes=[Bzt[zi]])
            S.op("dve", L("tensor_tensor", out=ybT[:, :, nb * 128:(nb + 1) * 128], in0=psum[:, ob, :].rearrange("p (i q) -> p i q", q=128),
                          in1=z3, op=ALU.mult), reads=[PB[ob], Bzt[zi]], writes=[Byb])

    if dbg:
        S.dma("sp", dbg_kb, KbT, reads=[BKb])
        S.dma("sp", dbg_vb, Vb.rearrange("p a b -> p (a b)"), reads=[BVb])
        S.dma("sp", dbg_qb, QbT.rearrange("p a b -> p (a b)"), reads=[BQ])
        S.dma("sp", dbg_yb, ybT.rearrange("p a b -> p (a b)"), reads=[Byb])
    if stop_after == 3:
        return finish()
    S.barrier()
    off[0] = mark_att2
    wout = alloc([128, 8, D], BF16); Bwo = Buf("wout")
    wr_b = alloc([128, 8, NE], BF16); wr_f = alloc([128, 8, NE]); br_b = alloc([1, NE], BF16); br_f = alloc([1, NE])
    Bwr = Buf("wr")
    x4 = [alloc([128, D]) for _ in range(2)]; Bx4 = [Buf("x4a"), Buf("x4b")]
    tm4 = alloc([128, D]); Btm4 = Buf("tm4")
    x1t = [alloc([128, D]) for _ in range(2)]; Bx1 = [Buf("x1a"), Buf("x1b")]
    h2 = alloc([128, D]); Bh2 = Buf("h2")
    hx2all = alloc([128, 16, D], BF16); Bhx2t = [Buf(f"hx2_{i}") for i in range(16)]
    hx2T = alloc([128, 8, 128], BF16); Bhx2T = Buf("hx2T")
    s4 = [alloc([128, 4]) for _ in range(2)]; Bs4 = [Buf("s4a"), Buf("s4b")]
    lg = alloc([128, NE]); m8 = alloc([128, 8]); i8 = alloc([128, 8], U32); ekf = alloc([128, 8]); nm0 = alloc([128, 1])
    ge = alloc([128, 4]); gz = alloc([128, 1]); maskb = alloc([128, NE], BF16); destf = alloc([128, NE]); prod = alloc([128, NE])
    dk = alloc([128, 4]); Brr = Buf("rr")
    Bx1s = Buf("x1scr"); Bxes = [Buf(f"xes{i}") for i in range(64)]

    woutv = w_out.rearrange("(c p) n -> p c n", p=128)
    S.dma("pool", wout[:, 0:4, :], woutv[:, 0:4, :], writes=[Bwo])
    for gi in range(4):
        for g in range(2):
            r0 = 512 + g * 256 + gi * 64
            S.dma("pool", wout[g * 64:(g + 1) * 64, 4 + gi, :], w_out[r0:r0 + 64, :], writes=[Bwo])
    S.dma("sp", wr_f, w_r.rearrange("(c p) n -> p c n", p=128), writes=[Bwr])
    S.dma("sp", br_f, b_r.rearrange("(o n) -> o n", o=1), writes=[Bwr])
    S.op("dve", L("tensor_copy", out=wr_b, in_=wr_f), reads=[Bwr], writes=[Bwr])
    S.op("dve", L("tensor_copy", out=br_b, in_=br_f), reads=[Bwr], writes=[Bwr])

    lgs = [alloc([128, NE]) for _ in range(2)]; Blg = [Buf("lg0"), Buf("lg1")]

    def stageA1(t):
        b = t % 2
        pb0 = 2 * b
        S.dma("sp", x4[b], xkv[128 + t * 128:128 + (t + 1) * 128, :], writes=[Bx4[b]])
        for hf in range(2):
            for c in range(8):
                lhs = yaT[:, c, t * 128:(t + 1) * 128] if c < 4 else ybT[:, c - 4, t * 128:(t + 1) * 128]
                S.op("pe", L("matmul", psum[:, pb0 + hf, :], lhsT=lhs, rhs=wout[:, c, hf * 512:(hf + 1) * 512],
                                                                             start=(c == 0), stop=(c == 7)),
                     reads=[Bya, Byb, Bwo], writes=[PB[pb0 + hf]])
        S.op("dve", L("tensor_tensor", out=tm4, in0=pflat(pb0, 2), in1=G1bc, op=ALU.mult), reads=[PB[pb0], PB[pb0 + 1], Bbc], writes=[Btm4])
        S.op("pool", L("tensor_tensor", out=x1t[b], in0=tm4, in1=x4[b], op=ALU.add), reads=[Btm4, Bx4[b]], writes=[Bx1[b]])
        S.dma("sp", x1scr[t * 128:(t + 1) * 128, :], x1t[b], reads=[Bx1[b]], writes=[Bx1s])
        if dbg:
            S.dma("sp", dbg_x1[t * 128:(t + 1) * 128, :], x1t[b], reads=[Bx1[b]])

    def stageA2(t):
        b = t % 2
        S.op("act", L("activation", out=junk, in_=x1t[b], func=AF.Square, accum_out=s4[b][:, 0:1]), reads=[Bx1[b]], writes=[Bjunk, Bs4[b]])
        S.op("act", L("activation", out=s4[b][:, 1:2], in_=s4[b][:, 0:1], func=AF.Sqrt, scale=1.0 / D, bias=EPS), reads=[Bs4[b]], writes=[Bs4[b]])
        S.op("dve", L("reciprocal", out=s4[b][:, 2:3], in_=s4[b][:, 1:2]), reads=[Bs4[b]], writes=[Bs4[b]])
        S.op("dve", L("scalar_tensor_tensor", out=h2, in0=x1t[b], scalar=s4[b][:, 2:3], in1=A2bc, op0=ALU.mult, op1=ALU.mult),
             reads=[Bx1[b], Bs4[b], Bbc], writes=[Bh2])
        S.op("pool", L("tensor_tensor", out=hx2all[:, t, :], in0=h2, in1=B2bc, op=ALU.add), reads=[Bh2, Bbc], writes=[Bhx2t[t]])

    def stageA3(t):
        b = t % 2
        for c in range(8):
            S.op("pe", L("transpose", out=pbf(4)[:, c, :], in_=hx2all[:, t, c * 128:(c + 1) * 128], identity=ident_b),
                 reads=[Bhx2t[t], Bc], writes=[PB[4]])
        S.op("act", L("activation", out=hx2T, in_=pbf(4), func=AF.Copy), reads=[PB[4]], writes=[Bhx2T])
        for c in range(8):
            S.op("pe", L("matmul", psum[:, 5, 0:NE], lhsT=hx2T[:, c, :], rhs=wr_b[:, c, :], start=(c == 0), stop=False),
                 reads=[Bhx2T, Bwr], writes=[PB[5]])
        S.op("pe", L("matmul", psum[:, 5, 0:NE], lhsT=ones_b[0:1, :], rhs=br_b, start=False, stop=True), reads=[Bwr, Bc], writes=[PB[5]])
        S.op("dve", L("tensor_copy", out=lgs[b], in_=psum[:, 5, 0:NE]), reads=[PB[5]], writes=[Blg[b]])

    def stageB(t):
        b = t % 2
        S.op("dve", L("max", out=m8, in_=lgs[b]), reads=[Brr, Blg[b]], writes=[Brr])
        S.op("dve", L("max_index", out=i8, in_max=m8, in_values=lgs[b]), reads=[Brr, Blg[b]], writes=[Brr])
        S.op("dve", L("tensor_copy", out=ek_all[:, t * 4:(t + 1) * 4], in_=i8[:, 0:4]), reads=[Brr], writes=[Brt])
        S.op("dve", L("tensor_scalar", out=nm0, in0=m8[:, 0:1], scalar1=-1.0, scalar2=None, op0=ALU.mult), reads=[Brr], writes=[Brr])
        S.op("act", L("activation", out=ge, in_=m8[:, 0:4], func=AF.Exp, bias=nm0, accum_out=gz), reads=[Brr], writes=[Brr])
        S.op("dve", L("reciprocal", out=gz, in_=gz), reads=[Brr], writes=[Brr])
        S.op("dve", L("tensor_scalar", out=gates[:, t * 4:(t + 1) * 4], in0=ge, scalar1=gz, scalar2=None, op0=ALU.mult), reads=[Brr], writes=[Brt])
        S.op("dve", L("tensor_scalar", out=maskb, in0=lgs[b], scalar1=m8[:, 3:4], scalar2=None, op0=ALU.is_ge), reads=[Brr, Blg[b]], writes=[Brr])
        S.op("pe", L("matmul", psum[:, 6, 0:NE], lhsT=U_b, rhs=maskb, start=True, stop=True), reads=[Brr, Bc], writes=[PB[6]])
        S.op("pe", L("matmul", psum[:, 7, 0:NE], lhsT=ones_b, rhs=maskb, start=True, stop=True), reads=[Brr, Bc], writes=[PB[7]])
        S.op("dve", L("tensor_tensor", out=destf, in0=psum[:, 6, 0:NE], in1=basecnt, op=ALU.add), reads=[PB[6], Brt], writes=[Brr])
        S.op("dve", L("tensor_tensor", out=basecnt, in0=psum[:, 7, 0:NE], in1=basecnt, op=ALU.add), reads=[PB[7], Brt], writes=[Brt])
        for k in range(4):
            col = t * 4 + k
            S.op("dve", L("scalar_tensor_tensor", out=prod, in0=iota_f, scalar=ek_all[:, col:col + 1], in1=destf, op0=ALU.is_equal, op1=ALU.mult,
                          accum_out=rk_all[:, col:col + 1]), reads=[Brr, Bc, Brt], writes=[Brr, Brt])

    stageA1(0); stageA1(1)
    if do_moe:
        stageA2(0)
    for t in range(16):
        if t + 2 < 16:
            stageA1(t + 2)
        if do_moe:
            if t + 1 < 16:
                stageA2(t + 1)
            stageA3(t)
            stageB(t)

    if do_moe:
        cntcol = alloc([32, 1]); tmp32 = alloc([32, 32]); G32 = alloc([32, 32]); T32 = alloc([32, 32]); poscol = alloc([32, 1]); pos2 = alloc([32, 1])
        PmT = alloc([32, 32]); OFFpos = alloc([128, 32]); offk = alloc([128, 64]); dst64 = alloc([128, 64]); t1p = alloc([128, 32])
        carr = alloc([128, 8]); idxf = alloc([128, 32, 8])
        Bso = Buf("sort")
        S.op("dve", L("tensor_tensor", out=tmp32, in0=basecnt[0:32, :], in1=ident_f[0:32, 0:32], op=ALU.mult), reads=[Brt, Bc], writes=[Bso])
        S.op("dve", L("reduce_sum", out=cntcol, in_=tmp32, axis=AX.X), reads=[Bso], writes=[Bso])
        S.op("dve", L("tensor_scalar", out=G32, in0=basecnt[0:32, :], scalar1=cntcol, scalar2=None, op0=ALU.is_gt), reads=[Brt, Bso], writes=[Bso])
        S.op("dve", L("scalar_tensor_tensor", out=T32, in0=basecnt[0:32, :], scalar=cntcol, in1=Lmask, op0=ALU.is_equal, op1=ALU.mult),
             reads=[Brt, Bso, Bc], writes=[Bso])
        S.op("dve", L("tensor_tensor", out=G32, in0=G32, in1=T32, op=ALU.add), reads=[Bso], writes=[Bso])
        S.op("dve", L("reduce_sum", out=poscol, in_=G32, axis=AX.X), reads=[Bso], writes=[Bso])
        S.op("dve", L("tensor_scalar", out=Pm, in0=iota_f[0:32, :], scalar1=poscol, scalar2=None, op0=ALU.is_equal), reads=[Bso, Bc], writes=[Brt])
        S.op("pe", L("matmul", psum[0:32, 0, 0:32], lhsT=Pm, rhs=ident_f[0:32, 0:32], start=True, stop=True), reads=[Brt, Bc], writes=[PB[0]])
        S.op("dve", L("tensor_copy", out=PmT, in_=psum[0:32, 0, 0:32]), reads=[PB[0]], writes=[Bso])
        S.op("pe", L("matmul", psum[:, 1, 0:32], lhsT=OFFrep, rhs=PmT, start=True, stop=True), reads=[Bso, Bc], writes=[PB[1]])
        S.op("dve", L("tensor_scalar", out=OFFpos, in0=psum[:, 1, 0:32], scalar1=128.0, scalar2=None, op0=ALU.mult), reads=[PB[1]], writes=[Bso])
        S.op("pe", L("matmul", psum[:, 2, 0:32], lhsT=pidx[0:32, :], rhs=Pm, start=True, stop=True), reads=[Brt, Bc], writes=[PB[2]])
        S.op("dve", L("tensor_copy", out=permbc, in_=psum[:, 2, 0:32]), reads=[PB[2]], writes=[Brt])
        for col in range(64):
            S.op("dve", L("scalar_tensor_tensor", out=prod, in0=iota_f, scalar=ek_all[:, col:col + 1], in1=OFFpos, op0=ALU.is_equal, op1=ALU.mult,
                          accum_out=offk[:, col:col + 1]), reads=[Brt, Bc, Bso], writes=[Brr, Bso])
        S.op("dve", L("tensor_tensor", out=dst64, in0=offk, in1=rk_all, op=ALU.add), reads=[Bso, Brt], writes=[Bso])
        S.op("dve", L("tensor_copy", out=desti, in_=dst64), reads=[Bso], writes=[Brt])
        S.op("dve", L("scalar_tensor_tensor", out=t1p, in0=permbc, scalar=1024.0, in1=pidx[:, 0:32], op0=ALU.mult, op1=ALU.add), reads=[Brt, Bc], writes=[Bso])
        S.op("dve", L("tensor_scalar", out=carr, in0=iota_f[:, 0:8], scalar1=128.0, scalar2=None, op0=ALU.mult), reads=[Bc], writes=[Bso])
        S.op("dve", L("tensor_tensor", out=idxf, in0=t1p.unsqueeze(2).to_broadcast([128, 32, 8]), in1=carr.unsqueeze(1).to_broadcast([128, 32, 8]), op=ALU.add),
             reads=[Bso], writes=[Bso])
        S.op("dve", L("tensor_copy", out=idxg, in_=idxf.rearrange("p a b -> p (a b)")), reads=[Bso], writes=[Brt])
        S.op("dve", L("tensor_copy", out=bdn_idx, in_=permbc), reads=[Brt], writes=[Brt])
        for col in range(64):
            t = col // 4
            S.op("pool", L("indirect_dma_start", out=xe[:, :], out_offset=bass.IndirectOffsetOnAxis(ap=desti[:, col:col + 1], axis=0),
                           in_=hx2all[:, t, :], in_offset=None), reads=[Bhx2t[t], Brt] + Bxe0, writes=[Bxes[col]], dma=True)

    if not do_moe:
        lastd = [o for o in S.ops["sp"] if o.dma][-4:]
        S.op("sp", None, deps=[o for o in S.ops["sp"] if o.dma][-40:])
        S.emit(nc)
        return nc

    S.barrier()
    off[0] = mark_persist

    XC = 1024
    wgu = [alloc([128, 8, 2 * D], BF16) for _ in range(2)]; wdn = [alloc([128, 8, D], BF16) for _ in range(2)]
    Bwgu = [[Buf(f"wgu{b}_{c}") for c in range(8)] for b in range(2)]; Bwdn = [[Buf(f"wdn{b}_{c}") for c in range(8)] for b in range(2)]
    bdn1 = alloc([128, D]); Bbdn1 = Buf("bdn")
    bgu_f = alloc([NE, 2 * D]); biasT = alloc([128, 16, NE]); Bbias = Buf("bias")
    xet = [alloc([128, D], BF16) for _ in range(3)]; Bxet = [Buf(f"xet{i}") for i in range(3)]
    xeT = alloc([128, 8, XC], BF16); BxeT = Buf("xeT")
    aT = alloc([128, 8, XC], BF16); BaT = Buf("aT")
    g1 = [alloc([128, 512]) for _ in range(2)]; sgm = [alloc([128, 512]) for _ in range(2)]
    u1 = [alloc([128, 512]) for _ in range(2)]; gsx = [alloc([128, 512]) for _ in range(2)]
    Bg1 = [Buf("g1a"), Buf("g1b")]; Bsgm = [Buf("sga"), Buf("sgb")]; Bu1 = [Buf("u1a"), Buf("u1b")]; Bgsx = [Buf("gsa"), Buf("gsb")]
    yst = [alloc([128, D]) for _ in range(2)]; Byst = [Buf("yst0"), Buf("yst1")]

    S.dma("sp", bgu_f, b_gu, writes=[Bbias])
    for m in range(8):
        for two in range(2):
            j = m * 2 + two
            S.op("pe", L("matmul", psum[:, 0, j * NE:(j + 1) * NE], lhsT=bgu_f[:, 2 * m * 128 + two:2 * (m + 1) * 128:2],
                         rhs=Pm, start=True, stop=True), reads=[Bbias, Brt], writes=[PB[0]])
    S.op("dve", L("tensor_copy", out=biasT, in_=psum[:, 0, :].rearrange("p (j n) -> p j n", n=NE)), reads=[PB[0]], writes=[Bbias])
    S.op("dve", L("tensor_scalar", out=biasT[:, 1:16:2, :], in0=biasT[:, 1:16:2, :], scalar1=1.0, scalar2=None, op0=ALU.add), reads=[Bbias], writes=[Bbias])

    wgu_rows = w_gu.rearrange("e k n -> (e k) n"); wdn_rows = w_dn.rearrange("e k n -> (e k) n")
    IO = bass.IndirectOffsetOnAxis

    def load_w(i):
        b = i % 2
        for c in range(8):
            S.op("pool", L("indirect_dma_start", out=wgu[b][:, c, :], out_offset=None, in_=wgu_rows[:, :], in_offset=IO(ap=idxg[:, i * 8 + c:i * 8 + c + 1], axis=0)),
                 reads=[Brt], writes=[Bwgu[b][c]], dma=True)
        for c in range(8):
            S.op("pool", L("indirect_dma_start", out=wdn[b][:, c, :], out_offset=None, in_=wdn_rows[:, :], in_offset=IO(ap=idxg[:, i * 8 + c:i * 8 + c + 1], axis=0)),
                 reads=[Brt], writes=[Bwdn[b][c]], dma=True)

    def load_bdn(i):
        S.op("pool", L("indirect_dma_start", out=bdn1, out_offset=None, in_=b_dn[:, :], in_offset=IO(ap=bdn_idx[:, i:i + 1], axis=0)),
             reads=[Brt], writes=[Bbdn1], dma=True)

    work = []
    for i in range(NE):
        n = NT[i]; o = OFFT[i]
        nitem = -(-n // (XC // 128))
        sizes = [n // nitem + (1 if q < n % nitem else 0) for q in range(nitem)]
        for q, k in enumerate(sizes):
            work.append((i, o, k, q == 0, q == nitem - 1)); o += k

    load_w(0); load_bdn(0)
    pc5 = [0]; xc5 = [0]; yc5 = [0]

    def stage_x(w):
        i, o, k, first, lastw = w
        for s in range(k):
            xi = xc5[0] % 3; xc5[0] += 1
            tb_ = xi % 2
            S.dma("sp", xet[xi], xe[(o + s) * 128:(o + s + 1) * 128, :], reads=Bxes, writes=[Bxet[xi]])
            for c in range(8):
                S.op("pe", L("transpose", out=pbf(tb_)[:, c, :], in_=xet[xi][:, c * 128:(c + 1) * 128], identity=ident_b),
                     reads=[Bxet[xi], Bc], writes=[PB[tb_]])
            S.op("act", L("activation", out=xeT[:, :, s * 128:(s + 1) * 128], in_=pbf(tb_), func=AF.Copy), reads=[PB[tb_]], writes=[BxeT])

    stage_x(work[0])
    for wi, (i, o, k, first, lastw) in enumerate(work):
        b = i % 2
        if first and i + 1 < NE:
            load_w(i + 1)
        ncols = k * 128
        for m in range(8):
            nch = -(-ncols // 512); cw = ncols // nch
            for c0 in range(0, ncols, cw):
                nn = cw
                ii = pc5[0] % 2; pc5[0] += 1
                bg_, bu_ = 2 + 2 * ii, 3 + 2 * ii
                cs = slice(c0, c0 + nn)
                for (bk, two) in ((bg_, 0), (bu_, 1)):
                    for c in range(8):
                        S.op("pe", L("matmul", psum[:, bk, 0:nn], lhsT=wgu[b][:, c, 2 * m * 128 + two:2 * (m + 1) * 128:2],
                                     rhs=xeT[:, c, cs], start=(c == 0), stop=(c == 7)),
                             reads=[Bwgu[b][c], BxeT], writes=[PB[bk]])
                S.op("dve", L("tensor_scalar", out=g1[ii][:, 0:nn], in0=psum[:, bg_, 0:nn], scalar1=biasT[:, 2 * m, i:i + 1], scalar2=7.0, op0=ALU.add, op1=ALU.min),
                     reads=[PB[bg_], Bbias], writes=[Bg1[ii]])
                S.op("act", L("activation", out=sgm[ii][:, 0:nn], in_=g1[ii][:, 0:nn], func=AF.Sigmoid, scale=1.702), reads=[Bg1[ii]], writes=[Bsgm[ii]])
                S.op("dve", L("tensor_scalar", out=u1[ii][:, 0:nn], in0=psum[:, bu_, 0:nn], scalar1=biasT[:, 2 * m + 1, i:i + 1], scalar2=8.0, op0=ALU.add, op1=ALU.min),
                     reads=[PB[bu_], Bbias], writes=[Bu1[ii]])
                S.op("dve", L("tensor_tensor", out=gsx[ii][:, 0:nn], in0=g1[ii][:, 0:nn], in1=sgm[ii][:, 0:nn], op=ALU.mult), reads=[Bg1[ii], Bsgm[ii]], writes=[Bgsx[ii]])
                S.op("dve", L("scalar_tensor_tensor", out=aT[:, m, cs], in0=u1[ii][:, 0:nn], scalar=-6.0, in1=gsx[ii][:, 0:nn], op0=ALU.max, op1=ALU.mult),
                     reads=[Bu1[ii], Bgsx[ii]], writes=[BaT])
        if wi + 1 < len(work):
            stage_x(work[wi + 1])
        for s in range(k):
            yb_ = yc5[0] % 2; yc5[0] += 1
            for hf in range(2):
                bk = 6 + hf
                for m in range(8):
                    S.op("pe", L("matmul", psum[:, bk, :], lhsT=aT[:, m, s * 128:(s + 1) * 128], rhs=wdn[b][:, m, hf * 512:(hf + 1) * 512],
                                 start=(m == 0), stop=(m == 7)),
                         reads=[BaT, Bwdn[b][m]], writes=[PB[bk]])
                S.op("dve", L("tensor_tensor", out=yst[yb_][:, hf * 512:(hf + 1) * 512], in0=psum[:, bk, :], in1=bdn1[:, hf * 512:(hf + 1) * 512], op=ALU.add),
                     reads=[PB[bk], Bbdn1], writes=[Byst[yb_]])
            S.dma("sp", ye[(o + s) * 128:(o + s + 1) * 128, :], yst[yb_], reads=[Byst[yb_]], writes=[Buf()])
        if lastw and i + 1 < NE:
            load_bdn(i + 1)

    S.barrier()
    off[0] = mark_persist

    yk = [alloc([128, D]) for _ in range(8)]; Byk = [Buf(f"yk{i}") for i in range(8)]
    x6 = [alloc([128, D]) for _ in range(2)]; Bx6 = [Buf("x6a"), Buf("x6b")]
    acc = [alloc([128, D]) for _ in range(2)]; Bacc = [Buf("acca"), Buf("accb")]
    acc2 = [alloc([128, D]) for _ in range(2)]; Bacc2 = [Buf("acc2a"), Buf("acc2b")]
    xo = [alloc([128, D]) for _ in range(2)]; Bxo = [Buf("xoa"), Buf("xob")]
    ot6 = [alloc([128, D]) for _ in range(2)]; Bot6 = [Buf("o6a"), Buf("o6b")]
    s6 = [alloc([128, 4]) for _ in range(2)]; Bs6 = [Buf("s6a"), Buf("s6b")]
    outs = []

    def loads6(t):
        b = t % 2
        S.dma("sp", x6[b], x1scr[t * 128:(t + 1) * 128, :], reads=[Bx1s], writes=[Bx6[b]])
        for k in range(4):
            col = t * 4 + k
            S.op("pool", L("indirect_dma_start", out=yk[4 * b + k], out_offset=None, in_=ye[:, :],
                           in_offset=bass.IndirectOffsetOnAxis(ap=desti[:, col:col + 1], axis=0)),
                 reads=[Brt], writes=[Byk[4 * b + k]], dma=True)

    loads6(0)
    for t in range(16):
        b = t % 2
        if t + 1 < 16:
            loads6(t + 1)
        S.op("act", L("activation", out=acc[b], in_=yk[4 * b], func=AF.Copy, scale=gates[:, t * 4:t * 4 + 1]), reads=[Byk[4 * b], Brt], writes=[Bacc[b]])
        for k in range(1, 4):
            S.op("dve", L("scalar_tensor_tensor", out=acc[b], in0=yk[4 * b + k], scalar=gates[:, t * 4 + k:t * 4 + k + 1], in1=acc[b], op0=ALU.mult, op1=ALU.add),
                 reads=[Byk[4 * b + k], Brt, Bacc[b]], writes=[Bacc[b]])
        S.op("dve", L("tensor_tensor", out=acc2[b], in0=acc[b], in1=G2bc, op=ALU.mult), reads=[Bacc[b], Bbc], writes=[Bacc2[b]])
        S.op("pool", L("tensor_tensor", out=xo[b], in0=acc2[b], in1=x6[b], op=ALU.add), reads=[Bacc2[b], Bx6[b]], writes=[Bxo[b]])
        S.op("act", L("activation", out=junk, in_=xo[b], func=AF.Square, accum_out=s6[b][:, 0:1]), reads=[Bxo[b]], writes=[Bjunk, Bs6[b]])
        S.op("act", L("activation", out=s6[b][:, 1:2], in_=s6[b][:, 0:1], func=AF.Sqrt, scale=1.0 / D, bias=EPS), reads=[Bs6[b]], writes=[Bs6[b]])
        S.op("dve", L("reciprocal", out=s6[b][:, 2:3], in_=s6[b][:, 1:2]), reads=[Bs6[b]], writes=[Bs6[b]])
        S.op("dve", L("scalar_tensor_tensor", out=ot6[b], in0=xo[b], scalar=s6[b][:, 2:3], in1=FGbc, op0=ALU.mult, op1=ALU.mult),
             reads=[Bxo[b], Bs6[b], Bbc], writes=[Bot6[b]])
        outs.append(S.dma("sp", outd[t * 128:(t + 1) * 128, :], ot6[b], reads=[Bot6[b]]))
    S.op("sp", None, deps=outs)
    S.emit(nc)
    return nc


def _rope_tables(pos):
    p = np.arange(128); d = p % 64; f = d % 16
    inv = (np.float32(10000.0) ** (-(np.arange(16, dtype=np.float32)) / np.float32(16))).astype(np.float32)
    row = (pos // 64).astype(np.float32); col = (pos % 64).astype(np.float32)
    coord = np.where((d < 32)[:, None], row[None, :], col[None, :]).astype(np.float32)
    ang = (coord * inv[f][:, None]).astype(np.float32)
    return np.cos(ang).astype(np.float32), np.sin(ang).astype(np.float32)


def _consts():
    c = np.zeros((128, 9, 128), np.float32)
    c[:, 0, :] = np.eye(128, dtype=np.float32)
    R = np.zeros((128, 128), np.float32)
    for m in range(128):
        if (m % 32) < 16: R[m, m + 16] = -1.0
        else: R[m, m - 16] = 1.0
    c[:, 1, :] = R.T
    j = np.arange(128)[:, None]; i = np.arange(128)[None, :]
    c[:, 2, :] = (j < i).astype(np.float32)
    c[:, 3, :] = (j >= i).astype(np.float32)
    c[:, 4, :] = (j <= i).astype(np.float32)
    c[:, 5, :] = np.arange(128, dtype=np.float32)[None, :]
    c[:, 6, :] = np.arange(128, dtype=np.float32)[:, None]
    c[0:NE, 7, :] = np.asarray(OFFT, np.float32)[:, None]
    c[:, 8, :] = (i < j).astype(np.float32)
    return c.reshape(128, 9 * 128)


def make_in_maps(inp, do_moe=True):
    x = np.asarray(inp["x"], np.float32); ctx = np.asarray(inp["ctx"], np.float32)
    c = np.asarray(inp["c"], np.float32); c_ctx = np.asarray(inp["c_ctx"], np.float32)
    consts = _consts()
    lam = np.stack([np.asarray(inp[k], np.float32)[0] for k in ("lam_q1", "lam_k1", "lam_q2", "lam_k2")], 0)
    shared = {
        "consts": consts, "w_mod": np.ascontiguousarray(inp["w_mod"][0]), "b_mod": np.ascontiguousarray(inp["b_mod"][0]),
        "norm1_g": np.ascontiguousarray(inp["norm1_g"][0]), "w_in": np.ascontiguousarray(inp["w_in"][0]), "lam": np.ascontiguousarray(lam),
        "subln_g": np.ascontiguousarray(inp["subln_g"][0]), "sink": np.ascontiguousarray(inp["sink"][0]),
        "w_out": np.ascontiguousarray(inp["w_out"][0]), "norm2_g": np.ascontiguousarray(inp["norm2_g"][0]),
        "w_router": np.ascontiguousarray(inp["w_router"][0]), "b_router": np.ascontiguousarray(inp["b_router"][0]),
        "final_g": np.ascontiguousarray(inp["final_g"]),
    }
    if do_moe:
        shared.update({"w_gate_up": np.ascontiguousarray(inp["w_gate_up"][0]), "b_gate_up": np.ascontiguousarray(inp["b_gate_up"][0]),
                       "w_down": np.ascontiguousarray(inp["w_down"][0]), "b_down": np.ascontiguousarray(inp["b_down"][0])})
    shared = {k: np.asarray(v, np.float32) for k, v in shared.items()}
    maps = []
    for core in range(8):
        b, j = core // 4, core % 4
        qs = j * NQ
        shift = qs - 128
        pos = (np.arange(S_LEN) + shift) % S_LEN
        cos, sin = _rope_tables(pos)
        cosT = np.concatenate([cos, np.ones((128, CTX), np.float32)], 1)
        sinT = np.concatenate([sin, np.zeros((128, CTX), np.float32)], 1)
        wmask = np.zeros((128, 2), np.float32)
        if j == 0: wmask[:, 0] = -1e30
        if j == 3: wmask[:, 1] = -1e30
        m = dict(shared)
        m.update({"xkv": np.ascontiguousarray(np.roll(x[b], -shift, axis=0)), "ctx": np.ascontiguousarray(ctx[b]),
                  "cvec": np.ascontiguousarray(np.stack([c[b], c_ctx], 0)), "cosT": cosT, "sinT": sinT, "wmask": wmask})
        maps.append(m)
    return maps


_NC_CACHE = {}


def kernel(**inputs):
    if "nc" not in _NC_CACHE:
        _NC_CACHE["nc"] = build_program(do_moe=True)
    nc = _NC_CACHE["nc"]
    maps = make_in_maps(inputs, do_moe=True)
    res = run_bass_kernel_spmd(nc, maps, core_ids=list(range(8)))
    out = np.empty((2, S_LEN, D), np.float32)
    for core in range(8):
        b, j = core // 4, core % 4
        out[b, j * NQ:(j + 1) * NQ, :] = res.results[core]["out"]
    return out
```
